# Optimizing a Trainium2 kernel written in Bass

```python
import jax, jax.numpy as jnp
from jax import lax
import numpy as np

D_MODEL = 1024
BATCH = 8
SEQ = 2048
DEPTH = 4

N_MIXERS = 2
N_LAYERS_A = (DEPTH + 1) // 2
N_LAYERS_B = DEPTH // 2
FOX_HEADS = 16
FOX_HEAD_DIM = D_MODEL // FOX_HEADS
Q_BLOCK = 128
FOX_IN = 4 * D_MODEL + FOX_HEADS
RET_HEADS = 4
RET_QK_DIM = D_MODEL // RET_HEADS
RET_V_DIM = 2 * D_MODEL // RET_HEADS
RET_CHUNK = 128
RET_IN = 2 * RET_HEADS * RET_QK_DIM + 2 * RET_HEADS * RET_V_DIM
N_GROUPS = 4
EXPERTS_PER_GROUP = 8
N_EXPERTS = N_GROUPS * EXPERTS_PER_GROUP
TOP_K = 2
EXPERT_FF = D_MODEL // 2
MOE_BLOCK = 128

EPS = 1e-6
NEG_INF = -1e30
ROPE_BASE = 10000.0

kernel_name = "fox_retnet_hier_moe_trunk"

F32 = jnp.float32


def rms_norm(x, g):
    xf = x.astype(F32)
    y = xf * lax.rsqrt(jnp.mean(xf * xf, axis=-1, keepdims=True) + EPS)
    return (y * g.astype(F32)).astype(x.dtype)


def forgetting_attention(h, w_in, b_f, w_out):
    B, S, _ = h.shape
    H, dh = FOX_HEADS, FOX_HEAD_DIM
    proj = h @ w_in
    q, k, v, gate = jnp.split(proj[..., :4 * D_MODEL], 4, axis=-1)
    f_logit = (proj[..., 4 * D_MODEL:] + b_f).astype(F32)
    cum = jnp.cumsum(jax.nn.log_sigmoid(f_logit), axis=1).transpose(0, 2, 1)
    heads = lambda t: t.reshape(B, S, H, dh).transpose(0, 2, 1, 3)
    q, k, v = heads(q), heads(k), heads(v)
    scale = dh ** -0.5
    k_pos = jnp.arange(S)

    def query_block(i):
        start = i * Q_BLOCK
        q_b = lax.dynamic_slice_in_dim(q, start, Q_BLOCK, axis=2)
        c_b = lax.dynamic_slice_in_dim(cum, start, Q_BLOCK, axis=2)
        s = jnp.einsum('bhqd,bhkd->bhqk', q_b, k).astype(F32) * scale
        s = s + c_b[..., :, None] - cum[..., None, :]
        q_pos = start + jnp.arange(Q_BLOCK)
        s = jnp.where(k_pos[None, :] <= q_pos[:, None], s, NEG_INF)
        p = jax.nn.softmax(s, axis=-1)
        return jnp.einsum('bhqk,bhkd->bhqd', p.astype(v.dtype), v)

    o = lax.map(query_block, jnp.arange(S // Q_BLOCK))
    o = o.transpose(1, 0, 3, 2, 4).reshape(B, S, D_MODEL)
    o = o * jax.nn.sigmoid(gate)
    return o @ w_out


def rotary(t, pos):
    d = t.shape[-1]
    inv = 1.0 / (ROPE_BASE ** jnp.linspace(0.0, 1.0, d // 2, dtype=F32))
    ang = pos.astype(F32)[:, None] * inv[None, :]
    cos, sin = jnp.cos(ang), jnp.sin(ang)
    t1, t2 = t[..., :d // 2].astype(F32), t[..., d // 2:].astype(F32)
    return jnp.concatenate([t1 * cos - t2 * sin, t1 * sin + t2 * cos], axis=-1)


def retention(h, w_in, gn_gain, w_out):
    B, S, _ = h.shape
    H, dk, dv, C = RET_HEADS, RET_QK_DIM, RET_V_DIM, RET_CHUNK
    proj = h @ w_in
    q, k, v, gate = jnp.split(proj, [H * dk, 2 * H * dk, 2 * H * dk + H * dv], axis=-1)
    heads = lambda t, d: t.reshape(B, S, H, d).transpose(0, 2, 1, 3)
    pos = jnp.arange(S)
    q = rotary(heads(q, dk), pos)
    k = rotary(heads(k, dk), pos) * (dk ** -0.5)
    v = heads(v, dv).astype(F32)

    log_g = jnp.log(1.0 - 2.0 ** (-5.0 - jnp.arange(H, dtype=F32)))
    idx = jnp.arange(C, dtype=F32)
    rel = idx[:, None] - idx[None, :]
    intra = jnp.where(rel >= 0, jnp.exp(log_g[:, None, None] * jnp.maximum(rel, 0.0)), 0.0)
    q_decay = jnp.exp(log_g[:, None] * (idx + 1.0))[None, :, :, None]
    k_decay = jnp.exp(log_g[:, None] * (C - 1.0 - idx))[None, :, :, None]
    chunk_decay = jnp.exp(log_g * C)[None, :, None, None]

    n_chunks = S // C
    to_chunks = lambda t: t.reshape(B, H, n_chunks, C, t.shape[-1]).transpose(2, 0, 1, 3, 4)

    def step(R, qkv):
        q_n, k_n, v_n = qkv
        s = jnp.einsum('bhid,bhjd->bhij', q_n, k_n) * intra
        o = jnp.einsum('bhij,bhje->bhie', s, v_n)
        o = o + jnp.einsum('bhid,bhde->bhie', q_n, R) * q_decay
        R = R * chunk_decay + jnp.einsum('bhjd,bhje->bhde', k_n * k_decay, v_n)
        return R, o

    R0 = jnp.zeros((B, H, dk, dv), F32)
    _, o = lax.scan(step, R0, (to_chunks(q), to_chunks(k), to_chunks(v)))
    o = o.transpose(1, 0, 3, 2, 4).reshape(B, S, H, dv)
    mu = jnp.mean(o, axis=-1, keepdims=True)
    var = jnp.mean(jnp.square(o - mu), axis=-1, keepdims=True)
    o = ((o - mu) * lax.rsqrt(var + EPS)).reshape(B, S, H * dv) * gn_gain.astype(F32)
    o = jax.nn.silu(gate.astype(F32)) * o
    return o.astype(h.dtype) @ w_out


def grouped_experts(xf, expert_id, gates, w_gu, w_down):
    T, D = xf.shape
    A = T * TOP_K
    flat_e = expert_id.reshape(A)
    order = jnp.argsort(flat_e)
    sorted_e = flat_e[order]
    counts = jnp.bincount(flat_e, length=N_EXPERTS)
    padded = (counts + MOE_BLOCK - 1) // MOE_BLOCK * MOE_BLOCK
    pad_end = jnp.cumsum(padded)
    pad_start = pad_end - padded
    start = jnp.cumsum(counts) - counts
    dest = pad_start[sorted_e] + jnp.arange(A) - start[sorted_e]
    n_blocks = (A + N_EXPERTS * (MOE_BLOCK - 1) + MOE_BLOCK - 1) // MOE_BLOCK
    n_rows = n_blocks * MOE_BLOCK
    tok_sorted = (order // TOP_K).astype(jnp.int32)
    row_tok = jnp.full((n_rows,), T, jnp.int32).at[dest].set(tok_sorted)
    x_pad = jnp.concatenate([xf, jnp.zeros((1, D), xf.dtype)], axis=0)
    x_rows = x_pad[row_tok].reshape(n_blocks, MOE_BLOCK, D)
    block_e = jnp.minimum(jnp.searchsorted(pad_end, jnp.arange(n_blocks) * MOE_BLOCK, side='right'),
                          N_EXPERTS - 1)

    def expert_block(args):
        xb, e = args
        a, b = jnp.split(xb @ w_gu[e], 2, axis=-1)
        return (jax.nn.silu(a) * b) @ w_down[e]

    y_rows = lax.map(expert_block, (x_rows, block_e)).reshape(n_rows, D)
    w_sorted = gates.reshape(A)[order].astype(y_rows.dtype)
    return jax.ops.segment_sum(y_rows[dest] * w_sorted[:, None], tok_sorted, num_segments=T)


def hier_moe(h, wr_g, br_g, wr_e, br_e, w_gu, w_down):
    B, S, D = h.shape
    T = B * S
    xf = h.reshape(T, D)
    p_group = jax.nn.softmax((xf @ wr_g).astype(F32) + br_g.astype(F32), axis=-1)
    pg, gi = lax.top_k(p_group, 1)
    e_logits = ((xf @ wr_e).astype(F32) + br_e.astype(F32)).reshape(T, N_GROUPS, EXPERTS_PER_GROUP)
    e_in = jnp.einsum('tge,tg->te', e_logits, jax.nn.one_hot(gi[:, 0], N_GROUPS, dtype=F32))
    pv, pi = lax.top_k(jax.nn.softmax(e_in, axis=-1), TOP_K)
    gates = pg * pv / jnp.sum(pv, axis=-1, keepdims=True)
    expert_id = gi * EXPERTS_PER_GROUP + pi
    return grouped_experts(xf, expert_id, gates, w_gu, w_down).reshape(B, S, D)


def setup_inputs(seed: int = 0) -> dict:
    key = jax.random.key(seed)
    ks = jax.random.split(key, 17)
    nrm = lambda k, shape, fan_in: jax.random.normal(k, shape, F32) * (fan_in ** -0.5)
    out_scale = (2.0 * DEPTH) ** -0.5
    return {
        "x": jax.random.normal(ks[0], (BATCH, SEQ, D_MODEL), F32),
        "fox_w_in": nrm(ks[1], (N_LAYERS_A, D_MODEL, FOX_IN), D_MODEL),
        "fox_b_f": 3.0 + 0.5 * jax.random.normal(ks[2], (N_LAYERS_A, FOX_HEADS), F32),
        "fox_w_out": nrm(ks[3], (N_LAYERS_A, D_MODEL, D_MODEL), D_MODEL) * out_scale,
        "ret_w_in": nrm(ks[4], (N_LAYERS_B, D_MODEL, RET_IN), D_MODEL),
        "ret_gn_gain": 1.0 + 0.02 * jax.random.normal(ks[5], (N_LAYERS_B, RET_HEADS * RET_V_DIM), F32),
        "ret_w_out": nrm(ks[6], (N_LAYERS_B, RET_HEADS * RET_V_DIM, D_MODEL), RET_HEADS * RET_V_DIM) * out_scale,
        "norm_mix": 1.0 + 0.02 * jax.random.normal(ks[7], (DEPTH, D_MODEL), F32),
        "norm_ffn": 1.0 + 0.02 * jax.random.normal(ks[8], (DEPTH, D_MODEL), F32),
        "router_group_w": nrm(ks[9], (DEPTH, D_MODEL, N_GROUPS), D_MODEL),
        "router_group_b": 0.01 * jax.random.normal(ks[10], (DEPTH, N_GROUPS), F32),
        "router_expert_w": nrm(ks[11], (DEPTH, D_MODEL, N_EXPERTS), D_MODEL),
        "router_expert_b": 0.01 * jax.random.normal(ks[12], (DEPTH, N_EXPERTS), F32),
        "expert_w_gu": nrm(ks[13], (DEPTH, N_EXPERTS, D_MODEL, 2 * EXPERT_FF), D_MODEL),
        "expert_w_down": nrm(ks[14], (DEPTH, N_EXPERTS, EXPERT_FF, D_MODEL), EXPERT_FF) * out_scale,
        "norm_final": 1.0 + 0.02 * jax.random.normal(ks[15], (D_MODEL,), F32),
    }


def reference(x, fox_w_in, fox_b_f, fox_w_out, ret_w_in, ret_gn_gain, ret_w_out,
              norm_mix, norm_ffn, router_group_w, router_group_b, router_expert_w,
              router_expert_b, expert_w_gu, expert_w_down, norm_final):
    h = x
    for i in range(DEPTH):
        hn = rms_norm(h, norm_mix[i])
        j = i // N_MIXERS
        if i % N_MIXERS == 0:
            h = h + forgetting_attention(hn, fox_w_in[j], fox_b_f[j], fox_w_out[j])
        else:
            h = h + retention(hn, ret_w_in[j], ret_gn_gain[j], ret_w_out[j])
        hn = rms_norm(h, norm_ffn[i])
        h = h + hier_moe(hn, router_group_w[i], router_group_b[i], router_expert_w[i],
                         router_expert_b[i], expert_w_gu[i], expert_w_down[i])
    return rms_norm(h, norm_final)
```

```python
import contextlib
import os
import numpy as np
import concourse.bass as bass
import concourse.mybir as mybir
from concourse.bass_utils import run_bass_kernel_spmd

F32 = mybir.dt.float32
BF16 = mybir.dt.bfloat16
I32 = mybir.dt.int32
AF = mybir.ActivationFunctionType
ALU = mybir.AluOpType
AX = mybir.AxisListType

D = 1024
S = 2048
NT = S // 128
DEPTH = 4
EPS = 1e-6
NCORES = 8

ENGINES = ("pe", "act", "dve", "pool", "sp")


class _Op:
    __slots__ = ("eng", "fn", "dma", "waits", "signal", "idx")

    def __init__(self, eng, fn, dma, idx):
        self.eng = eng
        self.fn = fn
        self.dma = dma
        self.waits = []
        self.signal = None
        self.idx = idx


class Prog:
    N_DMA_SEMS = 24

    def __init__(self, same_engine_sync=True):
        self.ops = []
        self.last_writer = {}
        self.readers = {}
        self.same_engine_sync = same_engine_sync
        self.dependents = {}
        self.deps = []
        self.forced = set()
        self.pending = {e: set() for e in ENGINES}
        self.last_op = {}
        self.unfenced_dma = set()

    def op(self, eng, fn, reads=(), writes=(), dma=False, force=False):
        idx = len(self.ops)
        o = _Op(eng, fn, dma, idx)
        if force:
            self.forced.add(idx)
        ps_reads = [k for k in reads if isinstance(k, tuple) and k[0] == "ps"]
        if ps_reads:
            reads = [k for k in reads if k not in ps_reads]
            writes = list(writes) + ps_reads
        deps = set()
        for k in reads:
            w = self.last_writer.get(k)
            if w is not None:
                deps.add(w)
        for k in writes:
            w = self.last_writer.get(k)
            if w is not None:
                deps.add(w)
            for r in self.readers.get(k, ()):
                deps.add(r)
        deps |= self.pending[eng]
        self.pending[eng] = set()
        deps.discard(idx)
        self.last_op[eng] = idx
        if dma:
            self.unfenced_dma.add(idx)
        for k in writes:
            self.last_writer[k] = idx
            self.readers[k] = []
        for k in reads:
            if k not in writes:
                self.readers.setdefault(k, []).append(idx)
        self.ops.append(o)
        self.deps.append(deps)
        return idx

    def barrier(self):
        deps = set(self.last_op.values()) | self.unfenced_dma
        self.unfenced_dma = set()
        for e in ENGINES:
            self.pending[e] |= deps

    def finalize(self):
        ops = self.ops
        needed = [i in self.forced for i in range(len(ops))]
        for o in ops:
            for d in self.deps[o.idx]:
                p = ops[d]
                if p.dma:
                    needed[d] = True
                elif p.eng == o.eng and not o.dma:
                    if p.eng == "pe":
                        continue
                    if self.same_engine_sync:
                        needed[d] = True
                else:
                    needed[d] = True
        eng_cnt = {e: 0 for e in ENGINES}
        dma_cnt = [0] * self.N_DMA_SEMS
        dma_rr = 0
        seen = {e: {} for e in ENGINES}
        for o in ops:
            waits = {}
            for d in self.deps[o.idx]:
                p = ops[d]
                if p.signal is None:
                    continue
                if (not p.dma) and p.eng == o.eng and not o.dma and (p.eng == "pe" or not self.same_engine_sync):
                    continue
                k, v, _ = p.signal
                if seen[o.eng].get(k, 0) >= v:
                    continue
                waits[k] = max(waits.get(k, 0), v)
            if o.dma and needed[o.idx]:
                j = dma_rr
                dma_rr = (dma_rr + 1) % self.N_DMA_SEMS
                k = ("dma", j)
                if dma_cnt[j] > 0 and seen[o.eng].get(k, 0) < dma_cnt[j]:
                    waits[k] = max(waits.get(k, 0), dma_cnt[j])
                dma_cnt[j] += 16
                o.signal = (k, dma_cnt[j], 16)
            elif needed[o.idx]:
                eng_cnt[o.eng] += 1
                o.signal = (("eng", o.eng), eng_cnt[o.eng], 1)
            for k, v in waits.items():
                seen[o.eng][k] = max(seen[o.eng].get(k, 0), v)
            o.waits = sorted(waits.items(), key=lambda kv: str(kv[0]))
        self.max_counts = dict(eng_cnt)

    def emit(self, nc, stack, final_waits):
        sems = {}
        for e in ENGINES:
            sems[("eng", e)] = stack.enter_context(nc.semaphore("sem_" + e))
        for j in range(self.N_DMA_SEMS):
            sems[("dma", j)] = stack.enter_context(nc.semaphore("sem_dma%d" % j))
        block = stack.enter_context(nc.Block())
        by_eng = {e: [o for o in self.ops if o.eng == e] for e in ENGINES}
        last_signal = {}
        for o in self.ops:
            if o.signal is not None:
                last_signal[o.signal[0]] = max(last_signal.get(o.signal[0], 0), o.signal[1])

        def run(engine, ename):
            for o in by_eng[ename]:
                for k, v in o.waits:
                    engine.wait_ge(sems[k], v)
                inst = o.fn(engine)
                if o.signal is not None:
                    inst.then_inc(sems[o.signal[0]], o.signal[2])
            if ename == final_waits:
                for k, v in last_signal.items():
                    if k[0] == "dma":
                        engine.wait_ge(sems[k], v)

        @block.tensor
        def _(e):
            run(e, "pe")

        @block.scalar
        def _(e):
            run(e, "act")

        @block.vector
        def _(e):
            run(e, "dve")

        @block.gpsimd
        def _(e):
            run(e, "pool")

        @block.sync
        def _(e):
            run(e, "sp")


NE = 32
CAP = 256
TRASH = NE * CAP
NSLOT = TRASH + 128
FF = 512
BIG = 30000.0


_DRAM_NAMES = {}
_LAYER_SETS = {}


class Builder:
    def __init__(self):
        self.nc = nc = bass.Bass("TRN2", target_bir_lowering=False)
        self.P = Prog()
        self.stack = contextlib.ExitStack()
        self.mem_stack = contextlib.ExitStack()
        self.dram = {}
        self.ps = [self.mem_stack.enter_context(nc.psum_tensor("ps%d" % b, [128, 512], F32)) for b in range(8)]
        sb = self.sb
        self.h = sb("h", [128, NT, D], F32)
        self.gb = sb("gb", [128, D], F32)
        self.ssq = sb("ssq", [128, NT], F32)
        self.rstd = sb("rstd", [128, NT], F32)
        self.junk = sb("junk", [128, D], BF16)
        self.ident_b = sb("ident_b", [128, 128], BF16)
        self.ident_f = sb("ident_f", [128, 128], F32)
        self.lst_b = sb("lst_b", [128, 128], BF16)
        self.ones_b = sb("ones_b", [128, 128], BF16)
        self.ebase = sb("ebase", [128, NE], F32)
        self.zeros_b = sb("zeros_b", [128, D], BF16)
        self.zeros_f = sb("zeros_f", [128, D], F32)
        self.arena_size = 126 * 1024
        self.arena_base, _ = nc.bump_sbuf(self.arena_size)
        self.arena_off = 0

    def sb(self, name, shape, dt):
        return self.mem_stack.enter_context(self.nc.sbuf_tensor(name, list(shape), dt))

    def arena_reset(self):
        self.arena_off = 0

    def ar(self, name, shape, dt):
        nbytes = int(np.prod(shape[1:])) * (4 if dt in (F32, I32) else 2)
        nbytes = (nbytes + 31) // 32 * 32
        assert self.arena_off + nbytes <= self.arena_size, (name, self.arena_off, nbytes)
        t = self.nc.alloc_sbuf_tensor_at(name, list(shape), dt, offset=self.arena_base + self.arena_off)
        self.arena_off += nbytes
        return t

    def din(self, name, shape, dt=F32):
        t = self.nc.dram_tensor(name, list(shape), dt, kind="ExternalInput").ap()
        self.dram[name] = t
        return t

    def dscratch(self, name, shape, dt):
        return self.nc.dram_tensor(name, list(shape), dt, kind="Internal").ap()

    def prologue(self):
        P = self.P
        x = self.din("x", [S, D])
        self.din("norm_mix", [DEPTH, D])
        self.din("norm_ffn", [DEPTH, D])
        self.din("norm_final", [1, D])
        cI = self.din("c_ident", [128, 128])
        cL = self.din("c_lst", [128, 128])
        cE = self.din("c_ebase", [1, NE])
        self.y = self.nc.dram_tensor("y", [S, D], F32, kind="ExternalOutput").ap()
        xv = x.rearrange("(n p) d -> p n d", p=128)
        h = self.h
        for q in range(4):
            sl = slice(q * 4, (q + 1) * 4)
            P.op("sp", lambda e, sl=sl: e.dma_start(out=h[:, sl, :], in_=xv[:, sl, :]),
                 writes=[("h", i) for i in range(q * 4, q * 4 + 4)], dma=True)
        P.op("sp", lambda e: e.dma_start(out=self.ident_f[:], in_=cI), writes=["ident_f"], dma=True)
        P.op("pool", lambda e: e.dma_start(out=self.ident_b[:], in_=cI), writes=["ident_b"], dma=True)
        P.op("pool", lambda e: e.dma_start(out=self.lst_b[:], in_=cL), writes=["lst_b"], dma=True)
        P.op("sp", lambda e: e.dma_start(out=self.ebase[:], in_=cE[0:1, :].partition_broadcast(128)),
             writes=["ebase"], dma=True)
        P.op("dve", lambda e: e.memset(self.ones_b[:], 1.0), writes=["ones_b"])
        P.op("dve", lambda e: e.memset(self.zeros_b[:], 0.0), writes=["zeros_b"])
        P.op("dve", lambda e: e.memset(self.zeros_f[:], 0.0), writes=["zeros_f"])

    def rms_stats(self, gain_row_ap):
        P, h, ssq, rstd, junk, gb = self.P, self.h, self.ssq, self.rstd, self.junk, self.gb
        P.op("sp", lambda e: e.dma_start(out=gb[:], in_=gain_row_ap.partition_broadcast(128)),
             writes=["gb"], dma=True)
        for i in range(NT):
            P.op("act", lambda e, i=i: e.activation(out=junk[:], in_=h[:, i, :], func=AF.Square,
                                                     accum_out=ssq[:, i:i + 1]),
                 reads=[("h", i)], writes=["junk", ("ssq", i)])
        P.op("dve", lambda e: e.tensor_scalar(out=rstd[:], in0=ssq[:], scalar1=1.0 / D, scalar2=EPS,
                                              op0=ALU.mult, op1=ALU.add),
             reads=[("ssq", i) for i in range(NT)], writes=["rstd"])
        P.op("act", lambda e: e.activation(out=rstd[:], in_=rstd[:], func=AF.Sqrt),
             reads=["rstd"], writes=["rstd"])
        P.op("dve", lambda e: e.reciprocal(out=rstd[:], in_=rstd[:]),
             reads=["rstd"], writes=["rstd"])

    def epilogue(self, final_norm=True):
        P, h = self.P, self.h
        P.barrier()
        self.arena_reset()
        outt = self.ar("outt", [128, 2, D], F32)
        yv = self.y.rearrange("(n p) d -> p n d", p=128)
        if final_norm:
            self.rms_stats(self.dram["norm_final"][0:1, :])
        for i in range(NT):
            b = i % 2
            if final_norm:
                P.op("dve", lambda e, i=i, b=b: e.scalar_tensor_tensor(
                    out=outt[:, b, :], in0=h[:, i, :], scalar=self.rstd[:, i:i + 1], in1=self.gb[:],
                    op0=ALU.mult, op1=ALU.mult),
                    reads=[("h", i), "rstd", "gb"], writes=[("outt", b)])
                P.op("sp", lambda e, i=i, b=b: e.dma_start(out=yv[:, i, :], in_=outt[:, b, :]),
                     reads=[("outt", b)], writes=[("y", i)], dma=True, force=True)
            else:
                P.op("sp", lambda e, i=i: e.dma_start(out=yv[:, i, :], in_=h[:, i, :]),
                     reads=[("h", i)], writes=[("y", i)], dma=True, force=True)

    def finish(self):
        _DRAM_NAMES[id(self.nc)] = list(self.dram.keys())
        self.P.finalize()
        self.P.emit(self.nc, self.stack, final_waits="sp")
        self.stack.close()
        return self.nc

    def moe_setup(self, layers):
        self.moe_layers = list(layers)
        nl = len(self.moe_layers)
        self.din("router_w", [nl, D, 36])
        self.din("router_b", [nl, 36])
        for p in range(nl):
            self.din("expert_w_gu%d" % p, [NE, D, 2 * FF])
            self.din("expert_w_down%d" % p, [NE, FF, D])
        self.Xs = self.dscratch("Xs", [NSLOT, D], BF16)
        self.Ys = self.dscratch("Ys", [NSLOT, D], F32)
        P = self.P
        xsv = self.Xs.rearrange("(r p) d -> p r d", p=128)
        ysv = self.Ys.rearrange("(r p) d -> p r d", p=128)
        nr = NSLOT // 128
        for r in range(nr):
            P.op("sp", lambda e, r=r: e.dma_start(out=xsv[:, r, :], in_=self.zeros_b[:]),
                 reads=["zeros_b"], writes=[("Xs_z", r)], dma=True)
        P.op("sp", lambda e: e.dma_start(out=ysv[:, nr - 1, :], in_=self.zeros_f[:]),
             reads=["zeros_f"], writes=["Ys_z"], dma=True)

    def moe_layer(self, layer):
        li = self.moe_layers.index(layer)
        P, h, ps = self.P, self.h, self.ps
        rstd, gb = self.rstd, self.gb
        P.barrier()
        self.arena_reset()
        ar = self.ar
        wgu = [ar("wgu%d" % s, [128, 8, 2 * FF], BF16) for s in range(2)]
        wdn = [ar("wdn%d" % s, [128, 4, D], BF16) for s in range(2)]
        xg = [ar("xg%d" % s, [128, 2, D], BF16) for s in range(2)]
        xT = [ar("xT%d" % s, [128, 8, CAP], BF16) for s in range(2)]
        hT = [ar("hT%d" % s, [128, 4, CAP], BF16) for s in range(2)]
        sA = [ar("sA%d" % s, [128, CAP], F32) for s in range(2)]
        yt = [ar("yt%d" % s, [128, 2, D], F32) for s in range(2)]
        yg = [ar("yg%d" % s, [128, D], F32) for s in range(2)]
        hn32 = ar("hn32", [128, D], F32)
        hnT32 = ar("hnT32", [128, 8, 128], F32)
        hnb = [ar("hnb%d" % s, [128, D], BF16) for s in range(2)]
        wr32 = ar("wr32", [128, 8, 36], F32)
        brb = ar("brb", [128, 36], F32)
        LG = ar("LG", [128, NT, 36], F32)
        Em = ar("Em", [128, NT, 32], F32)
        T1 = ar("T1", [128, NT, 32], F32)
        oh1 = ar("oh1", [128, NT, 32], F32)
        oh2 = ar("oh2", [128, NT, 32], F32)
        Em2 = ar("Em2", [128, NT, 32], F32)
        RK = ar("RK", [128, NT, 32], F32)
        Obf = ar("Obf", [128, NT, 32], BF16)
        Gm = ar("Gm", [128, NT, 4], F32)
        ohG = ar("ohG", [128, NT, 4], F32)
        pen = ar("pen", [128, NT, 4], F32)
        sm = {n: ar("sm_" + n, [128, NT], F32) for n in
              ("gmax", "sumG", "pg", "m1", "m2", "r", "den", "g1", "g2", "rk", "base", "valid", "sl")}
        slot_i = ar("slot_i", [128, 2, NT], I32)

        wr = self.dram["router_w"]
        br = self.dram["router_b"]
        wgu_d = self.dram["expert_w_gu%d" % li]
        wdn_d = self.dram["expert_w_down%d" % li]
        Xs, Ys = self.Xs, self.Ys

        self.rms_stats(self.dram["norm_ffn"][layer:layer + 1, :])
        P.op("sp", lambda e: e.dma_start(out=wr32[:], in_=wr[li].rearrange("(c p) j -> p c j", p=128)),
             writes=["wr32"], dma=True)
        P.op("sp", lambda e: e.dma_start(out=brb[:], in_=br[li:li + 1, :].partition_broadcast(128)),
             writes=["brb"], dma=True)

        def load_w(e_idx):
            s = e_idx % 2
            P.op("pool", lambda e: e.dma_start(out=wgu[s][:], in_=wgu_d[e_idx].rearrange("(c p) f -> p c f", p=128)),
                 writes=[("wgu", s)], dma=True)
            P.op("pool", lambda e: e.dma_start(out=wdn[s][:], in_=wdn_d[e_idx].rearrange("(c p) f -> p c f", p=128)),
                 writes=[("wdn", s)], dma=True)

        load_w(0)
        load_w(1)

        for t in range(NT):
            P.op("dve", lambda e, t=t: e.scalar_tensor_tensor(
                out=hn32[:], in0=h[:, t, :], scalar=rstd[:, t:t + 1], in1=gb[:], op0=ALU.mult, op1=ALU.mult),
                reads=[("h", t), "rstd", "gb"], writes=["hn32"])

            def tr(e):
                for c in range(8):
                    bank = ps[2 + c // 4]
                    inst = e.transpose(out=bank[:, (c % 4) * 128:(c % 4 + 1) * 128],
                                       in_=hn32[:, c * 128:(c + 1) * 128], identity=self.ident_f[:])
                return inst
            P.op("pe", tr, reads=["hn32", "ident_f"], writes=[("ps", 2), ("ps", 3)])
            P.op("act", lambda e: e.activation(out=hnT32[:, 0:4, :], in_=ps[2][:].rearrange("p (c t) -> p c t", c=4),
                                               func=AF.Copy),
                 reads=[("ps", 2)], writes=["hnT32a"])
            P.op("dve", lambda e: e.tensor_copy(out=hnT32[:, 4:8, :], in_=ps[3][:].rearrange("p (c t) -> p c t", c=4)),
                 reads=[("ps", 3)], writes=["hnT32b"])

            def rl(e):
                for c in range(8):
                    inst = e.matmul(ps[6][:, 0:36], hnT32[:, c, :], wr32[:, c, :], start=(c == 0), stop=(c == 7))
                return inst
            P.op("pe", rl, reads=["hnT32a", "hnT32b", "wr32"], writes=[("ps", 6)])
            P.op("dve", lambda e, t=t: e.tensor_tensor(out=LG[:, t, :], in0=ps[6][:, 0:36], in1=brb[:], op=ALU.add),
                 reads=[("ps", 6), "brb"], writes=["LG"])

        if int(os.environ.get('MOE_STOP', '99')) <= 1:
            return
        G = LG[:, :, 0:4]
        E4 = LG[:, :, 4:36].rearrange("p n (g e) -> p n g e", g=4)

        def bc(t2, n):
            return t2[:].unsqueeze(2).to_broadcast([128, NT, n])

        def dve(fn, reads, writes):
            P.op("dve", fn, reads=reads, writes=writes)

        dve(lambda e: e.tensor_reduce(out=sm["gmax"][:], in_=G, axis=AX.X, op=ALU.max), ["LG"], ["gmax"])
        dve(lambda e: e.tensor_tensor(out=Gm[:], in0=G, in1=bc(sm["gmax"], 4), op=ALU.subtract), ["LG", "gmax"], ["Gm"])
        dve(lambda e: e.tensor_single_scalar(out=ohG[:], in_=Gm[:], scalar=0.0, op=ALU.is_ge), ["Gm"], ["ohG"])
        P.op("act", lambda e: e.activation(out=Gm[:], in_=Gm[:], func=AF.Exp), reads=["Gm", "ohG"], writes=["Gm"])
        dve(lambda e: e.tensor_reduce(out=sm["sumG"][:], in_=Gm[:], axis=AX.X, op=ALU.add), ["Gm"], ["sumG"])
        dve(lambda e: e.reciprocal(out=sm["pg"][:], in_=sm["sumG"][:]), ["sumG"], ["pg"])
        dve(lambda e: e.tensor_scalar(out=pen[:], in0=ohG[:], scalar1=BIG, scalar2=-BIG, op0=ALU.mult, op1=ALU.add),
            ["ohG"], ["pen"])
        dve(lambda e: e.tensor_tensor(out=Em[:].rearrange("p n (g e) -> p n g e", g=4), in0=E4,
                                      in1=pen[:].unsqueeze(3).to_broadcast([128, NT, 4, 8]), op=ALU.add),
            ["LG", "pen"], ["Em"])
        dve(lambda e: e.tensor_reduce(out=sm["m1"][:], in_=Em[:], axis=AX.X, op=ALU.max), ["Em"], ["m1"])
        dve(lambda e: e.tensor_tensor(out=T1[:], in0=Em[:], in1=bc(sm["m1"], 32), op=ALU.subtract), ["Em", "m1"], ["T1"])
        dve(lambda e: e.tensor_single_scalar(out=oh1[:], in_=T1[:], scalar=0.0, op=ALU.is_ge), ["T1"], ["oh1"])
        dve(lambda e: e.scalar_tensor_tensor(out=Em2[:], in0=oh1[:], scalar=-BIG, in1=Em[:], op0=ALU.mult, op1=ALU.add),
            ["oh1", "Em"], ["Em2"])
        dve(lambda e: e.tensor_reduce(out=sm["m2"][:], in_=Em2[:], axis=AX.X, op=ALU.max), ["Em2"], ["m2"])
        dve(lambda e: e.tensor_tensor(out=T1[:], in0=Em2[:], in1=bc(sm["m2"], 32), op=ALU.subtract), ["Em2", "m2"], ["T1"])
        dve(lambda e: e.tensor_single_scalar(out=oh2[:], in_=T1[:], scalar=0.0, op=ALU.is_ge), ["T1"], ["oh2"])
        dve(lambda e: e.tensor_tensor(out=sm["r"][:], in0=sm["m2"][:], in1=sm["m1"][:], op=ALU.subtract), ["m1", "m2"], ["r"])
        P.op("act", lambda e: e.activation(out=sm["r"][:], in_=sm["r"][:], func=AF.Exp), reads=["r"], writes=["r"])
        dve(lambda e: e.tensor_scalar(out=sm["den"][:], in0=sm["r"][:], scalar1=1.0, scalar2=None, op0=ALU.add), ["r"], ["den"])
        dve(lambda e: e.reciprocal(out=sm["den"][:], in_=sm["den"][:]), ["den"], ["den"])
        dve(lambda e: e.tensor_tensor(out=sm["g1"][:], in0=sm["pg"][:], in1=sm["den"][:], op=ALU.mult), ["pg", "den"], ["g1"])
        dve(lambda e: e.tensor_tensor(out=sm["g2"][:], in0=sm["g1"][:], in1=sm["r"][:], op=ALU.mult), ["g1", "r"], ["g2"])
        dve(lambda e: e.tensor_tensor(out=Obf[:], in0=oh1[:], in1=oh2[:], op=ALU.add), ["oh1", "oh2"], ["Obf"])

        if int(os.environ.get('MOE_STOP', '99')) <= 2:
            return
        def ranks(e):
            for t in range(NT):
                o = ps[7][:, t * 32:(t + 1) * 32]
                inst = e.matmul(o, self.lst_b[:], Obf[:, t, :], start=True, stop=(t == 0))
                for j in range(t):
                    inst = e.matmul(o, self.ones_b[:], Obf[:, j, :], start=False, stop=(j == t - 1))
            return inst
        P.op("pe", ranks, reads=["Obf", "lst_b", "ones_b"], writes=[("ps", 7)])
        dve(lambda e: e.tensor_copy(out=RK[:], in_=ps[7][:].rearrange("p (n e) -> p n e", e=32)), [("ps", 7)], ["RK"])
        ebc = self.ebase[:].unsqueeze(1).to_broadcast([128, NT, 32])
        for k, (oh, ohn, gn) in enumerate(((oh1, "oh1", "g1"), (oh2, "oh2", "g2"))):
            dve(lambda e, oh=oh: e.tensor_tensor(out=T1[:], in0=oh[:], in1=RK[:], op=ALU.mult), [ohn, "RK"], ["T1"])
            dve(lambda e: e.tensor_reduce(out=sm["rk"][:], in_=T1[:], axis=AX.X, op=ALU.add), ["T1"], ["rk"])
            dve(lambda e, oh=oh: e.tensor_tensor(out=T1[:], in0=oh[:], in1=ebc, op=ALU.mult), [ohn, "ebase"], ["T1"])
            dve(lambda e: e.tensor_reduce(out=sm["base"][:], in_=T1[:], axis=AX.X, op=ALU.add), ["T1"], ["base"])
            dve(lambda e: e.tensor_single_scalar(out=sm["valid"][:], in_=sm["rk"][:], scalar=float(CAP), op=ALU.is_lt),
                ["rk"], ["valid"])
            dve(lambda e: e.scalar_tensor_tensor(out=sm["sl"][:], in0=sm["rk"][:], scalar=-float(TRASH), in1=sm["base"][:],
                                                 op0=ALU.add, op1=ALU.add), ["rk", "base"], ["sl"])
            dve(lambda e: e.tensor_tensor(out=sm["sl"][:], in0=sm["sl"][:], in1=sm["valid"][:], op=ALU.mult), ["sl", "valid"], ["sl"])
            dve(lambda e: e.tensor_scalar(out=sm["sl"][:], in0=sm["sl"][:], scalar1=float(TRASH), scalar2=None, op0=ALU.add),
                ["sl"], ["sl"])
            dve(lambda e, k=k: e.tensor_copy(out=slot_i[:, k, :], in_=sm["sl"][:]), ["sl"], [("slot", k)])
            dve(lambda e, gn=gn: e.tensor_tensor(out=sm[gn][:], in0=sm[gn][:], in1=sm["valid"][:], op=ALU.mult),
                [gn, "valid"], [gn])

        if int(os.environ.get('MOE_STOP', '99')) <= 3:
            return
        for t in range(NT):
            b = t % 2
            P.op("dve", lambda e, t=t, b=b: e.scalar_tensor_tensor(
                out=hnb[b][:], in0=h[:, t, :], scalar=rstd[:, t:t + 1], in1=gb[:], op0=ALU.mult, op1=ALU.mult),
                reads=[("h", t), "rstd", "gb"], writes=[("hnb", b)])
            for k in range(2):
                P.op("pool", lambda e, t=t, b=b, k=k: e.indirect_dma_start(
                    out=Xs[:, :], out_offset=bass.IndirectOffsetOnAxis(ap=slot_i[:, k, t:t + 1], axis=0),
                    in_=hnb[b][:], in_offset=None),
                    reads=[("hnb", b), ("slot", k)], writes=[("Xs_w", t, k)], dma=True)

        if int(os.environ.get('MOE_STOP', '99')) <= 4:
            dbg = [sm["sl"], sm["g1"], sm["g2"], sm["rk"], sm["base"], sm["valid"]]
            for q, tl in enumerate(dbg):
                P.op("dve", lambda e, q=q, tl=tl: e.tensor_copy(out=h[:, 0, q * 16:(q + 1) * 16], in_=tl[:]),
                     reads=["sl", "g1", "g2", "rk", "base", "valid"], writes=[("h", 0)])
            for k in range(2):
                P.op("dve", lambda e, k=k: e.tensor_copy(out=h[:, 0, 96 + k * 16:112 + k * 16], in_=slot_i[:, k, :]),
                     reads=[("slot", k)], writes=[("h", 0)])
            P.op("dve", lambda e: e.tensor_copy(out=h[:, 1, 0:576], in_=LG[:].rearrange("p n j -> p (n j)")),
                 reads=["LG"], writes=[("h", 1)])
            return
        xs_keys = [("Xs_w", t, k) for t in range(NT) for k in range(2)]

        def load_x(ex):
            s = ex % 2
            P.op("sp", lambda e: e.dma_start(
                out=xg[s][:], in_=Xs[ex * CAP:(ex + 1) * CAP, :].rearrange("(r p) d -> p r d", p=128)),
                reads=xs_keys, writes=[("xg", s)], dma=True)

        load_x(0)
        for ex in range(NE):
            s = ex % 2
            if ex + 1 < NE:
                load_x(ex + 1)
            for r in range(2):
                bank = ps[r]

                def tr2(e, r=r, s=s, bank=bank):
                    pv = bank[:].bitcast(BF16)
                    for c in range(8):
                        inst = e.transpose(out=pv[:, c * 128:(c + 1) * 128], in_=xg[s][:, r, c * 128:(c + 1) * 128],
                                           identity=self.ident_b[:])
                    return inst
                P.op("pe", tr2, reads=[("xg", s), "ident_b"], writes=[("ps", r)])
                src = lambda bank=bank: bank[:].bitcast(BF16).rearrange("p (c t) -> p c t", c=8)
                if r == 0:
                    P.op("act", lambda e, s=s, src=src: e.activation(out=xT[s][:, :, 0:128], in_=src(), func=AF.Copy),
                         reads=[("ps", 0)], writes=[("xT", s, 0)])
                else:
                    P.op("dve", lambda e, s=s, src=src: e.tensor_copy(out=xT[s][:, :, 128:256], in_=src()),
                         reads=[("ps", 1)], writes=[("xT", s, 1)])
            for m in range(4):
                bank = ps[2 + m % 2]
                bk = ("ps", 2 + m % 2)

                def gu(e, m=m, s=s, bank=bank):
                    for half in range(2):
                        col = half * FF + m * 128
                        for c in range(8):
                            inst = e.matmul(bank[:, half * CAP:(half + 1) * CAP], wgu[s][:, c, col:col + 128],
                                            xT[s][:, c, :], start=(c == 0), stop=(c == 7))
                    return inst
                P.op("pe", gu, reads=[("wgu", s), ("xT", s, 0), ("xT", s, 1)], writes=[bk])
                P.op("act", lambda e, m=m, bank=bank: e.activation(out=sA[m % 2][:], in_=bank[:, 0:CAP], func=AF.Silu),
                     reads=[bk], writes=[("sA", m % 2)])
                P.op("dve", lambda e, m=m, s=s, bank=bank: e.tensor_tensor(out=hT[s][:, m, :], in0=sA[m % 2][:],
                                                                          in1=bank[:, CAP:2 * CAP], op=ALU.mult),
                     reads=[bk, ("sA", m % 2)], writes=[("hT", s, m)])
            for r in range(2):
                for n in range(2):
                    q = r * 2 + n
                    bank = ps[4 + q % 2]
                    bk = ("ps", 4 + q % 2)

                    def dn(e, r=r, n=n, s=s, bank=bank):
                        for m in range(4):
                            inst = e.matmul(bank[:, :], hT[s][:, m, r * 128:(r + 1) * 128],
                                            wdn[s][:, m, n * 512:(n + 1) * 512], start=(m == 0), stop=(m == 3))
                        return inst
                    P.op("pe", dn, reads=[("wdn", s)] + [("hT", s, m) for m in range(4)], writes=[bk])
                    if q % 2 == 0:
                        P.op("act", lambda e, r=r, n=n, s=s, bank=bank: e.activation(
                            out=yt[s][:, r, n * 512:(n + 1) * 512], in_=bank[:, :], func=AF.Copy),
                            reads=[bk], writes=[("yt", s, q)])
                    else:
                        P.op("dve", lambda e, r=r, n=n, s=s, bank=bank: e.tensor_copy(
                            out=yt[s][:, r, n * 512:(n + 1) * 512], in_=bank[:, :]),
                            reads=[bk], writes=[("yt", s, q)])
            P.op("sp", lambda e, ex=ex, s=s: e.dma_start(
                out=Ys[ex * CAP:(ex + 1) * CAP, :].rearrange("(r p) d -> p r d", p=128), in_=yt[s][:]),
                reads=[("yt", s, q) for q in range(4)], writes=[("Ys_w", ex)], dma=True)
            if ex + 2 < NE:
                load_w(ex + 2)

        if int(os.environ.get('MOE_STOP', '99')) <= 5:
            return
        for t in range(NT):
            for k, gn in enumerate(("g1", "g2")):
                b = (t * 2 + k) % 2
                P.op("pool", lambda e, t=t, b=b, k=k: e.indirect_dma_start(
                    out=yg[b][:], out_offset=None, in_=Ys[:, :],
                    in_offset=bass.IndirectOffsetOnAxis(ap=slot_i[:, k, t:t + 1], axis=0)),
                    reads=[("Ys_w", ex) for ex in range(NE)] + [("slot", k)], writes=[("yg", b)], dma=True)
                P.op("dve", lambda e, t=t, b=b, gn=gn: e.scalar_tensor_tensor(
                    out=h[:, t, :], in0=yg[b][:], scalar=sm[gn][:, t:t + 1], in1=h[:, t, :], op0=ALU.mult, op1=ALU.add),
                    reads=[("yg", b), gn, ("h", t)], writes=[("h", t)])

    def norm_transpose(self, hnT, hnb):
        P, h, ps = self.P, self.h, self.ps
        for t in range(NT):
            b = t % 2
            P.op("dve", lambda e, t=t, b=b: e.scalar_tensor_tensor(
                out=hnb[b][:], in0=h[:, t, :], scalar=self.rstd[:, t:t + 1], in1=self.gb[:], op0=ALU.mult, op1=ALU.mult),
                reads=[("h", t), "rstd", "gb"], writes=[("hnb", b)])
            self.transpose_tile(hnb[b], ("hnb", b), hnT, t, b)

    def transpose_tile(self, src, src_key, dstT, t, b):
        P, ps = self.P, self.ps
        bank = ps[b]

        def tr(e):
            pv = bank[:].bitcast(BF16)
            for c in range(8):
                inst = e.transpose(out=pv[:, c * 128:(c + 1) * 128], in_=src[:, c * 128:(c + 1) * 128],
                                   identity=self.ident_b[:])
            return inst
        P.op("pe", tr, reads=[src_key, "ident_b"], writes=[("ps", b)])
        srcv = lambda: bank[:].bitcast(BF16).rearrange("p (c t) -> p c t", c=8)
        if b == 0:
            P.op("act", lambda e: e.activation(out=dstT[:, :, t * 128:(t + 1) * 128], in_=srcv(), func=AF.Copy),
                 reads=[("ps", b)], writes=[("xT", t)])
        else:
            P.op("dve", lambda e: e.tensor_copy(out=dstT[:, :, t * 128:(t + 1) * 128], in_=srcv()),
                 reads=[("ps", b)], writes=[("xT", t)])

    def out_proj(self, xT, kc, wout, n_k):
        P, h, ps = self.P, self.h, self.ps
        for t in range(NT):
            for n in range(2):
                bi = 2 + (t * 2 + n) % 2
                bank = ps[bi]

                def mm(e, t=t, n=n, bank=bank):
                    for c in range(kc):
                        inst = e.matmul(bank[:, :], xT[:, c, t * 128:(t + 1) * 128], wout[:, c, n * 512:(n + 1) * 512],
                                        start=(c == 0), stop=(c == kc - 1))
                    return inst
                P.op("pe", mm, reads=[("xT", t), "wout"] if n_k is None else n_k(t) + ["wout"], writes=[("ps", bi)])
                P.op("dve", lambda e, t=t, n=n, bank=bank: e.tensor_tensor(
                    out=h[:, t, n * 512:(n + 1) * 512], in0=h[:, t, n * 512:(n + 1) * 512], in1=bank[:, :], op=ALU.add),
                    reads=[("ps", bi), ("h", t)], writes=[("h", t)])

    def fox_setup(self, layers):
        self.fox_layers = list(layers)
        nl = len(layers)
        self.din("fox_w_in", [nl, D, 4112])
        self.din("fox_b_f", [nl, 16])
        self.din("fox_w_out", [nl, D, D])
        self.din("c_mask", [128, 128])
        self.cum3_d = self.dscratch("cum3_d", [16, 3, S], BF16)
        self.Og_d = self.dscratch("Og_d", [S, D], BF16)
        self.mask_b = self.sb("mask_b", [128, 128], BF16)
        self.P.op("pool", lambda e: e.dma_start(out=self.mask_b[:], in_=self.dram["c_mask"]), writes=["mask_b"], dma=True)

    def fox_layer(self, layer):
        j = self.fox_layers.index(layer)
        P, h, ps, ar = self.P, self.h, self.ps, self.ar
        w_in = self.dram["fox_w_in"]
        P.barrier()
        self.arena_reset()
        hnT = ar("f_hnT", [128, 8, S], BF16)
        hnb = [ar("f_hnb%d" % s, [128, D], BF16) for s in range(2)]
        wf = ar("f_wf", [128, 8, 16], BF16)
        bft = ar("f_bft", [128, 1], F32)
        negbf = ar("f_negbf", [128, 1], F32)
        save = self.arena_off
        Ft = ar("f_Ft", [128, S], F32)
        cumP = ar("f_cumP", [128, S], F32)
        r1 = ar("f_r1", [128, S], F32)
        cum3 = ar("f_cum3", [128, 3, S], BF16)
        self.arena_off = save
        wgrp = [ar("f_wgrp%d" % s, [128, 8, 4, 128], BF16) for s in range(2)]
        QTa = [ar("f_QTa%d" % s, [128, S], BF16) for s in range(2)]
        KTa = [ar("f_KTa%d" % s, [128, S], BF16) for s in range(2)]
        Vaug = ar("f_Vaug", [128, NT, 2, 65], BF16)
        Gs = ar("f_Gs", [128, NT, 128], BF16)
        NPT = 6
        PT = [ar("f_PT%d" % s, [128, 512], BF16) for s in range(NPT)]
        Og = [ar("f_Og%d" % s, [128, NT, 128], BF16) for s in range(2)]
        wout = ar("f_wout", [128, 8, D], BF16)
        rden = ar("f_rden", [128, 8], F32)

        self.rms_stats(self.dram["norm_mix"][layer:layer + 1, :])
        P.op("pool", lambda e: e.dma_start(out=wf[:], in_=w_in[j][:, 4096:4112].rearrange("(c p) f -> p c f", p=128)),
             writes=["wf"], dma=True)
        P.op("sp", lambda e: e.dma_start(out=bft[0:16, :], in_=self.dram["fox_b_f"][j].rearrange("(h o) -> h o", o=1)),
             writes=["bft"], dma=True)
        P.op("dve", lambda e: e.tensor_scalar(out=negbf[0:16, :], in0=bft[0:16, :], scalar1=-1.0, scalar2=None, op0=ALU.mult),
             reads=["bft"], writes=["negbf"])
        self.norm_transpose(hnT, hnb)
        xT_all = [("xT", t) for t in range(NT)]

        if int(os.environ.get('FOX_STOP', '99')) <= 1:
            return
        for qd in range(4):
            bank = ps[2 + qd % 2]
            bk = ("ps", 2 + qd % 2)

            def fm(e, qd=qd, bank=bank):
                for c in range(8):
                    inst = e.matmul(bank[0:16, :], wf[:, c, :], hnT[:, c, qd * 512:(qd + 1) * 512], start=(c == 0), stop=(c == 7))
                return inst
            P.op("pe", fm, reads=xT_all + ["wf"], writes=[bk])
            P.op("act", lambda e, qd=qd, bank=bank: e.activation(out=Ft[0:16, qd * 512:(qd + 1) * 512], in_=bank[0:16, :],
                                                                func=AF.Exp, scale=-1.0, bias=negbf[0:16, :]),
                 reads=[bk, "negbf"], writes=["Ft"])
        P.op("act", lambda e: e.activation(out=Ft[0:16, :], in_=Ft[0:16, :], func=AF.Ln, bias=1.0), reads=["Ft"], writes=["Ft"])
        P.op("dve", lambda e: e.tensor_scalar(out=Ft[0:16, :], in0=Ft[0:16, :], scalar1=0.5, scalar2=None, op0=ALU.mult),
             reads=["Ft"], writes=["Ft"])
        P.op("dve", lambda e: e.tensor_tensor_scan(out=cumP[0:16, :], data0=Ft[0:16, :], data1=Ft[0:16, :], initial=0.0,
                                                   op0=ALU.add, op1=ALU.add), reads=["Ft"], writes=["cumP"])
        P.op("dve", lambda e: e.tensor_copy(out=cum3[0:16, 0, :], in_=cumP[0:16, :]), reads=["cumP"], writes=["cum3"])
        P.op("dve", lambda e: e.tensor_tensor(out=r1[0:16, :], in0=cumP[0:16, :], in1=cum3[0:16, 0, :], op=ALU.subtract),
             reads=["cumP", "cum3"], writes=["r1"])
        P.op("dve", lambda e: e.tensor_copy(out=cum3[0:16, 1, :], in_=r1[0:16, :]), reads=["r1"], writes=["cum3"])
        P.op("dve", lambda e: e.tensor_tensor(out=r1[0:16, :], in0=r1[0:16, :], in1=cum3[0:16, 1, :], op=ALU.subtract),
             reads=["r1", "cum3"], writes=["r1"])
        P.op("dve", lambda e: e.tensor_copy(out=cum3[0:16, 2, :], in_=r1[0:16, :]), reads=["r1"], writes=["cum3"])
        P.op("sp", lambda e: e.dma_start(out=self.cum3_d, in_=cum3[0:16, :, :]), reads=["cum3"], writes=["cum3_d"], dma=True)
        if int(os.environ.get('FOX_STOP', '99')) <= 2:
            return
        P.barrier()

        P.op("pool", lambda e: e.dma_start(out=wout[:], in_=self.dram["fox_w_out"][j].rearrange("(c p) f -> p c f", p=128)),
             writes=["wout"], dma=True)
        for hh in range(2):
            P.op("dve", lambda e, hh=hh: e.memset(QTa[hh][64:128, :], 0.0), writes=[("QTa", hh, "aug")])
            P.op("dve", lambda e, hh=hh: e.memset(KTa[hh][64:128, :], 0.0), writes=[("KTa", hh, "aug")])
            P.op("dve", lambda e, hh=hh: e.memset(QTa[hh][64:70, :], 1.0), writes=[("QTa", hh, "aug")])
            P.op("dve", lambda e, hh=hh: e.memset(KTa[hh][64:70, :], -1.0), writes=[("KTa", hh, "aug")])
        P.op("dve", lambda e: e.memset(Vaug[:, :, :, 64:65], 1.0), writes=["Vaug1"])

        def load_grp(g):
            s = g % 2
            for seg in range(4):
                P.op("pool", lambda e, seg=seg: e.dma_start(
                    out=wgrp[s][:, :, seg, :],
                    in_=w_in[j][:, seg * 1024 + g * 128: seg * 1024 + (g + 1) * 128].rearrange("(c p) f -> p c f", p=128)),
                    writes=[("wgrp", s, seg)], dma=True)

        if int(os.environ.get('FOX_STOP', '99')) <= 3:
            return
        load_grp(0)
        pt_rr = [0]
        ogv = self.Og_d.rearrange("(n p) d -> p n d", p=128)
        NG = int(os.environ.get('FOX_NG', '8'))
        for g in range(NG):
            s = g % 2
            if g + 1 < NG:
                load_grp(g + 1)
            for hh in range(2):
                head = 2 * g + hh
                for qk in range(2):
                    for qd in range(4):
                        bi = (qk * 4 + qd) % 2
                        bank = ps[bi]

                        def pm(e, hh=hh, qk=qk, qd=qd, bank=bank, s=s):
                            for c in range(8):
                                inst = e.matmul(bank[0:64, :], wgrp[s][:, c, qk, hh * 64:(hh + 1) * 64],
                                                hnT[:, c, qd * 512:(qd + 1) * 512], start=(c == 0), stop=(c == 7))
                            return inst
                        P.op("pe", pm, reads=xT_all + [("wgrp", s, qk)], writes=[("ps", bi)])
                        if qk == 0:
                            P.op("act", lambda e, hh=hh, qd=qd, bank=bank: e.activation(
                                out=QTa[hh][0:64, qd * 512:(qd + 1) * 512], in_=bank[0:64, :], func=AF.Identity, scale=0.125),
                                reads=[("ps", bi)], writes=[("QTa", hh, qd)])
                        else:
                            P.op("dve", lambda e, hh=hh, qd=qd, bank=bank: e.tensor_copy(
                                out=KTa[hh][0:64, qd * 512:(qd + 1) * 512], in_=bank[0:64, :]),
                                reads=[("ps", bi)], writes=[("KTa", hh, qd)])
                P.op("sp", lambda e, hh=hh, head=head: e.dma_start(out=QTa[hh][64:67, :], in_=self.cum3_d[head]),
                     reads=["cum3_d"], writes=[("QTa", hh, "aug")], dma=True)
                P.op("sp", lambda e, hh=hh, head=head: e.dma_start(out=KTa[hh][67:70, :], in_=self.cum3_d[head]),
                     reads=["cum3_d"], writes=[("KTa", hh, "aug")], dma=True)
            if int(os.environ.get('FOX_STOP', '99')) <= 4:
                return
            for t in range(NT):
                bi = t % 2
                bank = ps[bi]

                def vm(e, t=t, bank=bank, s=s):
                    for c in range(8):
                        inst = e.matmul(bank[:, 0:256], hnT[:, c, t * 128:(t + 1) * 128], wgrp[s][:, c, 2:4, :].rearrange("p a b -> p (a b)"),
                                        start=(c == 0), stop=(c == 7))
                    return inst
                P.op("pe", vm, reads=[("xT", t), ("wgrp", s, 2), ("wgrp", s, 3)], writes=[("ps", bi)])
                if os.environ.get("FOX_DBG", "") != "noV":
                    P.op("dve", lambda e, t=t, bank=bank: e.tensor_copy(
                        out=Vaug[:, t, :, 0:64], in_=bank[:, 0:128].rearrange("p (a b) -> p a b", a=2)),
                        reads=[("ps", bi)], writes=[("Vaug", t)])
                if os.environ.get("FOX_DBG", "") != "noG":
                    P.op("act", lambda e, t=t, bank=bank: e.activation(out=Gs[:, t, :], in_=bank[:, 128:256], func=AF.Sigmoid),
                         reads=[("ps", bi)], writes=[("Gs", t)])
            if int(os.environ.get('FOX_STOP', '99')) <= 5:
                allk = [("QTa", 0, q) for q in range(4)] + [("KTa", 0, q) for q in range(4)] + [("QTa", 0, "aug"), ("KTa", 0, "aug"), "Vaug1"] + [("Vaug", t) for t in range(NT)] + [("Gs", t) for t in range(NT)]
                def dd(dst, src):
                    P.op("dve", lambda e: e.tensor_copy(out=dst, in_=src), reads=allk, writes=[("h", i) for i in range(NT)])
                dd(h[:, 0, :], QTa[0][:, 0:1024]); dd(h[:, 1, :], QTa[0][:, 1024:2048])
                dd(h[:, 2, :], KTa[0][:, 0:1024]); dd(h[:, 3, :], KTa[0][:, 1024:2048])
                dd(h[:, 4, 0:130], Vaug[:, 0, :, :].rearrange("p a b -> p (a b)")); dd(h[:, 4, 256:384], Gs[:, 0, :])
                return
            items = []
            for hh in range(2):
                for qg in range(4):
                    for kb in range(4 * (qg + 1)):
                        items.append((hh, qg, kb))

            def rec_qk(it):
                hh, qg, kb = it
                q0 = max(0, kb - 4 * qg) * 128
                slot = pt_rr[0] % NPT
                pt_rr[0] += 1
                bi = 2 + slot % 2
                bank = ps[bi]
                P.op("pe", lambda e: e.matmul(bank[:, q0:512], KTa[hh][0:70, kb * 128:(kb + 1) * 128],
                                             QTa[hh][0:70, qg * 512 + q0:(qg + 1) * 512], start=True, stop=True),
                     reads=[("KTa", hh, kb // 4), ("KTa", hh, "aug"), ("QTa", hh, qg), ("QTa", hh, "aug")],
                     writes=[("ps", bi)])
                P.op("act", lambda e: e.activation(out=PT[slot][:, q0:512], in_=bank[:, q0:512], func=AF.Exp),
                     reads=[("ps", bi)], writes=[("PT", slot)])
                if kb >= 4 * qg:
                    P.op("dve", lambda e: e.tensor_tensor(out=PT[slot][:, q0:q0 + 128], in0=PT[slot][:, q0:q0 + 128],
                                                          in1=self.mask_b[:], op=ALU.mult),
                         reads=[("PT", slot), "mask_b"], writes=[("PT", slot)])
                return slot

            def rec_pv(it, slot):
                hh, qg, kb = it
                for qt in range(4):
                    i = 4 * qg + qt
                    if kb > i:
                        continue
                    bi = 4 + qt
                    P.op("pe", lambda e, qt=qt, i=i, bi=bi: e.matmul(
                        ps[bi][:, 0:65], PT[slot][:, qt * 128:(qt + 1) * 128], Vaug[:, kb, hh, :],
                        start=(kb == 0), stop=(kb == i)),
                        reads=[("PT", slot), ("Vaug", kb), "Vaug1"], writes=[("ps", bi)])
                    if kb == i:
                        rs = (i + hh) % 8
                        P.op("dve", lambda e, bi=bi, rs=rs: e.reciprocal(out=rden[:, rs:rs + 1], in_=ps[bi][:, 64:65]),
                             reads=[("ps", bi)], writes=[("rden", rs)])
                        P.op("dve", lambda e, bi=bi, rs=rs, i=i, s=s: e.scalar_tensor_tensor(
                            out=Og[s][:, i, hh * 64:(hh + 1) * 64], in0=ps[bi][:, 0:64], scalar=rden[:, rs:rs + 1],
                            in1=Gs[:, i, hh * 64:(hh + 1) * 64], op0=ALU.mult, op1=ALU.mult),
                            reads=[("ps", bi), ("rden", rs), ("Gs", i)], writes=[("Og", s, i)])

            slots = {}
            slots[0] = rec_qk(items[0])
            for n, it in enumerate(items):
                if n + 1 < len(items):
                    slots[n + 1] = rec_qk(items[n + 1])
                rec_pv(it, slots[n])
            P.op("sp", lambda e, g=g, s=s: e.dma_start(out=ogv[:, :, g * 128:(g + 1) * 128], in_=Og[s][:]),
                 reads=[("Og", s, i) for i in range(NT)], writes=[("Og_d", g)], dma=True)
            if int(os.environ.get('FOX_STOP', '99')) <= 6:
                allk = [("Og", s, i) for i in range(NT)]
                P.op("dve", lambda e: e.tensor_copy(out=h[:, 0, :], in_=Og[0][:, 0:8, :].rearrange("p a b -> p (a b)")),
                     reads=allk, writes=[("h", 0)])
                P.op("dve", lambda e: e.tensor_copy(out=h[:, 1, :], in_=Og[0][:, 8:16, :].rearrange("p a b -> p (a b)")),
                     reads=allk, writes=[("h", 1)])
                return

        if int(os.environ.get('FOX_STOP', '99')) <= 7:
            return
        for t in range(NT):
            b = t % 2
            P.op("sp", lambda e, t=t, b=b: e.dma_start(out=hnb[b][:], in_=ogv[:, t, :]),
                 reads=[("Og_d", g) for g in range(8)], writes=[("hnb", b)], dma=True)
            self.transpose_tile(hnb[b], ("hnb", b), hnT, t, b)
        self.out_proj(hnT, NG, wout, None)

    def ret_setup(self, layers):
        self.ret_layers = list(layers)
        nl = len(layers)
        self.din("ret_w_in", [nl, D, 6144])
        self.din("ret_gn_gain", [nl, 2048])
        self.din("ret_w_out", [nl, 2048, D])
        self.din("c_cos", [128, S])
        self.din("c_sin", [128, S])
        self.din("c_intraT", [128, 4, 128])
        self.din("c_rdec", [128, 12])
        self.Og2_d = self.dscratch("Og2_d", [S, 2048], BF16)

    def ret_layer(self, layer):
        j = self.ret_layers.index(layer)
        P, h, ps, ar = self.P, self.h, self.ps, self.ar
        w_in = self.dram["ret_w_in"]
        P.barrier()
        self.arena_reset()
        hnT = ar("r_hnT", [128, 8, S], BF16)
        hnb = [ar("r_hnb%d" % s, [128, D], BF16) for s in range(2)]
        wqk = [ar("r_wqk%d" % s, [128, 8, 512], BF16) for s in range(2)]
        wvg = ar("r_wvg", [128, 8, 1024], BF16)
        QT = ar("r_QT", [128, 2, S], BF16)
        KT = ar("r_KT", [128, 2, S], BF16)
        cosb = ar("r_cos", [128, S], BF16)
        sinb = ar("r_sin", [128, S], BF16)
        rt = [ar("r_rt%d" % s, [128, 512], F32) for s in range(4)]
        intraT = ar("r_intraT", [128, 4, 128], BF16)
        rdec = ar("r_rdec", [128, 12], F32)
        gnb = ar("r_gnb", [128, 2048], BF16)
        R32 = ar("r_R32", [128, 2, 512], F32)
        Rb = ar("r_Rb", [128, 2, 512], BF16)
        Vt = [ar("r_Vt%d" % s, [128, 512], BF16) for s in range(2)]
        Kd = [ar("r_Kd%d" % s, [128, 256], BF16) for s in range(2)]
        PTt = [ar("r_PT%d" % s, [128, 128], BF16) for s in range(2)]
        on = [ar("r_on%d" % s, [128, 512], F32) for s in range(2)]
        sg = [ar("r_sg%d" % s, [128, 512], BF16) for s in range(2)]
        ogo = [ar("r_ogo%d" % s, [128, 512], BF16) for s in range(2)]
        st6 = ar("r_st6", [128, 2, 6], F32)
        mv = ar("r_mv", [128, 2, 2], F32)
        sm = ar("r_sm", [128, 2, 4], F32)

        self.rms_stats(self.dram["norm_mix"][layer:layer + 1, :])
        P.op("pool", lambda e: e.dma_start(out=cosb[:], in_=self.dram["c_cos"]), writes=["cosb"], dma=True)
        P.op("pool", lambda e: e.dma_start(out=sinb[:], in_=self.dram["c_sin"]), writes=["sinb"], dma=True)
        P.op("pool", lambda e: e.dma_start(out=intraT[:], in_=self.dram["c_intraT"]), writes=["intraT"], dma=True)
        P.op("sp", lambda e: e.dma_start(out=rdec[:], in_=self.dram["c_rdec"]), writes=["rdec"], dma=True)
        P.op("pool", lambda e: e.dma_start(out=gnb[:], in_=self.dram["ret_gn_gain"][j:j + 1, :].partition_broadcast(128)),
             writes=["gnb"], dma=True)
        self.norm_transpose(hnT, hnb)
        xT_all = [("xT", t) for t in range(NT)]
        og2v = self.Og2_d

        def load_qk(hd):
            s = hd % 2
            for qk in range(2):
                P.op("pool", lambda e, qk=qk: e.dma_start(
                    out=wqk[s][:, :, qk * 256:(qk + 1) * 256],
                    in_=w_in[j][:, qk * 1024 + hd * 256: qk * 1024 + (hd + 1) * 256].rearrange("(c p) f -> p c f", p=128)),
                    writes=[("wqk", s, qk)], dma=True)

        def load_vg(hd):
            for vg in range(2):
                P.op("pool", lambda e, vg=vg: e.dma_start(
                    out=wvg[:, :, vg * 512:(vg + 1) * 512],
                    in_=w_in[j][:, 2048 + vg * 2048 + hd * 512: 2048 + vg * 2048 + (hd + 1) * 512].rearrange("(c p) f -> p c f", p=128)),
                    writes=[("wvg", vg)], dma=True)

        load_qk(0)
        for hd in range(4):
            s = hd % 2
            gam = 1.0 - 2.0 ** (-5.0 - hd)
            gamC = float(np.float32(np.exp(np.float32(np.log(np.float32(gam))) * np.float32(128.0))))
            load_vg(hd)
            if hd + 1 < 4:
                load_qk(hd + 1)
            for qk in range(2):
                dst = QT if qk == 0 else KT
                dname = "QT" if qk == 0 else "KT"
                sc = 1.0 if qk == 0 else 0.0625
                for qd in range(4):
                    cs = slice(qd * 512, (qd + 1) * 512)
                    for half in range(2):
                        def pm(e, half=half, qk=qk, qd=qd, s=s):
                            col = qk * 256 + half * 128
                            for c in range(8):
                                inst = e.matmul(ps[half][:, :], wqk[s][:, c, col:col + 128], hnT[:, c, qd * 512:(qd + 1) * 512],
                                                start=(c == 0), stop=(c == 7))
                            return inst
                        P.op("pe", pm, reads=xT_all + [("wqk", s, qk)], writes=[("ps", half)])
                    for k4, (bank, tab, tn) in enumerate(((0, cosb, "cosb"), (1, sinb, "sinb"), (0, sinb, "sinb"), (1, cosb, "cosb"))):
                        P.op("dve", lambda e, k4=k4, bank=bank, tab=tab, cs=cs, sc=sc: e.scalar_tensor_tensor(
                            out=rt[k4][:], in0=ps[bank][:, :], scalar=sc, in1=tab[:, cs], op0=ALU.mult, op1=ALU.mult),
                            reads=[("ps", bank), tn], writes=[("rt", k4)])
                    P.op("pool", lambda e, dst=dst, cs=cs: e.tensor_tensor(out=dst[:, 0, cs], in0=rt[0][:], in1=rt[1][:], op=ALU.subtract),
                         reads=[("rt", 0), ("rt", 1)], writes=[(dname, qd)])
                    P.op("pool", lambda e, dst=dst, cs=cs: e.tensor_tensor(out=dst[:, 1, cs], in0=rt[2][:], in1=rt[3][:], op=ALU.add),
                         reads=[("rt", 2), ("rt", 3)], writes=[(dname, qd)])
            P.op("dve", lambda e: e.memset(R32[:], 0.0), writes=["R32"])
            P.op("pool", lambda e: e.memset(Rb[:], 0.0), writes=["Rb"])
            for n in range(NT):
                b = n % 2
                cols = slice(n * 128, (n + 1) * 128)
                qdk = n // 4

                def ktr(e, cols=cols, b=b):
                    pv = ps[0][:].bitcast(BF16)
                    for c in range(2):
                        inst = e.transpose(out=pv[:, c * 128:(c + 1) * 128], in_=KT[:, c, cols], identity=self.ident_b[:])
                    return inst
                P.op("pe", ktr, reads=[("KT", qdk), "ident_b"], writes=[("ps", 0)])
                P.op("act", lambda e, b=b, hd=hd: e.activation(out=Kd[b][:], in_=ps[0][:].bitcast(BF16)[:, 0:256], func=AF.Identity,
                                                               scale=rdec[:, 8 + hd:9 + hd]),
                     reads=[("ps", 0), "rdec"], writes=[("Kd", b)])

                def smm(e, cols=cols):
                    for c in range(2):
                        inst = e.matmul(ps[1][:, 0:128], KT[:, c, cols], QT[:, c, cols], start=(c == 0), stop=(c == 1))
                    return inst
                P.op("pe", smm, reads=[("KT", qdk), ("QT", qdk)], writes=[("ps", 1)])
                P.op("dve", lambda e, b=b, hd=hd: e.tensor_tensor(out=PTt[b][:], in0=ps[1][:, 0:128], in1=intraT[:, hd, :], op=ALU.mult),
                     reads=[("ps", 1), "intraT"], writes=[("PTt", b)])

                def vmm(e, cols=cols):
                    for c in range(8):
                        inst = e.matmul(ps[2][:, :], hnT[:, c, cols], wvg[:, c, 0:512], start=(c == 0), stop=(c == 7))
                    return inst
                P.op("pe", vmm, reads=[("xT", n), ("wvg", 0)], writes=[("ps", 2)])
                P.op("act", lambda e, b=b: e.activation(out=Vt[b][:], in_=ps[2][:, :], func=AF.Copy),
                     reads=[("ps", 2)], writes=[("Vt", b)])

                def gmm(e, cols=cols):
                    for c in range(8):
                        inst = e.matmul(ps[3][:, :], hnT[:, c, cols], wvg[:, c, 512:1024], start=(c == 0), stop=(c == 7))
                    return inst
                P.op("pe", gmm, reads=[("xT", n), ("wvg", 1)], writes=[("ps", 3)])
                P.op("act", lambda e, b=b: e.activation(out=sg[b][:], in_=ps[3][:, :], func=AF.Silu),
                     reads=[("ps", 3)], writes=[("sg", b)])

                def omm(e, cols=cols, b=b, n=n):
                    inst = e.matmul(ps[4][:, :], PTt[b][:], Vt[b][:], start=True, stop=(n == 0))
                    if n > 0:
                        for c in range(2):
                            inst = e.matmul(ps[4][:, :], QT[:, c, cols], Rb[:, c, :], start=False, stop=(c == 1))
                    return inst
                P.op("pe", omm, reads=[("PTt", b), ("Vt", b), ("QT", qdk), "Rb"], writes=[("ps", 4)])
                if n < NT - 1:
                    for c in range(2):
                        P.op("pe", lambda e, c=c, b=b: e.matmul(ps[5 + c][:, :], Kd[b][:, c * 128:(c + 1) * 128], Vt[b][:],
                                                               start=True, stop=True),
                             reads=[("Kd", b), ("Vt", b)], writes=[("ps", 5 + c)])
                        P.op("dve", lambda e, c=c, gamC=gamC: e.scalar_tensor_tensor(
                            out=R32[:, c, :], in0=R32[:, c, :], scalar=gamC, in1=ps[5 + c][:, :], op0=ALU.mult, op1=ALU.add),
                            reads=[("ps", 5 + c), "R32"], writes=["R32"])
                    P.op("pool", lambda e: e.tensor_copy(out=Rb[:], in_=R32[:]), reads=["R32"], writes=["Rb"])
                P.op("dve", lambda e, b=b: e.bn_stats(out=st6[:, b, :], in_=ps[4][:, :]), reads=[("ps", 4)], writes=[("st6", b)])
                P.op("dve", lambda e, b=b: e.bn_aggr(out=mv[:, b, :], in_=st6[:, b, :]), reads=[("st6", b)], writes=[("mv", b)])
                P.op("dve", lambda e, b=b, hd=hd: e.tensor_scalar(out=sm[:, b, 0:1], in0=mv[:, b, 1:2], scalar1=rdec[:, 4 + hd:5 + hd],
                                                                 scalar2=EPS, op0=ALU.mult, op1=ALU.add),
                     reads=[("mv", b), "rdec"], writes=[("sm", b, 0)])
                P.op("act", lambda e, b=b: e.activation(out=sm[:, b, 0:1], in_=sm[:, b, 0:1], func=AF.Sqrt),
                     reads=[("sm", b, 0)], writes=[("sm", b, 0)])
                P.op("dve", lambda e, b=b: e.reciprocal(out=sm[:, b, 1:2], in_=sm[:, b, 0:1]), reads=[("sm", b, 0)], writes=[("sm", b, 1)])
                P.op("dve", lambda e, b=b, hd=hd: e.tensor_tensor(out=sm[:, b, 2:3], in0=sm[:, b, 1:2], in1=rdec[:, hd:hd + 1], op=ALU.mult),
                     reads=[("sm", b, 1), "rdec"], writes=[("sm", b, 2)])
                P.op("dve", lambda e, b=b: e.scalar_tensor_tensor(out=sm[:, b, 3:4], in0=mv[:, b, 0:1], scalar=-1.0, in1=sm[:, b, 2:3],
                                                                 op0=ALU.mult, op1=ALU.mult),
                     reads=[("mv", b), ("sm", b, 2)], writes=[("sm", b, 3)])
                P.op("act", lambda e, b=b: e.activation(out=on[b][:], in_=ps[4][:, :], func=AF.Identity,
                                                        scale=sm[:, b, 2:3], bias=sm[:, b, 3:4]),
                     reads=[("ps", 4), ("sm", b, 2), ("sm", b, 3)], writes=[("on", b)])
                P.op("pool", lambda e, b=b, hd=hd: e.tensor_tensor(out=on[b][:], in0=on[b][:], in1=gnb[:, hd * 512:(hd + 1) * 512], op=ALU.mult),
                     reads=[("on", b), "gnb"], writes=[("on", b)])
                P.op("pool", lambda e, b=b: e.tensor_tensor(out=ogo[b][:], in0=on[b][:], in1=sg[b][:], op=ALU.mult),
                     reads=[("on", b), ("sg", b)], writes=[("ogo", b)])
                P.op("sp", lambda e, b=b, n=n, hd=hd: e.dma_start(out=og2v[n * 128:(n + 1) * 128, hd * 512:(hd + 1) * 512], in_=ogo[b][:]),
                     reads=[("ogo", b)], writes=[("Og2_d", n, hd)], dma=True)

        P.barrier()
        self.arena_reset()
        wout = ar("r_wout", [128, 16, D], BF16)
        ogt = [ar("r_ogt%d" % s, [128, 2048], BF16) for s in range(2)]
        OgT = [ar("r_OgT%d" % s, [128, 16, 128], BF16) for s in range(2)]
        P.op("pool", lambda e: e.dma_start(out=wout[:], in_=self.dram["ret_w_out"][j].rearrange("(c p) f -> p c f", p=128)),
             writes=["wout"], dma=True)
        for t in range(NT):
            b = t % 2
            P.op("sp", lambda e, t=t, b=b: e.dma_start(out=ogt[b][:], in_=og2v[t * 128:(t + 1) * 128, :]),
                 writes=[("ogt", b)], dma=True)
            for half in range(2):
                def tr(e, half=half, b=b):
                    pv = ps[half][:].bitcast(BF16)
                    for c in range(8):
                        cc = half * 8 + c
                        inst = e.transpose(out=pv[:, c * 128:(c + 1) * 128], in_=ogt[b][:, cc * 128:(cc + 1) * 128],
                                           identity=self.ident_b[:])
                    return inst
                P.op("pe", tr, reads=[("ogt", b), "ident_b"], writes=[("ps", half)])
                srcv = lambda half=half: ps[half][:].bitcast(BF16).rearrange("p (c t) -> p c t", c=8)
                if half == 0:
                    P.op("act", lambda e, b=b, srcv=srcv: e.activation(out=OgT[b][:, 0:8, :], in_=srcv(), func=AF.Copy),
                         reads=[("ps", 0)], writes=[("OgT", b, 0)])
                else:
                    P.op("dve", lambda e, b=b, srcv=srcv: e.tensor_copy(out=OgT[b][:, 8:16, :], in_=srcv()),
                         reads=[("ps", 1)], writes=[("OgT", b, 1)])
            for nn in range(2):
                bi = 2 + (t * 2 + nn) % 2

                def mm(e, nn=nn, b=b, bi=bi):
                    for c in range(16):
                        inst = e.matmul(ps[bi][:, :], OgT[b][:, c, :], wout[:, c, nn * 512:(nn + 1) * 512],
                                        start=(c == 0), stop=(c == 15))
                    return inst
                P.op("pe", mm, reads=[("OgT", b, 0), ("OgT", b, 1), "wout"], writes=[("ps", bi)])
                P.op("dve", lambda e, t=t, nn=nn, bi=bi: e.tensor_tensor(
                    out=h[:, t, nn * 512:(nn + 1) * 512], in0=h[:, t, nn * 512:(nn + 1) * 512], in1=ps[bi][:, :], op=ALU.add),
                    reads=[("ps", bi), ("h", t)], writes=[("h", t)])


def build(stages=None, final_norm=True):
    B = Builder()
    B.prologue()
    if stages is None:
        stages = []
        for i in range(DEPTH):
            stages += ["mix%d" % i, "moe%d" % i]
    moe_layers = sorted(set(int(s[3:]) for s in stages if s.startswith("moe")))
    if moe_layers:
        B.moe_setup(moe_layers)
    fox_layers = sorted(set(int(s[3:]) for s in stages if s.startswith("mix") and int(s[3:]) % 2 == 0))
    if fox_layers:
        B.fox_setup(fox_layers)
    ret_layers = sorted(set(int(s[3:]) for s in stages if s.startswith("mix") and int(s[3:]) % 2 == 1))
    if ret_layers:
        B.ret_setup(ret_layers)
    B.layer_sets = {"moe": moe_layers, "fox": fox_layers, "ret": ret_layers}
    for s in stages:
        li = int(s[3:])
        if s.startswith("moe"):
            B.moe_layer(li)
        elif li % 2 == 0:
            B.fox_layer(li)
        else:
            B.ret_layer(li)
    B.epilogue(final_norm=final_norm)
    nc = B.finish()
    _LAYER_SETS[id(nc)] = B.layer_sets
    return nc


def _consts():
    ident = np.eye(128, dtype=np.float32)
    lst = np.triu(np.ones((128, 128), np.float32), k=1)
    ebase = (np.arange(NE, dtype=np.float32) * CAP).reshape(1, NE)
    mask = np.triu(np.ones((128, 128), np.float32), k=0)
    inv = (np.float32(1.0) / (np.float32(10000.0) ** np.linspace(0.0, 1.0, 128, dtype=np.float32))).astype(np.float32)
    ang = (np.arange(S, dtype=np.float32)[None, :] * inv[:, None]).astype(np.float32)
    cosT = np.cos(ang).astype(np.float32)
    sinT = np.sin(ang).astype(np.float32)
    log_g = np.log(np.float32(1.0) - np.float32(2.0) ** (np.float32(-5.0) - np.arange(4, dtype=np.float32))).astype(np.float32)
    idx = np.arange(128, dtype=np.float32)
    intraT = np.zeros((128, 4, 128), np.float32)
    rdec = np.zeros((128, 12), np.float32)
    for hd in range(4):
        col = np.exp(-log_g[hd] * (idx + 1.0)).astype(np.float32)
        intraT[:, hd, :] = np.where(idx[None, :] >= idx[:, None], col[:, None], 0.0)
        qd = np.exp(log_g[hd] * (idx + 1.0)).astype(np.float32)
        rdec[:, hd] = qd
        rdec[:, 4 + hd] = qd * qd
        rdec[:, 8 + hd] = np.exp(log_g[hd] * (127.0 - idx)).astype(np.float32)
    return {"c_ident": ident, "c_lst": lst, "c_ebase": ebase, "c_mask": mask,
            "c_cos": cosT, "c_sin": sinT, "c_intraT": intraT, "c_rdec": rdec}


_CACHE = {}


def prep_inputs(inputs, nc_inputs, layer_sets):
    f = lambda a: np.ascontiguousarray(np.asarray(a), dtype=np.float32)
    shared = {}
    shared["norm_mix"] = f(inputs["norm_mix"])
    shared["norm_ffn"] = f(inputs["norm_ffn"])
    shared["norm_final"] = f(inputs["norm_final"]).reshape(1, D)
    shared.update(_consts())
    if "router_w" in nc_inputs:
        ml = layer_sets["moe"]
        shared["router_w"] = np.ascontiguousarray(
            np.concatenate([f(inputs["router_group_w"]), f(inputs["router_expert_w"])], axis=-1)[ml])
        shared["router_b"] = np.ascontiguousarray(
            np.concatenate([f(inputs["router_group_b"]), f(inputs["router_expert_b"])], axis=-1)[ml])
        for p, l in enumerate(ml):
            shared["expert_w_gu%d" % p] = f(inputs["expert_w_gu"][l])
            shared["expert_w_down%d" % p] = f(inputs["expert_w_down"][l])
    if "fox_w_in" in nc_inputs:
        fl = [l // 2 for l in layer_sets["fox"]]
        shared["fox_w_in"] = np.ascontiguousarray(f(inputs["fox_w_in"])[fl])
        shared["fox_b_f"] = np.ascontiguousarray(f(inputs["fox_b_f"])[fl])
        shared["fox_w_out"] = np.ascontiguousarray(f(inputs["fox_w_out"])[fl])
    if "ret_w_in" in nc_inputs:
        rl = [l // 2 for l in layer_sets["ret"]]
        shared["ret_w_in"] = np.ascontiguousarray(f(inputs["ret_w_in"])[rl])
        shared["ret_gn_gain"] = np.ascontiguousarray(f(inputs["ret_gn_gain"])[rl])
        shared["ret_w_out"] = np.ascontiguousarray(f(inputs["ret_w_out"])[rl])
    x = f(inputs["x"])
    in_maps = []
    for b in range(NCORES):
        m = {k: v for k, v in shared.items() if k in nc_inputs}
        m["x"] = x[b]
        in_maps.append(m)
    return in_maps


def run(inputs, stages=None, final_norm=True, trace=False):
    key = (tuple(stages) if stages is not None else None, final_norm)
    if key not in _CACHE:
        B_nc = build(stages, final_norm)
        _CACHE[key] = B_nc
    nc = _CACHE[key]
    names = set(_DRAM_NAMES[id(nc)])
    in_maps = prep_inputs(inputs, names, _LAYER_SETS[id(nc)])
    res = run_bass_kernel_spmd(nc, in_maps, core_ids=list(range(NCORES)), trace=trace)
    out = np.stack([np.asarray(r["y"]) for r in res.results], axis=0).astype(np.float32)
    return out, res


def kernel(**inputs):
    out, _ = run(inputs)
    return out
```

```python
import contextlib
import os
import numpy as np
import concourse.bass as bass
import concourse.mybir as mybir
from concourse.bass_utils import run_bass_kernel_spmd

F32 = mybir.dt.float32
BF16 = mybir.dt.bfloat16
I32 = mybir.dt.int32
AF = mybir.ActivationFunctionType
ALU = mybir.AluOpType
AX = mybir.AxisListType

D = 1024
S = 2048
NT = S // 128
DEPTH = 4
EPS = 1e-6
NCORES = 8

ENGINES = ("pe", "act", "dve", "pool", "sp")


class _Op:
    __slots__ = ("eng", "fn", "dma", "waits", "signal", "idx")

    def __init__(self, eng, fn, dma, idx):
        self.eng = eng
        self.fn = fn
        self.dma = dma
        self.waits = []
        self.signal = None
        self.idx = idx


class Prog:
    N_DMA_SEMS = 24

    def __init__(self, same_engine_sync=True):
        self.ops = []
        self.last_writer = {}
        self.readers = {}
        self.same_engine_sync = same_engine_sync
        self.dependents = {}
        self.deps = []
        self.forced = set()
        self.pending = {e: set() for e in ENGINES}
        self.last_op = {}
        self.unfenced_dma = set()

    def op(self, eng, fn, reads=(), writes=(), dma=False, force=False):
        idx = len(self.ops)
        o = _Op(eng, fn, dma, idx)
        if force:
            self.forced.add(idx)
        ps_reads = [k for k in reads if isinstance(k, tuple) and k[0] == "ps"]
        if ps_reads:
            reads = [k for k in reads if k not in ps_reads]
            writes = list(writes) + ps_reads
        deps = set()
        for k in reads:
            w = self.last_writer.get(k)
            if w is not None:
                deps.add(w)
        for k in writes:
            w = self.last_writer.get(k)
            if w is not None:
                deps.add(w)
            for r in self.readers.get(k, ()):
                deps.add(r)
        deps |= self.pending[eng]
        self.pending[eng] = set()
        deps.discard(idx)
        self.last_op[eng] = idx
        if dma:
            self.unfenced_dma.add(idx)
        for k in writes:
            self.last_writer[k] = idx
            self.readers[k] = []
        for k in reads:
            if k not in writes:
                self.readers.setdefault(k, []).append(idx)
        self.ops.append(o)
        self.deps.append(deps)
        return idx

    def barrier(self):
        deps = set(self.last_op.values()) | self.unfenced_dma
        self.unfenced_dma = set()
        for e in ENGINES:
            self.pending[e] |= deps

    def finalize(self):
        ops = self.ops
        needed = [i in self.forced for i in range(len(ops))]
        for o in ops:
            for d in self.deps[o.idx]:
                p = ops[d]
                if p.dma:
                    needed[d] = True
                elif p.eng == o.eng and not o.dma:
                    if p.eng == "pe":
                        continue
                    if self.same_engine_sync:
                        needed[d] = True
                else:
                    needed[d] = True
        eng_cnt = {e: 0 for e in ENGINES}
        dma_cnt = [0] * self.N_DMA_SEMS
        dma_rr = 0
        seen = {e: {} for e in ENGINES}
        for o in ops:
            waits = {}
            for d in self.deps[o.idx]:
                p = ops[d]
                if p.signal is None:
                    continue
                if (not p.dma) and p.eng == o.eng and not o.dma and (p.eng == "pe" or not self.same_engine_sync):
                    continue
                k, v, _ = p.signal
                if seen[o.eng].get(k, 0) >= v:
                    continue
                waits[k] = max(waits.get(k, 0), v)
            if o.dma and needed[o.idx]:
                j = dma_rr
                dma_rr = (dma_rr + 1) % self.N_DMA_SEMS
                k = ("dma", j)
                if dma_cnt[j] > 0 and seen[o.eng].get(k, 0) < dma_cnt[j]:
                    waits[k] = max(waits.get(k, 0), dma_cnt[j])
                dma_cnt[j] += 16
                o.signal = (k, dma_cnt[j], 16)
            elif needed[o.idx]:
                eng_cnt[o.eng] += 1
                o.signal = (("eng", o.eng), eng_cnt[o.eng], 1)
            for k, v in waits.items():
                seen[o.eng][k] = max(seen[o.eng].get(k, 0), v)
            o.waits = sorted(waits.items(), key=lambda kv: str(kv[0]))
        self.max_counts = dict(eng_cnt)

    def emit(self, nc, stack, final_waits):
        sems = {}
        for e in ENGINES:
            sems[("eng", e)] = stack.enter_context(nc.semaphore("sem_" + e))
        for j in range(self.N_DMA_SEMS):
            sems[("dma", j)] = stack.enter_context(nc.semaphore("sem_dma%d" % j))
        block = stack.enter_context(nc.Block())
        by_eng = {e: [o for o in self.ops if o.eng == e] for e in ENGINES}
        last_signal = {}
        for o in self.ops:
            if o.signal is not None:
                last_signal[o.signal[0]] = max(last_signal.get(o.signal[0], 0), o.signal[1])

        def run(engine, ename):
            for o in by_eng[ename]:
                for k, v in o.waits:
                    engine.wait_ge(sems[k], v)
                inst = o.fn(engine)
                if o.signal is not None:
                    inst.then_inc(sems[o.signal[0]], o.signal[2])
            if ename == final_waits:
                for k, v in last_signal.items():
                    if k[0] == "dma":
                        engine.wait_ge(sems[k], v)

        @block.tensor
        def _(e):
            run(e, "pe")

        @block.scalar
        def _(e):
            run(e, "act")

        @block.vector
        def _(e):
            run(e, "dve")

        @block.gpsimd
        def _(e):
            run(e, "pool")

        @block.sync
        def _(e):
            run(e, "sp")


NE = 32
CAP = 256
TRASH = NE * CAP
NSLOT = TRASH + 128
FF = 512
BIG = 30000.0


_DRAM_NAMES = {}
_LAYER_SETS = {}


class Builder:
    def __init__(self):
        self.nc = nc = bass.Bass("TRN2", target_bir_lowering=False)
        self.P = Prog()
        self.stack = contextlib.ExitStack()
        self.mem_stack = contextlib.ExitStack()
        self.dram = {}
        self.ps = [self.mem_stack.enter_context(nc.psum_tensor("ps%d" % b, [128, 512], F32)) for b in range(8)]
        sb = self.sb
        self.h = sb("h", [128, NT, D], F32)
        self.gb = sb("gb", [128, D], F32)
        self.ssq = sb("ssq", [128, NT], F32)
        self.rstd = sb("rstd", [128, NT], F32)
        self.junk = sb("junk", [128, D], BF16)
        self.ident_b = sb("ident_b", [128, 128], BF16)
        self.ident_f = sb("ident_f", [128, 128], F32)
        self.lst_b = sb("lst_b", [128, 128], BF16)
        self.ones_b = sb("ones_b", [128, 128], BF16)
        self.ebase = sb("ebase", [128, NE], F32)
        self.negh = sb("negh", [128, NT], F32)
        self.arena_size = 131 * 1024
        self.arena_base, _ = nc.bump_sbuf(self.arena_size)
        self.arena_off = 0

    def sb(self, name, shape, dt):
        return self.mem_stack.enter_context(self.nc.sbuf_tensor(name, list(shape), dt))

    def arena_reset(self):
        self.arena_off = 0

    def ar(self, name, shape, dt):
        nbytes = int(np.prod(shape[1:])) * (4 if dt in (F32, I32) else 2)
        nbytes = (nbytes + 31) // 32 * 32
        assert self.arena_off + nbytes <= self.arena_size, (name, self.arena_off, nbytes)
        t = self.nc.alloc_sbuf_tensor_at(name, list(shape), dt, offset=self.arena_base + self.arena_off)
        self.arena_off += nbytes
        return t

    def din(self, name, shape, dt=F32):
        t = self.nc.dram_tensor(name, list(shape), dt, kind="ExternalInput").ap()
        self.dram[name] = t
        return t

    def dscratch(self, name, shape, dt):
        return self.nc.dram_tensor(name, list(shape), dt, kind="Internal").ap()

    def prologue(self):
        P = self.P
        x = self.din("x", [S, D])
        self.din("norm_mix", [DEPTH, D])
        self.din("norm_ffn", [DEPTH, D])
        self.din("norm_final", [1, D])
        cI = self.din("c_ident", [128, 128])
        cL = self.din("c_lst", [128, 128])
        cE = self.din("c_ebase", [1, NE])
        self.y = self.nc.dram_tensor("y", [S, D], F32, kind="ExternalOutput").ap()
        xv = x.rearrange("(n p) d -> p n d", p=128)
        h = self.h
        for q in range(4):
            sl = slice(q * 4, (q + 1) * 4)
            P.op("sp", lambda e, sl=sl: e.dma_start(out=h[:, sl, :], in_=xv[:, sl, :]),
                 writes=[("h", i) for i in range(q * 4, q * 4 + 4)], dma=True)
        P.op("sp", lambda e: e.dma_start(out=self.ident_f[:], in_=cI), writes=["ident_f"], dma=True)
        P.op("pool", lambda e: e.dma_start(out=self.ident_b[:], in_=cI), writes=["ident_b"], dma=True)
        P.op("pool", lambda e: e.dma_start(out=self.lst_b[:], in_=cL), writes=["lst_b"], dma=True)
        P.op("sp", lambda e: e.dma_start(out=self.ebase[:], in_=cE[0:1, :].partition_broadcast(128)),
             writes=["ebase"], dma=True)
        P.op("dve", lambda e: e.memset(self.ones_b[:], 1.0), writes=["ones_b"])
        P.op("dve", lambda e: e.memset(self.negh[:], -0.5), writes=["negh"])

    def rms_stats(self, gain_row_ap):
        P, h, ssq, rstd, junk, gb = self.P, self.h, self.ssq, self.rstd, self.junk, self.gb
        P.op("sp", lambda e: e.dma_start(out=gb[:], in_=gain_row_ap.partition_broadcast(128)),
             writes=["gb"], dma=True)
        for i in range(NT):
            P.op("act", lambda e, i=i: e.activation(out=junk[:], in_=h[:, i, :], func=AF.Square,
                                                     accum_out=ssq[:, i:i + 1]),
                 reads=[("h", i)], writes=["junk", ("ssq", i)])
        P.op("dve", lambda e: e.tensor_scalar(out=rstd[:], in0=ssq[:], scalar1=1.0 / D, scalar2=EPS,
                                              op0=ALU.mult, op1=ALU.add),
             reads=[("ssq", i) for i in range(NT)], writes=["rstd"])
        P.op("pool", lambda e: e.tensor_tensor(out=rstd[:], in0=rstd[:], in1=self.negh[:], op=ALU.pow),
             reads=["rstd", "negh"], writes=["rstd"])

    def epilogue(self, final_norm=True):
        P, h = self.P, self.h
        P.barrier()
        self.arena_reset()
        outt = self.ar("outt", [128, 2, D], F32)
        yv = self.y.rearrange("(n p) d -> p n d", p=128)
        if final_norm:
            self.rms_stats(self.dram["norm_final"][0:1, :])
        for i in range(NT):
            b = i % 2
            if final_norm:
                P.op("dve", lambda e, i=i, b=b: e.scalar_tensor_tensor(
                    out=outt[:, b, :], in0=h[:, i, :], scalar=self.rstd[:, i:i + 1], in1=self.gb[:],
                    op0=ALU.mult, op1=ALU.mult),
                    reads=[("h", i), "rstd", "gb"], writes=[("outt", b)])
                P.op("sp", lambda e, i=i, b=b: e.dma_start(out=yv[:, i, :], in_=outt[:, b, :]),
                     reads=[("outt", b)], writes=[("y", i)], dma=True, force=True)
            else:
                P.op("sp", lambda e, i=i: e.dma_start(out=yv[:, i, :], in_=h[:, i, :]),
                     reads=[("h", i)], writes=[("y", i)], dma=True, force=True)

    def finish(self):
        _DRAM_NAMES[id(self.nc)] = list(self.dram.keys())
        self.P.finalize()
        self.P.emit(self.nc, self.stack, final_waits="sp")
        self.stack.close()
        return self.nc

    def moe_setup(self, layers):
        self.moe_layers = list(layers)
        nl = len(self.moe_layers)
        self.din("router_w", [nl, D, 36])
        self.din("router_b", [nl, 36])
        for p in range(nl):
            self.din("expert_w_gu%d" % p, [NE, D, 2 * FF])
            self.din("expert_w_down%d" % p, [NE, FF, D])
        self.Xs = self.dscratch("Xs", [NSLOT, D], BF16)
        self.Ys = self.dscratch("Ys", [NSLOT, D], BF16)
        P = self.P
        self.arena_reset()
        self.zeros_b = self.ar("zeros_b", [128, D], BF16)
        P.op("dve", lambda e: e.memset(self.zeros_b[:], 0.0), writes=["zeros_b"])
        xsv = self.Xs.rearrange("(r p) d -> p r d", p=128)
        ysv = self.Ys.rearrange("(r p) d -> p r d", p=128)
        nr = NSLOT // 128
        for r in range(nr):
            P.op("sp", lambda e, r=r: e.dma_start(out=xsv[:, r, :], in_=self.zeros_b[:]),
                 reads=["zeros_b"], writes=[("Xs_z", r)], dma=True)
        P.op("sp", lambda e: e.dma_start(out=ysv[:, nr - 1, :], in_=self.zeros_b[:]),
             reads=["zeros_b"], writes=["Ys_z"], dma=True)

    def moe_layer(self, layer):
        li = self.moe_layers.index(layer)
        P, h, ps = self.P, self.h, self.ps
        rstd, gb = self.rstd, self.gb
        P.barrier()
        self.arena_reset()
        ar = self.ar
        wgu = [ar("wgu%d" % s, [128, 8, 2 * FF], BF16) for s in range(2)]
        wdn = [ar("wdn%d" % s, [128, 4, D], BF16) for s in range(2)]
        xg = [ar("xg%d" % s, [128, 2, D], BF16) for s in range(2)]
        xT = [ar("xT%d" % s, [128, 8, CAP], BF16) for s in range(2)]
        hT = [ar("hT%d" % s, [128, 4, CAP], BF16) for s in range(2)]
        sA = [ar("sA%d" % s, [128, CAP], F32) for s in range(2)]
        yt = [ar("yt%d" % s, [128, 2, D], BF16) for s in range(2)]
        NYG = 8
        yg = [self.nc.alloc_sbuf_tensor_at("yg%d_%d" % (layer, s), [128, D], BF16, offset=self.arena_base + s * 2048)
              for s in range(NYG)]
        hn32 = [ar("hn32_%d" % s, [128, D], F32) for s in range(2)]
        hnT32 = [ar("hnT32_%d" % s, [128, 8, 128], F32) for s in range(2)]
        NHB = 4
        hnb = [ar("hnb%d" % s, [128, D], BF16) for s in range(NHB)]
        fence = ar("fence", [128, 8], F32)
        wr32 = ar("wr32", [128, 8, 36], F32)
        brb = ar("brb", [128, 36], F32)
        LG = ar("LG", [128, NT, 36], F32)
        Em = ar("Em", [128, NT, 32], F32)
        T1 = ar("T1", [128, NT, 32], F32)
        oh1 = ar("oh1", [128, NT, 32], F32)
        oh2 = ar("oh2", [128, NT, 32], F32)
        Em2 = ar("Em2", [128, NT, 32], F32)
        RK = ar("RK", [128, NT, 32], F32)
        Obf = ar("Obf", [128, NT, 32], BF16)
        Gm = ar("Gm", [128, NT, 4], F32)
        ohG = ar("ohG", [128, NT, 4], F32)
        pen = ar("pen", [128, NT, 4], F32)
        sm = {n: ar("sm_" + n, [128, NT], F32) for n in
              ("gmax", "sumG", "pg", "m1", "m2", "r", "den", "g1", "g2", "rk", "base", "valid", "sl")}
        slot_i = ar("slot_i", [128, 2, NT], I32)

        wr = self.dram["router_w"]
        br = self.dram["router_b"]
        wgu_d = self.dram["expert_w_gu%d" % li]
        wdn_d = self.dram["expert_w_down%d" % li]
        Xs, Ys = self.Xs, self.Ys

        self.rms_stats(self.dram["norm_ffn"][layer:layer + 1, :])
        P.op("sp", lambda e: e.dma_start(out=wr32[:], in_=wr[li].rearrange("(c p) j -> p c j", p=128)),
             writes=["wr32"], dma=True)
        P.op("sp", lambda e: e.dma_start(out=brb[:], in_=br[li:li + 1, :].partition_broadcast(128)),
             writes=["brb"], dma=True)

        def load_w(e_idx):
            s = e_idx % 2
            P.op("pool", lambda e: e.dma_start(out=wgu[s][:], in_=wgu_d[e_idx].rearrange("(c p) f -> p c f", p=128)),
                 writes=[("wgu", s)], dma=True)
            P.op("pool", lambda e: e.dma_start(out=wdn[s][:], in_=wdn_d[e_idx].rearrange("(c p) f -> p c f", p=128)),
                 writes=[("wdn", s)], dma=True)

        load_w(0)
        load_w(1)

        for t in range(NT):
            b = t % 2
            P.op("dve", lambda e, t=t, b=b: e.scalar_tensor_tensor(
                out=hn32[b][:], in0=h[:, t, :], scalar=rstd[:, t:t + 1], in1=gb[:], op0=ALU.mult, op1=ALU.mult),
                reads=[("h", t), "rstd", "gb"], writes=[("hn32", b)])

            def tr(e, b=b):
                for c in range(8):
                    bank = ps[2 * b + c // 4]
                    inst = e.transpose(out=bank[:, (c % 4) * 128:(c % 4 + 1) * 128],
                                       in_=hn32[b][:, c * 128:(c + 1) * 128], identity=self.ident_f[:])
                return inst
            P.op("pe", tr, reads=[("hn32", b), "ident_f"], writes=[("ps", 2 * b), ("ps", 2 * b + 1)])
            P.op("act", lambda e, b=b: e.activation(out=hnT32[b][:, 0:4, :], in_=ps[2 * b][:].rearrange("p (c t) -> p c t", c=4),
                                                    func=AF.Copy),
                 reads=[("ps", 2 * b)], writes=[("hnT32a", b)])
            P.op("dve", lambda e, b=b: e.tensor_copy(out=hnT32[b][:, 4:8, :], in_=ps[2 * b + 1][:].rearrange("p (c t) -> p c t", c=4)),
                 reads=[("ps", 2 * b + 1)], writes=[("hnT32b", b)])

            def rl(e, b=b):
                for c in range(8):
                    inst = e.matmul(ps[4 + b][:, 0:36], hnT32[b][:, c, :], wr32[:, c, :], start=(c == 0), stop=(c == 7))
                return inst
            P.op("pe", rl, reads=[("hnT32a", b), ("hnT32b", b), "wr32"], writes=[("ps", 4 + b)])
            P.op("dve", lambda e, t=t, b=b: e.tensor_tensor(out=LG[:, t, :], in0=ps[4 + b][:, 0:36], in1=brb[:], op=ALU.add),
                 reads=[("ps", 4 + b), "brb"], writes=[("LG", t)])

        if int(os.environ.get('MOE_STOP', '99')) <= 1:
            return
        G = LG[:, :, 0:4]
        E4 = LG[:, :, 4:36].rearrange("p n (g e) -> p n g e", g=4)

        def bc(t2, n):
            return t2[:].unsqueeze(2).to_broadcast([128, NT, n])

        def dve(fn, reads, writes):
            P.op("dve", fn, reads=reads, writes=writes)

        P.op("dve", lambda e: e.memset(fence[:], 0.0), reads=[("LG", t) for t in range(NT)], writes=["LG"])
        dve(lambda e: e.tensor_reduce(out=sm["gmax"][:], in_=G, axis=AX.X, op=ALU.max), ["LG"], ["gmax"])
        dve(lambda e: e.tensor_tensor(out=Gm[:], in0=G, in1=bc(sm["gmax"], 4), op=ALU.subtract), ["LG", "gmax"], ["Gm"])
        dve(lambda e: e.tensor_single_scalar(out=ohG[:], in_=Gm[:], scalar=0.0, op=ALU.is_ge), ["Gm"], ["ohG"])
        P.op("act", lambda e: e.activation(out=Gm[:], in_=Gm[:], func=AF.Exp), reads=["Gm", "ohG"], writes=["Gm"])
        dve(lambda e: e.tensor_reduce(out=sm["sumG"][:], in_=Gm[:], axis=AX.X, op=ALU.add), ["Gm"], ["sumG"])
        dve(lambda e: e.reciprocal(out=sm["pg"][:], in_=sm["sumG"][:]), ["sumG"], ["pg"])
        dve(lambda e: e.tensor_scalar(out=pen[:], in0=ohG[:], scalar1=BIG, scalar2=-BIG, op0=ALU.mult, op1=ALU.add),
            ["ohG"], ["pen"])
        dve(lambda e: e.tensor_tensor(out=Em[:].rearrange("p n (g e) -> p n g e", g=4), in0=E4,
                                      in1=pen[:].unsqueeze(3).to_broadcast([128, NT, 4, 8]), op=ALU.add),
            ["LG", "pen"], ["Em"])
        dve(lambda e: e.tensor_reduce(out=sm["m1"][:], in_=Em[:], axis=AX.X, op=ALU.max), ["Em"], ["m1"])
        dve(lambda e: e.tensor_tensor(out=T1[:], in0=Em[:], in1=bc(sm["m1"], 32), op=ALU.subtract), ["Em", "m1"], ["T1"])
        dve(lambda e: e.tensor_single_scalar(out=oh1[:], in_=T1[:], scalar=0.0, op=ALU.is_ge), ["T1"], ["oh1"])
        dve(lambda e: e.scalar_tensor_tensor(out=Em2[:], in0=oh1[:], scalar=-BIG, in1=Em[:], op0=ALU.mult, op1=ALU.add),
            ["oh1", "Em"], ["Em2"])
        dve(lambda e: e.tensor_reduce(out=sm["m2"][:], in_=Em2[:], axis=AX.X, op=ALU.max), ["Em2"], ["m2"])
        dve(lambda e: e.tensor_tensor(out=T1[:], in0=Em2[:], in1=bc(sm["m2"], 32), op=ALU.subtract), ["Em2", "m2"], ["T1"])
        dve(lambda e: e.tensor_single_scalar(out=oh2[:], in_=T1[:], scalar=0.0, op=ALU.is_ge), ["T1"], ["oh2"])
        dve(lambda e: e.tensor_tensor(out=sm["r"][:], in0=sm["m2"][:], in1=sm["m1"][:], op=ALU.subtract), ["m1", "m2"], ["r"])
        P.op("act", lambda e: e.activation(out=sm["r"][:], in_=sm["r"][:], func=AF.Exp), reads=["r"], writes=["r"])
        dve(lambda e: e.tensor_scalar(out=sm["den"][:], in0=sm["r"][:], scalar1=1.0, scalar2=None, op0=ALU.add), ["r"], ["den"])
        dve(lambda e: e.reciprocal(out=sm["den"][:], in_=sm["den"][:]), ["den"], ["den"])
        dve(lambda e: e.tensor_tensor(out=sm["g1"][:], in0=sm["pg"][:], in1=sm["den"][:], op=ALU.mult), ["pg", "den"], ["g1"])
        dve(lambda e: e.tensor_tensor(out=sm["g2"][:], in0=sm["g1"][:], in1=sm["r"][:], op=ALU.mult), ["g1", "r"], ["g2"])
        dve(lambda e: e.tensor_tensor(out=Obf[:], in0=oh1[:], in1=oh2[:], op=ALU.add), ["oh1", "oh2"], ["Obf"])

        if int(os.environ.get('MOE_STOP', '99')) <= 2:
            return
        def ranks(e):
            for t in range(NT):
                o = ps[7][:, t * 32:(t + 1) * 32]
                inst = e.matmul(o, self.lst_b[:], Obf[:, t, :], start=True, stop=(t == 0))
                for j in range(t):
                    inst = e.matmul(o, self.ones_b[:], Obf[:, j, :], start=False, stop=(j == t - 1))
            return inst
        P.op("pe", ranks, reads=["Obf", "lst_b", "ones_b"], writes=[("ps", 7)])
        dve(lambda e: e.tensor_copy(out=RK[:], in_=ps[7][:].rearrange("p (n e) -> p n e", e=32)), [("ps", 7)], ["RK"])
        ebc = self.ebase[:].unsqueeze(1).to_broadcast([128, NT, 32])
        for k, (oh, ohn, gn) in enumerate(((oh1, "oh1", "g1"), (oh2, "oh2", "g2"))):
            dve(lambda e, oh=oh: e.tensor_tensor(out=T1[:], in0=oh[:], in1=RK[:], op=ALU.mult), [ohn, "RK"], ["T1"])
            dve(lambda e: e.tensor_reduce(out=sm["rk"][:], in_=T1[:], axis=AX.X, op=ALU.add), ["T1"], ["rk"])
            dve(lambda e, oh=oh: e.tensor_tensor(out=T1[:], in0=oh[:], in1=ebc, op=ALU.mult), [ohn, "ebase"], ["T1"])
            dve(lambda e: e.tensor_reduce(out=sm["base"][:], in_=T1[:], axis=AX.X, op=ALU.add), ["T1"], ["base"])
            dve(lambda e: e.tensor_single_scalar(out=sm["valid"][:], in_=sm["rk"][:], scalar=float(CAP), op=ALU.is_lt),
                ["rk"], ["valid"])
            dve(lambda e: e.scalar_tensor_tensor(out=sm["sl"][:], in0=sm["rk"][:], scalar=-float(TRASH), in1=sm["base"][:],
                                                 op0=ALU.add, op1=ALU.add), ["rk", "base"], ["sl"])
            dve(lambda e: e.tensor_tensor(out=sm["sl"][:], in0=sm["sl"][:], in1=sm["valid"][:], op=ALU.mult), ["sl", "valid"], ["sl"])
            dve(lambda e: e.tensor_scalar(out=sm["sl"][:], in0=sm["sl"][:], scalar1=float(TRASH), scalar2=None, op0=ALU.add),
                ["sl"], ["sl"])
            dve(lambda e, k=k: e.tensor_copy(out=slot_i[:, k, :], in_=sm["sl"][:]), ["sl"], [("slot", k)])
            dve(lambda e, gn=gn: e.tensor_tensor(out=sm[gn][:], in0=sm[gn][:], in1=sm["valid"][:], op=ALU.mult),
                [gn, "valid"], [gn])

        if int(os.environ.get('MOE_STOP', '99')) <= 3:
            return
        for t in range(NT):
            b = t % NHB
            P.op("dve", lambda e, t=t, b=b: e.scalar_tensor_tensor(
                out=hnb[b][:], in0=h[:, t, :], scalar=rstd[:, t:t + 1], in1=gb[:], op0=ALU.mult, op1=ALU.mult),
                reads=[("h", t), "rstd", "gb"], writes=[("hnb", b)])
            for k in range(2):
                P.op("pool", lambda e, t=t, b=b, k=k: e.indirect_dma_start(
                    out=Xs[:, :], out_offset=bass.IndirectOffsetOnAxis(ap=slot_i[:, k, t:t + 1], axis=0),
                    in_=hnb[b][:], in_offset=None),
                    reads=[("hnb", b), ("slot", k)], writes=[("Xs_w", t, k)], dma=True)

        if int(os.environ.get('MOE_STOP', '99')) <= 4:
            dbg = [sm["sl"], sm["g1"], sm["g2"], sm["rk"], sm["base"], sm["valid"]]
            for q, tl in enumerate(dbg):
                P.op("dve", lambda e, q=q, tl=tl: e.tensor_copy(out=h[:, 0, q * 16:(q + 1) * 16], in_=tl[:]),
                     reads=["sl", "g1", "g2", "rk", "base", "valid"], writes=[("h", 0)])
            for k in range(2):
                P.op("dve", lambda e, k=k: e.tensor_copy(out=h[:, 0, 96 + k * 16:112 + k * 16], in_=slot_i[:, k, :]),
                     reads=[("slot", k)], writes=[("h", 0)])
            P.op("dve", lambda e: e.tensor_copy(out=h[:, 1, 0:576], in_=LG[:].rearrange("p n j -> p (n j)")),
                 reads=["LG"], writes=[("h", 1)])
            return
        xs_keys = [("Xs_w", t, k) for t in range(NT) for k in range(2)]

        def load_x(ex):
            s = ex % 2
            P.op("sp", lambda e: e.dma_start(
                out=xg[s][:], in_=Xs[ex * CAP:(ex + 1) * CAP, :].rearrange("(r p) d -> p r d", p=128)),
                reads=xs_keys, writes=[("xg", s)], dma=True)

        load_x(0)
        for ex in range(NE):
            s = ex % 2
            if ex + 1 < NE:
                load_x(ex + 1)
            for r in range(2):
                bank = ps[r]

                def tr2(e, r=r, s=s, bank=bank):
                    pv = bank[:].bitcast(BF16)
                    for c in range(8):
                        inst = e.transpose(out=pv[:, c * 128:(c + 1) * 128], in_=xg[s][:, r, c * 128:(c + 1) * 128],
                                           identity=self.ident_b[:])
                    return inst
                P.op("pe", tr2, reads=[("xg", s), "ident_b"], writes=[("ps", r)])
                src = lambda bank=bank: bank[:].bitcast(BF16).rearrange("p (c t) -> p c t", c=8)
                if r == 0:
                    P.op("act", lambda e, s=s, src=src: e.activation(out=xT[s][:, :, 0:128], in_=src(), func=AF.Copy),
                         reads=[("ps", 0)], writes=[("xT", s, 0)])
                else:
                    P.op("dve", lambda e, s=s, src=src: e.tensor_copy(out=xT[s][:, :, 128:256], in_=src()),
                         reads=[("ps", 1)], writes=[("xT", s, 1)])
            for m in range(4):
                bank = ps[2 + m % 2]
                bk = ("ps", 2 + m % 2)

                def gu(e, m=m, s=s, bank=bank):
                    for half in range(2):
                        col = half * FF + m * 128
                        for c in range(8):
                            inst = e.matmul(bank[:, half * CAP:(half + 1) * CAP], wgu[s][:, c, col:col + 128],
                                            xT[s][:, c, :], start=(c == 0), stop=(c == 7))
                    return inst
                P.op("pe", gu, reads=[("wgu", s), ("xT", s, 0), ("xT", s, 1)], writes=[bk])
                P.op("act", lambda e, m=m, bank=bank: e.activation(out=sA[m % 2][:], in_=bank[:, 0:CAP], func=AF.Silu),
                     reads=[bk], writes=[("sA", m % 2)])
                P.op("dve", lambda e, m=m, s=s, bank=bank: e.tensor_tensor(out=hT[s][:, m, :], in0=sA[m % 2][:],
                                                                          in1=bank[:, CAP:2 * CAP], op=ALU.mult),
                     reads=[bk, ("sA", m % 2)], writes=[("hT", s, m)])
            for r in range(2):
                for n in range(2):
                    q = r * 2 + n
                    bank = ps[4 + q % 2]
                    bk = ("ps", 4 + q % 2)

                    def dn(e, r=r, n=n, s=s, bank=bank):
                        for m in range(4):
                            inst = e.matmul(bank[:, :], hT[s][:, m, r * 128:(r + 1) * 128],
                                            wdn[s][:, m, n * 512:(n + 1) * 512], start=(m == 0), stop=(m == 3))
                        return inst
                    P.op("pe", dn, reads=[("wdn", s)] + [("hT", s, m) for m in range(4)], writes=[bk])
                    if q % 2 == 0:
                        P.op("act", lambda e, r=r, n=n, s=s, bank=bank: e.activation(
                            out=yt[s][:, r, n * 512:(n + 1) * 512], in_=bank[:, :], func=AF.Copy),
                            reads=[bk], writes=[("yt", s, q)])
                    else:
                        P.op("dve", lambda e, r=r, n=n, s=s, bank=bank: e.tensor_copy(
                            out=yt[s][:, r, n * 512:(n + 1) * 512], in_=bank[:, :]),
                            reads=[bk], writes=[("yt", s, q)])
            P.op("sp", lambda e, ex=ex, s=s: e.dma_start(
                out=Ys[ex * CAP:(ex + 1) * CAP, :].rearrange("(r p) d -> p r d", p=128), in_=yt[s][:]),
                reads=[("yt", s, q) for q in range(4)], writes=[("Ys_w", ex)], dma=True)
            if ex + 2 < NE:
                load_w(ex + 2)

        if int(os.environ.get('MOE_STOP', '99')) <= 5:
            return
        P.op("pool", lambda e: e.memset(fence[:], 0.0), writes=[("wgu", 0)] + [("yg", b) for b in range(NYG)])
        for t in range(NT):
            for k, gn in enumerate(("g1", "g2")):
                b = (t * 2 + k) % NYG
                P.op("pool", lambda e, t=t, b=b, k=k: e.indirect_dma_start(
                    out=yg[b][:], out_offset=None, in_=Ys[:, :],
                    in_offset=bass.IndirectOffsetOnAxis(ap=slot_i[:, k, t:t + 1], axis=0)),
                    reads=[("Ys_w", ex) for ex in range(NE)] + [("slot", k)], writes=[("yg", b)], dma=True)
                P.op("dve", lambda e, t=t, b=b, gn=gn: e.scalar_tensor_tensor(
                    out=h[:, t, :], in0=yg[b][:], scalar=sm[gn][:, t:t + 1], in1=h[:, t, :], op0=ALU.mult, op1=ALU.add),
                    reads=[("yg", b), gn, ("h", t)], writes=[("h", t)])

    def norm_transpose(self, hnT, hnb):
        P, h, ps = self.P, self.h, self.ps
        for t in range(NT):
            b = t % 2
            P.op("dve", lambda e, t=t, b=b: e.scalar_tensor_tensor(
                out=hnb[b][:], in0=h[:, t, :], scalar=self.rstd[:, t:t + 1], in1=self.gb[:], op0=ALU.mult, op1=ALU.mult),
                reads=[("h", t), "rstd", "gb"], writes=[("hnb", b)])
            self.transpose_tile(hnb[b], ("hnb", b), hnT, t, b)

    def transpose_tile(self, src, src_key, dstT, t, b):
        P, ps = self.P, self.ps
        bank = ps[b]

        def tr(e):
            pv = bank[:].bitcast(BF16)
            for c in range(8):
                inst = e.transpose(out=pv[:, c * 128:(c + 1) * 128], in_=src[:, c * 128:(c + 1) * 128],
                                   identity=self.ident_b[:])
            return inst
        P.op("pe", tr, reads=[src_key, "ident_b"], writes=[("ps", b)])
        srcv = lambda: bank[:].bitcast(BF16).rearrange("p (c t) -> p c t", c=8)
        if b == 0:
            P.op("act", lambda e: e.activation(out=dstT[:, :, t * 128:(t + 1) * 128], in_=srcv(), func=AF.Copy),
                 reads=[("ps", b)], writes=[("xT", t)])
        else:
            P.op("dve", lambda e: e.tensor_copy(out=dstT[:, :, t * 128:(t + 1) * 128], in_=srcv()),
                 reads=[("ps", b)], writes=[("xT", t)])

    def out_proj(self, xT, kc, wout, n_k):
        P, h, ps = self.P, self.h, self.ps
        for t in range(NT):
            for n in range(2):
                bi = 2 + (t * 2 + n) % 2
                bank = ps[bi]

                def mm(e, t=t, n=n, bank=bank):
                    for c in range(kc):
                        inst = e.matmul(bank[:, :], xT[:, c, t * 128:(t + 1) * 128], wout[:, c, n * 512:(n + 1) * 512],
                                        start=(c == 0), stop=(c == kc - 1))
                    return inst
                P.op("pe", mm, reads=[("xT", t), "wout"] if n_k is None else n_k(t) + ["wout"], writes=[("ps", bi)])
                P.op("dve", lambda e, t=t, n=n, bank=bank: e.tensor_tensor(
                    out=h[:, t, n * 512:(n + 1) * 512], in0=h[:, t, n * 512:(n + 1) * 512], in1=bank[:, :], op=ALU.add),
                    reads=[("ps", bi), ("h", t)], writes=[("h", t)])

    def fox_setup(self, layers):
        self.fox_layers = list(layers)
        nl = len(layers)
        self.din("fox_w_in", [nl, D, 4112])
        self.din("fox_b_f", [nl, 16])
        self.din("fox_w_out", [nl, D, D])
        self.din("c_mask", [128, 128])
        self.cum3_d = self.dscratch("cum3_d", [16, 3, S], BF16)
        self.Og_d = self.dscratch("Og_d", [S, D], BF16)
        self.mask_b = self.sb("mask_b", [128, 128], BF16)
        self.P.op("pool", lambda e: e.dma_start(out=self.mask_b[:], in_=self.dram["c_mask"]), writes=["mask_b"], dma=True)

    def fox_layer(self, layer):
        j = self.fox_layers.index(layer)
        P, h, ps, ar = self.P, self.h, self.ps, self.ar
        w_in = self.dram["fox_w_in"]
        P.barrier()
        self.arena_reset()
        hnT = ar("f_hnT", [128, 8, S], BF16)
        hnb = [ar("f_hnb%d" % s, [128, D], BF16) for s in range(2)]
        wf = ar("f_wf", [128, 8, 16], BF16)
        bft = ar("f_bft", [128, 1], F32)
        negbf = ar("f_negbf", [128, 1], F32)
        save = self.arena_off
        Ft = ar("f_Ft", [128, S], F32)
        cumP = ar("f_cumP", [128, S], F32)
        r1 = ar("f_r1", [128, S], F32)
        cum3 = ar("f_cum3", [128, 3, S], BF16)
        self.arena_off = save
        wgrp = [ar("f_wgrp%d" % s, [128, 8, 4, 128], BF16) for s in range(2)]
        QTa = [ar("f_QTa%d" % s, [128, S], BF16) for s in range(2)]
        KTa = [ar("f_KTa%d" % s, [128, S], BF16) for s in range(2)]
        Tst = [ar("f_Tst%d" % s, [128, S], BF16) for s in range(2)]
        Vaug = ar("f_Vaug", [128, NT, 2, 65], BF16)
        Gs = ar("f_Gs", [128, NT, 128], BF16)
        NPT = 6
        PT = [ar("f_PT%d" % s, [128, 512], BF16) for s in range(NPT)]
        Og = [ar("f_Og%d" % s, [128, NT, 128], BF16) for s in range(2)]
        wout = ar("f_wout", [128, 8, D], BF16)
        rden = ar("f_rden", [128, 8], F32)

        self.rms_stats(self.dram["norm_mix"][layer:layer + 1, :])
        P.op("pool", lambda e: e.dma_start(out=wf[:], in_=w_in[j][:, 4096:4112].rearrange("(c p) f -> p c f", p=128)),
             writes=["wf"], dma=True)
        P.op("sp", lambda e: e.dma_start(out=bft[0:16, :], in_=self.dram["fox_b_f"][j].rearrange("(h o) -> h o", o=1)),
             writes=["bft"], dma=True)
        P.op("dve", lambda e: e.tensor_scalar(out=negbf[0:16, :], in0=bft[0:16, :], scalar1=-1.0, scalar2=None, op0=ALU.mult),
             reads=["bft"], writes=["negbf"])
        self.norm_transpose(hnT, hnb)
        xT_all = [("xT", t) for t in range(NT)]

        if int(os.environ.get('FOX_STOP', '99')) <= 1:
            return
        for qd in range(4):
            bank = ps[2 + qd % 2]
            bk = ("ps", 2 + qd % 2)

            def fm(e, qd=qd, bank=bank):
                for c in range(8):
                    inst = e.matmul(bank[0:16, :], wf[:, c, :], hnT[:, c, qd * 512:(qd + 1) * 512], start=(c == 0), stop=(c == 7))
                return inst
            P.op("pe", fm, reads=xT_all + ["wf"], writes=[bk])
            P.op("act", lambda e, qd=qd, bank=bank: e.activation(out=Ft[0:16, qd * 512:(qd + 1) * 512], in_=bank[0:16, :],
                                                                func=AF.Exp, scale=-1.0, bias=negbf[0:16, :]),
                 reads=[bk, "negbf"], writes=["Ft"])
        P.op("act", lambda e: e.activation(out=Ft[0:16, :], in_=Ft[0:16, :], func=AF.Ln, bias=1.0), reads=["Ft"], writes=["Ft"])
        P.op("dve", lambda e: e.tensor_scalar(out=Ft[0:16, :], in0=Ft[0:16, :], scalar1=0.5, scalar2=None, op0=ALU.mult),
             reads=["Ft"], writes=["Ft"])
        P.op("dve", lambda e: e.tensor_tensor_scan(out=cumP[0:16, :], data0=Ft[0:16, :], data1=Ft[0:16, :], initial=0.0,
                                                   op0=ALU.add, op1=ALU.add), reads=["Ft"], writes=["cumP"])
        P.op("dve", lambda e: e.tensor_copy(out=cum3[0:16, 0, :], in_=cumP[0:16, :]), reads=["cumP"], writes=["cum3"])
        P.op("dve", lambda e: e.tensor_tensor(out=r1[0:16, :], in0=cumP[0:16, :], in1=cum3[0:16, 0, :], op=ALU.subtract),
             reads=["cumP", "cum3"], writes=["r1"])
        P.op("dve", lambda e: e.tensor_copy(out=cum3[0:16, 1, :], in_=r1[0:16, :]), reads=["r1"], writes=["cum3"])
        P.op("dve", lambda e: e.tensor_tensor(out=r1[0:16, :], in0=r1[0:16, :], in1=cum3[0:16, 1, :], op=ALU.subtract),
             reads=["r1", "cum3"], writes=["r1"])
        P.op("dve", lambda e: e.tensor_copy(out=cum3[0:16, 2, :], in_=r1[0:16, :]), reads=["r1"], writes=["cum3"])
        P.op("sp", lambda e: e.dma_start(out=self.cum3_d, in_=cum3[0:16, :, :]), reads=["cum3"], writes=["cum3_d"], dma=True)
        if int(os.environ.get('FOX_STOP', '99')) <= 2:
            return
        P.barrier()

        P.op("pool", lambda e: e.dma_start(out=wout[:], in_=self.dram["fox_w_out"][j].rearrange("(c p) f -> p c f", p=128)),
             writes=["wout"], dma=True)
        for hh in range(2):
            P.op("dve", lambda e, hh=hh: e.memset(QTa[hh][64:128, :], 0.0), writes=[("QTa", hh, "aug")])
            P.op("dve", lambda e, hh=hh: e.memset(KTa[hh][64:128, :], 0.0), writes=[("KTa", hh, "aug")])
            P.op("dve", lambda e, hh=hh: e.memset(QTa[hh][64:70, :], 1.0), writes=[("QTa", hh, "aug")])
            P.op("dve", lambda e, hh=hh: e.memset(KTa[hh][64:70, :], -1.0), writes=[("KTa", hh, "aug")])
        P.op("dve", lambda e: e.memset(Vaug[:, :, :, 64:65], 1.0), writes=["Vaug1"])

        def load_grp(g):
            s = g % 2
            for seg in range(4):
                P.op("pool", lambda e, seg=seg: e.dma_start(
                    out=wgrp[s][:, :, seg, :],
                    in_=w_in[j][:, seg * 1024 + g * 128: seg * 1024 + (g + 1) * 128].rearrange("(c p) f -> p c f", p=128)),
                    writes=[("wgrp", s, seg)], dma=True)

        if int(os.environ.get('FOX_STOP', '99')) <= 3:
            return
        load_grp(0)
        pt_rr = [0]
        ogv = self.Og_d.rearrange("(n p) d -> p n d", p=128)
        NG = int(os.environ.get('FOX_NG', '8'))
        for g in range(NG):
            s = g % 2
            if g + 1 < NG:
                load_grp(g + 1)
            for qk in range(2):
                dstT = QTa if qk == 0 else KTa
                dn = "QTa" if qk == 0 else "KTa"
                for qd in range(4):
                    bi = (qk * 4 + qd) % 2
                    bank = ps[bi]
                    cs = slice(qd * 512, (qd + 1) * 512)

                    def pm(e, qk=qk, qd=qd, bank=bank, s=s):
                        for c in range(8):
                            inst = e.matmul(bank[:, :], wgrp[s][:, c, qk, :],
                                            hnT[:, c, qd * 512:(qd + 1) * 512], start=(c == 0), stop=(c == 7))
                        return inst
                    P.op("pe", pm, reads=xT_all + [("wgrp", s, qk)], writes=[("ps", bi)])
                    if qk == 0:
                        P.op("act", lambda e, cs=cs, bank=bank: e.activation(
                            out=QTa[0][0:64, cs], in_=bank[0:64, :], func=AF.Identity, scale=0.125),
                            reads=[("ps", bi)], writes=[("QTa", 0, qd)])
                        P.op("act", lambda e, cs=cs, bank=bank: e.activation(
                            out=Tst[0][64:128, cs], in_=bank[64:128, :], func=AF.Identity, scale=0.125),
                            reads=[("ps", bi)], writes=[("Tst", 0, qd)])
                    else:
                        P.op("dve", lambda e, cs=cs, bank=bank: e.tensor_copy(out=KTa[0][0:64, cs], in_=bank[0:64, :]),
                             reads=[("ps", bi)], writes=[("KTa", 0, qd)])
                        P.op("dve", lambda e, cs=cs, bank=bank: e.tensor_copy(out=Tst[1][64:128, cs], in_=bank[64:128, :]),
                             reads=[("ps", bi)], writes=[("Tst", 1, qd)])
                P.op("sp", lambda e, qk=qk, dstT=dstT: e.dma_start(out=dstT[1][0:64, :], in_=Tst[qk][64:128, :]),
                     reads=[("Tst", qk, qd) for qd in range(4)], writes=[(dn, 1, qd) for qd in range(4)], dma=True)
            for hh in range(2):
                head = 2 * g + hh
                P.op("sp", lambda e, hh=hh, head=head: e.dma_start(out=QTa[hh][64:67, :], in_=self.cum3_d[head]),
                     reads=["cum3_d"], writes=[("QTa", hh, "aug")], dma=True)
                P.op("sp", lambda e, hh=hh, head=head: e.dma_start(out=KTa[hh][67:70, :], in_=self.cum3_d[head]),
                     reads=["cum3_d"], writes=[("KTa", hh, "aug")], dma=True)
            if int(os.environ.get('FOX_STOP', '99')) <= 4:
                return
            for t in range(NT):
                bi = t % 2
                bank = ps[bi]

                def vm(e, t=t, bank=bank, s=s):
                    for c in range(8):
                        inst = e.matmul(bank[:, 0:256], hnT[:, c, t * 128:(t + 1) * 128], wgrp[s][:, c, 2:4, :].rearrange("p a b -> p (a b)"),
                                        start=(c == 0), stop=(c == 7))
                    return inst
                P.op("pe", vm, reads=[("xT", t), ("wgrp", s, 2), ("wgrp", s, 3)], writes=[("ps", bi)])
                if os.environ.get("FOX_DBG", "") != "noV":
                    P.op("dve", lambda e, t=t, bank=bank: e.tensor_copy(
                        out=Vaug[:, t, :, 0:64], in_=bank[:, 0:128].rearrange("p (a b) -> p a b", a=2)),
                        reads=[("ps", bi)], writes=[("Vaug", t)])
                if os.environ.get("FOX_DBG", "") != "noG":
                    P.op("act", lambda e, t=t, bank=bank: e.activation(out=Gs[:, t, :], in_=bank[:, 128:256], func=AF.Sigmoid),
                         reads=[("ps", bi)], writes=[("Gs", t)])
            if int(os.environ.get('FOX_STOP', '99')) <= 5:
                allk = [("QTa", 0, q) for q in range(4)] + [("KTa", 0, q) for q in range(4)] + [("QTa", 0, "aug"), ("KTa", 0, "aug"), "Vaug1"] + [("Vaug", t) for t in range(NT)] + [("Gs", t) for t in range(NT)]
                def dd(dst, src):
                    P.op("dve", lambda e: e.tensor_copy(out=dst, in_=src), reads=allk, writes=[("h", i) for i in range(NT)])
                dd(h[:, 0, :], QTa[0][:, 0:1024]); dd(h[:, 1, :], QTa[0][:, 1024:2048])
                dd(h[:, 2, :], KTa[0][:, 0:1024]); dd(h[:, 3, :], KTa[0][:, 1024:2048])
                dd(h[:, 4, 0:130], Vaug[:, 0, :, :].rearrange("p a b -> p (a b)")); dd(h[:, 4, 256:384], Gs[:, 0, :])
                return
            items = []
            for hh in range(2):
                for qg in range(4):
                    for kb in range(4 * (qg + 1)):
                        items.append((hh, qg, kb))

            def rec_qk(it):
                hh, qg, kb = it
                q0 = max(0, kb - 4 * qg) * 128
                slot = pt_rr[0] % NPT
                pt_rr[0] += 1
                bi = 2 + slot % 2
                bank = ps[bi]
                P.op("pe", lambda e: e.matmul(bank[:, q0:512], KTa[hh][0:70, kb * 128:(kb + 1) * 128],
                                             QTa[hh][0:70, qg * 512 + q0:(qg + 1) * 512], start=True, stop=True),
                     reads=[("KTa", hh, kb // 4), ("KTa", hh, "aug"), ("QTa", hh, qg), ("QTa", hh, "aug")],
                     writes=[("ps", bi)])
                P.op("act", lambda e: e.activation(out=PT[slot][:, q0:512], in_=bank[:, q0:512], func=AF.Exp),
                     reads=[("ps", bi)], writes=[("PT", slot)])
                if kb >= 4 * qg:
                    P.op("dve", lambda e: e.tensor_tensor(out=PT[slot][:, q0:q0 + 128], in0=PT[slot][:, q0:q0 + 128],
                                                          in1=self.mask_b[:], op=ALU.mult),
                         reads=[("PT", slot), "mask_b"], writes=[("PT", slot)])
                return slot

            def rec_pv(it, slot):
                hh, qg, kb = it
                for qt in range(4):
                    i = 4 * qg + qt
                    if kb > i:
                        continue
                    bi = 4 + qt
                    P.op("pe", lambda e, qt=qt, i=i, bi=bi: e.matmul(
                        ps[bi][:, 0:65], PT[slot][:, qt * 128:(qt + 1) * 128], Vaug[:, kb, hh, :],
                        start=(kb == 0), stop=(kb == i)),
                        reads=[("PT", slot), ("Vaug", kb), "Vaug1"], writes=[("ps", bi)])
                    if kb == i:
                        rs = (i + hh) % 8
                        P.op("dve", lambda e, bi=bi, rs=rs: e.reciprocal(out=rden[:, rs:rs + 1], in_=ps[bi][:, 64:65]),
                             reads=[("ps", bi)], writes=[("rden", rs)])
                        P.op("dve", lambda e, bi=bi, rs=rs, i=i, s=s: e.scalar_tensor_tensor(
                            out=Og[s][:, i, hh * 64:(hh + 1) * 64], in0=ps[bi][:, 0:64], scalar=rden[:, rs:rs + 1],
                            in1=Gs[:, i, hh * 64:(hh + 1) * 64], op0=ALU.mult, op1=ALU.mult),
                            reads=[("ps", bi), ("rden", rs), ("Gs", i)], writes=[("Og", s, i)])

            slots = {}
            slots[0] = rec_qk(items[0])
            for n, it in enumerate(items):
                if n + 1 < len(items):
                    slots[n + 1] = rec_qk(items[n + 1])
                rec_pv(it, slots[n])
            P.op("sp", lambda e, g=g, s=s: e.dma_start(out=ogv[:, :, g * 128:(g + 1) * 128], in_=Og[s][:]),
                 reads=[("Og", s, i) for i in range(NT)], writes=[("Og_d", g)], dma=True)
            if int(os.environ.get('FOX_STOP', '99')) <= 6:
                allk = [("Og", s, i) for i in range(NT)]
                P.op("dve", lambda e: e.tensor_copy(out=h[:, 0, :], in_=Og[0][:, 0:8, :].rearrange("p a b -> p (a b)")),
                     reads=allk, writes=[("h", 0)])
                P.op("dve", lambda e: e.tensor_copy(out=h[:, 1, :], in_=Og[0][:, 8:16, :].rearrange("p a b -> p (a b)")),
                     reads=allk, writes=[("h", 1)])
                return

        if int(os.environ.get('FOX_STOP', '99')) <= 7:
            return
        for t in range(NT):
            b = t % 2
            P.op("sp", lambda e, t=t, b=b: e.dma_start(out=hnb[b][:], in_=ogv[:, t, :]),
                 reads=[("Og_d", g) for g in range(8)], writes=[("hnb", b)], dma=True)
            self.transpose_tile(hnb[b], ("hnb", b), hnT, t, b)
        self.out_proj(hnT, NG, wout, None)

    def ret_setup(self, layers):
        self.ret_layers = list(layers)
        nl = len(layers)
        self.din("ret_w_in", [nl, D, 6144])
        self.din("ret_gn_gain", [nl, 2048])
        self.din("ret_w_out", [nl, 2048, D])
        self.din("c_cos", [128, S])
        self.din("c_sin", [128, S])
        self.din("c_intraT", [128, 4, 128])
        self.din("c_rdec", [128, 12])
        self.Og2_d = self.dscratch("Og2_d", [S, 2048], BF16)

    def ret_layer(self, layer):
        j = self.ret_layers.index(layer)
        P, h, ps, ar = self.P, self.h, self.ps, self.ar
        w_in = self.dram["ret_w_in"]
        P.barrier()
        self.arena_reset()
        hnT = ar("r_hnT", [128, 8, S], BF16)
        hnb = [ar("r_hnb%d" % s, [128, D], BF16) for s in range(2)]
        wqk = [ar("r_wqk%d" % s, [128, 8, 512], BF16) for s in range(2)]
        wvg = ar("r_wvg", [128, 8, 1024], BF16)
        QT = ar("r_QT", [128, 2, S], BF16)
        KT = ar("r_KT", [128, 2, S], BF16)
        cosb = ar("r_cos", [128, S], BF16)
        sinb = ar("r_sin", [128, S], BF16)
        rt = [ar("r_rt%d" % s, [128, 512], F32) for s in range(4)]
        intraT = ar("r_intraT", [128, 4, 128], BF16)
        rdec = ar("r_rdec", [128, 12], F32)
        R32 = ar("r_R32", [128, 2, 512], F32)
        Rb = [ar("r_Rb%d" % s, [128, 2, 512], BF16) for s in range(2)]
        Vt = [ar("r_Vt%d" % s, [128, 512], BF16) for s in range(2)]
        Kd = [ar("r_Kd%d" % s, [128, 256], BF16) for s in range(2)]
        PTt = [ar("r_PT%d" % s, [128, 128], BF16) for s in range(2)]
        on = [ar("r_on%d" % s, [128, 512], F32) for s in range(2)]
        sg = [ar("r_sg%d" % s, [128, 512], BF16) for s in range(2)]
        ogo = [ar("r_ogo%d" % s, [128, 512], BF16) for s in range(2)]
        st6 = ar("r_st6", [128, 2, 6], F32)
        mv = ar("r_mv", [128, 2, 2], F32)
        sm = ar("r_sm", [128, 2, 4], F32)

        self.rms_stats(self.dram["norm_mix"][layer:layer + 1, :])
        P.op("pool", lambda e: e.dma_start(out=cosb[:], in_=self.dram["c_cos"]), writes=["cosb"], dma=True)
        P.op("pool", lambda e: e.dma_start(out=sinb[:], in_=self.dram["c_sin"]), writes=["sinb"], dma=True)
        P.op("pool", lambda e: e.dma_start(out=intraT[:], in_=self.dram["c_intraT"]), writes=["intraT"], dma=True)
        P.op("sp", lambda e: e.dma_start(out=rdec[:], in_=self.dram["c_rdec"]), writes=["rdec"], dma=True)
        self.norm_transpose(hnT, hnb)
        xT_all = [("xT", t) for t in range(NT)]
        og2v = self.Og2_d

        def load_qk(hd):
            s = hd % 2
            for qk in range(2):
                P.op("pool", lambda e, qk=qk: e.dma_start(
                    out=wqk[s][:, :, qk * 256:(qk + 1) * 256],
                    in_=w_in[j][:, qk * 1024 + hd * 256: qk * 1024 + (hd + 1) * 256].rearrange("(c p) f -> p c f", p=128)),
                    writes=[("wqk", s, qk)], dma=True)

        def load_vg(hd):
            for vg in range(2):
                P.op("pool", lambda e, vg=vg: e.dma_start(
                    out=wvg[:, :, vg * 512:(vg + 1) * 512],
                    in_=w_in[j][:, 2048 + vg * 2048 + hd * 512: 2048 + vg * 2048 + (hd + 1) * 512].rearrange("(c p) f -> p c f", p=128)),
                    writes=[("wvg", vg)], dma=True)

        load_qk(0)
        for hd in range(4):
            s = hd % 2
            gam = 1.0 - 2.0 ** (-5.0 - hd)
            gamC = float(np.float32(np.exp(np.float32(np.log(np.float32(gam))) * np.float32(128.0))))
            load_vg(hd)
            if hd + 1 < 4:
                load_qk(hd + 1)
            for qk in range(2):
                dst = QT if qk == 0 else KT
                dname = "QT" if qk == 0 else "KT"
                sc = 1.0 if qk == 0 else 0.0625
                for qd in range(4):
                    cs = slice(qd * 512, (qd + 1) * 512)
                    for half in range(2):
                        def pm(e, half=half, qk=qk, qd=qd, s=s):
                            col = qk * 256 + half * 128
                            for c in range(8):
                                inst = e.matmul(ps[half][:, :], wqk[s][:, c, col:col + 128], hnT[:, c, qd * 512:(qd + 1) * 512],
                                                start=(c == 0), stop=(c == 7))
                            return inst
                        P.op("pe", pm, reads=xT_all + [("wqk", s, qk)], writes=[("ps", half)])
                    for k4, (bank, tab, tn) in enumerate(((0, cosb, "cosb"), (1, sinb, "sinb"), (0, sinb, "sinb"), (1, cosb, "cosb"))):
                        P.op("dve", lambda e, k4=k4, bank=bank, tab=tab, cs=cs, sc=sc: e.scalar_tensor_tensor(
                            out=rt[k4][:], in0=ps[bank][:, :], scalar=sc, in1=tab[:, cs], op0=ALU.mult, op1=ALU.mult),
                            reads=[("ps", bank), tn], writes=[("rt", k4)])
                    P.op("pool", lambda e, dst=dst, cs=cs: e.tensor_tensor(out=dst[:, 0, cs], in0=rt[0][:], in1=rt[1][:], op=ALU.subtract),
                         reads=[("rt", 0), ("rt", 1)], writes=[(dname, qd)])
                    P.op("pool", lambda e, dst=dst, cs=cs: e.tensor_tensor(out=dst[:, 1, cs], in0=rt[2][:], in1=rt[3][:], op=ALU.add),
                         reads=[("rt", 2), ("rt", 3)], writes=[(dname, qd)])
            P.op("dve", lambda e: e.memset(R32[:], 0.0), writes=[("R32", 0), ("R32", 1)])
            for n in range(NT):
                b = n % 2
                cols = slice(n * 128, (n + 1) * 128)
                qdk = n // 4

                def ktr(e, cols=cols, b=b):
                    pv = ps[0][:].bitcast(BF16)
                    for c in range(2):
                        inst = e.transpose(out=pv[:, c * 128:(c + 1) * 128], in_=KT[:, c, cols], identity=self.ident_b[:])
                    return inst
                P.op("pe", ktr, reads=[("KT", qdk), "ident_b"], writes=[("ps", 0)])
                P.op("act", lambda e, b=b, hd=hd: e.activation(out=Kd[b][:], in_=ps[0][:].bitcast(BF16)[:, 0:256], func=AF.Identity,
                                                               scale=rdec[:, 8 + hd:9 + hd]),
                     reads=[("ps", 0), "rdec"], writes=[("Kd", b)])

                def smm(e, cols=cols):
                    for c in range(2):
                        inst = e.matmul(ps[1][:, 0:128], KT[:, c, cols], QT[:, c, cols], start=(c == 0), stop=(c == 1))
                    return inst
                P.op("pe", smm, reads=[("KT", qdk), ("QT", qdk)], writes=[("ps", 1)])
                P.op("dve", lambda e, b=b, hd=hd: e.tensor_tensor(out=PTt[b][:], in0=ps[1][:, 0:128], in1=intraT[:, hd, :], op=ALU.mult),
                     reads=[("ps", 1), "intraT"], writes=[("PTt", b)])

                def vmm(e, cols=cols):
                    for c in range(8):
                        inst = e.matmul(ps[2][:, :], hnT[:, c, cols], wvg[:, c, 0:512], start=(c == 0), stop=(c == 7))
                    return inst
                P.op("pe", vmm, reads=[("xT", n), ("wvg", 0)], writes=[("ps", 2)])
                P.op("act", lambda e, b=b: e.activation(out=Vt[b][:], in_=ps[2][:, :], func=AF.Copy),
                     reads=[("ps", 2)], writes=[("Vt", b)])

                def gmm(e, cols=cols):
                    for c in range(8):
                        inst = e.matmul(ps[3][:, :], hnT[:, c, cols], wvg[:, c, 512:1024], start=(c == 0), stop=(c == 7))
                    return inst
                P.op("pe", gmm, reads=[("xT", n), ("wvg", 1)], writes=[("ps", 3)])
                P.op("act", lambda e, b=b: e.activation(out=sg[b][:], in_=ps[3][:, :], func=AF.Silu),
                     reads=[("ps", 3)], writes=[("sg", b)])

                if n < NT - 1:
                    rbn = (n + 1) % 2
                    for c in range(2):
                        P.op("pe", lambda e, c=c, b=b: e.matmul(ps[5 + c][:, :], Kd[b][:, c * 128:(c + 1) * 128], Vt[b][:],
                                                               start=True, stop=True),
                             reads=[("Kd", b), ("Vt", b)], writes=[("ps", 5 + c)])
                        P.op("dve", lambda e, c=c, gamC=gamC: e.scalar_tensor_tensor(
                            out=R32[:, c, :], in0=R32[:, c, :], scalar=gamC, in1=ps[5 + c][:, :], op0=ALU.mult, op1=ALU.add),
                            reads=[("ps", 5 + c), ("R32", c)], writes=[("R32", c)])
                        P.op("act", lambda e, c=c, rbn=rbn: e.activation(out=Rb[rbn][:, c, :], in_=R32[:, c, :], func=AF.Copy),
                             reads=[("R32", c)], writes=[("Rb", rbn, c)])

                def omm(e, cols=cols, b=b, n=n):
                    inst = e.matmul(ps[4][:, :], PTt[b][:], Vt[b][:], start=True, stop=(n == 0))
                    if n > 0:
                        for c in range(2):
                            inst = e.matmul(ps[4][:, :], QT[:, c, cols], Rb[n % 2][:, c, :], start=False, stop=(c == 1))
                    return inst
                P.op("pe", omm, reads=[("PTt", b), ("Vt", b), ("QT", qdk), ("Rb", n % 2, 0), ("Rb", n % 2, 1)], writes=[("ps", 4)])
                P.op("dve", lambda e, b=b: e.bn_stats(out=st6[:, b, :], in_=ps[4][:, :]), reads=[("ps", 4)], writes=[("st6", b)])
                P.op("dve", lambda e, b=b: e.bn_aggr(out=mv[:, b, :], in_=st6[:, b, :]), reads=[("st6", b)], writes=[("mv", b)])
                P.op("dve", lambda e, b=b, hd=hd: e.tensor_scalar(out=sm[:, b, 0:1], in0=mv[:, b, 1:2], scalar1=rdec[:, 4 + hd:5 + hd],
                                                                 scalar2=EPS, op0=ALU.mult, op1=ALU.add),
                     reads=[("mv", b), "rdec"], writes=[("sm", b, 0)])
                P.op("pool", lambda e, b=b: e.tensor_tensor(out=sm[:, b, 1:2], in0=sm[:, b, 0:1], in1=self.negh[:, 0:1], op=ALU.pow),
                     reads=[("sm", b, 0), "negh"], writes=[("sm", b, 1)])
                P.op("dve", lambda e, b=b, hd=hd: e.tensor_tensor(out=sm[:, b, 2:3], in0=sm[:, b, 1:2], in1=rdec[:, hd:hd + 1], op=ALU.mult),
                     reads=[("sm", b, 1), "rdec"], writes=[("sm", b, 2)])
                P.op("dve", lambda e, b=b: e.scalar_tensor_tensor(out=sm[:, b, 3:4], in0=mv[:, b, 0:1], scalar=-1.0, in1=sm[:, b, 2:3],
                                                                 op0=ALU.mult, op1=ALU.mult),
                     reads=[("mv", b), ("sm", b, 2)], writes=[("sm", b, 3)])
                P.op("act", lambda e, b=b: e.activation(out=on[b][:], in_=ps[4][:, :], func=AF.Identity,
                                                        scale=sm[:, b, 2:3], bias=sm[:, b, 3:4]),
                     reads=[("ps", 4), ("sm", b, 2), ("sm", b, 3)], writes=[("on", b)])
                P.op("pool", lambda e, b=b: e.tensor_tensor(out=ogo[b][:], in0=on[b][:], in1=sg[b][:], op=ALU.mult),
                     reads=[("on", b), ("sg", b)], writes=[("ogo", b)])
                P.op("sp", lambda e, b=b, n=n, hd=hd: e.dma_start(out=og2v[n * 128:(n + 1) * 128, hd * 512:(hd + 1) * 512], in_=ogo[b][:]),
                     reads=[("ogo", b)], writes=[("Og2_d", n, hd)], dma=True)

        P.barrier()
        self.arena_reset()
        wout = ar("r_wout", [128, 16, D], BF16)
        ogt = [ar("r_ogt%d" % s, [128, 2048], BF16) for s in range(2)]
        OgT = [ar("r_OgT%d" % s, [128, 16, 128], BF16) for s in range(2)]
        gcol = ar("r_gcol", [128, 16], F32)
        P.op("pool", lambda e: e.dma_start(out=wout[:], in_=self.dram["ret_w_out"][j].rearrange("(c p) f -> p c f", p=128)),
             writes=["wout"], dma=True)
        P.op("sp", lambda e: e.dma_start(out=gcol[:], in_=self.dram["ret_gn_gain"][j].rearrange("(c p) -> p c", p=128),
                                         allow_slow_non_contiguous=True), writes=["gcol"], dma=True)
        for c in range(16):
            P.op("dve", lambda e, c=c: e.tensor_scalar(out=wout[:, c, :], in0=wout[:, c, :], scalar1=gcol[:, c:c + 1], scalar2=None,
                                                     op0=ALU.mult), reads=["wout", "gcol"], writes=["wout"])
        for t in range(NT):
            b = t % 2
            P.op("sp", lambda e, t=t, b=b: e.dma_start(out=ogt[b][:], in_=og2v[t * 128:(t + 1) * 128, :]),
                 writes=[("ogt", b)], dma=True)
            for half in range(2):
                def tr(e, half=half, b=b):
                    pv = ps[half][:].bitcast(BF16)
                    for c in range(8):
                        cc = half * 8 + c
                        inst = e.transpose(out=pv[:, c * 128:(c + 1) * 128], in_=ogt[b][:, cc * 128:(cc + 1) * 128],
                                           identity=self.ident_b[:])
                    return inst
                P.op("pe", tr, reads=[("ogt", b), "ident_b"], writes=[("ps", half)])
                srcv = lambda half=half: ps[half][:].bitcast(BF16).rearrange("p (c t) -> p c t", c=8)
                if half == 0:
                    P.op("act", lambda e, b=b, srcv=srcv: e.activation(out=OgT[b][:, 0:8, :], in_=srcv(), func=AF.Copy),
                         reads=[("ps", 0)], writes=[("OgT", b, 0)])
                else:
                    P.op("dve", lambda e, b=b, srcv=srcv: e.tensor_copy(out=OgT[b][:, 8:16, :], in_=srcv()),
                         reads=[("ps", 1)], writes=[("OgT", b, 1)])
            for nn in range(2):
                bi = 2 + (t * 2 + nn) % 2

                def mm(e, nn=nn, b=b, bi=bi):
                    for c in range(16):
                        inst = e.matmul(ps[bi][:, :], OgT[b][:, c, :], wout[:, c, nn * 512:(nn + 1) * 512],
                                        start=(c == 0), stop=(c == 15))
                    return inst
                P.op("pe", mm, reads=[("OgT", b, 0), ("OgT", b, 1), "wout"], writes=[("ps", bi)])
                P.op("dve", lambda e, t=t, nn=nn, bi=bi: e.tensor_tensor(
                    out=h[:, t, nn * 512:(nn + 1) * 512], in0=h[:, t, nn * 512:(nn + 1) * 512], in1=ps[bi][:, :], op=ALU.add),
                    reads=[("ps", bi), ("h", t)], writes=[("h", t)])


def build(stages=None, final_norm=True):
    B = Builder()
    B.prologue()
    if stages is None:
        stages = []
        for i in range(DEPTH):
            stages += ["mix%d" % i, "moe%d" % i]
    moe_layers = sorted(set(int(s[3:]) for s in stages if s.startswith("moe")))
    if moe_layers:
        B.moe_setup(moe_layers)
    fox_layers = sorted(set(int(s[3:]) for s in stages if s.startswith("mix") and int(s[3:]) % 2 == 0))
    if fox_layers:
        B.fox_setup(fox_layers)
    ret_layers = sorted(set(int(s[3:]) for s in stages if s.startswith("mix") and int(s[3:]) % 2 == 1))
    if ret_layers:
        B.ret_setup(ret_layers)
    B.layer_sets = {"moe": moe_layers, "fox": fox_layers, "ret": ret_layers}
    for s in stages:
        li = int(s[3:])
        if s.startswith("moe"):
            B.moe_layer(li)
        elif li % 2 == 0:
            B.fox_layer(li)
        else:
            B.ret_layer(li)
    B.epilogue(final_norm=final_norm)
    nc = B.finish()
    _LAYER_SETS[id(nc)] = B.layer_sets
    return nc


def _consts():
    ident = np.eye(128, dtype=np.float32)
    lst = np.triu(np.ones((128, 128), np.float32), k=1)
    ebase = (np.arange(NE, dtype=np.float32) * CAP).reshape(1, NE)
    mask = np.triu(np.ones((128, 128), np.float32), k=0)
    inv = (np.float32(1.0) / (np.float32(10000.0) ** np.linspace(0.0, 1.0, 128, dtype=np.float32))).astype(np.float32)
    ang = (np.arange(S, dtype=np.float32)[None, :] * inv[:, None]).astype(np.float32)
    cosT = np.cos(ang).astype(np.float32)
    sinT = np.sin(ang).astype(np.float32)
    log_g = np.log(np.float32(1.0) - np.float32(2.0) ** (np.float32(-5.0) - np.arange(4, dtype=np.float32))).astype(np.float32)
    idx = np.arange(128, dtype=np.float32)
    intraT = np.zeros((128, 4, 128), np.float32)
    rdec = np.zeros((128, 12), np.float32)
    for hd in range(4):
        col = np.exp(-log_g[hd] * (idx + 1.0)).astype(np.float32)
        intraT[:, hd, :] = np.where(idx[None, :] >= idx[:, None], col[:, None], 0.0)
        qd = np.exp(log_g[hd] * (idx + 1.0)).astype(np.float32)
        rdec[:, hd] = qd
        rdec[:, 4 + hd] = qd * qd
        rdec[:, 8 + hd] = np.exp(log_g[hd] * (127.0 - idx)).astype(np.float32)
    return {"c_ident": ident, "c_lst": lst, "c_ebase": ebase, "c_mask": mask,
            "c_cos": cosT, "c_sin": sinT, "c_intraT": intraT, "c_rdec": rdec}


_CACHE = {}


def prep_inputs(inputs, nc_inputs, layer_sets):
    f = lambda a: np.ascontiguousarray(np.asarray(a), dtype=np.float32)
    shared = {}
    shared["norm_mix"] = f(inputs["norm_mix"])
    shared["norm_ffn"] = f(inputs["norm_ffn"])
    shared["norm_final"] = f(inputs["norm_final"]).reshape(1, D)
    shared.update(_consts())
    if "router_w" in nc_inputs:
        ml = layer_sets["moe"]
        shared["router_w"] = np.ascontiguousarray(
            np.concatenate([f(inputs["router_group_w"]), f(inputs["router_expert_w"])], axis=-1)[ml])
        shared["router_b"] = np.ascontiguousarray(
            np.concatenate([f(inputs["router_group_b"]), f(inputs["router_expert_b"])], axis=-1)[ml])
        for p, l in enumerate(ml):
            shared["expert_w_gu%d" % p] = f(inputs["expert_w_gu"][l])
            shared["expert_w_down%d" % p] = f(inputs["expert_w_down"][l])
    if "fox_w_in" in nc_inputs:
        fl = [l // 2 for l in layer_sets["fox"]]
        shared["fox_w_in"] = np.ascontiguousarray(f(inputs["fox_w_in"])[fl])
        shared["fox_b_f"] = np.ascontiguousarray(f(inputs["fox_b_f"])[fl])
        shared["fox_w_out"] = np.ascontiguousarray(f(inputs["fox_w_out"])[fl])
    if "ret_w_in" in nc_inputs:
        rl = [l // 2 for l in layer_sets["ret"]]
        shared["ret_w_in"] = np.ascontiguousarray(f(inputs["ret_w_in"])[rl])
        shared["ret_gn_gain"] = np.ascontiguousarray(f(inputs["ret_gn_gain"])[rl])
        shared["ret_w_out"] = np.ascontiguousarray(f(inputs["ret_w_out"])[rl])
    x = f(inputs["x"])
    in_maps = []
    for b in range(NCORES):
        m = {k: v for k, v in shared.items() if k in nc_inputs}
        m["x"] = x[b]
        in_maps.append(m)
    return in_maps


def run(inputs, stages=None, final_norm=True, trace=False):
    key = (tuple(stages) if stages is not None else None, final_norm)
    if key not in _CACHE:
        B_nc = build(stages, final_norm)
        _CACHE[key] = B_nc
    nc = _CACHE[key]
    names = set(_DRAM_NAMES[id(nc)])
    in_maps = prep_inputs(inputs, names, _LAYER_SETS[id(nc)])
    res = run_bass_kernel_spmd(nc, in_maps, core_ids=list(range(NCORES)), trace=trace)
    out = np.stack([np.asarray(r["y"]) for r in res.results], axis=0).astype(np.float32)
    return out, res


def kernel(**inputs):
    out, _ = run(inputs)
    return out
```

```python
import contextlib
import os
import numpy as np
import concourse.bass as bass
import concourse.mybir as mybir
from concourse.bass_utils import run_bass_kernel_spmd

F32 = mybir.dt.float32
BF16 = mybir.dt.bfloat16
I32 = mybir.dt.int32
AF = mybir.ActivationFunctionType
ALU = mybir.AluOpType
AX = mybir.AxisListType

D = 1024
S = 2048
NT = S // 128
DEPTH = 4
EPS = 1e-6
NCORES = 8

ENGINES = ("pe", "act", "dve", "pool", "sp")


class _Op:
    __slots__ = ("eng", "fn", "dma", "waits", "signal", "idx")

    def __init__(self, eng, fn, dma, idx):
        self.eng = eng
        self.fn = fn
        self.dma = dma
        self.waits = []
        self.signal = None
        self.idx = idx


class Prog:
    N_DMA_SEMS = 24

    def __init__(self, same_engine_sync=True):
        self.ops = []
        self.last_writer = {}
        self.readers = {}
        self.same_engine_sync = same_engine_sync
        self.dependents = {}
        self.deps = []
        self.forced = set()
        self.pending = {e: set() for e in ENGINES}
        self.last_op = {}
        self.unfenced_dma = set()

    def op(self, eng, fn, reads=(), writes=(), dma=False, force=False):
        idx = len(self.ops)
        o = _Op(eng, fn, dma, idx)
        if force:
            self.forced.add(idx)
        ps_reads = [k for k in reads if isinstance(k, tuple) and k[0] == "ps"]
        if ps_reads:
            reads = [k for k in reads if k not in ps_reads]
            writes = list(writes) + ps_reads
        deps = set()
        for k in reads:
            w = self.last_writer.get(k)
            if w is not None:
                deps.add(w)
        for k in writes:
            w = self.last_writer.get(k)
            if w is not None:
                deps.add(w)
            for r in self.readers.get(k, ()):
                deps.add(r)
        deps |= self.pending[eng]
        self.pending[eng] = set()
        deps.discard(idx)
        self.last_op[eng] = idx
        if dma:
            self.unfenced_dma.add(idx)
        for k in writes:
            self.last_writer[k] = idx
            self.readers[k] = []
        for k in reads:
            if k not in writes:
                self.readers.setdefault(k, []).append(idx)
        self.ops.append(o)
        self.deps.append(deps)
        return idx

    def barrier(self):
        carry = getattr(self, "carry_dma", set())
        deps = set(self.last_op.values()) | (self.unfenced_dma - carry)
        self.unfenced_dma = set(carry)
        self.carry_dma = set()
        for e in ENGINES:
            self.pending[e] |= deps

    def finalize(self):
        ops = self.ops
        needed = [i in self.forced for i in range(len(ops))]
        for o in ops:
            for d in self.deps[o.idx]:
                p = ops[d]
                if p.dma:
                    needed[d] = True
                elif p.eng == o.eng and not o.dma:
                    if p.eng == "pe":
                        continue
                    if self.same_engine_sync:
                        needed[d] = True
                else:
                    needed[d] = True
        eng_cnt = {e: 0 for e in ENGINES}
        dma_cnt = [0] * self.N_DMA_SEMS
        dma_rr = 0
        seen = {e: {} for e in ENGINES}
        for o in ops:
            waits = {}
            for d in self.deps[o.idx]:
                p = ops[d]
                if p.signal is None:
                    continue
                if (not p.dma) and p.eng == o.eng and not o.dma and (p.eng == "pe" or not self.same_engine_sync):
                    continue
                k, v, _ = p.signal
                if seen[o.eng].get(k, 0) >= v:
                    continue
                waits[k] = max(waits.get(k, 0), v)
            if o.dma and needed[o.idx]:
                j = dma_rr
                dma_rr = (dma_rr + 1) % self.N_DMA_SEMS
                k = ("dma", j)
                if dma_cnt[j] > 0 and seen[o.eng].get(k, 0) < dma_cnt[j]:
                    waits[k] = max(waits.get(k, 0), dma_cnt[j])
                dma_cnt[j] += 16
                o.signal = (k, dma_cnt[j], 16)
            elif needed[o.idx]:
                eng_cnt[o.eng] += 1
                o.signal = (("eng", o.eng), eng_cnt[o.eng], 1)
            for k, v in waits.items():
                seen[o.eng][k] = max(seen[o.eng].get(k, 0), v)
            o.waits = sorted(waits.items(), key=lambda kv: str(kv[0]))
        self.max_counts = dict(eng_cnt)

    def emit(self, nc, stack, final_waits):
        sems = {}
        for e in ENGINES:
            sems[("eng", e)] = stack.enter_context(nc.semaphore("sem_" + e))
        for j in range(self.N_DMA_SEMS):
            sems[("dma", j)] = stack.enter_context(nc.semaphore("sem_dma%d" % j))
        block = stack.enter_context(nc.Block())
        by_eng = {e: [o for o in self.ops if o.eng == e] for e in ENGINES}
        last_signal = {}
        for o in self.ops:
            if o.signal is not None:
                last_signal[o.signal[0]] = max(last_signal.get(o.signal[0], 0), o.signal[1])

        def run(engine, ename):
            for o in by_eng[ename]:
                for k, v in o.waits:
                    engine.wait_ge(sems[k], v)
                inst = o.fn(engine)
                if o.signal is not None:
                    inst.then_inc(sems[o.signal[0]], o.signal[2])
            if ename == final_waits:
                for k, v in last_signal.items():
                    if k[0] == "dma":
                        engine.wait_ge(sems[k], v)

        @block.tensor
        def _(e):
            run(e, "pe")

        @block.scalar
        def _(e):
            run(e, "act")

        @block.vector
        def _(e):
            run(e, "dve")

        @block.gpsimd
        def _(e):
            run(e, "pool")

        @block.sync
        def _(e):
            run(e, "sp")


NE = 32
CAP = 256
TRASH = NE * CAP
NSLOT = TRASH + 128
FF = 512
BIG = 30000.0


_DRAM_NAMES = {}
_LAYER_SETS = {}


class Builder:
    def __init__(self):
        self.nc = nc = bass.Bass("TRN2", target_bir_lowering=False)
        self.P = Prog()
        self.stack = contextlib.ExitStack()
        self.mem_stack = contextlib.ExitStack()
        self.dram = {}
        self.ps = [self.mem_stack.enter_context(nc.psum_tensor("ps%d" % b, [128, 512], F32)) for b in range(8)]
        sb = self.sb
        self.h = sb("h", [128, NT, D], F32)
        self.gb = sb("gb", [128, D], F32)
        self.ssq = sb("ssq", [128, NT], F32)
        self.rstd = sb("rstd", [128, NT], F32)
        self.junk = sb("junk", [128, D], BF16)
        self.ident_b = sb("ident_b", [128, 128], BF16)
        self.ident_f = sb("ident_f", [128, 128], F32)
        self.lst_b = sb("lst_b", [128, 128], BF16)
        self.ones_b = sb("ones_b", [128, 128], BF16)
        self.ebase = sb("ebase", [128, NE], F32)
        self.negh = sb("negh", [128, NT], F32)
        self.arena_size = 131 * 1024
        self.arena_base, _ = nc.bump_sbuf(self.arena_size)
        self.arena_off = 0

    def sb(self, name, shape, dt):
        return self.mem_stack.enter_context(self.nc.sbuf_tensor(name, list(shape), dt))

    def arena_reset(self):
        self.arena_off = 0

    def ar(self, name, shape, dt):
        nbytes = int(np.prod(shape[1:])) * (4 if dt in (F32, I32) else 2)
        nbytes = (nbytes + 31) // 32 * 32
        assert self.arena_off + nbytes <= self.arena_size, (name, self.arena_off, nbytes)
        t = self.nc.alloc_sbuf_tensor_at(name, list(shape), dt, offset=self.arena_base + self.arena_off)
        self.arena_off += nbytes
        return t

    def din(self, name, shape, dt=F32):
        t = self.nc.dram_tensor(name, list(shape), dt, kind="ExternalInput").ap()
        self.dram[name] = t
        return t

    def dscratch(self, name, shape, dt):
        return self.nc.dram_tensor(name, list(shape), dt, kind="Internal").ap()

    def prologue(self):
        P = self.P
        x = self.din("x", [S, D])
        self.din("norm_mix", [DEPTH, D])
        self.din("norm_ffn", [DEPTH, D])
        self.din("norm_final", [1, D])
        cI = self.din("c_ident", [128, 128])
        cL = self.din("c_lst", [128, 128])
        cE = self.din("c_ebase", [1, NE])
        self.y = self.nc.dram_tensor("y", [S, D], F32, kind="ExternalOutput").ap()
        xv = x.rearrange("(n p) d -> p n d", p=128)
        h = self.h
        for q in range(4):
            sl = slice(q * 4, (q + 1) * 4)
            P.op("sp", lambda e, sl=sl: e.dma_start(out=h[:, sl, :], in_=xv[:, sl, :]),
                 writes=[("h", i) for i in range(q * 4, q * 4 + 4)], dma=True)
        P.op("sp", lambda e: e.dma_start(out=self.ident_f[:], in_=cI), writes=["ident_f"], dma=True)
        P.op("pool", lambda e: e.dma_start(out=self.ident_b[:], in_=cI), writes=["ident_b"], dma=True)
        P.op("pool", lambda e: e.dma_start(out=self.lst_b[:], in_=cL), writes=["lst_b"], dma=True)
        P.op("sp", lambda e: e.dma_start(out=self.ebase[:], in_=cE[0:1, :].partition_broadcast(128)),
             writes=["ebase"], dma=True)
        P.op("dve", lambda e: e.memset(self.ones_b[:], 1.0), writes=["ones_b"])
        P.op("dve", lambda e: e.memset(self.negh[:], -0.5), writes=["negh"])

    def rms_stats(self, gain_row_ap):
        P, h, ssq, rstd, junk, gb = self.P, self.h, self.ssq, self.rstd, self.junk, self.gb
        P.op("sp", lambda e: e.dma_start(out=gb[:], in_=gain_row_ap.partition_broadcast(128)),
             writes=["gb"], dma=True)
        for i in range(NT):
            P.op("act", lambda e, i=i: e.activation(out=junk[:], in_=h[:, i, :], func=AF.Square,
                                                     accum_out=ssq[:, i:i + 1]),
                 reads=[("h", i)], writes=["junk", ("ssq", i)])
        P.op("dve", lambda e: e.tensor_scalar(out=rstd[:], in0=ssq[:], scalar1=1.0 / D, scalar2=EPS,
                                              op0=ALU.mult, op1=ALU.add),
             reads=[("ssq", i) for i in range(NT)], writes=["rstd"])
        P.op("pool", lambda e: e.tensor_tensor(out=rstd[:], in0=rstd[:], in1=self.negh[:], op=ALU.pow),
             reads=["rstd", "negh"], writes=["rstd"])

    def epilogue(self, final_norm=True):
        P, h = self.P, self.h
        P.barrier()
        self.arena_reset()
        outt = self.ar("outt", [128, 2, D], F32)
        yv = self.y.rearrange("(n p) d -> p n d", p=128)
        if final_norm:
            self.rms_stats(self.dram["norm_final"][0:1, :])
        for i in range(NT):
            b = i % 2
            if final_norm:
                P.op("dve", lambda e, i=i, b=b: e.scalar_tensor_tensor(
                    out=outt[:, b, :], in0=h[:, i, :], scalar=self.rstd[:, i:i + 1], in1=self.gb[:],
                    op0=ALU.mult, op1=ALU.mult),
                    reads=[("h", i), "rstd", "gb"], writes=[("outt", b)])
                P.op("sp", lambda e, i=i, b=b: e.dma_start(out=yv[:, i, :], in_=outt[:, b, :]),
                     reads=[("outt", b)], writes=[("y", i)], dma=True, force=True)
            else:
                P.op("sp", lambda e, i=i: e.dma_start(out=yv[:, i, :], in_=h[:, i, :]),
                     reads=[("h", i)], writes=[("y", i)], dma=True, force=True)

    def finish(self):
        _DRAM_NAMES[id(self.nc)] = list(self.dram.keys())
        self.P.finalize()
        self.P.emit(self.nc, self.stack, final_waits="sp")
        self.stack.close()
        return self.nc

    def moe_setup(self, layers):
        self.moe_layers = list(layers)
        nl = len(self.moe_layers)
        self.din("router_w", [nl, D, 36])
        self.din("router_b", [nl, 36])
        for p in range(nl):
            self.din("expert_w_gu%d" % p, [NE, D, 2 * FF])
            self.din("expert_w_down%d" % p, [NE, FF, D])
        self.Xs = self.dscratch("Xs", [NSLOT, D], BF16)
        self.Ys = self.dscratch("Ys", [NSLOT, D], BF16)
        P = self.P
        self.arena_reset()
        self.zeros_b = self.ar("zeros_b", [128, D], BF16)
        P.op("dve", lambda e: e.memset(self.zeros_b[:], 0.0), writes=["zeros_b"])
        xsv = self.Xs.rearrange("(r p) d -> p r d", p=128)
        ysv = self.Ys.rearrange("(r p) d -> p r d", p=128)
        nr = NSLOT // 128
        P.carry_dma = set()
        for r0 in range(0, nr, 13):
            P.carry_dma.add(P.op("pool", lambda e, r0=r0: e.dma_start(
                out=xsv[:, r0:r0 + 13, :], in_=self.zeros_b[:].unsqueeze(1).to_broadcast([128, 13, D])),
                reads=["zeros_b"], writes=[("Xs_z", r0)], dma=True))
        P.carry_dma.add(P.op("sp", lambda e: e.dma_start(out=ysv[:, nr - 1, :], in_=self.zeros_b[:]),
                             reads=["zeros_b"], writes=["Ys_z"], dma=True))

    def moe_layer(self, layer):
        li = self.moe_layers.index(layer)
        P, h, ps = self.P, self.h, self.ps
        rstd, gb = self.rstd, self.gb
        P.barrier()
        self.arena_reset()
        ar = self.ar
        wgu = [ar("wgu%d" % s, [128, 8, 2 * FF], BF16) for s in range(2)]
        wdn = [ar("wdn%d" % s, [128, 4, D], BF16) for s in range(2)]
        NYG = 8
        yg = [self.nc.alloc_sbuf_tensor_at("yg%d_%d" % (layer, s), [128, D], BF16, offset=self.arena_base + s * 2048)
              for s in range(NYG)]
        save = self.arena_off
        NRB = 3
        hn32 = [ar("hn32_%d" % s, [128, D], F32) for s in range(NRB)]
        hhi = [ar("hhi%d" % s, [128, D], BF16) for s in range(NRB)]
        hlo = [ar("hlo%d" % s, [128, D], BF16) for s in range(NRB)]
        hiT = [ar("hiT%d" % s, [128, 8, 128], BF16) for s in range(NRB)]
        loT = [ar("loT%d" % s, [128, 8, 128], BF16) for s in range(NRB)]
        end1 = self.arena_off
        self.arena_off = save
        xg = [ar("xg%d" % s, [128, 2, D], BF16) for s in range(2)]
        xT = [ar("xT%d" % s, [128, 8, CAP], BF16) for s in range(2)]
        hT = [ar("hT%d" % s, [128, 4, CAP], BF16) for s in range(2)]
        sA = [ar("sA%d" % s, [128, CAP], F32) for s in range(2)]
        yt = [ar("yt%d" % s, [128, 2, D], BF16) for s in range(2)]
        self.arena_off = max(self.arena_off, end1)
        NHB = 4
        hnb = [ar("hnb%d" % s, [128, D], BF16) for s in range(NHB)]
        fence = ar("fence", [128, 8], F32)
        wr_hi = ar("wr_hi", [128, 8, 36], BF16)
        wr_lo = ar("wr_lo", [128, 8, 36], BF16)
        wr32 = ar("wr32", [128, 8, 36], F32)
        brb = ar("brb", [128, 36], F32)
        LG = ar("LG", [128, NT, 36], F32)
        Em = ar("Em", [128, NT, 32], F32)
        T1 = ar("T1", [128, NT, 32], F32)
        oh1 = ar("oh1", [128, NT, 32], F32)
        oh2 = ar("oh2", [128, NT, 32], F32)
        Em2 = ar("Em2", [128, NT, 32], F32)
        RK = ar("RK", [128, NT, 32], F32)
        Obf = ar("Obf", [128, NT, 32], BF16)
        Gm = ar("Gm", [128, NT, 4], F32)
        ohG = ar("ohG", [128, NT, 4], F32)
        pen = ar("pen", [128, NT, 4], F32)
        sm = {n: ar("sm_" + n, [128, NT], F32) for n in
              ("gmax", "sumG", "pg", "m1", "m2", "r", "den", "g1", "g2", "rk", "base", "valid", "sl")}
        slot_i = ar("slot_i", [128, 2, NT], I32)

        wr = self.dram["router_w"]
        br = self.dram["router_b"]
        wgu_d = self.dram["expert_w_gu%d" % li]
        wdn_d = self.dram["expert_w_down%d" % li]
        Xs, Ys = self.Xs, self.Ys

        self.rms_stats(self.dram["norm_ffn"][layer:layer + 1, :])
        P.op("sp", lambda e: e.dma_start(out=wr32[:], in_=wr[li].rearrange("(c p) j -> p c j", p=128)),
             writes=["wr32"], dma=True)
        P.op("sp", lambda e: e.dma_start(out=brb[:], in_=br[li:li + 1, :].partition_broadcast(128)),
             writes=["brb"], dma=True)

        def load_w(e_idx):
            s = e_idx % 2
            P.op("pool", lambda e: e.dma_start(out=wgu[s][:], in_=wgu_d[e_idx].rearrange("(c p) f -> p c f", p=128)),
                 writes=[("wgu", s)], dma=True)
            P.op("pool", lambda e: e.dma_start(out=wdn[s][:], in_=wdn_d[e_idx].rearrange("(c p) f -> p c f", p=128)),
                 writes=[("wdn", s)], dma=True)

        load_w(0)
        load_w(1)

        P.op("act", lambda e: e.activation(out=wr_hi[:], in_=wr32[:], func=AF.Copy), reads=["wr32"], writes=["wr_hi"])
        P.op("pool", lambda e: e.tensor_tensor(out=wr_lo[:], in0=wr32[:], in1=wr_hi[:], op=ALU.subtract),
             reads=["wr32", "wr_hi"], writes=["wr_lo"])
        def st1(t):
            b = t % NRB
            P.op("dve", lambda e: e.scalar_tensor_tensor(
                out=hn32[b][:], in0=h[:, t, :], scalar=rstd[:, t:t + 1], in1=gb[:], op0=ALU.mult, op1=ALU.mult),
                reads=[("h", t), "rstd", "gb"], writes=[("hn32", b)])
            P.op("act", lambda e: e.activation(out=hhi[b][:], in_=hn32[b][:], func=AF.Copy),
                 reads=[("hn32", b)], writes=[("hhi", b)])
            P.op("pool", lambda e: e.tensor_tensor(out=hlo[b][:], in0=hn32[b][:], in1=hhi[b][:], op=ALU.subtract),
                 reads=[("hn32", b), ("hhi", b)], writes=[("hlo", b)])

        def st2(t):
            b = t % NRB
            for w, (src, sk, dstT, dk) in enumerate(((hhi, "hhi", hiT, "hiT"), (hlo, "hlo", loT, "loT"))):
                bi = 2 * (t % 2) + w

                def tr(e, src=src, bi=bi):
                    pv = ps[bi][:].bitcast(BF16)
                    for c in range(8):
                        inst = e.transpose(out=pv[:, c * 128:(c + 1) * 128], in_=src[b][:, c * 128:(c + 1) * 128],
                                           identity=self.ident_b[:])
                    return inst
                P.op("pe", tr, reads=[(sk, b), "ident_b"], writes=[("ps", bi)])
                srcv = lambda bi=bi: ps[bi][:].bitcast(BF16).rearrange("p (c t) -> p c t", c=8)
                if w == 0:
                    P.op("act", lambda e, srcv=srcv, dstT=dstT: e.activation(out=dstT[b][:], in_=srcv(), func=AF.Copy),
                         reads=[("ps", bi)], writes=[(dk, b)])
                else:
                    P.op("dve", lambda e, srcv=srcv, dstT=dstT: e.tensor_copy(out=dstT[b][:], in_=srcv()),
                         reads=[("ps", bi)], writes=[(dk, b)])

        def st3(t):
            b = t % NRB
            pb = 4 + t % 2

            def rl(e):
                k = 0
                for (xt, wt) in ((hiT, wr_hi), (loT, wr_hi), (hiT, wr_lo)):
                    for c in range(8):
                        inst = e.matmul(ps[pb][:, 0:36], xt[b][:, c, :], wt[:, c, :], start=(k == 0), stop=(k == 23))
                        k += 1
                return inst
            P.op("pe", rl, reads=[("hiT", b), ("loT", b), "wr_hi", "wr_lo"], writes=[("ps", pb)])
            P.op("dve", lambda e: e.tensor_tensor(out=LG[:, t, :], in0=ps[pb][:, 0:36], in1=brb[:], op=ALU.add),
                 reads=[("ps", pb), "brb"], writes=[("LG", t)])

        for i in range(NT + 2):
            if i < NT:
                st1(i)
            if 0 <= i - 1 < NT:
                st2(i - 1)
            if 0 <= i - 2 < NT:
                st3(i - 2)

        if int(os.environ.get('MOE_STOP', '99')) <= 1:
            return
        G = LG[:, :, 0:4]
        E4 = LG[:, :, 4:36].rearrange("p n (g e) -> p n g e", g=4)

        def bc(t2, n):
            return t2[:].unsqueeze(2).to_broadcast([128, NT, n])

        def dve(fn, reads, writes):
            P.op("dve", fn, reads=reads, writes=writes)

        P.op("dve", lambda e: e.memset(fence[:], 0.0), reads=[("LG", t) for t in range(NT)], writes=["LG"])
        dve(lambda e: e.tensor_reduce(out=sm["gmax"][:], in_=G, axis=AX.X, op=ALU.max), ["LG"], ["gmax"])
        dve(lambda e: e.tensor_tensor(out=Gm[:], in0=G, in1=bc(sm["gmax"], 4), op=ALU.subtract), ["LG", "gmax"], ["Gm"])
        dve(lambda e: e.tensor_single_scalar(out=ohG[:], in_=Gm[:], scalar=0.0, op=ALU.is_ge), ["Gm"], ["ohG"])
        P.op("act", lambda e: e.activation(out=Gm[:], in_=Gm[:], func=AF.Exp), reads=["Gm", "ohG"], writes=["Gm"])
        dve(lambda e: e.tensor_reduce(out=sm["sumG"][:], in_=Gm[:], axis=AX.X, op=ALU.add), ["Gm"], ["sumG"])
        dve(lambda e: e.reciprocal(out=sm["pg"][:], in_=sm["sumG"][:]), ["sumG"], ["pg"])
        dve(lambda e: e.tensor_scalar(out=pen[:], in0=ohG[:], scalar1=BIG, scalar2=-BIG, op0=ALU.mult, op1=ALU.add),
            ["ohG"], ["pen"])
        dve(lambda e: e.tensor_tensor(out=Em[:].rearrange("p n (g e) -> p n g e", g=4), in0=E4,
                                      in1=pen[:].unsqueeze(3).to_broadcast([128, NT, 4, 8]), op=ALU.add),
            ["LG", "pen"], ["Em"])
        dve(lambda e: e.tensor_reduce(out=sm["m1"][:], in_=Em[:], axis=AX.X, op=ALU.max), ["Em"], ["m1"])
        dve(lambda e: e.tensor_tensor(out=T1[:], in0=Em[:], in1=bc(sm["m1"], 32), op=ALU.subtract), ["Em", "m1"], ["T1"])
        dve(lambda e: e.tensor_single_scalar(out=oh1[:], in_=T1[:], scalar=0.0, op=ALU.is_ge), ["T1"], ["oh1"])
        dve(lambda e: e.scalar_tensor_tensor(out=Em2[:], in0=oh1[:], scalar=-BIG, in1=Em[:], op0=ALU.mult, op1=ALU.add),
            ["oh1", "Em"], ["Em2"])
        dve(lambda e: e.tensor_reduce(out=sm["m2"][:], in_=Em2[:], axis=AX.X, op=ALU.max), ["Em2"], ["m2"])
        dve(lambda e: e.tensor_tensor(out=T1[:], in0=Em2[:], in1=bc(sm["m2"], 32), op=ALU.subtract), ["Em2", "m2"], ["T1"])
        dve(lambda e: e.tensor_single_scalar(out=oh2[:], in_=T1[:], scalar=0.0, op=ALU.is_ge), ["T1"], ["oh2"])
        dve(lambda e: e.tensor_tensor(out=sm["r"][:], in0=sm["m2"][:], in1=sm["m1"][:], op=ALU.subtract), ["m1", "m2"], ["r"])
        P.op("act", lambda e: e.activation(out=sm["r"][:], in_=sm["r"][:], func=AF.Exp), reads=["r"], writes=["r"])
        dve(lambda e: e.tensor_scalar(out=sm["den"][:], in0=sm["r"][:], scalar1=1.0, scalar2=None, op0=ALU.add), ["r"], ["den"])
        dve(lambda e: e.reciprocal(out=sm["den"][:], in_=sm["den"][:]), ["den"], ["den"])
        dve(lambda e: e.tensor_tensor(out=sm["g1"][:], in0=sm["pg"][:], in1=sm["den"][:], op=ALU.mult), ["pg", "den"], ["g1"])
        dve(lambda e: e.tensor_tensor(out=sm["g2"][:], in0=sm["g1"][:], in1=sm["r"][:], op=ALU.mult), ["g1", "r"], ["g2"])
        dve(lambda e: e.tensor_tensor(out=Obf[:], in0=oh1[:], in1=oh2[:], op=ALU.add), ["oh1", "oh2"], ["Obf"])

        if int(os.environ.get('MOE_STOP', '99')) <= 2:
            return
        def ranks(e):
            for t in range(NT):
                o = ps[7][:, t * 32:(t + 1) * 32]
                inst = e.matmul(o, self.lst_b[:], Obf[:, t, :], start=True, stop=(t == 0))
                for j in range(t):
                    inst = e.matmul(o, self.ones_b[:], Obf[:, j, :], start=False, stop=(j == t - 1))
            return inst
        P.op("pe", ranks, reads=["Obf", "lst_b", "ones_b"], writes=[("ps", 7)])
        dve(lambda e: e.tensor_copy(out=RK[:], in_=ps[7][:].rearrange("p (n e) -> p n e", e=32)), [("ps", 7)], ["RK"])
        ebc = self.ebase[:].unsqueeze(1).to_broadcast([128, NT, 32])
        for k, (oh, ohn, gn) in enumerate(((oh1, "oh1", "g1"), (oh2, "oh2", "g2"))):
            dve(lambda e, oh=oh: e.tensor_tensor(out=T1[:], in0=oh[:], in1=RK[:], op=ALU.mult), [ohn, "RK"], ["T1"])
            dve(lambda e: e.tensor_reduce(out=sm["rk"][:], in_=T1[:], axis=AX.X, op=ALU.add), ["T1"], ["rk"])
            dve(lambda e, oh=oh: e.tensor_tensor(out=T1[:], in0=oh[:], in1=ebc, op=ALU.mult), [ohn, "ebase"], ["T1"])
            dve(lambda e: e.tensor_reduce(out=sm["base"][:], in_=T1[:], axis=AX.X, op=ALU.add), ["T1"], ["base"])
            dve(lambda e: e.tensor_single_scalar(out=sm["valid"][:], in_=sm["rk"][:], scalar=float(CAP), op=ALU.is_lt),
                ["rk"], ["valid"])
            dve(lambda e: e.scalar_tensor_tensor(out=sm["sl"][:], in0=sm["rk"][:], scalar=-float(TRASH), in1=sm["base"][:],
                                                 op0=ALU.add, op1=ALU.add), ["rk", "base"], ["sl"])
            dve(lambda e: e.tensor_tensor(out=sm["sl"][:], in0=sm["sl"][:], in1=sm["valid"][:], op=ALU.mult), ["sl", "valid"], ["sl"])
            dve(lambda e: e.tensor_scalar(out=sm["sl"][:], in0=sm["sl"][:], scalar1=float(TRASH), scalar2=None, op0=ALU.add),
                ["sl"], ["sl"])
            dve(lambda e, k=k: e.tensor_copy(out=slot_i[:, k, :], in_=sm["sl"][:]), ["sl"], [("slot", k)])
            dve(lambda e, gn=gn: e.tensor_tensor(out=sm[gn][:], in0=sm[gn][:], in1=sm["valid"][:], op=ALU.mult),
                [gn, "valid"], [gn])

        if int(os.environ.get('MOE_STOP', '99')) <= 3:
            return
        for t in range(NT):
            b = t % NHB
            P.op("dve", lambda e, t=t, b=b: e.scalar_tensor_tensor(
                out=hnb[b][:], in0=h[:, t, :], scalar=rstd[:, t:t + 1], in1=gb[:], op0=ALU.mult, op1=ALU.mult),
                reads=[("h", t), "rstd", "gb"], writes=[("hnb", b)])
            for k in range(2):
                P.op("pool", lambda e, t=t, b=b, k=k: e.indirect_dma_start(
                    out=Xs[:, :], out_offset=bass.IndirectOffsetOnAxis(ap=slot_i[:, k, t:t + 1], axis=0),
                    in_=hnb[b][:], in_offset=None),
                    reads=[("hnb", b), ("slot", k)], writes=[("Xs_w", t, k)], dma=True)

        if int(os.environ.get('MOE_STOP', '99')) <= 4:
            dbg = [sm["sl"], sm["g1"], sm["g2"], sm["rk"], sm["base"], sm["valid"]]
            for q, tl in enumerate(dbg):
                P.op("dve", lambda e, q=q, tl=tl: e.tensor_copy(out=h[:, 0, q * 16:(q + 1) * 16], in_=tl[:]),
                     reads=["sl", "g1", "g2", "rk", "base", "valid"], writes=[("h", 0)])
            for k in range(2):
                P.op("dve", lambda e, k=k: e.tensor_copy(out=h[:, 0, 96 + k * 16:112 + k * 16], in_=slot_i[:, k, :]),
                     reads=[("slot", k)], writes=[("h", 0)])
            P.op("dve", lambda e: e.tensor_copy(out=h[:, 1, 0:576], in_=LG[:].rearrange("p n j -> p (n j)")),
                 reads=["LG"], writes=[("h", 1)])
            return
        P.barrier()
        xs_keys = [("Xs_w", t, k) for t in range(NT) for k in range(2)]

        def load_x(ex):
            s = ex % 2
            P.op("sp", lambda e: e.dma_start(
                out=xg[s][:], in_=Xs[ex * CAP:(ex + 1) * CAP, :].rearrange("(r p) d -> p r d", p=128)),
                reads=xs_keys, writes=[("xg", s)], dma=True)

        load_x(0)
        for ex in range(NE):
            s = ex % 2
            if ex + 1 < NE:
                load_x(ex + 1)
            for r in range(2):
                bank = ps[r]

                def tr2(e, r=r, s=s, bank=bank):
                    pv = bank[:].bitcast(BF16)
                    for c in range(8):
                        inst = e.transpose(out=pv[:, c * 128:(c + 1) * 128], in_=xg[s][:, r, c * 128:(c + 1) * 128],
                                           identity=self.ident_b[:])
                    return inst
                P.op("pe", tr2, reads=[("xg", s), "ident_b"], writes=[("ps", r)])
                src = lambda bank=bank: bank[:].bitcast(BF16).rearrange("p (c t) -> p c t", c=8)
                if r == 0:
                    P.op("act", lambda e, s=s, src=src: e.activation(out=xT[s][:, :, 0:128], in_=src(), func=AF.Copy),
                         reads=[("ps", 0)], writes=[("xT", s, 0)])
                else:
                    P.op("dve", lambda e, s=s, src=src: e.tensor_copy(out=xT[s][:, :, 128:256], in_=src()),
                         reads=[("ps", 1)], writes=[("xT", s, 1)])
            for m in range(4):
                bank = ps[2 + m % 2]
                bk = ("ps", 2 + m % 2)

                def gu(e, m=m, s=s, bank=bank):
                    for half in range(2):
                        col = half * FF + m * 128
                        for c in range(8):
                            inst = e.matmul(bank[:, half * CAP:(half + 1) * CAP], wgu[s][:, c, col:col + 128],
                                            xT[s][:, c, :], start=(c == 0), stop=(c == 7))
                    return inst
                P.op("pe", gu, reads=[("wgu", s), ("xT", s, 0), ("xT", s, 1)], writes=[bk])
                P.op("act", lambda e, m=m, bank=bank: e.activation(out=sA[m % 2][:], in_=bank[:, 0:CAP], func=AF.Silu),
                     reads=[bk], writes=[("sA", m % 2)])
                P.op("dve", lambda e, m=m, s=s, bank=bank: e.tensor_tensor(out=hT[s][:, m, :], in0=sA[m % 2][:],
                                                                          in1=bank[:, CAP:2 * CAP], op=ALU.mult),
                     reads=[bk, ("sA", m % 2)], writes=[("hT", s, m)])
            for r in range(2):
                for n in range(2):
                    q = r * 2 + n
                    bank = ps[4 + q % 2]
                    bk = ("ps", 4 + q % 2)

                    def dn(e, r=r, n=n, s=s, bank=bank):
                        for m in range(4):
                            inst = e.matmul(bank[:, :], hT[s][:, m, r * 128:(r + 1) * 128],
                                            wdn[s][:, m, n * 512:(n + 1) * 512], start=(m == 0), stop=(m == 3))
                        return inst
                    P.op("pe", dn, reads=[("wdn", s)] + [("hT", s, m) for m in range(4)], writes=[bk])
                    if q % 2 == 0:
                        P.op("act", lambda e, r=r, n=n, s=s, bank=bank: e.activation(
                            out=yt[s][:, r, n * 512:(n + 1) * 512], in_=bank[:, :], func=AF.Copy),
                            reads=[bk], writes=[("yt", s, q)])
                    else:
                        P.op("dve", lambda e, r=r, n=n, s=s, bank=bank: e.tensor_copy(
                            out=yt[s][:, r, n * 512:(n + 1) * 512], in_=bank[:, :]),
                            reads=[bk], writes=[("yt", s, q)])
            P.op("sp", lambda e, ex=ex, s=s: e.dma_start(
                out=Ys[ex * CAP:(ex + 1) * CAP, :].rearrange("(r p) d -> p r d", p=128), in_=yt[s][:]),
                reads=[("yt", s, q) for q in range(4)], writes=[("Ys_w", ex)], dma=True)
            if ex + 2 < NE:
                load_w(ex + 2)

        if int(os.environ.get('MOE_STOP', '99')) <= 5:
            return
        P.op("pool", lambda e: e.memset(fence[:], 0.0), writes=[("wgu", 0)] + [("yg", b) for b in range(NYG)])
        for t in range(NT):
            for k, gn in enumerate(("g1", "g2")):
                b = (t * 2 + k) % NYG
                P.op("pool", lambda e, t=t, b=b, k=k: e.indirect_dma_start(
                    out=yg[b][:], out_offset=None, in_=Ys[:, :],
                    in_offset=bass.IndirectOffsetOnAxis(ap=slot_i[:, k, t:t + 1], axis=0)),
                    reads=[("Ys_w", ex) for ex in range(NE)] + [("slot", k)], writes=[("yg", b)], dma=True)
                P.op("dve", lambda e, t=t, b=b, gn=gn: e.scalar_tensor_tensor(
                    out=h[:, t, :], in0=yg[b][:], scalar=sm[gn][:, t:t + 1], in1=h[:, t, :], op0=ALU.mult, op1=ALU.add),
                    reads=[("yg", b), gn, ("h", t)], writes=[("h", t)])

    def norm_transpose(self, hnT, hnb):
        P, h, ps = self.P, self.h, self.ps
        for t in range(NT):
            b = t % 2
            P.op("dve", lambda e, t=t, b=b: e.scalar_tensor_tensor(
                out=hnb[b][:], in0=h[:, t, :], scalar=self.rstd[:, t:t + 1], in1=self.gb[:], op0=ALU.mult, op1=ALU.mult),
                reads=[("h", t), "rstd", "gb"], writes=[("hnb", b)])
            self.transpose_tile(hnb[b], ("hnb", b), hnT, t, b)

    def transpose_tile(self, src, src_key, dstT, t, b):
        P, ps = self.P, self.ps
        bank = ps[b]

        def tr(e):
            pv = bank[:].bitcast(BF16)
            for c in range(8):
                inst = e.transpose(out=pv[:, c * 128:(c + 1) * 128], in_=src[:, c * 128:(c + 1) * 128],
                                   identity=self.ident_b[:])
            return inst
        P.op("pe", tr, reads=[src_key, "ident_b"], writes=[("ps", b)])
        srcv = lambda: bank[:].bitcast(BF16).rearrange("p (c t) -> p c t", c=8)
        if b == 0:
            P.op("act", lambda e: e.activation(out=dstT[:, :, t * 128:(t + 1) * 128], in_=srcv(), func=AF.Copy),
                 reads=[("ps", b)], writes=[("xT", t)])
        else:
            P.op("dve", lambda e: e.tensor_copy(out=dstT[:, :, t * 128:(t + 1) * 128], in_=srcv()),
                 reads=[("ps", b)], writes=[("xT", t)])

    def out_proj(self, xT, kc, wout, n_k):
        P, h, ps = self.P, self.h, self.ps
        for t in range(NT):
            for n in range(2):
                bi = 2 + (t * 2 + n) % 2
                bank = ps[bi]

                def mm(e, t=t, n=n, bank=bank):
                    for c in range(kc):
                        inst = e.matmul(bank[:, :], xT[:, c, t * 128:(t + 1) * 128], wout[:, c, n * 512:(n + 1) * 512],
                                        start=(c == 0), stop=(c == kc - 1))
                    return inst
                P.op("pe", mm, reads=[("xT", t), "wout"] if n_k is None else n_k(t) + ["wout"], writes=[("ps", bi)])
                P.op("dve", lambda e, t=t, n=n, bank=bank: e.tensor_tensor(
                    out=h[:, t, n * 512:(n + 1) * 512], in0=h[:, t, n * 512:(n + 1) * 512], in1=bank[:, :], op=ALU.add),
                    reads=[("ps", bi), ("h", t)], writes=[("h", t)])

    def fox_setup(self, layers):
        self.fox_layers = list(layers)
        nl = len(layers)
        self.din("fox_w_in", [nl, D, 4112])
        self.din("fox_b_f", [nl, 16])
        self.din("fox_w_out", [nl, D, D])
        self.din("c_mask", [128, 128])
        self.cum3_d = self.dscratch("cum3_d", [16, 3, S], BF16)
        self.Og_d = self.dscratch("Og_d", [S, D], BF16)
        self.mask_b = self.sb("mask_b", [128, 128], BF16)
        self.P.op("pool", lambda e: e.dma_start(out=self.mask_b[:], in_=self.dram["c_mask"]), writes=["mask_b"], dma=True)

    def fox_layer(self, layer):
        j = self.fox_layers.index(layer)
        P, h, ps, ar = self.P, self.h, self.ps, self.ar
        w_in = self.dram["fox_w_in"]
        P.barrier()
        self.arena_reset()
        hnT = ar("f_hnT", [128, 8, S], BF16)
        hnb = [ar("f_hnb%d" % s, [128, D], BF16) for s in range(2)]
        wf = ar("f_wf", [128, 8, 16], BF16)
        bft = ar("f_bft", [128, 1], F32)
        negbf = ar("f_negbf", [128, 1], F32)
        save = self.arena_off
        Ft = ar("f_Ft", [128, S], F32)
        cumP = ar("f_cumP", [128, S], F32)
        r1 = ar("f_r1", [128, S], F32)
        cum3 = ar("f_cum3", [128, 3, S], BF16)
        self.arena_off = save
        wgrp = [ar("f_wgrp%d" % s, [128, 8, 4, 128], BF16) for s in range(2)]
        QTa = [ar("f_QTa%d" % s, [128, S], BF16) for s in range(2)]
        KTa = [ar("f_KTa%d" % s, [128, S], BF16) for s in range(2)]
        Tst = [ar("f_Tst%d" % s, [128, S], BF16) for s in range(2)]
        Vaug = ar("f_Vaug", [128, NT, 2, 65], BF16)
        Gs = ar("f_Gs", [128, NT, 128], BF16)
        NPT = 6
        PT = [ar("f_PT%d" % s, [128, 512], BF16) for s in range(NPT)]
        Og = [ar("f_Og%d" % s, [128, NT, 128], BF16) for s in range(2)]
        wout = ar("f_wout", [128, 8, D], BF16)
        rden = ar("f_rden", [128, 8], F32)

        self.rms_stats(self.dram["norm_mix"][layer:layer + 1, :])
        P.op("pool", lambda e: e.dma_start(out=wf[:], in_=w_in[j][:, 4096:4112].rearrange("(c p) f -> p c f", p=128)),
             writes=["wf"], dma=True)
        P.op("sp", lambda e: e.dma_start(out=bft[0:16, :], in_=self.dram["fox_b_f"][j].rearrange("(h o) -> h o", o=1)),
             writes=["bft"], dma=True)
        P.op("dve", lambda e: e.tensor_scalar(out=negbf[0:16, :], in0=bft[0:16, :], scalar1=-1.0, scalar2=None, op0=ALU.mult),
             reads=["bft"], writes=["negbf"])
        self.norm_transpose(hnT, hnb)
        xT_all = [("xT", t) for t in range(NT)]

        if int(os.environ.get('FOX_STOP', '99')) <= 1:
            return
        for qd in range(4):
            bank = ps[2 + qd % 2]
            bk = ("ps", 2 + qd % 2)

            def fm(e, qd=qd, bank=bank):
                for c in range(8):
                    inst = e.matmul(bank[0:16, :], wf[:, c, :], hnT[:, c, qd * 512:(qd + 1) * 512], start=(c == 0), stop=(c == 7))
                return inst
            P.op("pe", fm, reads=xT_all + ["wf"], writes=[bk])
            P.op("act", lambda e, qd=qd, bank=bank: e.activation(out=Ft[0:16, qd * 512:(qd + 1) * 512], in_=bank[0:16, :],
                                                                func=AF.Exp, scale=-1.0, bias=negbf[0:16, :]),
                 reads=[bk, "negbf"], writes=["Ft"])
        P.op("act", lambda e: e.activation(out=Ft[0:16, :], in_=Ft[0:16, :], func=AF.Ln, bias=1.0), reads=["Ft"], writes=["Ft"])
        P.op("dve", lambda e: e.tensor_scalar(out=Ft[0:16, :], in0=Ft[0:16, :], scalar1=0.5, scalar2=None, op0=ALU.mult),
             reads=["Ft"], writes=["Ft"])
        P.op("dve", lambda e: e.tensor_tensor_scan(out=cumP[0:16, :], data0=Ft[0:16, :], data1=Ft[0:16, :], initial=0.0,
                                                   op0=ALU.add, op1=ALU.add), reads=["Ft"], writes=["cumP"])
        P.op("dve", lambda e: e.tensor_copy(out=cum3[0:16, 0, :], in_=cumP[0:16, :]), reads=["cumP"], writes=["cum3"])
        P.op("dve", lambda e: e.tensor_tensor(out=r1[0:16, :], in0=cumP[0:16, :], in1=cum3[0:16, 0, :], op=ALU.subtract),
             reads=["cumP", "cum3"], writes=["r1"])
        P.op("dve", lambda e: e.tensor_copy(out=cum3[0:16, 1, :], in_=r1[0:16, :]), reads=["r1"], writes=["cum3"])
        P.op("dve", lambda e: e.tensor_tensor(out=r1[0:16, :], in0=r1[0:16, :], in1=cum3[0:16, 1, :], op=ALU.subtract),
             reads=["r1", "cum3"], writes=["r1"])
        P.op("dve", lambda e: e.tensor_copy(out=cum3[0:16, 2, :], in_=r1[0:16, :]), reads=["r1"], writes=["cum3"])
        P.op("sp", lambda e: e.dma_start(out=self.cum3_d, in_=cum3[0:16, :, :]), reads=["cum3"], writes=["cum3_d"], dma=True)
        if int(os.environ.get('FOX_STOP', '99')) <= 2:
            return
        P.barrier()

        P.op("pool", lambda e: e.dma_start(out=wout[:], in_=self.dram["fox_w_out"][j].rearrange("(c p) f -> p c f", p=128)),
             writes=["wout"], dma=True)
        for hh in range(2):
            P.op("dve", lambda e, hh=hh: e.memset(QTa[hh][64:128, :], 0.0), writes=[("QTa", hh, "aug")])
            P.op("dve", lambda e, hh=hh: e.memset(KTa[hh][64:128, :], 0.0), writes=[("KTa", hh, "aug")])
            P.op("dve", lambda e, hh=hh: e.memset(QTa[hh][64:70, :], 1.0), writes=[("QTa", hh, "aug")])
            P.op("dve", lambda e, hh=hh: e.memset(KTa[hh][64:70, :], -1.0), writes=[("KTa", hh, "aug")])
        P.op("dve", lambda e: e.memset(Vaug[:, :, :, 64:65], 1.0), writes=["Vaug1"])

        def load_grp(g):
            s = g % 2
            for seg in range(4):
                P.op("pool", lambda e, seg=seg: e.dma_start(
                    out=wgrp[s][:, :, seg, :],
                    in_=w_in[j][:, seg * 1024 + g * 128: seg * 1024 + (g + 1) * 128].rearrange("(c p) f -> p c f", p=128)),
                    writes=[("wgrp", s, seg)], dma=True)

        if int(os.environ.get('FOX_STOP', '99')) <= 3:
            return
        load_grp(0)
        pt_rr = [0]
        ogv = self.Og_d.rearrange("(n p) d -> p n d", p=128)
        NG = int(os.environ.get('FOX_NG', '8'))
        for g in range(NG):
            s = g % 2
            if g + 1 < NG:
                load_grp(g + 1)
            for qk in range(2):
                dstT = QTa if qk == 0 else KTa
                dn = "QTa" if qk == 0 else "KTa"
                for qd in range(4):
                    bi = (qk * 4 + qd) % 2
                    bank = ps[bi]
                    cs = slice(qd * 512, (qd + 1) * 512)

                    def pm(e, qk=qk, qd=qd, bank=bank, s=s):
                        for c in range(8):
                            inst = e.matmul(bank[:, :], wgrp[s][:, c, qk, :],
                                            hnT[:, c, qd * 512:(qd + 1) * 512], start=(c == 0), stop=(c == 7))
                        return inst
                    P.op("pe", pm, reads=xT_all + [("wgrp", s, qk)], writes=[("ps", bi)])
                    if qk == 0:
                        P.op("act", lambda e, cs=cs, bank=bank: e.activation(
                            out=QTa[0][0:64, cs], in_=bank[0:64, :], func=AF.Identity, scale=0.125),
                            reads=[("ps", bi)], writes=[("QTa", 0, qd)])
                        P.op("act", lambda e, cs=cs, bank=bank: e.activation(
                            out=Tst[0][64:128, cs], in_=bank[64:128, :], func=AF.Identity, scale=0.125),
                            reads=[("ps", bi)], writes=[("Tst", 0, qd)])
                    else:
                        P.op("dve", lambda e, cs=cs, bank=bank: e.tensor_copy(out=KTa[0][0:64, cs], in_=bank[0:64, :]),
                             reads=[("ps", bi)], writes=[("KTa", 0, qd)])
                        P.op("dve", lambda e, cs=cs, bank=bank: e.tensor_copy(out=Tst[1][64:128, cs], in_=bank[64:128, :]),
                             reads=[("ps", bi)], writes=[("Tst", 1, qd)])
                P.op("sp", lambda e, qk=qk, dstT=dstT: e.dma_start(out=dstT[1][0:64, :], in_=Tst[qk][64:128, :]),
                     reads=[("Tst", qk, qd) for qd in range(4)], writes=[(dn, 1, qd) for qd in range(4)], dma=True)
            for hh in range(2):
                head = 2 * g + hh
                P.op("sp", lambda e, hh=hh, head=head: e.dma_start(out=QTa[hh][64:67, :], in_=self.cum3_d[head]),
                     reads=["cum3_d"], writes=[("QTa", hh, "aug")], dma=True)
                P.op("sp", lambda e, hh=hh, head=head: e.dma_start(out=KTa[hh][67:70, :], in_=self.cum3_d[head]),
                     reads=["cum3_d"], writes=[("KTa", hh, "aug")], dma=True)
            if int(os.environ.get('FOX_STOP', '99')) <= 4:
                return
            for t in range(NT):
                bi = t % 2
                bank = ps[bi]

                def vm(e, t=t, bank=bank, s=s):
                    for c in range(8):
                        inst = e.matmul(bank[:, 0:256], hnT[:, c, t * 128:(t + 1) * 128], wgrp[s][:, c, 2:4, :].rearrange("p a b -> p (a b)"),
                                        start=(c == 0), stop=(c == 7))
                    return inst
                P.op("pe", vm, reads=[("xT", t), ("wgrp", s, 2), ("wgrp", s, 3)], writes=[("ps", bi)])
                if os.environ.get("FOX_DBG", "") != "noV":
                    P.op("dve", lambda e, t=t, bank=bank: e.tensor_copy(
                        out=Vaug[:, t, :, 0:64], in_=bank[:, 0:128].rearrange("p (a b) -> p a b", a=2)),
                        reads=[("ps", bi)], writes=[("Vaug", t)])
                if os.environ.get("FOX_DBG", "") != "noG":
                    P.op("act", lambda e, t=t, bank=bank: e.activation(out=Gs[:, t, :], in_=bank[:, 128:256], func=AF.Sigmoid),
                         reads=[("ps", bi)], writes=[("Gs", t)])
            if int(os.environ.get('FOX_STOP', '99')) <= 5:
                allk = [("QTa", 0, q) for q in range(4)] + [("KTa", 0, q) for q in range(4)] + [("QTa", 0, "aug"), ("KTa", 0, "aug"), "Vaug1"] + [("Vaug", t) for t in range(NT)] + [("Gs", t) for t in range(NT)]
                def dd(dst, src):
                    P.op("dve", lambda e: e.tensor_copy(out=dst, in_=src), reads=allk, writes=[("h", i) for i in range(NT)])
                dd(h[:, 0, :], QTa[0][:, 0:1024]); dd(h[:, 1, :], QTa[0][:, 1024:2048])
                dd(h[:, 2, :], KTa[0][:, 0:1024]); dd(h[:, 3, :], KTa[0][:, 1024:2048])
                dd(h[:, 4, 0:130], Vaug[:, 0, :, :].rearrange("p a b -> p (a b)")); dd(h[:, 4, 256:384], Gs[:, 0, :])
                return
            items = []
            for hh in range(2):
                for qg in range(4):
                    for kb in range(4 * (qg + 1)):
                        items.append((hh, qg, kb))

            def rec_qk(it):
                hh, qg, kb = it
                q0 = max(0, kb - 4 * qg) * 128
                slot = pt_rr[0] % NPT
                pt_rr[0] += 1
                bi = 2 + slot % 2
                bank = ps[bi]
                P.op("pe", lambda e: e.matmul(bank[:, q0:512], KTa[hh][0:96, kb * 128:(kb + 1) * 128],
                                             QTa[hh][0:96, qg * 512 + q0:(qg + 1) * 512], start=True, stop=True),
                     reads=[("KTa", hh, kb // 4), ("KTa", hh, "aug"), ("QTa", hh, qg), ("QTa", hh, "aug")],
                     writes=[("ps", bi)])
                P.op("act", lambda e: e.activation(out=PT[slot][:, q0:512], in_=bank[:, q0:512], func=AF.Exp),
                     reads=[("ps", bi)], writes=[("PT", slot)])
                if kb >= 4 * qg:
                    P.op("dve", lambda e: e.tensor_tensor(out=PT[slot][:, q0:q0 + 128], in0=PT[slot][:, q0:q0 + 128],
                                                          in1=self.mask_b[:], op=ALU.mult),
                         reads=[("PT", slot), "mask_b"], writes=[("PT", slot)])
                return slot

            def rec_pv(it, slot):
                hh, qg, kb = it
                for qt in range(4):
                    i = 4 * qg + qt
                    if kb > i:
                        continue
                    bi = 4 + qt
                    P.op("pe", lambda e, qt=qt, i=i, bi=bi: e.matmul(
                        ps[bi][:, 0:65], PT[slot][:, qt * 128:(qt + 1) * 128], Vaug[:, kb, hh, :],
                        start=(kb == 0), stop=(kb == i)),
                        reads=[("PT", slot), ("Vaug", kb), "Vaug1"], writes=[("ps", bi)])
                    if kb == i:
                        rs = (i + hh) % 8
                        P.op("dve", lambda e, bi=bi, rs=rs: e.reciprocal(out=rden[:, rs:rs + 1], in_=ps[bi][:, 64:65]),
                             reads=[("ps", bi)], writes=[("rden", rs)])
                        P.op("dve", lambda e, bi=bi, rs=rs, i=i, s=s: e.scalar_tensor_tensor(
                            out=Og[s][:, i, hh * 64:(hh + 1) * 64], in0=ps[bi][:, 0:64], scalar=rden[:, rs:rs + 1],
                            in1=Gs[:, i, hh * 64:(hh + 1) * 64], op0=ALU.mult, op1=ALU.mult),
                            reads=[("ps", bi), ("rden", rs), ("Gs", i)], writes=[("Og", s, i)])

            slots = {}
            slots[0] = rec_qk(items[0])
            for n, it in enumerate(items):
                if n + 1 < len(items):
                    slots[n + 1] = rec_qk(items[n + 1])
                rec_pv(it, slots[n])
            P.op("sp", lambda e, g=g, s=s: e.dma_start(out=ogv[:, :, g * 128:(g + 1) * 128], in_=Og[s][:]),
                 reads=[("Og", s, i) for i in range(NT)], writes=[("Og_d", g)], dma=True)
            if int(os.environ.get('FOX_STOP', '99')) <= 6:
                allk = [("Og", s, i) for i in range(NT)]
                P.op("dve", lambda e: e.tensor_copy(out=h[:, 0, :], in_=Og[0][:, 0:8, :].rearrange("p a b -> p (a b)")),
                     reads=allk, writes=[("h", 0)])
                P.op("dve", lambda e: e.tensor_copy(out=h[:, 1, :], in_=Og[0][:, 8:16, :].rearrange("p a b -> p (a b)")),
                     reads=allk, writes=[("h", 1)])
                return

        if int(os.environ.get('FOX_STOP', '99')) <= 7:
            return
        for t in range(NT):
            b = t % 2
            P.op("sp", lambda e, t=t, b=b: e.dma_start(out=hnb[b][:], in_=ogv[:, t, :]),
                 reads=[("Og_d", g) for g in range(8)], writes=[("hnb", b)], dma=True)
            self.transpose_tile(hnb[b], ("hnb", b), hnT, t, b)
        self.out_proj(hnT, NG, wout, None)

    def ret_setup(self, layers):
        self.ret_layers = list(layers)
        nl = len(layers)
        self.din("ret_w_in", [nl, D, 6144])
        self.din("ret_gn_gain", [nl, 2048])
        self.din("ret_w_out", [nl, 2048, D])
        self.din("c_cos", [128, S])
        self.din("c_sin", [128, S])
        self.din("c_intraT", [128, 4, 128])
        self.din("c_rdec", [128, 12])
        self.Og2_d = self.dscratch("Og2_d", [S, 2048], BF16)

    def ret_layer(self, layer):
        j = self.ret_layers.index(layer)
        P, h, ps, ar = self.P, self.h, self.ps, self.ar
        w_in = self.dram["ret_w_in"]
        P.barrier()
        self.arena_reset()
        hnT = ar("r_hnT", [128, 8, S], BF16)
        hnb = [ar("r_hnb%d" % s, [128, D], BF16) for s in range(2)]
        wqk = [ar("r_wqk%d" % s, [128, 8, 512], BF16) for s in range(2)]
        wvg = ar("r_wvg", [128, 8, 1024], BF16)
        QT = ar("r_QT", [128, 2, S], BF16)
        KT = ar("r_KT", [128, 2, S], BF16)
        cosb = ar("r_cos", [128, S], BF16)
        sinb = ar("r_sin", [128, S], BF16)
        rt = [ar("r_rt%d" % s, [128, 512], F32) for s in range(4)]
        intraT = ar("r_intraT", [128, 4, 128], BF16)
        rdec = ar("r_rdec", [128, 12], F32)
        R32 = ar("r_R32", [128, 2, 512], F32)
        Rb = [ar("r_Rb%d" % s, [128, 2, 512], BF16) for s in range(2)]
        Vt = [ar("r_Vt%d" % s, [128, 512], BF16) for s in range(2)]
        Kd = [ar("r_Kd%d" % s, [128, 256], BF16) for s in range(2)]
        PTt = [ar("r_PT%d" % s, [128, 128], BF16) for s in range(2)]
        on = [ar("r_on%d" % s, [128, 512], F32) for s in range(2)]
        sg = [ar("r_sg%d" % s, [128, 512], BF16) for s in range(2)]
        ogo = [ar("r_ogo%d" % s, [128, 512], BF16) for s in range(2)]
        st6 = ar("r_st6", [128, 2, 6], F32)
        mv = ar("r_mv", [128, 2, 2], F32)
        sm = ar("r_sm", [128, 2, 4], F32)

        self.rms_stats(self.dram["norm_mix"][layer:layer + 1, :])
        P.op("pool", lambda e: e.dma_start(out=cosb[:], in_=self.dram["c_cos"]), writes=["cosb"], dma=True)
        P.op("pool", lambda e: e.dma_start(out=sinb[:], in_=self.dram["c_sin"]), writes=["sinb"], dma=True)
        P.op("pool", lambda e: e.dma_start(out=intraT[:], in_=self.dram["c_intraT"]), writes=["intraT"], dma=True)
        P.op("sp", lambda e: e.dma_start(out=rdec[:], in_=self.dram["c_rdec"]), writes=["rdec"], dma=True)
        self.norm_transpose(hnT, hnb)
        xT_all = [("xT", t) for t in range(NT)]
        og2v = self.Og2_d

        def load_qk(hd):
            s = hd % 2
            for qk in range(2):
                P.op("pool", lambda e, qk=qk: e.dma_start(
                    out=wqk[s][:, :, qk * 256:(qk + 1) * 256],
                    in_=w_in[j][:, qk * 1024 + hd * 256: qk * 1024 + (hd + 1) * 256].rearrange("(c p) f -> p c f", p=128)),
                    writes=[("wqk", s, qk)], dma=True)

        def load_vg(hd):
            for vg in range(2):
                P.op("pool", lambda e, vg=vg: e.dma_start(
                    out=wvg[:, :, vg * 512:(vg + 1) * 512],
                    in_=w_in[j][:, 2048 + vg * 2048 + hd * 512: 2048 + vg * 2048 + (hd + 1) * 512].rearrange("(c p) f -> p c f", p=128)),
                    writes=[("wvg", vg)], dma=True)

        load_qk(0)
        for hd in range(4):
            s = hd % 2
            gam = 1.0 - 2.0 ** (-5.0 - hd)
            gamC = float(np.float32(np.exp(np.float32(np.log(np.float32(gam))) * np.float32(128.0))))
            load_vg(hd)
            if hd + 1 < 4:
                load_qk(hd + 1)
            for qk in range(2):
                dst = QT if qk == 0 else KT
                dname = "QT" if qk == 0 else "KT"
                sc = 1.0 if qk == 0 else 0.0625
                for qd in range(4):
                    cs = slice(qd * 512, (qd + 1) * 512)
                    for half in range(2):
                        def pm(e, half=half, qk=qk, qd=qd, s=s):
                            col = qk * 256 + half * 128
                            for c in range(8):
                                inst = e.matmul(ps[half][:, :], wqk[s][:, c, col:col + 128], hnT[:, c, qd * 512:(qd + 1) * 512],
                                                start=(c == 0), stop=(c == 7))
                            return inst
                        P.op("pe", pm, reads=xT_all + [("wqk", s, qk)], writes=[("ps", half)])
                    for k4, (bank, tab, tn) in enumerate(((0, cosb, "cosb"), (1, sinb, "sinb"), (0, sinb, "sinb"), (1, cosb, "cosb"))):
                        P.op("dve", lambda e, k4=k4, bank=bank, tab=tab, cs=cs, sc=sc: e.scalar_tensor_tensor(
                            out=rt[k4][:], in0=ps[bank][:, :], scalar=sc, in1=tab[:, cs], op0=ALU.mult, op1=ALU.mult),
                            reads=[("ps", bank), tn], writes=[("rt", k4)])
                    P.op("pool", lambda e, dst=dst, cs=cs: e.tensor_tensor(out=dst[:, 0, cs], in0=rt[0][:], in1=rt[1][:], op=ALU.subtract),
                         reads=[("rt", 0), ("rt", 1)], writes=[(dname, qd)])
                    P.op("pool", lambda e, dst=dst, cs=cs: e.tensor_tensor(out=dst[:, 1, cs], in0=rt[2][:], in1=rt[3][:], op=ALU.add),
                         reads=[("rt", 2), ("rt", 3)], writes=[(dname, qd)])
            P.op("dve", lambda e: e.memset(R32[:], 0.0), writes=[("R32", 0), ("R32", 1)])
            for n in range(NT):
                b = n % 2
                cols = slice(n * 128, (n + 1) * 128)
                qdk = n // 4

                def ktr(e, cols=cols, b=b):
                    pv = ps[0][:].bitcast(BF16)
                    for c in range(2):
                        inst = e.transpose(out=pv[:, c * 128:(c + 1) * 128], in_=KT[:, c, cols], identity=self.ident_b[:])
                    return inst
                P.op("pe", ktr, reads=[("KT", qdk), "ident_b"], writes=[("ps", 0)])
                P.op("act", lambda e, b=b, hd=hd: e.activation(out=Kd[b][:], in_=ps[0][:].bitcast(BF16)[:, 0:256], func=AF.Identity,
                                                               scale=rdec[:, 8 + hd:9 + hd]),
                     reads=[("ps", 0), "rdec"], writes=[("Kd", b)])

                def smm(e, cols=cols):
                    for c in range(2):
                        inst = e.matmul(ps[1][:, 0:128], KT[:, c, cols], QT[:, c, cols], start=(c == 0), stop=(c == 1))
                    return inst
                P.op("pe", smm, reads=[("KT", qdk), ("QT", qdk)], writes=[("ps", 1)])
                P.op("dve", lambda e, b=b, hd=hd: e.tensor_tensor(out=PTt[b][:], in0=ps[1][:, 0:128], in1=intraT[:, hd, :], op=ALU.mult),
                     reads=[("ps", 1), "intraT"], writes=[("PTt", b)])

                def vmm(e, cols=cols):
                    for c in range(8):
                        inst = e.matmul(ps[2][:, :], hnT[:, c, cols], wvg[:, c, 0:512], start=(c == 0), stop=(c == 7))
                    return inst
                P.op("pe", vmm, reads=[("xT", n), ("wvg", 0)], writes=[("ps", 2)])
                P.op("act", lambda e, b=b: e.activation(out=Vt[b][:], in_=ps[2][:, :], func=AF.Copy),
                     reads=[("ps", 2)], writes=[("Vt", b)])

                def gmm(e, cols=cols):
                    for c in range(8):
                        inst = e.matmul(ps[3][:, :], hnT[:, c, cols], wvg[:, c, 512:1024], start=(c == 0), stop=(c == 7))
                    return inst
                P.op("pe", gmm, reads=[("xT", n), ("wvg", 1)], writes=[("ps", 3)])
                P.op("act", lambda e, b=b: e.activation(out=sg[b][:], in_=ps[3][:, :], func=AF.Silu),
                     reads=[("ps", 3)], writes=[("sg", b)])

                if n < NT - 1:
                    rbn = (n + 1) % 2
                    for c in range(2):
                        P.op("pe", lambda e, c=c, b=b: e.matmul(ps[5 + c][:, :], Kd[b][:, c * 128:(c + 1) * 128], Vt[b][:],
                                                               start=True, stop=True),
                             reads=[("Kd", b), ("Vt", b)], writes=[("ps", 5 + c)])
                        P.op("dve", lambda e, c=c, gamC=gamC: e.scalar_tensor_tensor(
                            out=R32[:, c, :], in0=R32[:, c, :], scalar=gamC, in1=ps[5 + c][:, :], op0=ALU.mult, op1=ALU.add),
                            reads=[("ps", 5 + c), ("R32", c)], writes=[("R32", c)])
                        P.op("act", lambda e, c=c, rbn=rbn: e.activation(out=Rb[rbn][:, c, :], in_=R32[:, c, :], func=AF.Copy),
                             reads=[("R32", c)], writes=[("Rb", rbn, c)])

                def omm(e, cols=cols, b=b, n=n):
                    inst = e.matmul(ps[4][:, :], PTt[b][:], Vt[b][:], start=True, stop=(n == 0))
                    if n > 0:
                        for c in range(2):
                            inst = e.matmul(ps[4][:, :], QT[:, c, cols], Rb[n % 2][:, c, :], start=False, stop=(c == 1))
                    return inst
                P.op("pe", omm, reads=[("PTt", b), ("Vt", b), ("QT", qdk), ("Rb", n % 2, 0), ("Rb", n % 2, 1)], writes=[("ps", 4)])
                P.op("dve", lambda e, b=b: e.bn_stats(out=st6[:, b, :], in_=ps[4][:, :]), reads=[("ps", 4)], writes=[("st6", b)])
                P.op("dve", lambda e, b=b: e.bn_aggr(out=mv[:, b, :], in_=st6[:, b, :]), reads=[("st6", b)], writes=[("mv", b)])
                P.op("dve", lambda e, b=b, hd=hd: e.tensor_scalar(out=sm[:, b, 0:1], in0=mv[:, b, 1:2], scalar1=rdec[:, 4 + hd:5 + hd],
                                                                 scalar2=EPS, op0=ALU.mult, op1=ALU.add),
                     reads=[("mv", b), "rdec"], writes=[("sm", b, 0)])
                P.op("pool", lambda e, b=b: e.tensor_tensor(out=sm[:, b, 1:2], in0=sm[:, b, 0:1], in1=self.negh[:, 0:1], op=ALU.pow),
                     reads=[("sm", b, 0), "negh"], writes=[("sm", b, 1)])
                P.op("dve", lambda e, b=b, hd=hd: e.tensor_tensor(out=sm[:, b, 2:3], in0=sm[:, b, 1:2], in1=rdec[:, hd:hd + 1], op=ALU.mult),
                     reads=[("sm", b, 1), "rdec"], writes=[("sm", b, 2)])
                P.op("dve", lambda e, b=b: e.scalar_tensor_tensor(out=sm[:, b, 3:4], in0=mv[:, b, 0:1], scalar=-1.0, in1=sm[:, b, 2:3],
                                                                 op0=ALU.mult, op1=ALU.mult),
                     reads=[("mv", b), ("sm", b, 2)], writes=[("sm", b, 3)])
                P.op("act", lambda e, b=b: e.activation(out=on[b][:], in_=ps[4][:, :], func=AF.Identity,
                                                        scale=sm[:, b, 2:3], bias=sm[:, b, 3:4]),
                     reads=[("ps", 4), ("sm", b, 2), ("sm", b, 3)], writes=[("on", b)])
                P.op("pool", lambda e, b=b: e.tensor_tensor(out=ogo[b][:], in0=on[b][:], in1=sg[b][:], op=ALU.mult),
                     reads=[("on", b), ("sg", b)], writes=[("ogo", b)])
                P.op("sp", lambda e, b=b, n=n, hd=hd: e.dma_start(out=og2v[n * 128:(n + 1) * 128, hd * 512:(hd + 1) * 512], in_=ogo[b][:]),
                     reads=[("ogo", b)], writes=[("Og2_d", n, hd)], dma=True)

        P.barrier()
        self.arena_reset()
        wout = ar("r_wout", [128, 16, D], BF16)
        ogt = [ar("r_ogt%d" % s, [128, 2048], BF16) for s in range(2)]
        OgT = [ar("r_OgT%d" % s, [128, 16, 128], BF16) for s in range(2)]
        gcol = ar("r_gcol", [128, 16], F32)
        P.op("pool", lambda e: e.dma_start(out=wout[:], in_=self.dram["ret_w_out"][j].rearrange("(c p) f -> p c f", p=128)),
             writes=["wout"], dma=True)
        P.op("sp", lambda e: e.dma_start(out=gcol[:], in_=self.dram["ret_gn_gain"][j].rearrange("(c p) -> p c", p=128),
                                         allow_slow_non_contiguous=True), writes=["gcol"], dma=True)
        for c in range(16):
            P.op("dve", lambda e, c=c: e.tensor_scalar(out=wout[:, c, :], in0=wout[:, c, :], scalar1=gcol[:, c:c + 1], scalar2=None,
                                                     op0=ALU.mult), reads=["wout", "gcol"], writes=["wout"])
        for t in range(NT):
            b = t % 2
            P.op("sp", lambda e, t=t, b=b: e.dma_start(out=ogt[b][:], in_=og2v[t * 128:(t + 1) * 128, :]),
                 writes=[("ogt", b)], dma=True)
            for half in range(2):
                def tr(e, half=half, b=b):
                    pv = ps[half][:].bitcast(BF16)
                    for c in range(8):
                        cc = half * 8 + c
                        inst = e.transpose(out=pv[:, c * 128:(c + 1) * 128], in_=ogt[b][:, cc * 128:(cc + 1) * 128],
                                           identity=self.ident_b[:])
                    return inst
                P.op("pe", tr, reads=[("ogt", b), "ident_b"], writes=[("ps", half)])
                srcv = lambda half=half: ps[half][:].bitcast(BF16).rearrange("p (c t) -> p c t", c=8)
                if half == 0:
                    P.op("act", lambda e, b=b, srcv=srcv: e.activation(out=OgT[b][:, 0:8, :], in_=srcv(), func=AF.Copy),
                         reads=[("ps", 0)], writes=[("OgT", b, 0)])
                else:
                    P.op("dve", lambda e, b=b, srcv=srcv: e.tensor_copy(out=OgT[b][:, 8:16, :], in_=srcv()),
                         reads=[("ps", 1)], writes=[("OgT", b, 1)])
            for nn in range(2):
                bi = 2 + (t * 2 + nn) % 2

                def mm(e, nn=nn, b=b, bi=bi):
                    for c in range(16):
                        inst = e.matmul(ps[bi][:, :], OgT[b][:, c, :], wout[:, c, nn * 512:(nn + 1) * 512],
                                        start=(c == 0), stop=(c == 15))
                    return inst
                P.op("pe", mm, reads=[("OgT", b, 0), ("OgT", b, 1), "wout"], writes=[("ps", bi)])
                P.op("dve", lambda e, t=t, nn=nn, bi=bi: e.tensor_tensor(
                    out=h[:, t, nn * 512:(nn + 1) * 512], in0=h[:, t, nn * 512:(nn + 1) * 512], in1=ps[bi][:, :], op=ALU.add),
                    reads=[("ps", bi), ("h", t)], writes=[("h", t)])


def build(stages=None, final_norm=True):
    B = Builder()
    B.prologue()
    if stages is None:
        stages = []
        for i in range(DEPTH):
            stages += ["mix%d" % i, "moe%d" % i]
    moe_layers = sorted(set(int(s[3:]) for s in stages if s.startswith("moe")))
    if moe_layers:
        B.moe_setup(moe_layers)
    fox_layers = sorted(set(int(s[3:]) for s in stages if s.startswith("mix") and int(s[3:]) % 2 == 0))
    if fox_layers:
        B.fox_setup(fox_layers)
    ret_layers = sorted(set(int(s[3:]) for s in stages if s.startswith("mix") and int(s[3:]) % 2 == 1))
    if ret_layers:
        B.ret_setup(ret_layers)
    B.layer_sets = {"moe": moe_layers, "fox": fox_layers, "ret": ret_layers}
    for s in stages:
        li = int(s[3:])
        if s.startswith("moe"):
            B.moe_layer(li)
        elif li % 2 == 0:
            B.fox_layer(li)
        else:
            B.ret_layer(li)
    B.epilogue(final_norm=final_norm)
    nc = B.finish()
    _LAYER_SETS[id(nc)] = B.layer_sets
    return nc


def _consts():
    ident = np.eye(128, dtype=np.float32)
    lst = np.triu(np.ones((128, 128), np.float32), k=1)
    ebase = (np.arange(NE, dtype=np.float32) * CAP).reshape(1, NE)
    mask = np.triu(np.ones((128, 128), np.float32), k=0)
    inv = (np.float32(1.0) / (np.float32(10000.0) ** np.linspace(0.0, 1.0, 128, dtype=np.float32))).astype(np.float32)
    ang = (np.arange(S, dtype=np.float32)[None, :] * inv[:, None]).astype(np.float32)
    cosT = np.cos(ang).astype(np.float32)
    sinT = np.sin(ang).astype(np.float32)
    log_g = np.log(np.float32(1.0) - np.float32(2.0) ** (np.float32(-5.0) - np.arange(4, dtype=np.float32))).astype(np.float32)
    idx = np.arange(128, dtype=np.float32)
    intraT = np.zeros((128, 4, 128), np.float32)
    rdec = np.zeros((128, 12), np.float32)
    for hd in range(4):
        col = np.exp(-log_g[hd] * (idx + 1.0)).astype(np.float32)
        intraT[:, hd, :] = np.where(idx[None, :] >= idx[:, None], col[:, None], 0.0)
        qd = np.exp(log_g[hd] * (idx + 1.0)).astype(np.float32)
        rdec[:, hd] = qd
        rdec[:, 4 + hd] = qd * qd
        rdec[:, 8 + hd] = np.exp(log_g[hd] * (127.0 - idx)).astype(np.float32)
    return {"c_ident": ident, "c_lst": lst, "c_ebase": ebase, "c_mask": mask,
            "c_cos": cosT, "c_sin": sinT, "c_intraT": intraT, "c_rdec": rdec}


_CACHE = {}


def prep_inputs(inputs, nc_inputs, layer_sets):
    f = lambda a: np.ascontiguousarray(np.asarray(a), dtype=np.float32)
    shared = {}
    shared["norm_mix"] = f(inputs["norm_mix"])
    shared["norm_ffn"] = f(inputs["norm_ffn"])
    shared["norm_final"] = f(inputs["norm_final"]).reshape(1, D)
    shared.update(_consts())
    if "router_w" in nc_inputs:
        ml = layer_sets["moe"]
        shared["router_w"] = np.ascontiguousarray(
            np.concatenate([f(inputs["router_group_w"]), f(inputs["router_expert_w"])], axis=-1)[ml])
        shared["router_b"] = np.ascontiguousarray(
            np.concatenate([f(inputs["router_group_b"]), f(inputs["router_expert_b"])], axis=-1)[ml])
        for p, l in enumerate(ml):
            shared["expert_w_gu%d" % p] = f(inputs["expert_w_gu"][l])
            shared["expert_w_down%d" % p] = f(inputs["expert_w_down"][l])
    if "fox_w_in" in nc_inputs:
        fl = [l // 2 for l in layer_sets["fox"]]
        shared["fox_w_in"] = np.ascontiguousarray(f(inputs["fox_w_in"])[fl])
        shared["fox_b_f"] = np.ascontiguousarray(f(inputs["fox_b_f"])[fl])
        shared["fox_w_out"] = np.ascontiguousarray(f(inputs["fox_w_out"])[fl])
    if "ret_w_in" in nc_inputs:
        rl = [l // 2 for l in layer_sets["ret"]]
        shared["ret_w_in"] = np.ascontiguousarray(f(inputs["ret_w_in"])[rl])
        shared["ret_gn_gain"] = np.ascontiguousarray(f(inputs["ret_gn_gain"])[rl])
        shared["ret_w_out"] = np.ascontiguousarray(f(inputs["ret_w_out"])[rl])
    x = f(inputs["x"])
    in_maps = []
    for b in range(NCORES):
        m = {k: v for k, v in shared.items() if k in nc_inputs}
        m["x"] = x[b]
        in_maps.append(m)
    return in_maps


def run(inputs, stages=None, final_norm=True, trace=False):
    key = (tuple(stages) if stages is not None else None, final_norm)
    if key not in _CACHE:
        B_nc = build(stages, final_norm)
        _CACHE[key] = B_nc
    nc = _CACHE[key]
    names = set(_DRAM_NAMES[id(nc)])
    in_maps = prep_inputs(inputs, names, _LAYER_SETS[id(nc)])
    res = run_bass_kernel_spmd(nc, in_maps, core_ids=list(range(NCORES)), trace=trace)
    out = np.stack([np.asarray(r["y"]) for r in res.results], axis=0).astype(np.float32)
    return out, res


def kernel(**inputs):
    out, _ = run(inputs)
    return out
```

```python
import contextlib
import os
import numpy as np
import concourse.bass as bass
import concourse.mybir as mybir
from concourse.bass_utils import run_bass_kernel_spmd

F32 = mybir.dt.float32
BF16 = mybir.dt.bfloat16
I32 = mybir.dt.int32
AF = mybir.ActivationFunctionType
ALU = mybir.AluOpType
AX = mybir.AxisListType

D = 1024
S = 2048
NT = S // 128
DEPTH = 4
EPS = 1e-6
NCORES = 8

ENGINES = ("pe", "act", "dve", "pool", "sp")


class _Op:
    __slots__ = ("eng", "fn", "dma", "waits", "signal", "idx")

    def __init__(self, eng, fn, dma, idx):
        self.eng = eng
        self.fn = fn
        self.dma = dma
        self.waits = []
        self.signal = None
        self.idx = idx


class Prog:
    N_DMA_SEMS = 24

    def __init__(self, same_engine_sync=True):
        self.ops = []
        self.last_writer = {}
        self.readers = {}
        self.same_engine_sync = same_engine_sync
        self.dependents = {}
        self.deps = []
        self.forced = set()
        self.pending = {e: set() for e in ENGINES}
        self.last_op = {}
        self.unfenced_dma = set()

    def op(self, eng, fn, reads=(), writes=(), dma=False, force=False):
        idx = len(self.ops)
        o = _Op(eng, fn, dma, idx)
        if force:
            self.forced.add(idx)
        ps_reads = [k for k in reads if isinstance(k, tuple) and k[0] == "ps"]
        if ps_reads:
            reads = [k for k in reads if k not in ps_reads]
            writes = list(writes) + ps_reads
        deps = set()
        for k in reads:
            w = self.last_writer.get(k)
            if w is not None:
                deps.add(w)
        for k in writes:
            w = self.last_writer.get(k)
            if w is not None:
                deps.add(w)
            for r in self.readers.get(k, ()):
                deps.add(r)
        deps |= self.pending[eng]
        self.pending[eng] = set()
        deps.discard(idx)
        self.last_op[eng] = idx
        if dma:
            self.unfenced_dma.add(idx)
        for k in writes:
            self.last_writer[k] = idx
            self.readers[k] = []
        for k in reads:
            if k not in writes:
                self.readers.setdefault(k, []).append(idx)
        self.ops.append(o)
        self.deps.append(deps)
        return idx

    def barrier(self):
        carry = getattr(self, "carry_dma", set())
        deps = set(self.last_op.values()) | (self.unfenced_dma - carry)
        self.unfenced_dma = set(carry)
        self.carry_dma = set()
        for e in ENGINES:
            self.pending[e] |= deps

    def finalize(self):
        ops = self.ops
        needed = [i in self.forced for i in range(len(ops))]
        for o in ops:
            for d in self.deps[o.idx]:
                p = ops[d]
                if p.dma:
                    needed[d] = True
                elif p.eng == o.eng and not o.dma:
                    if p.eng == "pe":
                        continue
                    if self.same_engine_sync:
                        needed[d] = True
                else:
                    needed[d] = True
        eng_cnt = {e: 0 for e in ENGINES}
        dma_cnt = [0] * self.N_DMA_SEMS
        dma_rr = 0
        seen = {e: {} for e in ENGINES}
        for o in ops:
            waits = {}
            for d in self.deps[o.idx]:
                p = ops[d]
                if p.signal is None:
                    continue
                if (not p.dma) and p.eng == o.eng and not o.dma and (p.eng == "pe" or not self.same_engine_sync):
                    continue
                k, v, _ = p.signal
                if seen[o.eng].get(k, 0) >= v:
                    continue
                waits[k] = max(waits.get(k, 0), v)
            if o.dma and needed[o.idx]:
                j = dma_rr
                dma_rr = (dma_rr + 1) % self.N_DMA_SEMS
                k = ("dma", j)
                if dma_cnt[j] > 0 and seen[o.eng].get(k, 0) < dma_cnt[j]:
                    waits[k] = max(waits.get(k, 0), dma_cnt[j])
                dma_cnt[j] += 16
                o.signal = (k, dma_cnt[j], 16)
            elif needed[o.idx]:
                eng_cnt[o.eng] += 1
                o.signal = (("eng", o.eng), eng_cnt[o.eng], 1)
            for k, v in waits.items():
                seen[o.eng][k] = max(seen[o.eng].get(k, 0), v)
            o.waits = sorted(waits.items(), key=lambda kv: str(kv[0]))
        self.max_counts = dict(eng_cnt)

    def emit(self, nc, stack, final_waits):
        sems = {}
        for e in ENGINES:
            sems[("eng", e)] = stack.enter_context(nc.semaphore("sem_" + e))
        for j in range(self.N_DMA_SEMS):
            sems[("dma", j)] = stack.enter_context(nc.semaphore("sem_dma%d" % j))
        block = stack.enter_context(nc.Block())
        by_eng = {e: [o for o in self.ops if o.eng == e] for e in ENGINES}
        last_signal = {}
        for o in self.ops:
            if o.signal is not None:
                last_signal[o.signal[0]] = max(last_signal.get(o.signal[0], 0), o.signal[1])

        def run(engine, ename):
            for o in by_eng[ename]:
                for k, v in o.waits:
                    engine.wait_ge(sems[k], v)
                inst = o.fn(engine)
                if o.signal is not None:
                    inst.then_inc(sems[o.signal[0]], o.signal[2])
            if ename == final_waits:
                for k, v in last_signal.items():
                    if k[0] == "dma":
                        engine.wait_ge(sems[k], v)

        @block.tensor
        def _(e):
            run(e, "pe")

        @block.scalar
        def _(e):
            run(e, "act")

        @block.vector
        def _(e):
            run(e, "dve")

        @block.gpsimd
        def _(e):
            run(e, "pool")

        @block.sync
        def _(e):
            run(e, "sp")


NE = 32
CAP = 256
TRASH = NE * CAP
NSLOT = TRASH + 128
FF = 512
BIG = 30000.0


_DRAM_NAMES = {}
_LAYER_SETS = {}


class Builder:
    def __init__(self):
        self.nc = nc = bass.Bass("TRN2", target_bir_lowering=False)
        self.P = Prog()
        self.stack = contextlib.ExitStack()
        self.mem_stack = contextlib.ExitStack()
        self.dram = {}
        self.ps = [self.mem_stack.enter_context(nc.psum_tensor("ps%d" % b, [128, 512], F32)) for b in range(8)]
        sb = self.sb
        self.h = sb("h", [128, NT, D], F32)
        self.gb = sb("gb", [128, D], F32)
        self.ssq = sb("ssq", [128, NT], F32)
        self.rstd = sb("rstd", [128, NT], F32)
        self.junk = sb("junk", [128, D], BF16)
        self.ident_b = sb("ident_b", [128, 128], BF16)
        self.ident_f = sb("ident_f", [128, 128], F32)
        self.lst_b = sb("lst_b", [128, 128], BF16)
        self.ones_b = sb("ones_b", [128, 128], BF16)
        self.ebase = sb("ebase", [128, NE], F32)
        self.negh = sb("negh", [128, NT], F32)
        self.arena_size = 131 * 1024
        self.arena_base, _ = nc.bump_sbuf(self.arena_size)
        self.arena_off = 0

    def sb(self, name, shape, dt):
        return self.mem_stack.enter_context(self.nc.sbuf_tensor(name, list(shape), dt))

    def arena_reset(self):
        self.arena_off = 0

    def ar(self, name, shape, dt):
        nbytes = int(np.prod(shape[1:])) * (4 if dt in (F32, I32) else 2)
        nbytes = (nbytes + 31) // 32 * 32
        assert self.arena_off + nbytes <= self.arena_size, (name, self.arena_off, nbytes)
        t = self.nc.alloc_sbuf_tensor_at(name, list(shape), dt, offset=self.arena_base + self.arena_off)
        self.arena_off += nbytes
        return t

    def din(self, name, shape, dt=F32):
        t = self.nc.dram_tensor(name, list(shape), dt, kind="ExternalInput").ap()
        self.dram[name] = t
        return t

    def dscratch(self, name, shape, dt):
        return self.nc.dram_tensor(name, list(shape), dt, kind="Internal").ap()

    def prologue(self):
        P = self.P
        x = self.din("x", [S, D])
        self.din("norm_mix", [DEPTH, D])
        self.din("norm_ffn", [DEPTH, D])
        self.din("norm_final", [1, D])
        cI = self.din("c_ident", [128, 128])
        cL = self.din("c_lst", [128, 128])
        cE = self.din("c_ebase", [1, NE])
        self.y = self.nc.dram_tensor("y", [S, D], F32, kind="ExternalOutput").ap()
        xv = x.rearrange("(n p) d -> p n d", p=128)
        h = self.h
        for q in range(4):
            sl = slice(q * 4, (q + 1) * 4)
            P.op("sp", lambda e, sl=sl: e.dma_start(out=h[:, sl, :], in_=xv[:, sl, :]),
                 writes=[("h", i) for i in range(q * 4, q * 4 + 4)], dma=True)
        P.op("sp", lambda e: e.dma_start(out=self.ident_f[:], in_=cI), writes=["ident_f"], dma=True)
        P.op("pool", lambda e: e.dma_start(out=self.ident_b[:], in_=cI), writes=["ident_b"], dma=True)
        P.op("pool", lambda e: e.dma_start(out=self.lst_b[:], in_=cL), writes=["lst_b"], dma=True)
        P.op("sp", lambda e: e.dma_start(out=self.ebase[:], in_=cE[0:1, :].partition_broadcast(128)),
             writes=["ebase"], dma=True)
        P.op("dve", lambda e: e.memset(self.ones_b[:], 1.0), writes=["ones_b"])
        P.op("dve", lambda e: e.memset(self.negh[:], -0.5), writes=["negh"])

    def rms_stats(self, gain_row_ap):
        P, h, ssq, rstd, junk, gb = self.P, self.h, self.ssq, self.rstd, self.junk, self.gb
        P.op("sp", lambda e: e.dma_start(out=gb[:], in_=gain_row_ap.partition_broadcast(128)),
             writes=["gb"], dma=True)
        for i in range(NT):
            P.op("act", lambda e, i=i: e.activation(out=junk[:], in_=h[:, i, :], func=AF.Square,
                                                     accum_out=ssq[:, i:i + 1]),
                 reads=[("h", i)], writes=["junk", ("ssq", i)])
        P.op("dve", lambda e: e.tensor_scalar(out=rstd[:], in0=ssq[:], scalar1=1.0 / D, scalar2=EPS,
                                              op0=ALU.mult, op1=ALU.add),
             reads=[("ssq", i) for i in range(NT)], writes=["rstd"])
        P.op("pool", lambda e: e.tensor_tensor(out=rstd[:], in0=rstd[:], in1=self.negh[:], op=ALU.pow),
             reads=["rstd", "negh"], writes=["rstd"])

    def epilogue(self, final_norm=True):
        P, h = self.P, self.h
        P.barrier()
        self.arena_reset()
        outt = self.ar("outt", [128, 2, D], F32)
        yv = self.y.rearrange("(n p) d -> p n d", p=128)
        if final_norm:
            self.rms_stats(self.dram["norm_final"][0:1, :])
        for i in range(NT):
            b = i % 2
            if final_norm:
                P.op("dve", lambda e, i=i, b=b: e.scalar_tensor_tensor(
                    out=outt[:, b, :], in0=h[:, i, :], scalar=self.rstd[:, i:i + 1], in1=self.gb[:],
                    op0=ALU.mult, op1=ALU.mult),
                    reads=[("h", i), "rstd", "gb"], writes=[("outt", b)])
                P.op("sp", lambda e, i=i, b=b: e.dma_start(out=yv[:, i, :], in_=outt[:, b, :]),
                     reads=[("outt", b)], writes=[("y", i)], dma=True, force=True)
            else:
                P.op("sp", lambda e, i=i: e.dma_start(out=yv[:, i, :], in_=h[:, i, :]),
                     reads=[("h", i)], writes=[("y", i)], dma=True, force=True)

    def finish(self):
        _DRAM_NAMES[id(self.nc)] = list(self.dram.keys())
        self.P.finalize()
        self.P.emit(self.nc, self.stack, final_waits="sp")
        self.stack.close()
        return self.nc

    def moe_setup(self, layers):
        self.moe_layers = list(layers)
        nl = len(self.moe_layers)
        self.din("router_w", [nl, D, 36])
        self.din("router_b", [nl, 36])
        for p in range(nl):
            self.din("expert_w_gu%d" % p, [NE, D, 2 * FF])
            self.din("expert_w_down%d" % p, [NE, FF, D])
        self.Xs = self.dscratch("Xs", [NSLOT, D], BF16)
        self.Ys = self.dscratch("Ys", [NSLOT, D], BF16)
        P = self.P
        self.arena_reset()
        self.zeros_b = self.ar("zeros_b", [128, D], BF16)
        P.op("dve", lambda e: e.memset(self.zeros_b[:], 0.0), writes=["zeros_b"])
        self.zeroed = False

    def moe_layer(self, layer):
        li = self.moe_layers.index(layer)
        P, h, ps = self.P, self.h, self.ps
        rstd, gb = self.rstd, self.gb
        P.barrier()
        self.arena_reset()
        ar = self.ar
        wgu = [ar("wgu%d" % s, [128, 8, 2 * FF], BF16) for s in range(2)]
        wdn = [ar("wdn%d" % s, [128, 4, D], BF16) for s in range(2)]
        NYG = 8
        yg = [self.nc.alloc_sbuf_tensor_at("yg%d_%d" % (layer, s), [128, D], BF16, offset=self.arena_base + s * 2048)
              for s in range(NYG)]
        save = self.arena_off
        NRB = 3
        hn32 = [ar("hn32_%d" % s, [128, D], F32) for s in range(NRB)]
        hhi = [ar("hhi%d" % s, [128, D], BF16) for s in range(NRB)]
        hlo = [ar("hlo%d" % s, [128, D], BF16) for s in range(NRB)]
        hiT = [ar("hiT%d" % s, [128, 8, 128], BF16) for s in range(NRB)]
        loT = [ar("loT%d" % s, [128, 8, 128], BF16) for s in range(NRB)]
        end1 = self.arena_off
        self.arena_off = save
        xg = [ar("xg%d" % s, [128, 2, D], BF16) for s in range(2)]
        xT = [ar("xT%d" % s, [128, 8, CAP], BF16) for s in range(2)]
        hT = [ar("hT%d" % s, [128, 4, CAP], BF16) for s in range(2)]
        sA = [ar("sA%d" % s, [128, CAP], F32) for s in range(2)]
        yt = [ar("yt%d" % s, [128, 2, D], BF16) for s in range(2)]
        self.arena_off = max(self.arena_off, end1)
        NHB = 4
        hnb = [ar("hnb%d" % s, [128, D], BF16) for s in range(NHB)]
        fence = ar("fence", [128, 8], F32)
        fence2 = ar("fence2", [128, D], BF16)
        wr_hi = ar("wr_hi", [128, 8, 36], BF16)
        wr_lo = ar("wr_lo", [128, 8, 36], BF16)
        wr32 = ar("wr32", [128, 8, 36], F32)
        brb = ar("brb", [128, 36], F32)
        LG = ar("LG", [128, NT, 36], F32)
        Em = ar("Em", [128, NT, 32], F32)
        T1 = ar("T1", [128, NT, 32], F32)
        oh1 = ar("oh1", [128, NT, 32], F32)
        oh2 = ar("oh2", [128, NT, 32], F32)
        Em2 = ar("Em2", [128, NT, 32], F32)
        RK = ar("RK", [128, NT, 32], F32)
        Obf = ar("Obf", [128, NT, 32], BF16)
        Gm = ar("Gm", [128, NT, 4], F32)
        ohG = ar("ohG", [128, NT, 4], F32)
        pen = ar("pen", [128, NT, 4], F32)
        sm = {n: ar("sm_" + n, [128, NT], F32) for n in
              ("gmax", "sumG", "pg", "m1", "m2", "r", "den", "g1", "g2", "rk", "base", "valid", "sl")}
        slot_i = ar("slot_i", [128, 2, NT], I32)

        wr = self.dram["router_w"]
        br = self.dram["router_b"]
        wgu_d = self.dram["expert_w_gu%d" % li]
        wdn_d = self.dram["expert_w_down%d" % li]
        Xs, Ys = self.Xs, self.Ys

        self.rms_stats(self.dram["norm_ffn"][layer:layer + 1, :])
        P.op("sp", lambda e: e.dma_start(out=wr32[:], in_=wr[li].rearrange("(c p) j -> p c j", p=128)),
             writes=["wr32"], dma=True)
        P.op("sp", lambda e: e.dma_start(out=brb[:], in_=br[li:li + 1, :].partition_broadcast(128)),
             writes=["brb"], dma=True)

        def load_w(e_idx):
            s = e_idx % 2
            P.op("pool", lambda e: e.dma_start(out=wgu[s][:], in_=wgu_d[e_idx].rearrange("(c p) f -> p c f", p=128)),
                 writes=[("wgu", s)], dma=True)
            P.op("pool", lambda e: e.dma_start(out=wdn[s][:], in_=wdn_d[e_idx].rearrange("(c p) f -> p c f", p=128)),
                 writes=[("wdn", s)], dma=True)

        load_w(0)
        load_w(1)
        nr = NSLOT // 128
        xs_z_keys = [("Xs_z", r0) for r0 in range(0, nr, 13)] + ["Ys_z"]
        if not self.zeroed:
            self.zeroed = True
            P.op("dve", lambda e: e.memset(fence2[:], 0.0), writes=["zeros_b"])
            xsv = Xs.rearrange("(r p) d -> p r d", p=128)
            ysv = Ys.rearrange("(r p) d -> p r d", p=128)
            for r0 in range(0, nr, 13):
                P.op("sp", lambda e, r0=r0: e.dma_start(
                    out=xsv[:, r0:r0 + 13, :], in_=fence2[:].unsqueeze(1).to_broadcast([128, 13, D])),
                    reads=["zeros_b"], writes=[("Xs_z", r0)], dma=True)
            P.op("sp", lambda e: e.dma_start(out=ysv[:, nr - 1, :], in_=fence2[:]),
                 reads=["zeros_b"], writes=["Ys_z"], dma=True)

        P.op("act", lambda e: e.activation(out=wr_hi[:], in_=wr32[:], func=AF.Copy), reads=["wr32"], writes=["wr_hi"])
        P.op("pool", lambda e: e.tensor_tensor(out=wr_lo[:], in0=wr32[:], in1=wr_hi[:], op=ALU.subtract),
             reads=["wr32", "wr_hi"], writes=["wr_lo"])
        def st1(t):
            b = t % NRB
            P.op("dve", lambda e: e.scalar_tensor_tensor(
                out=hn32[b][:], in0=h[:, t, :], scalar=rstd[:, t:t + 1], in1=gb[:], op0=ALU.mult, op1=ALU.mult),
                reads=[("h", t), "rstd", "gb"], writes=[("hn32", b)])
            P.op("act", lambda e: e.activation(out=hhi[b][:], in_=hn32[b][:], func=AF.Copy),
                 reads=[("hn32", b)], writes=[("hhi", b)])
            P.op("pool", lambda e: e.tensor_tensor(out=hlo[b][:], in0=hn32[b][:], in1=hhi[b][:], op=ALU.subtract),
                 reads=[("hn32", b), ("hhi", b)], writes=[("hlo", b)])

        def st2(t):
            b = t % NRB
            for w, (src, sk, dstT, dk) in enumerate(((hhi, "hhi", hiT, "hiT"), (hlo, "hlo", loT, "loT"))):
                bi = 2 * (t % 2) + w

                def tr(e, src=src, bi=bi):
                    pv = ps[bi][:].bitcast(BF16)
                    for c in range(8):
                        inst = e.transpose(out=pv[:, c * 128:(c + 1) * 128], in_=src[b][:, c * 128:(c + 1) * 128],
                                           identity=self.ident_b[:])
                    return inst
                P.op("pe", tr, reads=[(sk, b), "ident_b"], writes=[("ps", bi)])
                srcv = lambda bi=bi: ps[bi][:].bitcast(BF16).rearrange("p (c t) -> p c t", c=8)
                if w == 0:
                    P.op("act", lambda e, srcv=srcv, dstT=dstT: e.activation(out=dstT[b][:], in_=srcv(), func=AF.Copy),
                         reads=[("ps", bi)], writes=[(dk, b)])
                else:
                    P.op("dve", lambda e, srcv=srcv, dstT=dstT: e.tensor_copy(out=dstT[b][:], in_=srcv()),
                         reads=[("ps", bi)], writes=[(dk, b)])

        def st3(t):
            b = t % NRB
            pb = 4 + t % 2

            def rl(e):
                k = 0
                for (xt, wt) in ((hiT, wr_hi), (loT, wr_hi), (hiT, wr_lo)):
                    for c in range(8):
                        inst = e.matmul(ps[pb][:, 0:36], xt[b][:, c, :], wt[:, c, :], start=(k == 0), stop=(k == 23))
                        k += 1
                return inst
            P.op("pe", rl, reads=[("hiT", b), ("loT", b), "wr_hi", "wr_lo"], writes=[("ps", pb)])
            P.op("dve", lambda e: e.tensor_tensor(out=LG[:, t, :], in0=ps[pb][:, 0:36], in1=brb[:], op=ALU.add),
                 reads=[("ps", pb), "brb"], writes=[("LG", t)])

        for i in range(NT + 2):
            if i < NT:
                st1(i)
            if 0 <= i - 1 < NT:
                st2(i - 1)
            if 0 <= i - 2 < NT:
                st3(i - 2)

        if int(os.environ.get('MOE_STOP', '99')) <= 1:
            return
        G = LG[:, :, 0:4]
        E4 = LG[:, :, 4:36].rearrange("p n (g e) -> p n g e", g=4)

        def bc(t2, n):
            return t2[:].unsqueeze(2).to_broadcast([128, NT, n])

        def dve(fn, reads, writes):
            P.op("dve", fn, reads=reads, writes=writes)

        P.op("dve", lambda e: e.memset(fence[:], 0.0), reads=[("LG", t) for t in range(NT)], writes=["LG"])
        dve(lambda e: e.tensor_reduce(out=sm["gmax"][:], in_=G, axis=AX.X, op=ALU.max), ["LG"], ["gmax"])
        dve(lambda e: e.tensor_tensor(out=Gm[:], in0=G, in1=bc(sm["gmax"], 4), op=ALU.subtract), ["LG", "gmax"], ["Gm"])
        dve(lambda e: e.tensor_single_scalar(out=ohG[:], in_=Gm[:], scalar=0.0, op=ALU.is_ge), ["Gm"], ["ohG"])
        P.op("act", lambda e: e.activation(out=Gm[:], in_=Gm[:], func=AF.Exp), reads=["Gm", "ohG"], writes=["Gm"])
        dve(lambda e: e.tensor_reduce(out=sm["sumG"][:], in_=Gm[:], axis=AX.X, op=ALU.add), ["Gm"], ["sumG"])
        dve(lambda e: e.reciprocal(out=sm["pg"][:], in_=sm["sumG"][:]), ["sumG"], ["pg"])
        dve(lambda e: e.tensor_scalar(out=pen[:], in0=ohG[:], scalar1=BIG, scalar2=-BIG, op0=ALU.mult, op1=ALU.add),
            ["ohG"], ["pen"])
        dve(lambda e: e.tensor_tensor(out=Em[:].rearrange("p n (g e) -> p n g e", g=4), in0=E4,
                                      in1=pen[:].unsqueeze(3).to_broadcast([128, NT, 4, 8]), op=ALU.add),
            ["LG", "pen"], ["Em"])
        dve(lambda e: e.tensor_reduce(out=sm["m1"][:], in_=Em[:], axis=AX.X, op=ALU.max), ["Em"], ["m1"])
        dve(lambda e: e.tensor_tensor(out=T1[:], in0=Em[:], in1=bc(sm["m1"], 32), op=ALU.subtract), ["Em", "m1"], ["T1"])
        dve(lambda e: e.tensor_single_scalar(out=oh1[:], in_=T1[:], scalar=0.0, op=ALU.is_ge), ["T1"], ["oh1"])
        dve(lambda e: e.scalar_tensor_tensor(out=Em2[:], in0=oh1[:], scalar=-BIG, in1=Em[:], op0=ALU.mult, op1=ALU.add),
            ["oh1", "Em"], ["Em2"])
        dve(lambda e: e.tensor_reduce(out=sm["m2"][:], in_=Em2[:], axis=AX.X, op=ALU.max), ["Em2"], ["m2"])
        dve(lambda e: e.tensor_tensor(out=T1[:], in0=Em2[:], in1=bc(sm["m2"], 32), op=ALU.subtract), ["Em2", "m2"], ["T1"])
        dve(lambda e: e.tensor_single_scalar(out=oh2[:], in_=T1[:], scalar=0.0, op=ALU.is_ge), ["T1"], ["oh2"])
        dve(lambda e: e.tensor_tensor(out=sm["r"][:], in0=sm["m2"][:], in1=sm["m1"][:], op=ALU.subtract), ["m1", "m2"], ["r"])
        P.op("act", lambda e: e.activation(out=sm["r"][:], in_=sm["r"][:], func=AF.Exp), reads=["r"], writes=["r"])
        dve(lambda e: e.tensor_scalar(out=sm["den"][:], in0=sm["r"][:], scalar1=1.0, scalar2=None, op0=ALU.add), ["r"], ["den"])
        dve(lambda e: e.reciprocal(out=sm["den"][:], in_=sm["den"][:]), ["den"], ["den"])
        dve(lambda e: e.tensor_tensor(out=sm["g1"][:], in0=sm["pg"][:], in1=sm["den"][:], op=ALU.mult), ["pg", "den"], ["g1"])
        dve(lambda e: e.tensor_tensor(out=sm["g2"][:], in0=sm["g1"][:], in1=sm["r"][:], op=ALU.mult), ["g1", "r"], ["g2"])
        dve(lambda e: e.tensor_tensor(out=Obf[:], in0=oh1[:], in1=oh2[:], op=ALU.add), ["oh1", "oh2"], ["Obf"])

        if int(os.environ.get('MOE_STOP', '99')) <= 2:
            return
        def ranks(e):
            for t in range(NT):
                o = ps[7][:, t * 32:(t + 1) * 32]
                inst = e.matmul(o, self.lst_b[:], Obf[:, t, :], start=True, stop=(t == 0))
                for j in range(t):
                    inst = e.matmul(o, self.ones_b[:], Obf[:, j, :], start=False, stop=(j == t - 1))
            return inst
        P.op("pe", ranks, reads=["Obf", "lst_b", "ones_b"], writes=[("ps", 7)])
        dve(lambda e: e.tensor_copy(out=RK[:], in_=ps[7][:].rearrange("p (n e) -> p n e", e=32)), [("ps", 7)], ["RK"])
        ebc = self.ebase[:].unsqueeze(1).to_broadcast([128, NT, 32])
        for k, (oh, ohn, gn) in enumerate(((oh1, "oh1", "g1"), (oh2, "oh2", "g2"))):
            dve(lambda e, oh=oh: e.tensor_tensor(out=T1[:], in0=oh[:], in1=RK[:], op=ALU.mult), [ohn, "RK"], ["T1"])
            dve(lambda e: e.tensor_reduce(out=sm["rk"][:], in_=T1[:], axis=AX.X, op=ALU.add), ["T1"], ["rk"])
            dve(lambda e, oh=oh: e.tensor_tensor(out=T1[:], in0=oh[:], in1=ebc, op=ALU.mult), [ohn, "ebase"], ["T1"])
            dve(lambda e: e.tensor_reduce(out=sm["base"][:], in_=T1[:], axis=AX.X, op=ALU.add), ["T1"], ["base"])
            dve(lambda e: e.tensor_single_scalar(out=sm["valid"][:], in_=sm["rk"][:], scalar=float(CAP), op=ALU.is_lt),
                ["rk"], ["valid"])
            dve(lambda e: e.scalar_tensor_tensor(out=sm["sl"][:], in0=sm["rk"][:], scalar=-float(TRASH), in1=sm["base"][:],
                                                 op0=ALU.add, op1=ALU.add), ["rk", "base"], ["sl"])
            dve(lambda e: e.tensor_tensor(out=sm["sl"][:], in0=sm["sl"][:], in1=sm["valid"][:], op=ALU.mult), ["sl", "valid"], ["sl"])
            dve(lambda e: e.tensor_scalar(out=sm["sl"][:], in0=sm["sl"][:], scalar1=float(TRASH), scalar2=None, op0=ALU.add),
                ["sl"], ["sl"])
            dve(lambda e, k=k: e.tensor_copy(out=slot_i[:, k, :], in_=sm["sl"][:]), ["sl"], [("slot", k)])
            dve(lambda e, gn=gn: e.tensor_tensor(out=sm[gn][:], in0=sm[gn][:], in1=sm["valid"][:], op=ALU.mult),
                [gn, "valid"], [gn])

        if int(os.environ.get('MOE_STOP', '99')) <= 3:
            return
        for t in range(NT):
            b = t % NHB
            P.op("dve", lambda e, t=t, b=b: e.scalar_tensor_tensor(
                out=hnb[b][:], in0=h[:, t, :], scalar=rstd[:, t:t + 1], in1=gb[:], op0=ALU.mult, op1=ALU.mult),
                reads=[("h", t), "rstd", "gb"], writes=[("hnb", b)])
            for k in range(2):
                P.op("pool", lambda e, t=t, b=b, k=k: e.indirect_dma_start(
                    out=Xs[:, :], out_offset=bass.IndirectOffsetOnAxis(ap=slot_i[:, k, t:t + 1], axis=0),
                    in_=hnb[b][:], in_offset=None),
                    reads=[("hnb", b), ("slot", k)] + xs_z_keys, writes=[("Xs_w", t, k)], dma=True)

        if int(os.environ.get('MOE_STOP', '99')) <= 4:
            dbg = [sm["sl"], sm["g1"], sm["g2"], sm["rk"], sm["base"], sm["valid"]]
            for q, tl in enumerate(dbg):
                P.op("dve", lambda e, q=q, tl=tl: e.tensor_copy(out=h[:, 0, q * 16:(q + 1) * 16], in_=tl[:]),
                     reads=["sl", "g1", "g2", "rk", "base", "valid"], writes=[("h", 0)])
            for k in range(2):
                P.op("dve", lambda e, k=k: e.tensor_copy(out=h[:, 0, 96 + k * 16:112 + k * 16], in_=slot_i[:, k, :]),
                     reads=[("slot", k)], writes=[("h", 0)])
            P.op("dve", lambda e: e.tensor_copy(out=h[:, 1, 0:576], in_=LG[:].rearrange("p n j -> p (n j)")),
                 reads=["LG"], writes=[("h", 1)])
            return
        P.barrier()
        xs_keys = [("Xs_w", t, k) for t in range(NT) for k in range(2)]

        def load_x(ex):
            s = ex % 2
            P.op("sp", lambda e: e.dma_start(
                out=xg[s][:], in_=Xs[ex * CAP:(ex + 1) * CAP, :].rearrange("(r p) d -> p r d", p=128)),
                reads=xs_keys, writes=[("xg", s)], dma=True)

        load_x(0)
        for ex in range(NE):
            s = ex % 2
            if ex + 1 < NE:
                load_x(ex + 1)
            for r in range(2):
                bank = ps[r]

                def tr2(e, r=r, s=s, bank=bank):
                    pv = bank[:].bitcast(BF16)
                    for c in range(8):
                        inst = e.transpose(out=pv[:, c * 128:(c + 1) * 128], in_=xg[s][:, r, c * 128:(c + 1) * 128],
                                           identity=self.ident_b[:])
                    return inst
                P.op("pe", tr2, reads=[("xg", s), "ident_b"], writes=[("ps", r)])
                src = lambda bank=bank: bank[:].bitcast(BF16).rearrange("p (c t) -> p c t", c=8)
                if r == 0:
                    P.op("act", lambda e, s=s, src=src: e.activation(out=xT[s][:, :, 0:128], in_=src(), func=AF.Copy),
                         reads=[("ps", 0)], writes=[("xT", s, 0)])
                else:
                    P.op("dve", lambda e, s=s, src=src: e.tensor_copy(out=xT[s][:, :, 128:256], in_=src()),
                         reads=[("ps", 1)], writes=[("xT", s, 1)])
            for m in range(4):
                bank = ps[2 + m % 2]
                bk = ("ps", 2 + m % 2)

                def gu(e, m=m, s=s, bank=bank):
                    for half in range(2):
                        col = half * FF + m * 128
                        for c in range(8):
                            inst = e.matmul(bank[:, half * CAP:(half + 1) * CAP], wgu[s][:, c, col:col + 128],
                                            xT[s][:, c, :], start=(c == 0), stop=(c == 7))
                    return inst
                P.op("pe", gu, reads=[("wgu", s), ("xT", s, 0), ("xT", s, 1)], writes=[bk])
                P.op("act", lambda e, m=m, bank=bank: e.activation(out=sA[m % 2][:], in_=bank[:, 0:CAP], func=AF.Silu),
                     reads=[bk], writes=[("sA", m % 2)])
                P.op("dve", lambda e, m=m, s=s, bank=bank: e.tensor_tensor(out=hT[s][:, m, :], in0=sA[m % 2][:],
                                                                          in1=bank[:, CAP:2 * CAP], op=ALU.mult),
                     reads=[bk, ("sA", m % 2)], writes=[("hT", s, m)])
            for r in range(2):
                for n in range(2):
                    q = r * 2 + n
                    bank = ps[4 + q % 2]
                    bk = ("ps", 4 + q % 2)

                    def dn(e, r=r, n=n, s=s, bank=bank):
                        for m in range(4):
                            inst = e.matmul(bank[:, :], hT[s][:, m, r * 128:(r + 1) * 128],
                                            wdn[s][:, m, n * 512:(n + 1) * 512], start=(m == 0), stop=(m == 3))
                        return inst
                    P.op("pe", dn, reads=[("wdn", s)] + [("hT", s, m) for m in range(4)], writes=[bk])
                    if q % 2 == 0:
                        P.op("act", lambda e, r=r, n=n, s=s, bank=bank: e.activation(
                            out=yt[s][:, r, n * 512:(n + 1) * 512], in_=bank[:, :], func=AF.Copy),
                            reads=[bk], writes=[("yt", s, q)])
                    else:
                        P.op("dve", lambda e, r=r, n=n, s=s, bank=bank: e.tensor_copy(
                            out=yt[s][:, r, n * 512:(n + 1) * 512], in_=bank[:, :]),
                            reads=[bk], writes=[("yt", s, q)])
            P.op("sp", lambda e, ex=ex, s=s: e.dma_start(
                out=Ys[ex * CAP:(ex + 1) * CAP, :].rearrange("(r p) d -> p r d", p=128), in_=yt[s][:]),
                reads=[("yt", s, q) for q in range(4)], writes=[("Ys_w", ex)], dma=True)
            if ex + 2 < NE:
                load_w(ex + 2)

        if int(os.environ.get('MOE_STOP', '99')) <= 5:
            return
        P.op("pool", lambda e: e.memset(fence[:], 0.0), writes=[("wgu", 0)] + [("yg", b) for b in range(NYG)])
        for t in range(NT):
            for k, gn in enumerate(("g1", "g2")):
                b = (t * 2 + k) % NYG
                P.op("pool", lambda e, t=t, b=b, k=k: e.indirect_dma_start(
                    out=yg[b][:], out_offset=None, in_=Ys[:, :],
                    in_offset=bass.IndirectOffsetOnAxis(ap=slot_i[:, k, t:t + 1], axis=0)),
                    reads=[("Ys_w", ex) for ex in range(NE)] + [("slot", k), "Ys_z"], writes=[("yg", b)], dma=True)
                P.op("dve", lambda e, t=t, b=b, gn=gn: e.scalar_tensor_tensor(
                    out=h[:, t, :], in0=yg[b][:], scalar=sm[gn][:, t:t + 1], in1=h[:, t, :], op0=ALU.mult, op1=ALU.add),
                    reads=[("yg", b), gn, ("h", t)], writes=[("h", t)])

    def norm_transpose(self, hnT, hnb):
        P, h, ps = self.P, self.h, self.ps
        def st1(t):
            b = t % 2
            P.op("dve", lambda e: e.scalar_tensor_tensor(
                out=hnb[b][:], in0=h[:, t, :], scalar=self.rstd[:, t:t + 1], in1=self.gb[:], op0=ALU.mult, op1=ALU.mult),
                reads=[("h", t), "rstd", "gb"], writes=[("hnb", b)])
        st1(0)
        for t in range(NT):
            if t + 1 < NT:
                st1(t + 1)
            self.transpose_tile(hnb[t % 2], ("hnb", t % 2), hnT, t, t % 2)

    def transpose_tile(self, src, src_key, dstT, t, b):
        P, ps = self.P, self.ps
        bank = ps[b]

        def tr(e):
            pv = bank[:].bitcast(BF16)
            for c in range(8):
                inst = e.transpose(out=pv[:, c * 128:(c + 1) * 128], in_=src[:, c * 128:(c + 1) * 128],
                                   identity=self.ident_b[:])
            return inst
        P.op("pe", tr, reads=[src_key, "ident_b"], writes=[("ps", b)])
        srcv = lambda: bank[:].bitcast(BF16).rearrange("p (c t) -> p c t", c=8)
        if b == 0:
            P.op("act", lambda e: e.activation(out=dstT[:, :, t * 128:(t + 1) * 128], in_=srcv(), func=AF.Copy),
                 reads=[("ps", b)], writes=[("xT", t)])
        else:
            P.op("dve", lambda e: e.tensor_copy(out=dstT[:, :, t * 128:(t + 1) * 128], in_=srcv()),
                 reads=[("ps", b)], writes=[("xT", t)])

    def out_proj(self, xT, kc, wout, n_k):
        P, h, ps = self.P, self.h, self.ps
        for t in range(NT):
            for n in range(2):
                bi = 2 + (t * 2 + n) % 2
                bank = ps[bi]

                def mm(e, t=t, n=n, bank=bank):
                    for c in range(kc):
                        inst = e.matmul(bank[:, :], xT[:, c, t * 128:(t + 1) * 128], wout[:, c, n * 512:(n + 1) * 512],
                                        start=(c == 0), stop=(c == kc - 1))
                    return inst
                P.op("pe", mm, reads=[("xT", t), "wout"] if n_k is None else n_k(t) + ["wout"], writes=[("ps", bi)])
                P.op("dve", lambda e, t=t, n=n, bank=bank: e.tensor_tensor(
                    out=h[:, t, n * 512:(n + 1) * 512], in0=h[:, t, n * 512:(n + 1) * 512], in1=bank[:, :], op=ALU.add),
                    reads=[("ps", bi), ("h", t)], writes=[("h", t)])

    def fox_setup(self, layers):
        self.fox_layers = list(layers)
        nl = len(layers)
        self.din("fox_w_in", [nl, D, 4112])
        self.din("fox_b_f", [nl, 16])
        self.din("fox_w_out", [nl, D, D])
        self.din("c_mask", [128, 128])
        self.cum3_d = self.dscratch("cum3_d", [16, 3, S], BF16)
        self.Og_d = self.dscratch("Og_d", [S, D], BF16)
        self.mask_b = self.sb("mask_b", [128, 128], BF16)
        self.P.op("pool", lambda e: e.dma_start(out=self.mask_b[:], in_=self.dram["c_mask"]), writes=["mask_b"], dma=True)

    def fox_layer(self, layer):
        j = self.fox_layers.index(layer)
        P, h, ps, ar = self.P, self.h, self.ps, self.ar
        w_in = self.dram["fox_w_in"]
        P.barrier()
        self.arena_reset()
        hnT = ar("f_hnT", [128, 8, S], BF16)
        hnb = [ar("f_hnb%d" % s, [128, D], BF16) for s in range(2)]
        wf = ar("f_wf", [128, 8, 16], BF16)
        bft = ar("f_bft", [128, 1], F32)
        negbf = ar("f_negbf", [128, 1], F32)
        save = self.arena_off
        Ft = ar("f_Ft", [128, S], F32)
        cumP = ar("f_cumP", [128, S], F32)
        r1 = ar("f_r1", [128, S], F32)
        cum3 = ar("f_cum3", [128, 3, S], BF16)
        self.arena_off = save
        wgrp = [ar("f_wgrp%d" % s, [128, 8, 4, 128], BF16) for s in range(2)]
        QTa = [ar("f_QTa%d" % s, [128, S], BF16) for s in range(2)]
        KTa = [ar("f_KTa%d" % s, [128, S], BF16) for s in range(2)]
        Tst = [ar("f_Tst%d" % s, [128, S], BF16) for s in range(2)]
        Vaug = ar("f_Vaug", [128, NT, 2, 65], BF16)
        Gs = ar("f_Gs", [128, NT, 128], BF16)
        NPT = 6
        PT = [ar("f_PT%d" % s, [128, 512], BF16) for s in range(NPT)]
        Og = [ar("f_Og%d" % s, [128, NT, 128], BF16) for s in range(2)]
        wout = ar("f_wout", [128, 8, D], BF16)
        rden = ar("f_rden", [128, 8], F32)

        self.rms_stats(self.dram["norm_mix"][layer:layer + 1, :])
        P.op("pool", lambda e: e.dma_start(out=wf[:], in_=w_in[j][:, 4096:4112].rearrange("(c p) f -> p c f", p=128)),
             writes=["wf"], dma=True)
        P.op("sp", lambda e: e.dma_start(out=bft[0:16, :], in_=self.dram["fox_b_f"][j].rearrange("(h o) -> h o", o=1)),
             writes=["bft"], dma=True)
        P.op("dve", lambda e: e.tensor_scalar(out=negbf[0:16, :], in0=bft[0:16, :], scalar1=-1.0, scalar2=None, op0=ALU.mult),
             reads=["bft"], writes=["negbf"])
        self.norm_transpose(hnT, hnb)
        xT_all = [("xT", t) for t in range(NT)]

        if int(os.environ.get('FOX_STOP', '99')) <= 1:
            return
        for qd in range(4):
            bank = ps[2 + qd % 2]
            bk = ("ps", 2 + qd % 2)

            def fm(e, qd=qd, bank=bank):
                for c in range(8):
                    inst = e.matmul(bank[0:16, :], wf[:, c, :], hnT[:, c, qd * 512:(qd + 1) * 512], start=(c == 0), stop=(c == 7))
                return inst
            P.op("pe", fm, reads=xT_all + ["wf"], writes=[bk])
            P.op("act", lambda e, qd=qd, bank=bank: e.activation(out=Ft[0:16, qd * 512:(qd + 1) * 512], in_=bank[0:16, :],
                                                                func=AF.Exp, scale=-1.0, bias=negbf[0:16, :]),
                 reads=[bk, "negbf"], writes=["Ft"])
        P.op("act", lambda e: e.activation(out=Ft[0:16, :], in_=Ft[0:16, :], func=AF.Ln, bias=1.0), reads=["Ft"], writes=["Ft"])
        P.op("dve", lambda e: e.tensor_scalar(out=Ft[0:16, :], in0=Ft[0:16, :], scalar1=0.5, scalar2=None, op0=ALU.mult),
             reads=["Ft"], writes=["Ft"])
        P.op("dve", lambda e: e.tensor_tensor_scan(out=cumP[0:16, :], data0=Ft[0:16, :], data1=Ft[0:16, :], initial=0.0,
                                                   op0=ALU.add, op1=ALU.add), reads=["Ft"], writes=["cumP"])
        P.op("dve", lambda e: e.tensor_copy(out=cum3[0:16, 0, :], in_=cumP[0:16, :]), reads=["cumP"], writes=["cum3"])
        P.op("dve", lambda e: e.tensor_tensor(out=r1[0:16, :], in0=cumP[0:16, :], in1=cum3[0:16, 0, :], op=ALU.subtract),
             reads=["cumP", "cum3"], writes=["r1"])
        P.op("dve", lambda e: e.tensor_copy(out=cum3[0:16, 1, :], in_=r1[0:16, :]), reads=["r1"], writes=["cum3"])
        P.op("dve", lambda e: e.tensor_tensor(out=r1[0:16, :], in0=r1[0:16, :], in1=cum3[0:16, 1, :], op=ALU.subtract),
             reads=["r1", "cum3"], writes=["r1"])
        P.op("dve", lambda e: e.tensor_copy(out=cum3[0:16, 2, :], in_=r1[0:16, :]), reads=["r1"], writes=["cum3"])
        P.op("sp", lambda e: e.dma_start(out=self.cum3_d, in_=cum3[0:16, :, :]), reads=["cum3"], writes=["cum3_d"], dma=True)
        if int(os.environ.get('FOX_STOP', '99')) <= 2:
            return
        P.barrier()

        P.op("pool", lambda e: e.dma_start(out=wout[:], in_=self.dram["fox_w_out"][j].rearrange("(c p) f -> p c f", p=128)),
             writes=["wout"], dma=True)
        for hh in range(2):
            P.op("dve", lambda e, hh=hh: e.memset(QTa[hh][64:128, :], 0.0), writes=[("QTa", hh, "aug")])
            P.op("dve", lambda e, hh=hh: e.memset(KTa[hh][64:128, :], 0.0), writes=[("KTa", hh, "aug")])
            P.op("dve", lambda e, hh=hh: e.memset(QTa[hh][64:70, :], 1.0), writes=[("QTa", hh, "aug")])
            P.op("dve", lambda e, hh=hh: e.memset(KTa[hh][64:70, :], -1.0), writes=[("KTa", hh, "aug")])
        P.op("dve", lambda e: e.memset(Vaug[:, :, :, 64:65], 1.0), writes=["Vaug1"])

        def load_grp(g):
            s = g % 2
            for seg in range(4):
                P.op("pool", lambda e, seg=seg: e.dma_start(
                    out=wgrp[s][:, :, seg, :],
                    in_=w_in[j][:, seg * 1024 + g * 128: seg * 1024 + (g + 1) * 128].rearrange("(c p) f -> p c f", p=128)),
                    writes=[("wgrp", s, seg)], dma=True)

        if int(os.environ.get('FOX_STOP', '99')) <= 3:
            return
        load_grp(0)
        pt_rr = [0]
        ogv = self.Og_d.rearrange("(n p) d -> p n d", p=128)
        NG = int(os.environ.get('FOX_NG', '8'))
        for g in range(NG):
            s = g % 2
            if g + 1 < NG:
                load_grp(g + 1)
            for qk in range(2):
                dstT = QTa if qk == 0 else KTa
                dn = "QTa" if qk == 0 else "KTa"
                for qd in range(4):
                    bi = (qk * 4 + qd) % 2
                    bank = ps[bi]
                    cs = slice(qd * 512, (qd + 1) * 512)

                    def pm(e, qk=qk, qd=qd, bank=bank, s=s):
                        for c in range(8):
                            inst = e.matmul(bank[:, :], wgrp[s][:, c, qk, :],
                                            hnT[:, c, qd * 512:(qd + 1) * 512], start=(c == 0), stop=(c == 7))
                        return inst
                    P.op("pe", pm, reads=xT_all + [("wgrp", s, qk)], writes=[("ps", bi)])
                    if qk == 0:
                        P.op("act", lambda e, cs=cs, bank=bank: e.activation(
                            out=QTa[0][0:64, cs], in_=bank[0:64, :], func=AF.Identity, scale=0.125),
                            reads=[("ps", bi)], writes=[("QTa", 0, qd)])
                        P.op("act", lambda e, cs=cs, bank=bank: e.activation(
                            out=Tst[0][64:128, cs], in_=bank[64:128, :], func=AF.Identity, scale=0.125),
                            reads=[("ps", bi)], writes=[("Tst", 0, qd)])
                    else:
                        P.op("dve", lambda e, cs=cs, bank=bank: e.tensor_copy(out=KTa[0][0:64, cs], in_=bank[0:64, :]),
                             reads=[("ps", bi)], writes=[("KTa", 0, qd)])
                        P.op("dve", lambda e, cs=cs, bank=bank: e.tensor_copy(out=Tst[1][64:128, cs], in_=bank[64:128, :]),
                             reads=[("ps", bi)], writes=[("Tst", 1, qd)])
                P.op("sp", lambda e, qk=qk, dstT=dstT: e.dma_start(out=dstT[1][0:64, :], in_=Tst[qk][64:128, :]),
                     reads=[("Tst", qk, qd) for qd in range(4)], writes=[(dn, 1, qd) for qd in range(4)], dma=True)
            for hh in range(2):
                head = 2 * g + hh
                P.op("sp", lambda e, hh=hh, head=head: e.dma_start(out=QTa[hh][64:67, :], in_=self.cum3_d[head]),
                     reads=["cum3_d"], writes=[("QTa", hh, "aug")], dma=True)
                P.op("sp", lambda e, hh=hh, head=head: e.dma_start(out=KTa[hh][67:70, :], in_=self.cum3_d[head]),
                     reads=["cum3_d"], writes=[("KTa", hh, "aug")], dma=True)
            if int(os.environ.get('FOX_STOP', '99')) <= 4:
                return
            for t in range(NT):
                bi = t % 2
                bank = ps[bi]

                def vm(e, t=t, bank=bank, s=s):
                    for c in range(8):
                        inst = e.matmul(bank[:, 0:256], hnT[:, c, t * 128:(t + 1) * 128], wgrp[s][:, c, 2:4, :].rearrange("p a b -> p (a b)"),
                                        start=(c == 0), stop=(c == 7))
                    return inst
                P.op("pe", vm, reads=[("xT", t), ("wgrp", s, 2), ("wgrp", s, 3)], writes=[("ps", bi)])
                if os.environ.get("FOX_DBG", "") != "noV":
                    P.op("dve", lambda e, t=t, bank=bank: e.tensor_copy(
                        out=Vaug[:, t, :, 0:64], in_=bank[:, 0:128].rearrange("p (a b) -> p a b", a=2)),
                        reads=[("ps", bi)], writes=[("Vaug", t)])
                if os.environ.get("FOX_DBG", "") != "noG":
                    P.op("act", lambda e, t=t, bank=bank: e.activation(out=Gs[:, t, :], in_=bank[:, 128:256], func=AF.Sigmoid),
                         reads=[("ps", bi)], writes=[("Gs", t)])
            if int(os.environ.get('FOX_STOP', '99')) <= 5:
                allk = [("QTa", 0, q) for q in range(4)] + [("KTa", 0, q) for q in range(4)] + [("QTa", 0, "aug"), ("KTa", 0, "aug"), "Vaug1"] + [("Vaug", t) for t in range(NT)] + [("Gs", t) for t in range(NT)]
                def dd(dst, src):
                    P.op("dve", lambda e: e.tensor_copy(out=dst, in_=src), reads=allk, writes=[("h", i) for i in range(NT)])
                dd(h[:, 0, :], QTa[0][:, 0:1024]); dd(h[:, 1, :], QTa[0][:, 1024:2048])
                dd(h[:, 2, :], KTa[0][:, 0:1024]); dd(h[:, 3, :], KTa[0][:, 1024:2048])
                dd(h[:, 4, 0:130], Vaug[:, 0, :, :].rearrange("p a b -> p (a b)")); dd(h[:, 4, 256:384], Gs[:, 0, :])
                return
            items = []
            for hh in range(2):
                for qg in range(4):
                    for kb in range(4 * (qg + 1)):
                        items.append((hh, qg, kb))

            def rec_qk(it):
                hh, qg, kb = it
                q0 = max(0, kb - 4 * qg) * 128
                slot = pt_rr[0] % NPT
                pt_rr[0] += 1
                bi = 2 + slot % 2
                bank = ps[bi]
                P.op("pe", lambda e: e.matmul(bank[:, q0:512], KTa[hh][0:96, kb * 128:(kb + 1) * 128],
                                             QTa[hh][0:96, qg * 512 + q0:(qg + 1) * 512], start=True, stop=True),
                     reads=[("KTa", hh, kb // 4), ("KTa", hh, "aug"), ("QTa", hh, qg), ("QTa", hh, "aug")],
                     writes=[("ps", bi)])
                P.op("act", lambda e: e.activation(out=PT[slot][:, q0:512], in_=bank[:, q0:512], func=AF.Exp),
                     reads=[("ps", bi)], writes=[("PT", slot)])
                if kb >= 4 * qg:
                    P.op("dve", lambda e: e.tensor_tensor(out=PT[slot][:, q0:q0 + 128], in0=PT[slot][:, q0:q0 + 128],
                                                          in1=self.mask_b[:], op=ALU.mult),
                         reads=[("PT", slot), "mask_b"], writes=[("PT", slot)])
                return slot

            def rec_pv(it, slot):
                hh, qg, kb = it
                for qt in range(4):
                    i = 4 * qg + qt
                    if kb > i:
                        continue
                    bi = 4 + qt
                    P.op("pe", lambda e, qt=qt, i=i, bi=bi: e.matmul(
                        ps[bi][:, 0:65], PT[slot][:, qt * 128:(qt + 1) * 128], Vaug[:, kb, hh, :],
                        start=(kb == 0), stop=(kb == i)),
                        reads=[("PT", slot), ("Vaug", kb), "Vaug1"], writes=[("ps", bi)])
                    if kb == i:
                        rs = (i + hh) % 8
                        P.op("dve", lambda e, bi=bi, rs=rs: e.reciprocal(out=rden[:, rs:rs + 1], in_=ps[bi][:, 64:65]),
                             reads=[("ps", bi)], writes=[("rden", rs)])
                        P.op("dve", lambda e, bi=bi, rs=rs, i=i, s=s: e.scalar_tensor_tensor(
                            out=Og[s][:, i, hh * 64:(hh + 1) * 64], in0=ps[bi][:, 0:64], scalar=rden[:, rs:rs + 1],
                            in1=Gs[:, i, hh * 64:(hh + 1) * 64], op0=ALU.mult, op1=ALU.mult),
                            reads=[("ps", bi), ("rden", rs), ("Gs", i)], writes=[("Og", s, i)])

            slots = {}
            slots[0] = rec_qk(items[0])
            for n, it in enumerate(items):
                if n + 1 < len(items):
                    slots[n + 1] = rec_qk(items[n + 1])
                rec_pv(it, slots[n])
            P.op("sp", lambda e, g=g, s=s: e.dma_start(out=ogv[:, :, g * 128:(g + 1) * 128], in_=Og[s][:]),
                 reads=[("Og", s, i) for i in range(NT)], writes=[("Og_d", g)], dma=True)
            if int(os.environ.get('FOX_STOP', '99')) <= 6:
                allk = [("Og", s, i) for i in range(NT)]
                P.op("dve", lambda e: e.tensor_copy(out=h[:, 0, :], in_=Og[0][:, 0:8, :].rearrange("p a b -> p (a b)")),
                     reads=allk, writes=[("h", 0)])
                P.op("dve", lambda e: e.tensor_copy(out=h[:, 1, :], in_=Og[0][:, 8:16, :].rearrange("p a b -> p (a b)")),
                     reads=allk, writes=[("h", 1)])
                return

        if int(os.environ.get('FOX_STOP', '99')) <= 7:
            return
        for t in range(NT):
            b = t % 2
            P.op("sp", lambda e, t=t, b=b: e.dma_start(out=hnb[b][:], in_=ogv[:, t, :]),
                 reads=[("Og_d", g) for g in range(8)], writes=[("hnb", b)], dma=True)
            self.transpose_tile(hnb[b], ("hnb", b), hnT, t, b)
        self.out_proj(hnT, NG, wout, None)

    def ret_setup(self, layers):
        self.ret_layers = list(layers)
        nl = len(layers)
        self.din("ret_w_in", [nl, D, 6144])
        self.din("ret_gn_gain", [nl, 2048])
        self.din("ret_w_out", [nl, 2048, D])
        self.din("c_cos", [128, S])
        self.din("c_sin", [128, S])
        self.din("c_intraT", [128, 4, 128])
        self.din("c_rdec", [128, 12])
        self.Og2_d = self.dscratch("Og2_d", [S, 2048], BF16)

    def ret_layer(self, layer):
        j = self.ret_layers.index(layer)
        P, h, ps, ar = self.P, self.h, self.ps, self.ar
        w_in = self.dram["ret_w_in"]
        P.barrier()
        self.arena_reset()
        hnT = ar("r_hnT", [128, 8, S], BF16)
        hnb = [ar("r_hnb%d" % s, [128, D], BF16) for s in range(2)]
        wqk = [ar("r_wqk%d" % s, [128, 8, 512], BF16) for s in range(2)]
        wvg = ar("r_wvg", [128, 8, 1024], BF16)
        QT = ar("r_QT", [128, 2, S], BF16)
        KT = ar("r_KT", [128, 2, S], BF16)
        cosb = ar("r_cos", [128, S], BF16)
        sinb = ar("r_sin", [128, S], BF16)
        rt = [ar("r_rt%d" % s, [128, 512], F32) for s in range(4)]
        intraT = ar("r_intraT", [128, 4, 128], BF16)
        rdec = ar("r_rdec", [128, 12], F32)
        R32 = ar("r_R32", [128, 2, 512], F32)
        Rb = [ar("r_Rb%d" % s, [128, 2, 512], BF16) for s in range(2)]
        Vt = [ar("r_Vt%d" % s, [128, 512], BF16) for s in range(2)]
        Kd = [ar("r_Kd%d" % s, [128, 256], BF16) for s in range(2)]
        PTt = [ar("r_PT%d" % s, [128, 128], BF16) for s in range(2)]
        on = [ar("r_on%d" % s, [128, 512], F32) for s in range(2)]
        sg = [ar("r_sg%d" % s, [128, 512], BF16) for s in range(2)]
        ogo = [ar("r_ogo%d" % s, [128, 512], BF16) for s in range(2)]
        st6 = ar("r_st6", [128, 2, 6], F32)
        mv = ar("r_mv", [128, 2, 2], F32)
        sm = ar("r_sm", [128, 2, 4], F32)

        self.rms_stats(self.dram["norm_mix"][layer:layer + 1, :])
        P.op("pool", lambda e: e.dma_start(out=cosb[:], in_=self.dram["c_cos"]), writes=["cosb"], dma=True)
        P.op("pool", lambda e: e.dma_start(out=sinb[:], in_=self.dram["c_sin"]), writes=["sinb"], dma=True)
        P.op("pool", lambda e: e.dma_start(out=intraT[:], in_=self.dram["c_intraT"]), writes=["intraT"], dma=True)
        P.op("sp", lambda e: e.dma_start(out=rdec[:], in_=self.dram["c_rdec"]), writes=["rdec"], dma=True)
        self.norm_transpose(hnT, hnb)
        xT_all = [("xT", t) for t in range(NT)]
        og2v = self.Og2_d

        def load_qk(hd):
            s = hd % 2
            for qk in range(2):
                P.op("pool", lambda e, qk=qk: e.dma_start(
                    out=wqk[s][:, :, qk * 256:(qk + 1) * 256],
                    in_=w_in[j][:, qk * 1024 + hd * 256: qk * 1024 + (hd + 1) * 256].rearrange("(c p) f -> p c f", p=128)),
                    writes=[("wqk", s, qk)], dma=True)

        def load_vg(hd):
            for vg in range(2):
                P.op("pool", lambda e, vg=vg: e.dma_start(
                    out=wvg[:, :, vg * 512:(vg + 1) * 512],
                    in_=w_in[j][:, 2048 + vg * 2048 + hd * 512: 2048 + vg * 2048 + (hd + 1) * 512].rearrange("(c p) f -> p c f", p=128)),
                    writes=[("wvg", vg)], dma=True)

        load_qk(0)
        for hd in range(4):
            s = hd % 2
            gam = 1.0 - 2.0 ** (-5.0 - hd)
            gamC = float(np.float32(np.exp(np.float32(np.log(np.float32(gam))) * np.float32(128.0))))
            load_vg(hd)
            if hd + 1 < 4:
                load_qk(hd + 1)
            for qk in range(2):
                dst = QT if qk == 0 else KT
                dname = "QT" if qk == 0 else "KT"
                sc = 1.0 if qk == 0 else 0.0625
                for qd in range(4):
                    cs = slice(qd * 512, (qd + 1) * 512)
                    for half in range(2):
                        def pm(e, half=half, qk=qk, qd=qd, s=s):
                            col = qk * 256 + half * 128
                            for c in range(8):
                                inst = e.matmul(ps[half][:, :], wqk[s][:, c, col:col + 128], hnT[:, c, qd * 512:(qd + 1) * 512],
                                                start=(c == 0), stop=(c == 7))
                            return inst
                        P.op("pe", pm, reads=xT_all + [("wqk", s, qk)], writes=[("ps", half)])
                    for k4, (bank, tab, tn) in enumerate(((0, cosb, "cosb"), (1, sinb, "sinb"), (0, sinb, "sinb"), (1, cosb, "cosb"))):
                        P.op("dve", lambda e, k4=k4, bank=bank, tab=tab, cs=cs, sc=sc: e.scalar_tensor_tensor(
                            out=rt[k4][:], in0=ps[bank][:, :], scalar=sc, in1=tab[:, cs], op0=ALU.mult, op1=ALU.mult),
                            reads=[("ps", bank), tn], writes=[("rt", k4)])
                    P.op("pool", lambda e, dst=dst, cs=cs: e.tensor_tensor(out=dst[:, 0, cs], in0=rt[0][:], in1=rt[1][:], op=ALU.subtract),
                         reads=[("rt", 0), ("rt", 1)], writes=[(dname, qd)])
                    P.op("pool", lambda e, dst=dst, cs=cs: e.tensor_tensor(out=dst[:, 1, cs], in0=rt[2][:], in1=rt[3][:], op=ALU.add),
                         reads=[("rt", 2), ("rt", 3)], writes=[(dname, qd)])
            P.op("dve", lambda e: e.memset(R32[:], 0.0), writes=[("R32", 0), ("R32", 1)])
            for n in range(NT):
                b = n % 2
                cols = slice(n * 128, (n + 1) * 128)
                qdk = n // 4

                def ktr(e, cols=cols, b=b):
                    pv = ps[0][:].bitcast(BF16)
                    for c in range(2):
                        inst = e.transpose(out=pv[:, c * 128:(c + 1) * 128], in_=KT[:, c, cols], identity=self.ident_b[:])
                    return inst
                P.op("pe", ktr, reads=[("KT", qdk), "ident_b"], writes=[("ps", 0)])
                P.op("act", lambda e, b=b, hd=hd: e.activation(out=Kd[b][:], in_=ps[0][:].bitcast(BF16)[:, 0:256], func=AF.Identity,
                                                               scale=rdec[:, 8 + hd:9 + hd]),
                     reads=[("ps", 0), "rdec"], writes=[("Kd", b)])

                def smm(e, cols=cols):
                    for c in range(2):
                        inst = e.matmul(ps[1][:, 0:128], KT[:, c, cols], QT[:, c, cols], start=(c == 0), stop=(c == 1))
                    return inst
                P.op("pe", smm, reads=[("KT", qdk), ("QT", qdk)], writes=[("ps", 1)])
                P.op("dve", lambda e, b=b, hd=hd: e.tensor_tensor(out=PTt[b][:], in0=ps[1][:, 0:128], in1=intraT[:, hd, :], op=ALU.mult),
                     reads=[("ps", 1), "intraT"], writes=[("PTt", b)])

                def vmm(e, cols=cols):
                    for c in range(8):
                        inst = e.matmul(ps[2][:, :], hnT[:, c, cols], wvg[:, c, 0:512], start=(c == 0), stop=(c == 7))
                    return inst
                P.op("pe", vmm, reads=[("xT", n), ("wvg", 0)], writes=[("ps", 2)])
                P.op("act", lambda e, b=b: e.activation(out=Vt[b][:], in_=ps[2][:, :], func=AF.Copy),
                     reads=[("ps", 2)], writes=[("Vt", b)])

                def gmm(e, cols=cols):
                    for c in range(8):
                        inst = e.matmul(ps[3][:, :], hnT[:, c, cols], wvg[:, c, 512:1024], start=(c == 0), stop=(c == 7))
                    return inst
                P.op("pe", gmm, reads=[("xT", n), ("wvg", 1)], writes=[("ps", 3)])
                P.op("act", lambda e, b=b: e.activation(out=sg[b][:], in_=ps[3][:, :], func=AF.Silu),
                     reads=[("ps", 3)], writes=[("sg", b)])

                if n < NT - 1:
                    rbn = (n + 1) % 2
                    for c in range(2):
                        P.op("pe", lambda e, c=c, b=b: e.matmul(ps[5 + c][:, :], Kd[b][:, c * 128:(c + 1) * 128], Vt[b][:],
                                                               start=True, stop=True),
                             reads=[("Kd", b), ("Vt", b)], writes=[("ps", 5 + c)])
                        P.op("dve", lambda e, c=c, gamC=gamC: e.scalar_tensor_tensor(
                            out=R32[:, c, :], in0=R32[:, c, :], scalar=gamC, in1=ps[5 + c][:, :], op0=ALU.mult, op1=ALU.add),
                            reads=[("ps", 5 + c), ("R32", c)], writes=[("R32", c)])
                        P.op("act", lambda e, c=c, rbn=rbn: e.activation(out=Rb[rbn][:, c, :], in_=R32[:, c, :], func=AF.Copy),
                             reads=[("R32", c)], writes=[("Rb", rbn, c)])

                def omm(e, cols=cols, b=b, n=n):
                    inst = e.matmul(ps[4][:, :], PTt[b][:], Vt[b][:], start=True, stop=(n == 0))
                    if n > 0:
                        for c in range(2):
                            inst = e.matmul(ps[4][:, :], QT[:, c, cols], Rb[n % 2][:, c, :], start=False, stop=(c == 1))
                    return inst
                P.op("pe", omm, reads=[("PTt", b), ("Vt", b), ("QT", qdk), ("Rb", n % 2, 0), ("Rb", n % 2, 1)], writes=[("ps", 4)])
                P.op("dve", lambda e, b=b: e.bn_stats(out=st6[:, b, :], in_=ps[4][:, :]), reads=[("ps", 4)], writes=[("st6", b)])
                P.op("dve", lambda e, b=b: e.bn_aggr(out=mv[:, b, :], in_=st6[:, b, :]), reads=[("st6", b)], writes=[("mv", b)])
                P.op("dve", lambda e, b=b, hd=hd: e.tensor_scalar(out=sm[:, b, 0:1], in0=mv[:, b, 1:2], scalar1=rdec[:, 4 + hd:5 + hd],
                                                                 scalar2=EPS, op0=ALU.mult, op1=ALU.add),
                     reads=[("mv", b), "rdec"], writes=[("sm", b, 0)])
                P.op("pool", lambda e, b=b: e.tensor_tensor(out=sm[:, b, 1:2], in0=sm[:, b, 0:1], in1=self.negh[:, 0:1], op=ALU.pow),
                     reads=[("sm", b, 0), "negh"], writes=[("sm", b, 1)])
                P.op("dve", lambda e, b=b, hd=hd: e.tensor_tensor(out=sm[:, b, 2:3], in0=sm[:, b, 1:2], in1=rdec[:, hd:hd + 1], op=ALU.mult),
                     reads=[("sm", b, 1), "rdec"], writes=[("sm", b, 2)])
                P.op("dve", lambda e, b=b: e.scalar_tensor_tensor(out=sm[:, b, 3:4], in0=mv[:, b, 0:1], scalar=-1.0, in1=sm[:, b, 2:3],
                                                                 op0=ALU.mult, op1=ALU.mult),
                     reads=[("mv", b), ("sm", b, 2)], writes=[("sm", b, 3)])
                P.op("act", lambda e, b=b: e.activation(out=on[b][:], in_=ps[4][:, :], func=AF.Identity,
                                                        scale=sm[:, b, 2:3], bias=sm[:, b, 3:4]),
                     reads=[("ps", 4), ("sm", b, 2), ("sm", b, 3)], writes=[("on", b)])
                P.op("pool", lambda e, b=b: e.tensor_tensor(out=ogo[b][:], in0=on[b][:], in1=sg[b][:], op=ALU.mult),
                     reads=[("on", b), ("sg", b)], writes=[("ogo", b)])
                P.op("sp", lambda e, b=b, n=n, hd=hd: e.dma_start(out=og2v[n * 128:(n + 1) * 128, hd * 512:(hd + 1) * 512], in_=ogo[b][:]),
                     reads=[("ogo", b)], writes=[("Og2_d", n, hd)], dma=True)

        P.barrier()
        self.arena_reset()
        wout = ar("r_wout", [128, 16, D], BF16)
        ogt = [ar("r_ogt%d" % s, [128, 2048], BF16) for s in range(2)]
        OgT = [ar("r_OgT%d" % s, [128, 16, 128], BF16) for s in range(2)]
        gcol = ar("r_gcol", [128, 16], F32)
        P.op("pool", lambda e: e.dma_start(out=wout[:], in_=self.dram["ret_w_out"][j].rearrange("(c p) f -> p c f", p=128)),
             writes=["wout"], dma=True)
        P.op("sp", lambda e: e.dma_start(out=gcol[:], in_=self.dram["ret_gn_gain"][j].rearrange("(c p) -> p c", p=128),
                                         allow_slow_non_contiguous=True), writes=["gcol"], dma=True)
        for c in range(16):
            P.op("dve", lambda e, c=c: e.tensor_scalar(out=wout[:, c, :], in0=wout[:, c, :], scalar1=gcol[:, c:c + 1], scalar2=None,
                                                     op0=ALU.mult), reads=["wout", "gcol"], writes=["wout"])
        for t in range(NT):
            b = t % 2
            P.op("sp", lambda e, t=t, b=b: e.dma_start(out=ogt[b][:], in_=og2v[t * 128:(t + 1) * 128, :]),
                 writes=[("ogt", b)], dma=True)
            for half in range(2):
                def tr(e, half=half, b=b):
                    pv = ps[half][:].bitcast(BF16)
                    for c in range(8):
                        cc = half * 8 + c
                        inst = e.transpose(out=pv[:, c * 128:(c + 1) * 128], in_=ogt[b][:, cc * 128:(cc + 1) * 128],
                                           identity=self.ident_b[:])
                    return inst
                P.op("pe", tr, reads=[("ogt", b), "ident_b"], writes=[("ps", half)])
                srcv = lambda half=half: ps[half][:].bitcast(BF16).rearrange("p (c t) -> p c t", c=8)
                if half == 0:
                    P.op("act", lambda e, b=b, srcv=srcv: e.activation(out=OgT[b][:, 0:8, :], in_=srcv(), func=AF.Copy),
                         reads=[("ps", 0)], writes=[("OgT", b, 0)])
                else:
                    P.op("dve", lambda e, b=b, srcv=srcv: e.tensor_copy(out=OgT[b][:, 8:16, :], in_=srcv()),
                         reads=[("ps", 1)], writes=[("OgT", b, 1)])
            for nn in range(2):
                bi = 2 + (t * 2 + nn) % 2

                def mm(e, nn=nn, b=b, bi=bi):
                    for c in range(16):
                        inst = e.matmul(ps[bi][:, :], OgT[b][:, c, :], wout[:, c, nn * 512:(nn + 1) * 512],
                                        start=(c == 0), stop=(c == 15))
                    return inst
                P.op("pe", mm, reads=[("OgT", b, 0), ("OgT", b, 1), "wout"], writes=[("ps", bi)])
                P.op("dve", lambda e, t=t, nn=nn, bi=bi: e.tensor_tensor(
                    out=h[:, t, nn * 512:(nn + 1) * 512], in0=h[:, t, nn * 512:(nn + 1) * 512], in1=ps[bi][:, :], op=ALU.add),
                    reads=[("ps", bi), ("h", t)], writes=[("h", t)])


def build(stages=None, final_norm=True):
    B = Builder()
    B.prologue()
    if stages is None:
        stages = []
        for i in range(DEPTH):
            stages += ["mix%d" % i, "moe%d" % i]
    moe_layers = sorted(set(int(s[3:]) for s in stages if s.startswith("moe")))
    if moe_layers:
        B.moe_setup(moe_layers)
    fox_layers = sorted(set(int(s[3:]) for s in stages if s.startswith("mix") and int(s[3:]) % 2 == 0))
    if fox_layers:
        B.fox_setup(fox_layers)
    ret_layers = sorted(set(int(s[3:]) for s in stages if s.startswith("mix") and int(s[3:]) % 2 == 1))
    if ret_layers:
        B.ret_setup(ret_layers)
    B.layer_sets = {"moe": moe_layers, "fox": fox_layers, "ret": ret_layers}
    for s in stages:
        li = int(s[3:])
        if s.startswith("moe"):
            B.moe_layer(li)
        elif li % 2 == 0:
            B.fox_layer(li)
        else:
            B.ret_layer(li)
    B.epilogue(final_norm=final_norm)
    nc = B.finish()
    _LAYER_SETS[id(nc)] = B.layer_sets
    return nc


def _consts():
    ident = np.eye(128, dtype=np.float32)
    lst = np.triu(np.ones((128, 128), np.float32), k=1)
    ebase = (np.arange(NE, dtype=np.float32) * CAP).reshape(1, NE)
    mask = np.triu(np.ones((128, 128), np.float32), k=0)
    inv = (np.float32(1.0) / (np.float32(10000.0) ** np.linspace(0.0, 1.0, 128, dtype=np.float32))).astype(np.float32)
    ang = (np.arange(S, dtype=np.float32)[None, :] * inv[:, None]).astype(np.float32)
    cosT = np.cos(ang).astype(np.float32)
    sinT = np.sin(ang).astype(np.float32)
    log_g = np.log(np.float32(1.0) - np.float32(2.0) ** (np.float32(-5.0) - np.arange(4, dtype=np.float32))).astype(np.float32)
    idx = np.arange(128, dtype=np.float32)
    intraT = np.zeros((128, 4, 128), np.float32)
    rdec = np.zeros((128, 12), np.float32)
    for hd in range(4):
        col = np.exp(-log_g[hd] * (idx + 1.0)).astype(np.float32)
        intraT[:, hd, :] = np.where(idx[None, :] >= idx[:, None], col[:, None], 0.0)
        qd = np.exp(log_g[hd] * (idx + 1.0)).astype(np.float32)
        rdec[:, hd] = qd
        rdec[:, 4 + hd] = qd * qd
        rdec[:, 8 + hd] = np.exp(log_g[hd] * (127.0 - idx)).astype(np.float32)
    return {"c_ident": ident, "c_lst": lst, "c_ebase": ebase, "c_mask": mask,
            "c_cos": cosT, "c_sin": sinT, "c_intraT": intraT, "c_rdec": rdec}


_CACHE = {}


def prep_inputs(inputs, nc_inputs, layer_sets):
    f = lambda a: np.ascontiguousarray(np.asarray(a), dtype=np.float32)
    shared = {}
    shared["norm_mix"] = f(inputs["norm_mix"])
    shared["norm_ffn"] = f(inputs["norm_ffn"])
    shared["norm_final"] = f(inputs["norm_final"]).reshape(1, D)
    shared.update(_consts())
    if "router_w" in nc_inputs:
        ml = layer_sets["moe"]
        shared["router_w"] = np.ascontiguousarray(
            np.concatenate([f(inputs["router_group_w"]), f(inputs["router_expert_w"])], axis=-1)[ml])
        shared["router_b"] = np.ascontiguousarray(
            np.concatenate([f(inputs["router_group_b"]), f(inputs["router_expert_b"])], axis=-1)[ml])
        for p, l in enumerate(ml):
            shared["expert_w_gu%d" % p] = f(inputs["expert_w_gu"][l])
            shared["expert_w_down%d" % p] = f(inputs["expert_w_down"][l])
    if "fox_w_in" in nc_inputs:
        fl = [l // 2 for l in layer_sets["fox"]]
        shared["fox_w_in"] = np.ascontiguousarray(f(inputs["fox_w_in"])[fl])
        shared["fox_b_f"] = np.ascontiguousarray(f(inputs["fox_b_f"])[fl])
        shared["fox_w_out"] = np.ascontiguousarray(f(inputs["fox_w_out"])[fl])
    if "ret_w_in" in nc_inputs:
        rl = [l // 2 for l in layer_sets["ret"]]
        shared["ret_w_in"] = np.ascontiguousarray(f(inputs["ret_w_in"])[rl])
        shared["ret_gn_gain"] = np.ascontiguousarray(f(inputs["ret_gn_gain"])[rl])
        shared["ret_w_out"] = np.ascontiguousarray(f(inputs["ret_w_out"])[rl])
    x = f(inputs["x"])
    in_maps = []
    for b in range(NCORES):
        m = {k: v for k, v in shared.items() if k in nc_inputs}
        m["x"] = x[b]
        in_maps.append(m)
    return in_maps


def run(inputs, stages=None, final_norm=True, trace=False):
    key = (tuple(stages) if stages is not None else None, final_norm)
    if key not in _CACHE:
        B_nc = build(stages, final_norm)
        _CACHE[key] = B_nc
    nc = _CACHE[key]
    names = set(_DRAM_NAMES[id(nc)])
    in_maps = prep_inputs(inputs, names, _LAYER_SETS[id(nc)])
    res = run_bass_kernel_spmd(nc, in_maps, core_ids=list(range(NCORES)), trace=trace)
    out = np.stack([np.asarray(r["y"]) for r in res.results], axis=0).astype(np.float32)
    return out, res


def kernel(**inputs):
    out, _ = run(inputs)
    return out
```

```python
import contextlib
import os
import numpy as np
import concourse.bass as bass
import concourse.mybir as mybir
from concourse.bass_utils import run_bass_kernel_spmd

F32 = mybir.dt.float32
BF16 = mybir.dt.bfloat16
I32 = mybir.dt.int32
AF = mybir.ActivationFunctionType
ALU = mybir.AluOpType
AX = mybir.AxisListType

D = 1024
S = 2048
NT = S // 128
DEPTH = 4
EPS = 1e-6
NCORES = 8

ENGINES = ("pe", "act", "dve", "pool", "sp")


class _Op:
    __slots__ = ("eng", "fn", "dma", "waits", "signal", "idx")

    def __init__(self, eng, fn, dma, idx):
        self.eng = eng
        self.fn = fn
        self.dma = dma
        self.waits = []
        self.signal = None
        self.idx = idx


class Prog:
    N_DMA_SEMS = 24

    def __init__(self, same_engine_sync=True):
        self.ops = []
        self.last_writer = {}
        self.readers = {}
        self.same_engine_sync = same_engine_sync
        self.dependents = {}
        self.deps = []
        self.forced = set()
        self.pending = {e: set() for e in ENGINES}
        self.last_op = {}
        self.unfenced_dma = set()

    def op(self, eng, fn, reads=(), writes=(), dma=False, force=False):
        idx = len(self.ops)
        o = _Op(eng, fn, dma, idx)
        if force:
            self.forced.add(idx)
        ps_reads = [k for k in reads if isinstance(k, tuple) and k[0] == "ps"]
        if ps_reads:
            reads = [k for k in reads if k not in ps_reads]
            writes = list(writes) + ps_reads
        deps = set()
        for k in reads:
            w = self.last_writer.get(k)
            if w is not None:
                deps.add(w)
        for k in writes:
            w = self.last_writer.get(k)
            if w is not None:
                deps.add(w)
            for r in self.readers.get(k, ()):
                deps.add(r)
        deps |= self.pending[eng]
        self.pending[eng] = set()
        deps.discard(idx)
        self.last_op[eng] = idx
        if dma:
            self.unfenced_dma.add(idx)
        for k in writes:
            self.last_writer[k] = idx
            self.readers[k] = []
        for k in reads:
            if k not in writes:
                self.readers.setdefault(k, []).append(idx)
        self.ops.append(o)
        self.deps.append(deps)
        return idx

    def barrier(self):
        carry = getattr(self, "carry_dma", set())
        deps = set(self.last_op.values()) | (self.unfenced_dma - carry)
        self.unfenced_dma = set(carry)
        self.carry_dma = set()
        for e in ENGINES:
            self.pending[e] |= deps

    def finalize(self):
        ops = self.ops
        needed = [i in self.forced for i in range(len(ops))]
        for o in ops:
            for d in self.deps[o.idx]:
                p = ops[d]
                if p.dma:
                    needed[d] = True
                elif p.eng == o.eng and not o.dma:
                    if p.eng == "pe":
                        continue
                    if self.same_engine_sync:
                        needed[d] = True
                else:
                    needed[d] = True
        eng_cnt = {e: 0 for e in ENGINES}
        dma_cnt = [0] * self.N_DMA_SEMS
        dma_rr = 0
        seen = {e: {} for e in ENGINES}
        for o in ops:
            waits = {}
            for d in self.deps[o.idx]:
                p = ops[d]
                if p.signal is None:
                    continue
                if (not p.dma) and p.eng == o.eng and not o.dma and (p.eng == "pe" or not self.same_engine_sync):
                    continue
                k, v, _ = p.signal
                if seen[o.eng].get(k, 0) >= v:
                    continue
                waits[k] = max(waits.get(k, 0), v)
            if o.dma and needed[o.idx]:
                j = dma_rr
                dma_rr = (dma_rr + 1) % self.N_DMA_SEMS
                k = ("dma", j)
                if dma_cnt[j] > 0 and seen[o.eng].get(k, 0) < dma_cnt[j]:
                    waits[k] = max(waits.get(k, 0), dma_cnt[j])
                dma_cnt[j] += 16
                o.signal = (k, dma_cnt[j], 16)
            elif needed[o.idx]:
                eng_cnt[o.eng] += 1
                o.signal = (("eng", o.eng), eng_cnt[o.eng], 1)
            for k, v in waits.items():
                seen[o.eng][k] = max(seen[o.eng].get(k, 0), v)
            o.waits = sorted(waits.items(), key=lambda kv: str(kv[0]))
        self.max_counts = dict(eng_cnt)

    def emit(self, nc, stack, final_waits):
        sems = {}
        for e in ENGINES:
            sems[("eng", e)] = stack.enter_context(nc.semaphore("sem_" + e))
        for j in range(self.N_DMA_SEMS):
            sems[("dma", j)] = stack.enter_context(nc.semaphore("sem_dma%d" % j))
        block = stack.enter_context(nc.Block())
        by_eng = {e: [o for o in self.ops if o.eng == e] for e in ENGINES}
        last_signal = {}
        for o in self.ops:
            if o.signal is not None:
                last_signal[o.signal[0]] = max(last_signal.get(o.signal[0], 0), o.signal[1])

        def run(engine, ename):
            for o in by_eng[ename]:
                for k, v in o.waits:
                    engine.wait_ge(sems[k], v)
                inst = o.fn(engine)
                if o.signal is not None:
                    inst.then_inc(sems[o.signal[0]], o.signal[2])
            if ename == final_waits:
                for k, v in last_signal.items():
                    if k[0] == "dma":
                        engine.wait_ge(sems[k], v)

        @block.tensor
        def _(e):
            run(e, "pe")

        @block.scalar
        def _(e):
            run(e, "act")

        @block.vector
        def _(e):
            run(e, "dve")

        @block.gpsimd
        def _(e):
            run(e, "pool")

        @block.sync
        def _(e):
            run(e, "sp")


NE = 32
CAP = 256
TRASH = NE * CAP
NSLOT = TRASH + 128
FF = 512
BIG = 30000.0


_DRAM_NAMES = {}
_LAYER_SETS = {}


class Builder:
    def __init__(self):
        self.nc = nc = bass.Bass("TRN2", target_bir_lowering=False)
        self.P = Prog()
        self.stack = contextlib.ExitStack()
        self.mem_stack = contextlib.ExitStack()
        self.dram = {}
        self.ps = [self.mem_stack.enter_context(nc.psum_tensor("ps%d" % b, [128, 512], F32)) for b in range(8)]
        sb = self.sb
        self.h = sb("h", [128, NT, D], F32)
        self.gb = sb("gb", [128, D], F32)
        self.ssq = sb("ssq", [128, NT], F32)
        self.rstd = sb("rstd", [128, NT], F32)
        self.junk = sb("junk", [128, D], BF16)
        self.ident_b = sb("ident_b", [128, 128], BF16)
        self.ident_f = sb("ident_f", [128, 128], F32)
        self.lst_b = sb("lst_b", [128, 128], BF16)
        self.ones_b = sb("ones_b", [128, 128], BF16)
        self.ebase = sb("ebase", [128, NE], F32)
        self.negh = sb("negh", [128, NT], F32)
        self.arena_size = 131 * 1024
        self.arena_base, _ = nc.bump_sbuf(self.arena_size)
        self.arena_off = 0

    def sb(self, name, shape, dt):
        return self.mem_stack.enter_context(self.nc.sbuf_tensor(name, list(shape), dt))

    def arena_reset(self):
        self.arena_off = 0

    def ar(self, name, shape, dt):
        nbytes = int(np.prod(shape[1:])) * (4 if dt in (F32, I32) else 2)
        nbytes = (nbytes + 31) // 32 * 32
        assert self.arena_off + nbytes <= self.arena_size, (name, self.arena_off, nbytes)
        t = self.nc.alloc_sbuf_tensor_at(name, list(shape), dt, offset=self.arena_base + self.arena_off)
        self.arena_off += nbytes
        return t

    def din(self, name, shape, dt=F32):
        t = self.nc.dram_tensor(name, list(shape), dt, kind="ExternalInput").ap()
        self.dram[name] = t
        return t

    def dscratch(self, name, shape, dt):
        return self.nc.dram_tensor(name, list(shape), dt, kind="Internal").ap()

    def prologue(self):
        P = self.P
        x = self.din("x", [S, D])
        self.din("norm_mix", [DEPTH, D])
        self.din("norm_ffn", [DEPTH, D])
        self.din("norm_final", [1, D])
        cI = self.din("c_ident", [128, 128])
        cL = self.din("c_lst", [128, 128])
        cE = self.din("c_ebase", [1, NE])
        self.y = self.nc.dram_tensor("y", [S, D], F32, kind="ExternalOutput").ap()
        xv = x.rearrange("(n p) d -> p n d", p=128)
        h = self.h
        for q in range(4):
            sl = slice(q * 4, (q + 1) * 4)
            P.op("sp", lambda e, sl=sl: e.dma_start(out=h[:, sl, :], in_=xv[:, sl, :]),
                 writes=[("h", i) for i in range(q * 4, q * 4 + 4)], dma=True)
        P.op("sp", lambda e: e.dma_start(out=self.ident_f[:], in_=cI), writes=["ident_f"], dma=True)
        P.op("pool", lambda e: e.dma_start(out=self.ident_b[:], in_=cI), writes=["ident_b"], dma=True)
        P.op("pool", lambda e: e.dma_start(out=self.lst_b[:], in_=cL), writes=["lst_b"], dma=True)
        P.op("sp", lambda e: e.dma_start(out=self.ebase[:], in_=cE[0:1, :].partition_broadcast(128)),
             writes=["ebase"], dma=True)
        P.op("dve", lambda e: e.memset(self.ones_b[:], 1.0), writes=["ones_b"])
        P.op("dve", lambda e: e.memset(self.negh[:], -0.5), writes=["negh"])

    def rms_stats(self, gain_row_ap):
        P, h, ssq, rstd, junk, gb = self.P, self.h, self.ssq, self.rstd, self.junk, self.gb
        P.op("sp", lambda e: e.dma_start(out=gb[:], in_=gain_row_ap.partition_broadcast(128)),
             writes=["gb"], dma=True)
        for i in range(NT):
            P.op("act", lambda e, i=i: e.activation(out=junk[:], in_=h[:, i, :], func=AF.Square,
                                                     accum_out=ssq[:, i:i + 1]),
                 reads=[("h", i)], writes=["junk", ("ssq", i)])
        P.op("dve", lambda e: e.tensor_scalar(out=rstd[:], in0=ssq[:], scalar1=1.0 / D, scalar2=EPS,
                                              op0=ALU.mult, op1=ALU.add),
             reads=[("ssq", i) for i in range(NT)], writes=["rstd"])
        P.op("pool", lambda e: e.tensor_tensor(out=rstd[:], in0=rstd[:], in1=self.negh[:], op=ALU.pow),
             reads=["rstd", "negh"], writes=["rstd"])

    def epilogue(self, final_norm=True):
        P, h = self.P, self.h
        P.barrier()
        self.arena_reset()
        outt = self.ar("outt", [128, 2, D], F32)
        yv = self.y.rearrange("(n p) d -> p n d", p=128)
        if final_norm:
            self.rms_stats(self.dram["norm_final"][0:1, :])
        for i in range(NT):
            b = i % 2
            if final_norm:
                P.op("dve", lambda e, i=i, b=b: e.scalar_tensor_tensor(
                    out=outt[:, b, :], in0=h[:, i, :], scalar=self.rstd[:, i:i + 1], in1=self.gb[:],
                    op0=ALU.mult, op1=ALU.mult),
                    reads=[("h", i), "rstd", "gb"], writes=[("outt", b)])
                P.op("sp", lambda e, i=i, b=b: e.dma_start(out=yv[:, i, :], in_=outt[:, b, :]),
                     reads=[("outt", b)], writes=[("y", i)], dma=True, force=True)
            else:
                P.op("sp", lambda e, i=i: e.dma_start(out=yv[:, i, :], in_=h[:, i, :]),
                     reads=[("h", i)], writes=[("y", i)], dma=True, force=True)

    def finish(self):
        _DRAM_NAMES[id(self.nc)] = list(self.dram.keys())
        self.P.finalize()
        self.P.emit(self.nc, self.stack, final_waits="sp")
        self.stack.close()
        return self.nc

    def moe_setup(self, layers):
        self.moe_layers = list(layers)
        nl = len(self.moe_layers)
        self.din("router_w", [nl, D, 36])
        self.din("router_b", [nl, 36])
        for p in range(nl):
            self.din("expert_w_gu%d" % p, [NE, D, 2 * FF])
            self.din("expert_w_down%d" % p, [NE, FF, D])
        self.Xs = self.dscratch("Xs", [NSLOT, D], BF16)
        self.Ys = self.dscratch("Ys", [NSLOT, D], BF16)
        P = self.P
        self.arena_reset()
        self.zeros_b = self.ar("zeros_b", [128, D], BF16)
        P.op("dve", lambda e: e.memset(self.zeros_b[:], 0.0), writes=["zeros_b"])
        self.zeroed = False

    def moe_layer(self, layer):
        li = self.moe_layers.index(layer)
        P, h, ps = self.P, self.h, self.ps
        rstd, gb = self.rstd, self.gb
        P.barrier()
        self.arena_reset()
        ar = self.ar
        wgu = [ar("wgu%d" % s, [128, 8, 2 * FF], BF16) for s in range(2)]
        wdn = [ar("wdn%d" % s, [128, 4, D], BF16) for s in range(2)]
        NYG = 8
        yg = [self.nc.alloc_sbuf_tensor_at("yg%d_%d" % (layer, s), [128, D], BF16, offset=self.arena_base + s * 2048)
              for s in range(NYG)]
        save = self.arena_off
        NRB = 3
        hn32 = [ar("hn32_%d" % s, [128, D], F32) for s in range(NRB)]
        hhi = [ar("hhi%d" % s, [128, D], BF16) for s in range(NRB)]
        hlo = [ar("hlo%d" % s, [128, D], BF16) for s in range(NRB)]
        hiT = [ar("hiT%d" % s, [128, 8, 128], BF16) for s in range(NRB)]
        loT = [ar("loT%d" % s, [128, 8, 128], BF16) for s in range(NRB)]
        end1 = self.arena_off
        self.arena_off = save
        xg = [ar("xg%d" % s, [128, 2, D], BF16) for s in range(2)]
        xT = [ar("xT%d" % s, [128, 8, CAP], BF16) for s in range(2)]
        hT = [ar("hT%d" % s, [128, 4, CAP], BF16) for s in range(2)]
        sA = [ar("sA%d" % s, [128, CAP], F32) for s in range(2)]
        yt = [ar("yt%d" % s, [128, 2, D], BF16) for s in range(2)]
        self.arena_off = max(self.arena_off, end1)
        NHB = 4
        hnb = [ar("hnb%d" % s, [128, D], BF16) for s in range(NHB)]
        fence = ar("fence", [128, 8], F32)
        fence2 = ar("fence2", [128, D], BF16)
        wr_hi = ar("wr_hi", [128, 8, 36], BF16)
        wr_lo = ar("wr_lo", [128, 8, 36], BF16)
        wr32 = ar("wr32", [128, 8, 36], F32)
        brb = ar("brb", [128, 36], F32)
        LG = ar("LG", [128, NT, 36], F32)
        Em = ar("Em", [128, NT, 32], F32)
        T1 = ar("T1", [128, NT, 32], F32)
        oh1 = ar("oh1", [128, NT, 32], F32)
        oh2 = ar("oh2", [128, NT, 32], F32)
        Em2 = ar("Em2", [128, NT, 32], F32)
        RK = ar("RK", [128, NT, 32], F32)
        Obf = ar("Obf", [128, NT, 32], BF16)
        Gm = ar("Gm", [128, NT, 4], F32)
        ohG = ar("ohG", [128, NT, 4], F32)
        pen = ar("pen", [128, NT, 4], F32)
        sm = {n: ar("sm_" + n, [128, NT], F32) for n in
              ("gmax", "sumG", "pg", "m1", "m2", "r", "den", "g1", "g2", "rk", "base", "valid", "sl")}
        slot_i = ar("slot_i", [128, 2, NT], I32)

        wr = self.dram["router_w"]
        br = self.dram["router_b"]
        wgu_d = self.dram["expert_w_gu%d" % li]
        wdn_d = self.dram["expert_w_down%d" % li]
        Xs, Ys = self.Xs, self.Ys

        self.rms_stats(self.dram["norm_ffn"][layer:layer + 1, :])
        P.op("sp", lambda e: e.dma_start(out=wr32[:], in_=wr[li].rearrange("(c p) j -> p c j", p=128)),
             writes=["wr32"], dma=True)
        P.op("sp", lambda e: e.dma_start(out=brb[:], in_=br[li:li + 1, :].partition_broadcast(128)),
             writes=["brb"], dma=True)

        def load_w(e_idx):
            s = e_idx % 2
            P.op("pool", lambda e: e.dma_start(out=wgu[s][:], in_=wgu_d[e_idx].rearrange("(c p) f -> p c f", p=128)),
                 writes=[("wgu", s)], dma=True)
            P.op("pool", lambda e: e.dma_start(out=wdn[s][:], in_=wdn_d[e_idx].rearrange("(c p) f -> p c f", p=128)),
                 writes=[("wdn", s)], dma=True)

        load_w(0)
        load_w(1)
        nr = NSLOT // 128
        xs_z_keys = [("Xs_z", r0) for r0 in range(0, nr, 13)] + ["Ys_z"]
        if not self.zeroed:
            self.zeroed = True
            P.op("dve", lambda e: e.memset(fence2[:], 0.0), writes=["zeros_b"])
            xsv = Xs.rearrange("(r p) d -> p r d", p=128)
            ysv = Ys.rearrange("(r p) d -> p r d", p=128)
            for r0 in range(0, nr, 13):
                P.op("sp", lambda e, r0=r0: e.dma_start(
                    out=xsv[:, r0:r0 + 13, :], in_=fence2[:].unsqueeze(1).to_broadcast([128, 13, D])),
                    reads=["zeros_b"], writes=[("Xs_z", r0)], dma=True)
            P.op("sp", lambda e: e.dma_start(out=ysv[:, nr - 1, :], in_=fence2[:]),
                 reads=["zeros_b"], writes=["Ys_z"], dma=True)

        P.op("act", lambda e: e.activation(out=wr_hi[:], in_=wr32[:], func=AF.Copy), reads=["wr32"], writes=["wr_hi"])
        P.op("pool", lambda e: e.tensor_tensor(out=wr_lo[:], in0=wr32[:], in1=wr_hi[:], op=ALU.subtract),
             reads=["wr32", "wr_hi"], writes=["wr_lo"])
        def st1(t):
            b = t % NRB
            P.op("dve", lambda e: e.scalar_tensor_tensor(
                out=hn32[b][:], in0=h[:, t, :], scalar=rstd[:, t:t + 1], in1=gb[:], op0=ALU.mult, op1=ALU.mult),
                reads=[("h", t), "rstd", "gb"], writes=[("hn32", b)])
            P.op("act", lambda e: e.activation(out=hhi[b][:], in_=hn32[b][:], func=AF.Copy),
                 reads=[("hn32", b)], writes=[("hhi", b)])
            P.op("pool", lambda e: e.tensor_tensor(out=hlo[b][:], in0=hn32[b][:], in1=hhi[b][:], op=ALU.subtract),
                 reads=[("hn32", b), ("hhi", b)], writes=[("hlo", b)])

        def st2(t):
            b = t % NRB
            for w, (src, sk, dstT, dk) in enumerate(((hhi, "hhi", hiT, "hiT"), (hlo, "hlo", loT, "loT"))):
                bi = 2 * (t % 2) + w

                def tr(e, src=src, bi=bi):
                    pv = ps[bi][:].bitcast(BF16)
                    for c in range(8):
                        inst = e.transpose(out=pv[:, c * 128:(c + 1) * 128], in_=src[b][:, c * 128:(c + 1) * 128],
                                           identity=self.ident_b[:])
                    return inst
                P.op("pe", tr, reads=[(sk, b), "ident_b"], writes=[("ps", bi)])
                srcv = lambda bi=bi: ps[bi][:].bitcast(BF16).rearrange("p (c t) -> p c t", c=8)
                if w == 0:
                    P.op("act", lambda e, srcv=srcv, dstT=dstT: e.activation(out=dstT[b][:], in_=srcv(), func=AF.Copy),
                         reads=[("ps", bi)], writes=[(dk, b)])
                else:
                    P.op("dve", lambda e, srcv=srcv, dstT=dstT: e.tensor_copy(out=dstT[b][:], in_=srcv()),
                         reads=[("ps", bi)], writes=[(dk, b)])

        def st3(t):
            b = t % NRB
            pb = 4 + t % 2

            def rl(e):
                k = 0
                for (xt, wt) in ((hiT, wr_hi), (loT, wr_hi), (hiT, wr_lo)):
                    for c in range(8):
                        inst = e.matmul(ps[pb][:, 0:36], xt[b][:, c, :], wt[:, c, :], start=(k == 0), stop=(k == 23))
                        k += 1
                return inst
            P.op("pe", rl, reads=[("hiT", b), ("loT", b), "wr_hi", "wr_lo"], writes=[("ps", pb)])
            P.op("dve", lambda e: e.tensor_tensor(out=LG[:, t, :], in0=ps[pb][:, 0:36], in1=brb[:], op=ALU.add),
                 reads=[("ps", pb), "brb"], writes=[("LG", t)])

        for i in range(NT + 2):
            if i < NT:
                st1(i)
            if 0 <= i - 1 < NT:
                st2(i - 1)
            if 0 <= i - 2 < NT:
                st3(i - 2)

        if int(os.environ.get('MOE_STOP', '99')) <= 1:
            return
        G = LG[:, :, 0:4]
        E4 = LG[:, :, 4:36].rearrange("p n (g e) -> p n g e", g=4)

        def bc(t2, n):
            return t2[:].unsqueeze(2).to_broadcast([128, NT, n])

        def dve(fn, reads, writes):
            P.op("dve", fn, reads=reads, writes=writes)

        P.op("dve", lambda e: e.memset(fence[:], 0.0), reads=[("LG", t) for t in range(NT)], writes=["LG"])
        dve(lambda e: e.tensor_reduce(out=sm["gmax"][:], in_=G, axis=AX.X, op=ALU.max), ["LG"], ["gmax"])
        dve(lambda e: e.tensor_tensor(out=Gm[:], in0=G, in1=bc(sm["gmax"], 4), op=ALU.subtract), ["LG", "gmax"], ["Gm"])
        dve(lambda e: e.tensor_single_scalar(out=ohG[:], in_=Gm[:], scalar=0.0, op=ALU.is_ge), ["Gm"], ["ohG"])
        P.op("act", lambda e: e.activation(out=Gm[:], in_=Gm[:], func=AF.Exp), reads=["Gm", "ohG"], writes=["Gm"])
        dve(lambda e: e.tensor_reduce(out=sm["sumG"][:], in_=Gm[:], axis=AX.X, op=ALU.add), ["Gm"], ["sumG"])
        dve(lambda e: e.reciprocal(out=sm["pg"][:], in_=sm["sumG"][:]), ["sumG"], ["pg"])
        dve(lambda e: e.tensor_scalar(out=pen[:], in0=ohG[:], scalar1=BIG, scalar2=-BIG, op0=ALU.mult, op1=ALU.add),
            ["ohG"], ["pen"])
        dve(lambda e: e.tensor_tensor(out=Em[:].rearrange("p n (g e) -> p n g e", g=4), in0=E4,
                                      in1=pen[:].unsqueeze(3).to_broadcast([128, NT, 4, 8]), op=ALU.add),
            ["LG", "pen"], ["Em"])
        dve(lambda e: e.tensor_reduce(out=sm["m1"][:], in_=Em[:], axis=AX.X, op=ALU.max), ["Em"], ["m1"])
        dve(lambda e: e.tensor_tensor(out=T1[:], in0=Em[:], in1=bc(sm["m1"], 32), op=ALU.subtract), ["Em", "m1"], ["T1"])
        dve(lambda e: e.tensor_single_scalar(out=oh1[:], in_=T1[:], scalar=0.0, op=ALU.is_ge), ["T1"], ["oh1"])
        dve(lambda e: e.scalar_tensor_tensor(out=Em2[:], in0=oh1[:], scalar=-BIG, in1=Em[:], op0=ALU.mult, op1=ALU.add),
            ["oh1", "Em"], ["Em2"])
        dve(lambda e: e.tensor_reduce(out=sm["m2"][:], in_=Em2[:], axis=AX.X, op=ALU.max), ["Em2"], ["m2"])
        dve(lambda e: e.tensor_tensor(out=T1[:], in0=Em2[:], in1=bc(sm["m2"], 32), op=ALU.subtract), ["Em2", "m2"], ["T1"])
        dve(lambda e: e.tensor_single_scalar(out=oh2[:], in_=T1[:], scalar=0.0, op=ALU.is_ge), ["T1"], ["oh2"])
        dve(lambda e: e.tensor_tensor(out=sm["r"][:], in0=sm["m2"][:], in1=sm["m1"][:], op=ALU.subtract), ["m1", "m2"], ["r"])
        P.op("act", lambda e: e.activation(out=sm["r"][:], in_=sm["r"][:], func=AF.Exp), reads=["r"], writes=["r"])
        dve(lambda e: e.tensor_scalar(out=sm["den"][:], in0=sm["r"][:], scalar1=1.0, scalar2=None, op0=ALU.add), ["r"], ["den"])
        dve(lambda e: e.reciprocal(out=sm["den"][:], in_=sm["den"][:]), ["den"], ["den"])
        dve(lambda e: e.tensor_tensor(out=sm["g1"][:], in0=sm["pg"][:], in1=sm["den"][:], op=ALU.mult), ["pg", "den"], ["g1"])
        dve(lambda e: e.tensor_tensor(out=sm["g2"][:], in0=sm["g1"][:], in1=sm["r"][:], op=ALU.mult), ["g1", "r"], ["g2"])
        dve(lambda e: e.tensor_tensor(out=Obf[:], in0=oh1[:], in1=oh2[:], op=ALU.add), ["oh1", "oh2"], ["Obf"])

        if int(os.environ.get('MOE_STOP', '99')) <= 2:
            return
        def ranks(e):
            for t in range(NT):
                o = ps[7][:, t * 32:(t + 1) * 32]
                inst = e.matmul(o, self.lst_b[:], Obf[:, t, :], start=True, stop=(t == 0))
                for j in range(t):
                    inst = e.matmul(o, self.ones_b[:], Obf[:, j, :], start=False, stop=(j == t - 1))
            return inst
        P.op("pe", ranks, reads=["Obf", "lst_b", "ones_b"], writes=[("ps", 7)])
        dve(lambda e: e.tensor_copy(out=RK[:], in_=ps[7][:].rearrange("p (n e) -> p n e", e=32)), [("ps", 7)], ["RK"])
        ebc = self.ebase[:].unsqueeze(1).to_broadcast([128, NT, 32])
        for k, (oh, ohn, gn) in enumerate(((oh1, "oh1", "g1"), (oh2, "oh2", "g2"))):
            dve(lambda e, oh=oh: e.tensor_tensor(out=T1[:], in0=oh[:], in1=RK[:], op=ALU.mult), [ohn, "RK"], ["T1"])
            dve(lambda e: e.tensor_reduce(out=sm["rk"][:], in_=T1[:], axis=AX.X, op=ALU.add), ["T1"], ["rk"])
            dve(lambda e, oh=oh: e.tensor_tensor(out=T1[:], in0=oh[:], in1=ebc, op=ALU.mult), [ohn, "ebase"], ["T1"])
            dve(lambda e: e.tensor_reduce(out=sm["base"][:], in_=T1[:], axis=AX.X, op=ALU.add), ["T1"], ["base"])
            dve(lambda e: e.tensor_single_scalar(out=sm["valid"][:], in_=sm["rk"][:], scalar=float(CAP), op=ALU.is_lt),
                ["rk"], ["valid"])
            dve(lambda e: e.scalar_tensor_tensor(out=sm["sl"][:], in0=sm["rk"][:], scalar=-float(TRASH), in1=sm["base"][:],
                                                 op0=ALU.add, op1=ALU.add), ["rk", "base"], ["sl"])
            dve(lambda e: e.tensor_tensor(out=sm["sl"][:], in0=sm["sl"][:], in1=sm["valid"][:], op=ALU.mult), ["sl", "valid"], ["sl"])
            dve(lambda e: e.tensor_scalar(out=sm["sl"][:], in0=sm["sl"][:], scalar1=float(TRASH), scalar2=None, op0=ALU.add),
                ["sl"], ["sl"])
            dve(lambda e, k=k: e.tensor_copy(out=slot_i[:, k, :], in_=sm["sl"][:]), ["sl"], [("slot", k)])
            dve(lambda e, gn=gn: e.tensor_tensor(out=sm[gn][:], in0=sm[gn][:], in1=sm["valid"][:], op=ALU.mult),
                [gn, "valid"], [gn])

        if int(os.environ.get('MOE_STOP', '99')) <= 3:
            return
        for t in range(NT):
            b = t % NHB
            P.op("dve", lambda e, t=t, b=b: e.scalar_tensor_tensor(
                out=hnb[b][:], in0=h[:, t, :], scalar=rstd[:, t:t + 1], in1=gb[:], op0=ALU.mult, op1=ALU.mult),
                reads=[("h", t), "rstd", "gb"], writes=[("hnb", b)])
            for k in range(2):
                P.op("pool", lambda e, t=t, b=b, k=k: e.indirect_dma_start(
                    out=Xs[:, :], out_offset=bass.IndirectOffsetOnAxis(ap=slot_i[:, k, t:t + 1], axis=0),
                    in_=hnb[b][:], in_offset=None),
                    reads=[("hnb", b), ("slot", k)] + xs_z_keys, writes=[("Xs_w", t, k)], dma=True)

        if int(os.environ.get('MOE_STOP', '99')) <= 4:
            dbg = [sm["sl"], sm["g1"], sm["g2"], sm["rk"], sm["base"], sm["valid"]]
            for q, tl in enumerate(dbg):
                P.op("dve", lambda e, q=q, tl=tl: e.tensor_copy(out=h[:, 0, q * 16:(q + 1) * 16], in_=tl[:]),
                     reads=["sl", "g1", "g2", "rk", "base", "valid"], writes=[("h", 0)])
            for k in range(2):
                P.op("dve", lambda e, k=k: e.tensor_copy(out=h[:, 0, 96 + k * 16:112 + k * 16], in_=slot_i[:, k, :]),
                     reads=[("slot", k)], writes=[("h", 0)])
            P.op("dve", lambda e: e.tensor_copy(out=h[:, 1, 0:576], in_=LG[:].rearrange("p n j -> p (n j)")),
                 reads=["LG"], writes=[("h", 1)])
            return
        P.barrier()
        xs_keys = [("Xs_w", t, k) for t in range(NT) for k in range(2)]

        def load_x(ex):
            s = ex % 2
            P.op("sp", lambda e: e.dma_start(
                out=xg[s][:], in_=Xs[ex * CAP:(ex + 1) * CAP, :].rearrange("(r p) d -> p r d", p=128)),
                reads=xs_keys, writes=[("xg", s)], dma=True)

        load_x(0)
        for ex in range(NE):
            s = ex % 2
            if ex + 1 < NE:
                load_x(ex + 1)
            for r in range(2):
                bank = ps[r]

                def tr2(e, r=r, s=s, bank=bank):
                    pv = bank[:].bitcast(BF16)
                    for c in range(8):
                        inst = e.transpose(out=pv[:, c * 128:(c + 1) * 128], in_=xg[s][:, r, c * 128:(c + 1) * 128],
                                           identity=self.ident_b[:])
                    return inst
                P.op("pe", tr2, reads=[("xg", s), "ident_b"], writes=[("ps", r)])
                src = lambda bank=bank: bank[:].bitcast(BF16).rearrange("p (c t) -> p c t", c=8)
                if r == 0:
                    P.op("act", lambda e, s=s, src=src: e.activation(out=xT[s][:, :, 0:128], in_=src(), func=AF.Copy),
                         reads=[("ps", 0)], writes=[("xT", s, 0)])
                else:
                    P.op("dve", lambda e, s=s, src=src: e.tensor_copy(out=xT[s][:, :, 128:256], in_=src()),
                         reads=[("ps", 1)], writes=[("xT", s, 1)])
            for m in range(4):
                bank = ps[2 + m % 2]
                bk = ("ps", 2 + m % 2)

                def gu(e, m=m, s=s, bank=bank):
                    for half in range(2):
                        col = half * FF + m * 128
                        for c in range(8):
                            inst = e.matmul(bank[:, half * CAP:(half + 1) * CAP], wgu[s][:, c, col:col + 128],
                                            xT[s][:, c, :], start=(c == 0), stop=(c == 7))
                    return inst
                P.op("pe", gu, reads=[("wgu", s), ("xT", s, 0), ("xT", s, 1)], writes=[bk])
                P.op("act", lambda e, m=m, bank=bank: e.activation(out=sA[m % 2][:], in_=bank[:, 0:CAP], func=AF.Silu),
                     reads=[bk], writes=[("sA", m % 2)])
                P.op("dve", lambda e, m=m, s=s, bank=bank: e.tensor_tensor(out=hT[s][:, m, :], in0=sA[m % 2][:],
                                                                          in1=bank[:, CAP:2 * CAP], op=ALU.mult),
                     reads=[bk, ("sA", m % 2)], writes=[("hT", s, m)])
            for r in range(2):
                for n in range(2):
                    q = r * 2 + n
                    bank = ps[4 + q % 2]
                    bk = ("ps", 4 + q % 2)

                    def dn(e, r=r, n=n, s=s, bank=bank):
                        for m in range(4):
                            inst = e.matmul(bank[:, :], hT[s][:, m, r * 128:(r + 1) * 128],
                                            wdn[s][:, m, n * 512:(n + 1) * 512], start=(m == 0), stop=(m == 3))
                        return inst
                    P.op("pe", dn, reads=[("wdn", s)] + [("hT", s, m) for m in range(4)], writes=[bk])
                    if q % 2 == 0:
                        P.op("act", lambda e, r=r, n=n, s=s, bank=bank: e.activation(
                            out=yt[s][:, r, n * 512:(n + 1) * 512], in_=bank[:, :], func=AF.Copy),
                            reads=[bk], writes=[("yt", s, q)])
                    else:
                        P.op("dve", lambda e, r=r, n=n, s=s, bank=bank: e.tensor_copy(
                            out=yt[s][:, r, n * 512:(n + 1) * 512], in_=bank[:, :]),
                            reads=[bk], writes=[("yt", s, q)])
            P.op("sp", lambda e, ex=ex, s=s: e.dma_start(
                out=Ys[ex * CAP:(ex + 1) * CAP, :].rearrange("(r p) d -> p r d", p=128), in_=yt[s][:]),
                reads=[("yt", s, q) for q in range(4)], writes=[("Ys_w", ex)], dma=True)
            if ex + 2 < NE:
                load_w(ex + 2)

        if int(os.environ.get('MOE_STOP', '99')) <= 5:
            return
        P.op("pool", lambda e: e.memset(fence[:], 0.0), writes=[("wgu", 0)] + [("yg", b) for b in range(NYG)])
        for t in range(NT):
            for k, gn in enumerate(("g1", "g2")):
                b = (t * 2 + k) % NYG
                P.op("pool", lambda e, t=t, b=b, k=k: e.indirect_dma_start(
                    out=yg[b][:], out_offset=None, in_=Ys[:, :],
                    in_offset=bass.IndirectOffsetOnAxis(ap=slot_i[:, k, t:t + 1], axis=0)),
                    reads=[("Ys_w", ex) for ex in range(NE)] + [("slot", k), "Ys_z"], writes=[("yg", b)], dma=True)
                P.op("dve", lambda e, t=t, b=b, gn=gn: e.scalar_tensor_tensor(
                    out=h[:, t, :], in0=yg[b][:], scalar=sm[gn][:, t:t + 1], in1=h[:, t, :], op0=ALU.mult, op1=ALU.add),
                    reads=[("yg", b), gn, ("h", t)], writes=[("h", t)])

    def norm_transpose(self, hnT, hnb):
        P, h, ps = self.P, self.h, self.ps
        def st1(t):
            b = t % 2
            P.op("dve", lambda e: e.scalar_tensor_tensor(
                out=hnb[b][:], in0=h[:, t, :], scalar=self.rstd[:, t:t + 1], in1=self.gb[:], op0=ALU.mult, op1=ALU.mult),
                reads=[("h", t), "rstd", "gb"], writes=[("hnb", b)])
        st1(0)
        for t in range(NT):
            if t + 1 < NT:
                st1(t + 1)
            self.transpose_tile(hnb[t % 2], ("hnb", t % 2), hnT, t, t % 2)

    def transpose_tile(self, src, src_key, dstT, t, b):
        P, ps = self.P, self.ps
        bank = ps[b]

        def tr(e):
            pv = bank[:].bitcast(BF16)
            for c in range(8):
                inst = e.transpose(out=pv[:, c * 128:(c + 1) * 128], in_=src[:, c * 128:(c + 1) * 128],
                                   identity=self.ident_b[:])
            return inst
        P.op("pe", tr, reads=[src_key, "ident_b"], writes=[("ps", b)])
        srcv = lambda: bank[:].bitcast(BF16).rearrange("p (c t) -> p c t", c=8)
        if b == 0:
            P.op("act", lambda e: e.activation(out=dstT[:, :, t * 128:(t + 1) * 128], in_=srcv(), func=AF.Copy),
                 reads=[("ps", b)], writes=[("xT", t)])
        else:
            P.op("dve", lambda e: e.tensor_copy(out=dstT[:, :, t * 128:(t + 1) * 128], in_=srcv()),
                 reads=[("ps", b)], writes=[("xT", t)])

    def out_proj(self, xT, kc, wout, n_k):
        P, h, ps = self.P, self.h, self.ps
        for t in range(NT):
            for n in range(2):
                bi = 2 + (t * 2 + n) % 2
                bank = ps[bi]

                def mm(e, t=t, n=n, bank=bank):
                    for c in range(kc):
                        inst = e.matmul(bank[:, :], xT[:, c, t * 128:(t + 1) * 128], wout[:, c, n * 512:(n + 1) * 512],
                                        start=(c == 0), stop=(c == kc - 1))
                    return inst
                P.op("pe", mm, reads=[("xT", t), "wout"] if n_k is None else n_k(t) + ["wout"], writes=[("ps", bi)])
                P.op("dve", lambda e, t=t, n=n, bank=bank: e.tensor_tensor(
                    out=h[:, t, n * 512:(n + 1) * 512], in0=h[:, t, n * 512:(n + 1) * 512], in1=bank[:, :], op=ALU.add),
                    reads=[("ps", bi), ("h", t)], writes=[("h", t)])

    def fox_setup(self, layers):
        self.fox_layers = list(layers)
        nl = len(layers)
        self.din("fox_w_in", [nl, D, 4112])
        self.din("fox_b_f", [nl, 16])
        self.din("fox_w_out", [nl, D, D])
        self.din("c_mask", [128, 128])
        self.cum3_d = self.dscratch("cum3_d", [16, 3, S], BF16)
        self.Og_d = self.dscratch("Og_d", [S, D], BF16)
        self.mask_b = self.sb("mask_b", [128, 128], BF16)
        self.P.op("pool", lambda e: e.dma_start(out=self.mask_b[:], in_=self.dram["c_mask"]), writes=["mask_b"], dma=True)

    def fox_layer(self, layer):
        j = self.fox_layers.index(layer)
        P, h, ps, ar = self.P, self.h, self.ps, self.ar
        w_in = self.dram["fox_w_in"]
        P.barrier()
        self.arena_reset()
        hnT = ar("f_hnT", [128, 8, S], BF16)
        hnb = [ar("f_hnb%d" % s, [128, D], BF16) for s in range(2)]
        wf = ar("f_wf", [128, 8, 16], BF16)
        bft = ar("f_bft", [128, 1], F32)
        negbf = ar("f_negbf", [128, 1], F32)
        save = self.arena_off
        Ft = ar("f_Ft", [128, S], F32)
        cumP = ar("f_cumP", [128, S], F32)
        r1 = ar("f_r1", [128, S], F32)
        cum3 = ar("f_cum3", [128, 3, S], BF16)
        self.arena_off = save
        wgrp = [ar("f_wgrp%d" % s, [128, 8, 4, 128], BF16) for s in range(2)]
        QTa = [ar("f_QTa%d" % s, [128, S], BF16) for s in range(2)]
        KTa = [ar("f_KTa%d" % s, [128, S], BF16) for s in range(2)]
        Tst = [ar("f_Tst%d" % s, [128, S], BF16) for s in range(2)]
        Vaug = ar("f_Vaug", [128, NT, 2, 65], BF16)
        Gs = ar("f_Gs", [128, NT, 128], BF16)
        NPT = 8
        PT = [ar("f_PT%d" % s, [128, 512], BF16) for s in range(NPT)]
        Og = [ar("f_Og%d" % s, [128, NT, 128], BF16) for s in range(2)]
        wout = ar("f_wout", [128, 8, D], BF16)
        rden = ar("f_rden", [128, 8], F32)

        self.rms_stats(self.dram["norm_mix"][layer:layer + 1, :])
        P.op("pool", lambda e: e.dma_start(out=wf[:], in_=w_in[j][:, 4096:4112].rearrange("(c p) f -> p c f", p=128)),
             writes=["wf"], dma=True)
        P.op("sp", lambda e: e.dma_start(out=bft[0:16, :], in_=self.dram["fox_b_f"][j].rearrange("(h o) -> h o", o=1)),
             writes=["bft"], dma=True)
        P.op("dve", lambda e: e.tensor_scalar(out=negbf[0:16, :], in0=bft[0:16, :], scalar1=-1.0, scalar2=None, op0=ALU.mult),
             reads=["bft"], writes=["negbf"])
        self.norm_transpose(hnT, hnb)
        xT_all = [("xT", t) for t in range(NT)]

        if int(os.environ.get('FOX_STOP', '99')) <= 1:
            return
        for qd in range(4):
            bank = ps[2 + qd % 2]
            bk = ("ps", 2 + qd % 2)

            def fm(e, qd=qd, bank=bank):
                for c in range(8):
                    inst = e.matmul(bank[0:16, :], wf[:, c, :], hnT[:, c, qd * 512:(qd + 1) * 512], start=(c == 0), stop=(c == 7))
                return inst
            P.op("pe", fm, reads=xT_all + ["wf"], writes=[bk])
            P.op("act", lambda e, qd=qd, bank=bank: e.activation(out=Ft[0:16, qd * 512:(qd + 1) * 512], in_=bank[0:16, :],
                                                                func=AF.Exp, scale=-1.0, bias=negbf[0:16, :]),
                 reads=[bk, "negbf"], writes=["Ft"])
        P.op("act", lambda e: e.activation(out=Ft[0:16, :], in_=Ft[0:16, :], func=AF.Ln, bias=1.0), reads=["Ft"], writes=["Ft"])
        P.op("dve", lambda e: e.tensor_scalar(out=Ft[0:16, :], in0=Ft[0:16, :], scalar1=0.5, scalar2=None, op0=ALU.mult),
             reads=["Ft"], writes=["Ft"])
        P.op("dve", lambda e: e.tensor_tensor_scan(out=cumP[0:16, :], data0=Ft[0:16, :], data1=Ft[0:16, :], initial=0.0,
                                                   op0=ALU.add, op1=ALU.add), reads=["Ft"], writes=["cumP"])
        P.op("dve", lambda e: e.tensor_copy(out=cum3[0:16, 0, :], in_=cumP[0:16, :]), reads=["cumP"], writes=["cum3"])
        P.op("dve", lambda e: e.tensor_tensor(out=r1[0:16, :], in0=cumP[0:16, :], in1=cum3[0:16, 0, :], op=ALU.subtract),
             reads=["cumP", "cum3"], writes=["r1"])
        P.op("dve", lambda e: e.tensor_copy(out=cum3[0:16, 1, :], in_=r1[0:16, :]), reads=["r1"], writes=["cum3"])
        P.op("dve", lambda e: e.tensor_tensor(out=r1[0:16, :], in0=r1[0:16, :], in1=cum3[0:16, 1, :], op=ALU.subtract),
             reads=["r1", "cum3"], writes=["r1"])
        P.op("dve", lambda e: e.tensor_copy(out=cum3[0:16, 2, :], in_=r1[0:16, :]), reads=["r1"], writes=["cum3"])
        P.op("sp", lambda e: e.dma_start(out=self.cum3_d, in_=cum3[0:16, :, :]), reads=["cum3"], writes=["cum3_d"], dma=True)
        if int(os.environ.get('FOX_STOP', '99')) <= 2:
            return
        P.barrier()

        P.op("pool", lambda e: e.dma_start(out=wout[:], in_=self.dram["fox_w_out"][j].rearrange("(c p) f -> p c f", p=128)),
             writes=["wout"], dma=True)
        for hh in range(2):
            P.op("dve", lambda e, hh=hh: e.memset(QTa[hh][64:128, :], 0.0), writes=[("QTa", hh, "aug")])
            P.op("dve", lambda e, hh=hh: e.memset(KTa[hh][64:128, :], 0.0), writes=[("KTa", hh, "aug")])
            P.op("dve", lambda e, hh=hh: e.memset(QTa[hh][64:70, :], 1.0), writes=[("QTa", hh, "aug")])
            P.op("dve", lambda e, hh=hh: e.memset(KTa[hh][64:70, :], -1.0), writes=[("KTa", hh, "aug")])
        P.op("dve", lambda e: e.memset(Vaug[:, :, :, 64:65], 1.0), writes=["Vaug1"])

        def load_grp(g):
            s = g % 2
            for seg in range(4):
                P.op("pool", lambda e, seg=seg: e.dma_start(
                    out=wgrp[s][:, :, seg, :],
                    in_=w_in[j][:, seg * 1024 + g * 128: seg * 1024 + (g + 1) * 128].rearrange("(c p) f -> p c f", p=128)),
                    writes=[("wgrp", s, seg)], dma=True)

        if int(os.environ.get('FOX_STOP', '99')) <= 3:
            return
        load_grp(0)
        pt_rr = [0]
        ogv = self.Og_d.rearrange("(n p) d -> p n d", p=128)
        NG = int(os.environ.get('FOX_NG', '8'))
        for g in range(NG):
            s = g % 2
            if g + 1 < NG:
                load_grp(g + 1)
            for qk in range(2):
                dstT = QTa if qk == 0 else KTa
                dn = "QTa" if qk == 0 else "KTa"
                for qd in range(4):
                    bi = (qk * 4 + qd) % 2
                    bank = ps[bi]
                    cs = slice(qd * 512, (qd + 1) * 512)

                    def pm(e, qk=qk, qd=qd, bank=bank, s=s):
                        for c in range(8):
                            inst = e.matmul(bank[:, :], wgrp[s][:, c, qk, :],
                                            hnT[:, c, qd * 512:(qd + 1) * 512], start=(c == 0), stop=(c == 7))
                        return inst
                    P.op("pe", pm, reads=xT_all + [("wgrp", s, qk)], writes=[("ps", bi)])
                    if qk == 0:
                        P.op("act", lambda e, cs=cs, bank=bank: e.activation(
                            out=QTa[0][0:64, cs], in_=bank[0:64, :], func=AF.Identity, scale=0.125),
                            reads=[("ps", bi)], writes=[("QTa", 0, qd)])
                        P.op("act", lambda e, cs=cs, bank=bank: e.activation(
                            out=Tst[0][64:128, cs], in_=bank[64:128, :], func=AF.Identity, scale=0.125),
                            reads=[("ps", bi)], writes=[("Tst", 0, qd)])
                    else:
                        P.op("dve", lambda e, cs=cs, bank=bank: e.tensor_copy(out=KTa[0][0:64, cs], in_=bank[0:64, :]),
                             reads=[("ps", bi)], writes=[("KTa", 0, qd)])
                        P.op("dve", lambda e, cs=cs, bank=bank: e.tensor_copy(out=Tst[1][64:128, cs], in_=bank[64:128, :]),
                             reads=[("ps", bi)], writes=[("Tst", 1, qd)])
                P.op("sp", lambda e, qk=qk, dstT=dstT: e.dma_start(out=dstT[1][0:64, :], in_=Tst[qk][64:128, :]),
                     reads=[("Tst", qk, qd) for qd in range(4)], writes=[(dn, 1, qd) for qd in range(4)], dma=True)
            for hh in range(2):
                head = 2 * g + hh
                P.op("sp", lambda e, hh=hh, head=head: e.dma_start(out=QTa[hh][64:67, :], in_=self.cum3_d[head]),
                     reads=["cum3_d"], writes=[("QTa", hh, "aug")], dma=True)
                P.op("sp", lambda e, hh=hh, head=head: e.dma_start(out=KTa[hh][67:70, :], in_=self.cum3_d[head]),
                     reads=["cum3_d"], writes=[("KTa", hh, "aug")], dma=True)
            if int(os.environ.get('FOX_STOP', '99')) <= 4:
                return
            for t in range(NT):
                bi = t % 2
                bank = ps[bi]

                def vm(e, t=t, bank=bank, s=s):
                    for c in range(8):
                        inst = e.matmul(bank[:, 0:256], hnT[:, c, t * 128:(t + 1) * 128], wgrp[s][:, c, 2:4, :].rearrange("p a b -> p (a b)"),
                                        start=(c == 0), stop=(c == 7))
                    return inst
                P.op("pe", vm, reads=[("xT", t), ("wgrp", s, 2), ("wgrp", s, 3)], writes=[("ps", bi)])
                if os.environ.get("FOX_DBG", "") != "noV":
                    P.op("dve", lambda e, t=t, bank=bank: e.tensor_copy(
                        out=Vaug[:, t, :, 0:64], in_=bank[:, 0:128].rearrange("p (a b) -> p a b", a=2)),
                        reads=[("ps", bi)], writes=[("Vaug", t)])
                if os.environ.get("FOX_DBG", "") != "noG":
                    P.op("act", lambda e, t=t, bank=bank: e.activation(out=Gs[:, t, :], in_=bank[:, 128:256], func=AF.Sigmoid),
                         reads=[("ps", bi)], writes=[("Gs", t)])
            if int(os.environ.get('FOX_STOP', '99')) <= 5:
                allk = [("QTa", 0, q) for q in range(4)] + [("KTa", 0, q) for q in range(4)] + [("QTa", 0, "aug"), ("KTa", 0, "aug"), "Vaug1"] + [("Vaug", t) for t in range(NT)] + [("Gs", t) for t in range(NT)]
                def dd(dst, src):
                    P.op("dve", lambda e: e.tensor_copy(out=dst, in_=src), reads=allk, writes=[("h", i) for i in range(NT)])
                dd(h[:, 0, :], QTa[0][:, 0:1024]); dd(h[:, 1, :], QTa[0][:, 1024:2048])
                dd(h[:, 2, :], KTa[0][:, 0:1024]); dd(h[:, 3, :], KTa[0][:, 1024:2048])
                dd(h[:, 4, 0:130], Vaug[:, 0, :, :].rearrange("p a b -> p (a b)")); dd(h[:, 4, 256:384], Gs[:, 0, :])
                return
            items = []
            for hh in range(2):
                for qg in range(4):
                    for kb in range(4 * (qg + 1)):
                        items.append((hh, qg, kb))

            def rec_qk(it):
                hh, qg, kb = it
                q0 = max(0, kb - 4 * qg) * 128
                slot = pt_rr[0] % NPT
                pt_rr[0] += 1
                bi = slot % 4
                bank = ps[bi]
                P.op("pe", lambda e: e.matmul(bank[:, q0:512], KTa[hh][0:96, kb * 128:(kb + 1) * 128],
                                             QTa[hh][0:96, qg * 512 + q0:(qg + 1) * 512], start=True, stop=True),
                     reads=[("KTa", hh, kb // 4), ("KTa", hh, "aug"), ("QTa", hh, qg), ("QTa", hh, "aug")],
                     writes=[("ps", bi)])
                P.op("act", lambda e: e.activation(out=PT[slot][:, q0:512], in_=bank[:, q0:512], func=AF.Exp),
                     reads=[("ps", bi)], writes=[("PT", slot)])
                if kb >= 4 * qg:
                    P.op("dve", lambda e: e.tensor_tensor(out=PT[slot][:, q0:q0 + 128], in0=PT[slot][:, q0:q0 + 128],
                                                          in1=self.mask_b[:], op=ALU.mult),
                         reads=[("PT", slot), "mask_b"], writes=[("PT", slot)])
                return slot

            def rec_pv(it, slot):
                hh, qg, kb = it
                for qt in range(4):
                    i = 4 * qg + qt
                    if kb > i:
                        continue
                    bi = 4 + qt
                    P.op("pe", lambda e, qt=qt, i=i, bi=bi: e.matmul(
                        ps[bi][:, 0:65], PT[slot][:, qt * 128:(qt + 1) * 128], Vaug[:, kb, hh, :],
                        start=(kb == 0), stop=(kb == i)),
                        reads=[("PT", slot), ("Vaug", kb), "Vaug1"], writes=[("ps", bi)])
                    if kb == i:
                        rs = (i + hh) % 8
                        P.op("dve", lambda e, bi=bi, rs=rs: e.reciprocal(out=rden[:, rs:rs + 1], in_=ps[bi][:, 64:65]),
                             reads=[("ps", bi)], writes=[("rden", rs)])
                        P.op("dve", lambda e, bi=bi, rs=rs, i=i, s=s: e.scalar_tensor_tensor(
                            out=Og[s][:, i, hh * 64:(hh + 1) * 64], in0=ps[bi][:, 0:64], scalar=rden[:, rs:rs + 1],
                            in1=Gs[:, i, hh * 64:(hh + 1) * 64], op0=ALU.mult, op1=ALU.mult),
                            reads=[("ps", bi), ("rden", rs), ("Gs", i)], writes=[("Og", s, i)])

            slots = {}
            LA = int(os.environ.get('FOX_LA', '4'))
            for n in range(min(LA, len(items))):
                slots[n] = rec_qk(items[n])
            for n, it in enumerate(items):
                if n + LA < len(items):
                    slots[n + LA] = rec_qk(items[n + LA])
                rec_pv(it, slots[n])
            P.op("sp", lambda e, g=g, s=s: e.dma_start(out=ogv[:, :, g * 128:(g + 1) * 128], in_=Og[s][:]),
                 reads=[("Og", s, i) for i in range(NT)], writes=[("Og_d", g)], dma=True)
            if int(os.environ.get('FOX_STOP', '99')) <= 6:
                allk = [("Og", s, i) for i in range(NT)]
                P.op("dve", lambda e: e.tensor_copy(out=h[:, 0, :], in_=Og[0][:, 0:8, :].rearrange("p a b -> p (a b)")),
                     reads=allk, writes=[("h", 0)])
                P.op("dve", lambda e: e.tensor_copy(out=h[:, 1, :], in_=Og[0][:, 8:16, :].rearrange("p a b -> p (a b)")),
                     reads=allk, writes=[("h", 1)])
                return

        if int(os.environ.get('FOX_STOP', '99')) <= 7:
            return
        for t in range(NT):
            b = t % 2
            P.op("sp", lambda e, t=t, b=b: e.dma_start(out=hnb[b][:], in_=ogv[:, t, :]),
                 reads=[("Og_d", g) for g in range(8)], writes=[("hnb", b)], dma=True)
            self.transpose_tile(hnb[b], ("hnb", b), hnT, t, b)
        self.out_proj(hnT, NG, wout, None)

    def ret_setup(self, layers):
        self.ret_layers = list(layers)
        nl = len(layers)
        self.din("ret_w_in", [nl, D, 6144])
        self.din("ret_gn_gain", [nl, 2048])
        self.din("ret_w_out", [nl, 2048, D])
        self.din("c_cos", [128, S])
        self.din("c_sin", [128, S])
        self.din("c_intraT", [128, 4, 128])
        self.din("c_rdec", [128, 12])
        self.Og2_d = self.dscratch("Og2_d", [S, 2048], BF16)

    def ret_layer(self, layer):
        j = self.ret_layers.index(layer)
        P, h, ps, ar = self.P, self.h, self.ps, self.ar
        w_in = self.dram["ret_w_in"]
        P.barrier()
        self.arena_reset()
        hnT = ar("r_hnT", [128, 8, S], BF16)
        hnb = [ar("r_hnb%d" % s, [128, D], BF16) for s in range(2)]
        wqk = [ar("r_wqk%d" % s, [128, 8, 512], BF16) for s in range(2)]
        wvg = ar("r_wvg", [128, 8, 1024], BF16)
        QT = ar("r_QT", [128, 2, S], BF16)
        KT = ar("r_KT", [128, 2, S], BF16)
        cosb = ar("r_cos", [128, S], BF16)
        sinb = ar("r_sin", [128, S], BF16)
        rt = [ar("r_rt%d" % s, [128, 512], F32) for s in range(4)]
        intraT = ar("r_intraT", [128, 4, 128], BF16)
        rdec = ar("r_rdec", [128, 12], F32)
        R32 = ar("r_R32", [128, 2, 512], F32)
        Rb = [ar("r_Rb%d" % s, [128, 2, 512], BF16) for s in range(2)]
        Vt = [ar("r_Vt%d" % s, [128, 512], BF16) for s in range(2)]
        Kd = [ar("r_Kd%d" % s, [128, 256], BF16) for s in range(2)]
        PTt = [ar("r_PT%d" % s, [128, 128], BF16) for s in range(2)]
        on = [ar("r_on%d" % s, [128, 512], F32) for s in range(2)]
        sg = [ar("r_sg%d" % s, [128, 512], BF16) for s in range(2)]
        ogo = [ar("r_ogo%d" % s, [128, 512], BF16) for s in range(2)]
        st6 = ar("r_st6", [128, 2, 6], F32)
        mv = ar("r_mv", [128, 2, 2], F32)
        sm = ar("r_sm", [128, 2, 4], F32)

        self.rms_stats(self.dram["norm_mix"][layer:layer + 1, :])
        P.op("pool", lambda e: e.dma_start(out=cosb[:], in_=self.dram["c_cos"]), writes=["cosb"], dma=True)
        P.op("pool", lambda e: e.dma_start(out=sinb[:], in_=self.dram["c_sin"]), writes=["sinb"], dma=True)
        P.op("pool", lambda e: e.dma_start(out=intraT[:], in_=self.dram["c_intraT"]), writes=["intraT"], dma=True)
        P.op("sp", lambda e: e.dma_start(out=rdec[:], in_=self.dram["c_rdec"]), writes=["rdec"], dma=True)
        self.norm_transpose(hnT, hnb)
        xT_all = [("xT", t) for t in range(NT)]
        og2v = self.Og2_d

        def load_qk(hd):
            s = hd % 2
            for qk in range(2):
                P.op("pool", lambda e, qk=qk: e.dma_start(
                    out=wqk[s][:, :, qk * 256:(qk + 1) * 256],
                    in_=w_in[j][:, qk * 1024 + hd * 256: qk * 1024 + (hd + 1) * 256].rearrange("(c p) f -> p c f", p=128)),
                    writes=[("wqk", s, qk)], dma=True)

        def load_vg(hd):
            for vg in range(2):
                P.op("pool", lambda e, vg=vg: e.dma_start(
                    out=wvg[:, :, vg * 512:(vg + 1) * 512],
                    in_=w_in[j][:, 2048 + vg * 2048 + hd * 512: 2048 + vg * 2048 + (hd + 1) * 512].rearrange("(c p) f -> p c f", p=128)),
                    writes=[("wvg", vg)], dma=True)

        load_qk(0)
        for hd in range(4):
            s = hd % 2
            gam = 1.0 - 2.0 ** (-5.0 - hd)
            gamC = float(np.float32(np.exp(np.float32(np.log(np.float32(gam))) * np.float32(128.0))))
            load_vg(hd)
            if hd + 1 < 4:
                load_qk(hd + 1)
            for qk in range(2):
                dst = QT if qk == 0 else KT
                dname = "QT" if qk == 0 else "KT"
                sc = 1.0 if qk == 0 else 0.0625
                for qd in range(4):
                    cs = slice(qd * 512, (qd + 1) * 512)
                    for half in range(2):
                        def pm(e, half=half, qk=qk, qd=qd, s=s):
                            col = qk * 256 + half * 128
                            for c in range(8):
                                inst = e.matmul(ps[half][:, :], wqk[s][:, c, col:col + 128], hnT[:, c, qd * 512:(qd + 1) * 512],
                                                start=(c == 0), stop=(c == 7))
                            return inst
                        P.op("pe", pm, reads=xT_all + [("wqk", s, qk)], writes=[("ps", half)])
                    for k4, (bank, tab, tn) in enumerate(((0, cosb, "cosb"), (1, sinb, "sinb"), (0, sinb, "sinb"), (1, cosb, "cosb"))):
                        P.op("dve", lambda e, k4=k4, bank=bank, tab=tab, cs=cs, sc=sc: e.scalar_tensor_tensor(
                            out=rt[k4][:], in0=ps[bank][:, :], scalar=sc, in1=tab[:, cs], op0=ALU.mult, op1=ALU.mult),
                            reads=[("ps", bank), tn], writes=[("rt", k4)])
                    P.op("pool", lambda e, dst=dst, cs=cs: e.tensor_tensor(out=dst[:, 0, cs], in0=rt[0][:], in1=rt[1][:], op=ALU.subtract),
                         reads=[("rt", 0), ("rt", 1)], writes=[(dname, qd)])
                    P.op("pool", lambda e, dst=dst, cs=cs: e.tensor_tensor(out=dst[:, 1, cs], in0=rt[2][:], in1=rt[3][:], op=ALU.add),
                         reads=[("rt", 2), ("rt", 3)], writes=[(dname, qd)])
            P.op("dve", lambda e: e.memset(R32[:], 0.0), writes=[("R32", 0), ("R32", 1)])
            def partA(n):
                b = n % 2
                cols = slice(n * 128, (n + 1) * 128)
                qdk = n // 4

                def ktr(e, cols=cols, b=b):
                    pv = ps[0][:].bitcast(BF16)
                    for c in range(2):
                        inst = e.transpose(out=pv[:, c * 128:(c + 1) * 128], in_=KT[:, c, cols], identity=self.ident_b[:])
                    return inst
                P.op("pe", ktr, reads=[("KT", qdk), "ident_b"], writes=[("ps", 0)])
                P.op("act", lambda e, b=b, hd=hd: e.activation(out=Kd[b][:], in_=ps[0][:].bitcast(BF16)[:, 0:256], func=AF.Identity,
                                                               scale=rdec[:, 8 + hd:9 + hd]),
                     reads=[("ps", 0), "rdec"], writes=[("Kd", b)])

                def smm(e, cols=cols):
                    for c in range(2):
                        inst = e.matmul(ps[1][:, 0:128], KT[:, c, cols], QT[:, c, cols], start=(c == 0), stop=(c == 1))
                    return inst
                P.op("pe", smm, reads=[("KT", qdk), ("QT", qdk)], writes=[("ps", 1)])
                P.op("dve", lambda e, b=b, hd=hd: e.tensor_tensor(out=PTt[b][:], in0=ps[1][:, 0:128], in1=intraT[:, hd, :], op=ALU.mult),
                     reads=[("ps", 1), "intraT"], writes=[("PTt", b)])

                def vmm(e, cols=cols):
                    for c in range(8):
                        inst = e.matmul(ps[2][:, :], hnT[:, c, cols], wvg[:, c, 0:512], start=(c == 0), stop=(c == 7))
                    return inst
                P.op("pe", vmm, reads=[("xT", n), ("wvg", 0)], writes=[("ps", 2)])
                P.op("act", lambda e, b=b: e.activation(out=Vt[b][:], in_=ps[2][:, :], func=AF.Copy),
                     reads=[("ps", 2)], writes=[("Vt", b)])

                def gmm(e, cols=cols):
                    for c in range(8):
                        inst = e.matmul(ps[3][:, :], hnT[:, c, cols], wvg[:, c, 512:1024], start=(c == 0), stop=(c == 7))
                    return inst
                P.op("pe", gmm, reads=[("xT", n), ("wvg", 1)], writes=[("ps", 3)])
                P.op("act", lambda e, b=b: e.activation(out=sg[b][:], in_=ps[3][:, :], func=AF.Silu),
                     reads=[("ps", 3)], writes=[("sg", b)])


            def partB(n):
                b = n % 2
                cols = slice(n * 128, (n + 1) * 128)
                qdk = n // 4
                if n < NT - 1:
                    rbn = (n + 1) % 2
                    for c in range(2):
                        P.op("pe", lambda e, c=c, b=b: e.matmul(ps[5 + c][:, :], Kd[b][:, c * 128:(c + 1) * 128], Vt[b][:],
                                                               start=True, stop=True),
                             reads=[("Kd", b), ("Vt", b)], writes=[("ps", 5 + c)])
                        P.op("dve", lambda e, c=c, gamC=gamC: e.scalar_tensor_tensor(
                            out=R32[:, c, :], in0=R32[:, c, :], scalar=gamC, in1=ps[5 + c][:, :], op0=ALU.mult, op1=ALU.add),
                            reads=[("ps", 5 + c), ("R32", c)], writes=[("R32", c)])
                        P.op("act", lambda e, c=c, rbn=rbn: e.activation(out=Rb[rbn][:, c, :], in_=R32[:, c, :], func=AF.Copy),
                             reads=[("R32", c)], writes=[("Rb", rbn, c)])

                def omm(e, cols=cols, b=b, n=n):
                    inst = e.matmul(ps[4][:, :], PTt[b][:], Vt[b][:], start=True, stop=(n == 0))
                    if n > 0:
                        for c in range(2):
                            inst = e.matmul(ps[4][:, :], QT[:, c, cols], Rb[n % 2][:, c, :], start=False, stop=(c == 1))
                    return inst
                P.op("pe", omm, reads=[("PTt", b), ("Vt", b), ("QT", qdk), ("Rb", n % 2, 0), ("Rb", n % 2, 1)], writes=[("ps", 4)])
                P.op("dve", lambda e, b=b: e.bn_stats(out=st6[:, b, :], in_=ps[4][:, :]), reads=[("ps", 4)], writes=[("st6", b)])
                P.op("dve", lambda e, b=b: e.bn_aggr(out=mv[:, b, :], in_=st6[:, b, :]), reads=[("st6", b)], writes=[("mv", b)])
                P.op("dve", lambda e, b=b, hd=hd: e.tensor_scalar(out=sm[:, b, 0:1], in0=mv[:, b, 1:2], scalar1=rdec[:, 4 + hd:5 + hd],
                                                                 scalar2=EPS, op0=ALU.mult, op1=ALU.add),
                     reads=[("mv", b), "rdec"], writes=[("sm", b, 0)])
                P.op("pool", lambda e, b=b: e.tensor_tensor(out=sm[:, b, 1:2], in0=sm[:, b, 0:1], in1=self.negh[:, 0:1], op=ALU.pow),
                     reads=[("sm", b, 0), "negh"], writes=[("sm", b, 1)])
                P.op("dve", lambda e, b=b, hd=hd: e.tensor_tensor(out=sm[:, b, 2:3], in0=sm[:, b, 1:2], in1=rdec[:, hd:hd + 1], op=ALU.mult),
                     reads=[("sm", b, 1), "rdec"], writes=[("sm", b, 2)])
                P.op("dve", lambda e, b=b: e.scalar_tensor_tensor(out=sm[:, b, 3:4], in0=mv[:, b, 0:1], scalar=-1.0, in1=sm[:, b, 2:3],
                                                                 op0=ALU.mult, op1=ALU.mult),
                     reads=[("mv", b), ("sm", b, 2)], writes=[("sm", b, 3)])
                P.op("act", lambda e, b=b: e.activation(out=on[b][:], in_=ps[4][:, :], func=AF.Identity,
                                                        scale=sm[:, b, 2:3], bias=sm[:, b, 3:4]),
                     reads=[("ps", 4), ("sm", b, 2), ("sm", b, 3)], writes=[("on", b)])
                P.op("pool", lambda e, b=b: e.tensor_tensor(out=ogo[b][:], in0=on[b][:], in1=sg[b][:], op=ALU.mult),
                     reads=[("on", b), ("sg", b)], writes=[("ogo", b)])
                P.op("sp", lambda e, b=b, n=n, hd=hd: e.dma_start(out=og2v[n * 128:(n + 1) * 128, hd * 512:(hd + 1) * 512], in_=ogo[b][:]),
                     reads=[("ogo", b)], writes=[("Og2_d", n, hd)], dma=True)

            partA(0)
            for n in range(NT):
                if n + 1 < NT:
                    partA(n + 1)
                partB(n)

        P.barrier()
        self.arena_reset()
        wout = ar("r_wout", [128, 16, D], BF16)
        ogt = [ar("r_ogt%d" % s, [128, 2048], BF16) for s in range(2)]
        OgT = [ar("r_OgT%d" % s, [128, 16, 128], BF16) for s in range(2)]
        gcol = ar("r_gcol", [128, 16], F32)
        P.op("pool", lambda e: e.dma_start(out=wout[:], in_=self.dram["ret_w_out"][j].rearrange("(c p) f -> p c f", p=128)),
             writes=["wout"], dma=True)
        P.op("sp", lambda e: e.dma_start(out=gcol[:], in_=self.dram["ret_gn_gain"][j].rearrange("(c p) -> p c", p=128),
                                         allow_slow_non_contiguous=True), writes=["gcol"], dma=True)
        for c in range(16):
            P.op("dve", lambda e, c=c: e.tensor_scalar(out=wout[:, c, :], in0=wout[:, c, :], scalar1=gcol[:, c:c + 1], scalar2=None,
                                                     op0=ALU.mult), reads=["wout", "gcol"], writes=["wout"])
        for t in range(NT):
            b = t % 2
            P.op("sp", lambda e, t=t, b=b: e.dma_start(out=ogt[b][:], in_=og2v[t * 128:(t + 1) * 128, :]),
                 writes=[("ogt", b)], dma=True)
            for half in range(2):
                def tr(e, half=half, b=b):
                    pv = ps[half][:].bitcast(BF16)
                    for c in range(8):
                        cc = half * 8 + c
                        inst = e.transpose(out=pv[:, c * 128:(c + 1) * 128], in_=ogt[b][:, cc * 128:(cc + 1) * 128],
                                           identity=self.ident_b[:])
                    return inst
                P.op("pe", tr, reads=[("ogt", b), "ident_b"], writes=[("ps", half)])
                srcv = lambda half=half: ps[half][:].bitcast(BF16).rearrange("p (c t) -> p c t", c=8)
                if half == 0:
                    P.op("act", lambda e, b=b, srcv=srcv: e.activation(out=OgT[b][:, 0:8, :], in_=srcv(), func=AF.Copy),
                         reads=[("ps", 0)], writes=[("OgT", b, 0)])
                else:
                    P.op("dve", lambda e, b=b, srcv=srcv: e.tensor_copy(out=OgT[b][:, 8:16, :], in_=srcv()),
                         reads=[("ps", 1)], writes=[("OgT", b, 1)])
            for nn in range(2):
                bi = 2 + (t * 2 + nn) % 2

                def mm(e, nn=nn, b=b, bi=bi):
                    for c in range(16):
                        inst = e.matmul(ps[bi][:, :], OgT[b][:, c, :], wout[:, c, nn * 512:(nn + 1) * 512],
                                        start=(c == 0), stop=(c == 15))
                    return inst
                P.op("pe", mm, reads=[("OgT", b, 0), ("OgT", b, 1), "wout"], writes=[("ps", bi)])
                P.op("dve", lambda e, t=t, nn=nn, bi=bi: e.tensor_tensor(
                    out=h[:, t, nn * 512:(nn + 1) * 512], in0=h[:, t, nn * 512:(nn + 1) * 512], in1=ps[bi][:, :], op=ALU.add),
                    reads=[("ps", bi), ("h", t)], writes=[("h", t)])


def build(stages=None, final_norm=True):
    B = Builder()
    B.prologue()
    if stages is None:
        stages = []
        for i in range(DEPTH):
            stages += ["mix%d" % i, "moe%d" % i]
    moe_layers = sorted(set(int(s[3:]) for s in stages if s.startswith("moe")))
    if moe_layers:
        B.moe_setup(moe_layers)
    fox_layers = sorted(set(int(s[3:]) for s in stages if s.startswith("mix") and int(s[3:]) % 2 == 0))
    if fox_layers:
        B.fox_setup(fox_layers)
    ret_layers = sorted(set(int(s[3:]) for s in stages if s.startswith("mix") and int(s[3:]) % 2 == 1))
    if ret_layers:
        B.ret_setup(ret_layers)
    B.layer_sets = {"moe": moe_layers, "fox": fox_layers, "ret": ret_layers}
    for s in stages:
        li = int(s[3:])
        if s.startswith("moe"):
            B.moe_layer(li)
        elif li % 2 == 0:
            B.fox_layer(li)
        else:
            B.ret_layer(li)
    B.epilogue(final_norm=final_norm)
    nc = B.finish()
    _LAYER_SETS[id(nc)] = B.layer_sets
    return nc


def _consts():
    ident = np.eye(128, dtype=np.float32)
    lst = np.triu(np.ones((128, 128), np.float32), k=1)
    ebase = (np.arange(NE, dtype=np.float32) * CAP).reshape(1, NE)
    mask = np.triu(np.ones((128, 128), np.float32), k=0)
    inv = (np.float32(1.0) / (np.float32(10000.0) ** np.linspace(0.0, 1.0, 128, dtype=np.float32))).astype(np.float32)
    ang = (np.arange(S, dtype=np.float32)[None, :] * inv[:, None]).astype(np.float32)
    cosT = np.cos(ang).astype(np.float32)
    sinT = np.sin(ang).astype(np.float32)
    log_g = np.log(np.float32(1.0) - np.float32(2.0) ** (np.float32(-5.0) - np.arange(4, dtype=np.float32))).astype(np.float32)
    idx = np.arange(128, dtype=np.float32)
    intraT = np.zeros((128, 4, 128), np.float32)
    rdec = np.zeros((128, 12), np.float32)
    for hd in range(4):
        col = np.exp(-log_g[hd] * (idx + 1.0)).astype(np.float32)
        intraT[:, hd, :] = np.where(idx[None, :] >= idx[:, None], col[:, None], 0.0)
        qd = np.exp(log_g[hd] * (idx + 1.0)).astype(np.float32)
        rdec[:, hd] = qd
        rdec[:, 4 + hd] = qd * qd
        rdec[:, 8 + hd] = np.exp(log_g[hd] * (127.0 - idx)).astype(np.float32)
    return {"c_ident": ident, "c_lst": lst, "c_ebase": ebase, "c_mask": mask,
            "c_cos": cosT, "c_sin": sinT, "c_intraT": intraT, "c_rdec": rdec}


_CACHE = {}


def prep_inputs(inputs, nc_inputs, layer_sets):
    f = lambda a: np.ascontiguousarray(np.asarray(a), dtype=np.float32)
    shared = {}
    shared["norm_mix"] = f(inputs["norm_mix"])
    shared["norm_ffn"] = f(inputs["norm_ffn"])
    shared["norm_final"] = f(inputs["norm_final"]).reshape(1, D)
    shared.update(_consts())
    if "router_w" in nc_inputs:
        ml = layer_sets["moe"]
        shared["router_w"] = np.ascontiguousarray(
            np.concatenate([f(inputs["router_group_w"]), f(inputs["router_expert_w"])], axis=-1)[ml])
        shared["router_b"] = np.ascontiguousarray(
            np.concatenate([f(inputs["router_group_b"]), f(inputs["router_expert_b"])], axis=-1)[ml])
        for p, l in enumerate(ml):
            shared["expert_w_gu%d" % p] = f(inputs["expert_w_gu"][l])
            shared["expert_w_down%d" % p] = f(inputs["expert_w_down"][l])
    if "fox_w_in" in nc_inputs:
        fl = [l // 2 for l in layer_sets["fox"]]
        shared["fox_w_in"] = np.ascontiguousarray(f(inputs["fox_w_in"])[fl])
        shared["fox_b_f"] = np.ascontiguousarray(f(inputs["fox_b_f"])[fl])
        shared["fox_w_out"] = np.ascontiguousarray(f(inputs["fox_w_out"])[fl])
    if "ret_w_in" in nc_inputs:
        rl = [l // 2 for l in layer_sets["ret"]]
        shared["ret_w_in"] = np.ascontiguousarray(f(inputs["ret_w_in"])[rl])
        shared["ret_gn_gain"] = np.ascontiguousarray(f(inputs["ret_gn_gain"])[rl])
        shared["ret_w_out"] = np.ascontiguousarray(f(inputs["ret_w_out"])[rl])
    x = f(inputs["x"])
    in_maps = []
    for b in range(NCORES):
        m = {k: v for k, v in shared.items() if k in nc_inputs}
        m["x"] = x[b]
        in_maps.append(m)
    return in_maps


def run(inputs, stages=None, final_norm=True, trace=False):
    key = (tuple(stages) if stages is not None else None, final_norm)
    if key not in _CACHE:
        B_nc = build(stages, final_norm)
        _CACHE[key] = B_nc
    nc = _CACHE[key]
    names = set(_DRAM_NAMES[id(nc)])
    in_maps = prep_inputs(inputs, names, _LAYER_SETS[id(nc)])
    res = run_bass_kernel_spmd(nc, in_maps, core_ids=list(range(NCORES)), trace=trace)
    out = np.stack([np.asarray(r["y"]) for r in res.results], axis=0).astype(np.float32)
    return out, res


def kernel(**inputs):
    out, _ = run(inputs)
    return out
```

```python
import contextlib
import os
import numpy as np
import concourse.bass as bass
import concourse.mybir as mybir
from concourse.bass_utils import run_bass_kernel_spmd

F32 = mybir.dt.float32
BF16 = mybir.dt.bfloat16
I32 = mybir.dt.int32
AF = mybir.ActivationFunctionType
ALU = mybir.AluOpType
AX = mybir.AxisListType

D = 1024
S = 2048
NT = S // 128
DEPTH = 4
EPS = 1e-6
NCORES = 8

ENGINES = ("pe", "act", "dve", "pool", "sp")


class _Op:
    __slots__ = ("eng", "fn", "dma", "waits", "signal", "idx")

    def __init__(self, eng, fn, dma, idx):
        self.eng = eng
        self.fn = fn
        self.dma = dma
        self.waits = []
        self.signal = None
        self.idx = idx


class Prog:
    N_DMA_SEMS = 24

    def __init__(self, same_engine_sync=True):
        self.ops = []
        self.last_writer = {}
        self.readers = {}
        self.same_engine_sync = same_engine_sync
        self.dependents = {}
        self.deps = []
        self.forced = set()
        self.pending = {e: set() for e in ENGINES}
        self.last_op = {}
        self.unfenced_dma = set()

    def op(self, eng, fn, reads=(), writes=(), dma=False, force=False):
        idx = len(self.ops)
        o = _Op(eng, fn, dma, idx)
        if force:
            self.forced.add(idx)
        ps_reads = [k for k in reads if isinstance(k, tuple) and k[0] == "ps"]
        if ps_reads:
            reads = [k for k in reads if k not in ps_reads]
            writes = list(writes) + ps_reads
        deps = set()
        for k in reads:
            w = self.last_writer.get(k)
            if w is not None:
                deps.add(w)
        for k in writes:
            w = self.last_writer.get(k)
            if w is not None:
                deps.add(w)
            for r in self.readers.get(k, ()):
                deps.add(r)
        deps |= self.pending[eng]
        self.pending[eng] = set()
        deps.discard(idx)
        self.last_op[eng] = idx
        if dma:
            self.unfenced_dma.add(idx)
        for k in writes:
            self.last_writer[k] = idx
            self.readers[k] = []
        for k in reads:
            if k not in writes:
                self.readers.setdefault(k, []).append(idx)
        self.ops.append(o)
        self.deps.append(deps)
        return idx

    def barrier(self):
        carry = getattr(self, "carry_dma", set())
        deps = set(self.last_op.values()) | (self.unfenced_dma - carry)
        self.unfenced_dma = set(carry)
        self.carry_dma = set()
        for e in ENGINES:
            self.pending[e] |= deps

    def finalize(self):
        ops = self.ops
        needed = [i in self.forced for i in range(len(ops))]
        for o in ops:
            for d in self.deps[o.idx]:
                p = ops[d]
                if p.dma:
                    needed[d] = True
                elif p.eng == o.eng and not o.dma:
                    if p.eng == "pe":
                        continue
                    if self.same_engine_sync:
                        needed[d] = True
                else:
                    needed[d] = True
        eng_cnt = {e: 0 for e in ENGINES}
        dma_cnt = [0] * self.N_DMA_SEMS
        dma_rr = 0
        seen = {e: {} for e in ENGINES}
        for o in ops:
            waits = {}
            for d in self.deps[o.idx]:
                p = ops[d]
                if p.signal is None:
                    continue
                if (not p.dma) and p.eng == o.eng and not o.dma and (p.eng == "pe" or not self.same_engine_sync):
                    continue
                k, v, _ = p.signal
                if seen[o.eng].get(k, 0) >= v:
                    continue
                waits[k] = max(waits.get(k, 0), v)
            if o.dma and needed[o.idx]:
                j = dma_rr
                dma_rr = (dma_rr + 1) % self.N_DMA_SEMS
                k = ("dma", j)
                if dma_cnt[j] > 0 and seen[o.eng].get(k, 0) < dma_cnt[j]:
                    waits[k] = max(waits.get(k, 0), dma_cnt[j])
                dma_cnt[j] += 16
                o.signal = (k, dma_cnt[j], 16)
            elif needed[o.idx]:
                eng_cnt[o.eng] += 1
                o.signal = (("eng", o.eng), eng_cnt[o.eng], 1)
            for k, v in waits.items():
                seen[o.eng][k] = max(seen[o.eng].get(k, 0), v)
            o.waits = sorted(waits.items(), key=lambda kv: str(kv[0]))
        self.max_counts = dict(eng_cnt)

    def emit(self, nc, stack, final_waits):
        sems = {}
        for e in ENGINES:
            sems[("eng", e)] = stack.enter_context(nc.semaphore("sem_" + e))
        for j in range(self.N_DMA_SEMS):
            sems[("dma", j)] = stack.enter_context(nc.semaphore("sem_dma%d" % j))
        block = stack.enter_context(nc.Block())
        by_eng = {e: [o for o in self.ops if o.eng == e] for e in ENGINES}
        last_signal = {}
        for o in self.ops:
            if o.signal is not None:
                last_signal[o.signal[0]] = max(last_signal.get(o.signal[0], 0), o.signal[1])

        def run(engine, ename):
            for o in by_eng[ename]:
                for k, v in o.waits:
                    engine.wait_ge(sems[k], v)
                inst = o.fn(engine)
                if o.signal is not None:
                    inst.then_inc(sems[o.signal[0]], o.signal[2])
            if ename == final_waits:
                for k, v in last_signal.items():
                    if k[0] == "dma":
                        engine.wait_ge(sems[k], v)

        @block.tensor
        def _(e):
            run(e, "pe")

        @block.scalar
        def _(e):
            run(e, "act")

        @block.vector
        def _(e):
            run(e, "dve")

        @block.gpsimd
        def _(e):
            run(e, "pool")

        @block.sync
        def _(e):
            run(e, "sp")


NE = 32
CAP = 256
TRASH = NE * CAP
NSLOT = TRASH + 128
FF = 512
BIG = 30000.0


_DRAM_NAMES = {}
_LAYER_SETS = {}


class Builder:
    def __init__(self):
        self.nc = nc = bass.Bass("TRN2", target_bir_lowering=False)
        self.P = Prog()
        self.stack = contextlib.ExitStack()
        self.mem_stack = contextlib.ExitStack()
        self.dram = {}
        self.sq_done = set()
        self.ps = [self.mem_stack.enter_context(nc.psum_tensor("ps%d" % b, [128, 512], F32)) for b in range(8)]
        sb = self.sb
        self.h = sb("h", [128, NT, D], F32)
        self.gb = sb("gb", [128, D], F32)
        self.ssq = sb("ssq", [128, NT], F32)
        self.rstd = sb("rstd", [128, NT], F32)
        self.junk = sb("junk", [128, D], BF16)
        self.ident_b = sb("ident_b", [128, 128], BF16)
        self.ident_f = sb("ident_f", [128, 128], F32)
        self.lst_b = sb("lst_b", [128, 128], BF16)
        self.ones_b = sb("ones_b", [128, 128], BF16)
        self.ebase = sb("ebase", [128, NE], F32)
        self.negh = sb("negh", [128, NT], F32)
        self.arena_size = 131 * 1024
        self.arena_base, _ = nc.bump_sbuf(self.arena_size)
        self.arena_off = 0

    def sb(self, name, shape, dt):
        return self.mem_stack.enter_context(self.nc.sbuf_tensor(name, list(shape), dt))

    def arena_reset(self):
        self.arena_off = 0

    def ar(self, name, shape, dt):
        nbytes = int(np.prod(shape[1:])) * (4 if dt in (F32, I32) else 2)
        nbytes = (nbytes + 31) // 32 * 32
        assert self.arena_off + nbytes <= self.arena_size, (name, self.arena_off, nbytes)
        t = self.nc.alloc_sbuf_tensor_at(name, list(shape), dt, offset=self.arena_base + self.arena_off)
        self.arena_off += nbytes
        return t

    def din(self, name, shape, dt=F32):
        t = self.nc.dram_tensor(name, list(shape), dt, kind="ExternalInput").ap()
        self.dram[name] = t
        return t

    def dscratch(self, name, shape, dt):
        return self.nc.dram_tensor(name, list(shape), dt, kind="Internal").ap()

    def prologue(self):
        P = self.P
        x = self.din("x", [S, D])
        self.din("norm_mix", [DEPTH, D])
        self.din("norm_ffn", [DEPTH, D])
        self.din("norm_final", [1, D])
        cI = self.din("c_ident", [128, 128])
        cL = self.din("c_lst", [128, 128])
        cE = self.din("c_ebase", [1, NE])
        self.y = self.nc.dram_tensor("y", [S, D], F32, kind="ExternalOutput").ap()
        xv = x.rearrange("(n p) d -> p n d", p=128)
        h = self.h
        for q in range(4):
            sl = slice(q * 4, (q + 1) * 4)
            P.op("sp", lambda e, sl=sl: e.dma_start(out=h[:, sl, :], in_=xv[:, sl, :]),
                 writes=[("h", i) for i in range(q * 4, q * 4 + 4)], dma=True)
        P.op("sp", lambda e: e.dma_start(out=self.ident_f[:], in_=cI), writes=["ident_f"], dma=True)
        P.op("pool", lambda e: e.dma_start(out=self.ident_b[:], in_=cI), writes=["ident_b"], dma=True)
        P.op("pool", lambda e: e.dma_start(out=self.lst_b[:], in_=cL), writes=["lst_b"], dma=True)
        P.op("sp", lambda e: e.dma_start(out=self.ebase[:], in_=cE[0:1, :].partition_broadcast(128)),
             writes=["ebase"], dma=True)
        P.op("dve", lambda e: e.memset(self.ones_b[:], 1.0), writes=["ones_b"])
        P.op("dve", lambda e: e.memset(self.negh[:], -0.5), writes=["negh"])

    def sq(self, i):
        h, ssq, junk = self.h, self.ssq, self.junk
        self.P.op("act", lambda e: e.activation(out=junk[:], in_=h[:, i, :], func=AF.Square, accum_out=ssq[:, i:i + 1]),
                  reads=[("h", i)], writes=["junk", ("ssq", i)])
        self.sq_done.add(i)

    def rms_stats(self, gain_row_ap):
        P, h, ssq, rstd, junk, gb = self.P, self.h, self.ssq, self.rstd, self.junk, self.gb
        P.op("sp", lambda e: e.dma_start(out=gb[:], in_=gain_row_ap.partition_broadcast(128)),
             writes=["gb"], dma=True)
        for i in range(NT):
            if i not in self.sq_done:
                self.sq(i)
        self.sq_done = set()
        P.op("dve", lambda e: e.tensor_scalar(out=rstd[:], in0=ssq[:], scalar1=1.0 / D, scalar2=EPS,
                                              op0=ALU.mult, op1=ALU.add),
             reads=[("ssq", i) for i in range(NT)], writes=["rstd"])
        P.op("pool", lambda e: e.tensor_tensor(out=rstd[:], in0=rstd[:], in1=self.negh[:], op=ALU.pow),
             reads=["rstd", "negh"], writes=["rstd"])

    def epilogue(self, final_norm=True):
        P, h = self.P, self.h
        P.barrier()
        self.arena_reset()
        outt = self.ar("outt", [128, 2, D], F32)
        yv = self.y.rearrange("(n p) d -> p n d", p=128)
        if final_norm:
            self.rms_stats(self.dram["norm_final"][0:1, :])
        for i in range(NT):
            b = i % 2
            if final_norm:
                P.op("dve", lambda e, i=i, b=b: e.scalar_tensor_tensor(
                    out=outt[:, b, :], in0=h[:, i, :], scalar=self.rstd[:, i:i + 1], in1=self.gb[:],
                    op0=ALU.mult, op1=ALU.mult),
                    reads=[("h", i), "rstd", "gb"], writes=[("outt", b)])
                P.op("sp", lambda e, i=i, b=b: e.dma_start(out=yv[:, i, :], in_=outt[:, b, :]),
                     reads=[("outt", b)], writes=[("y", i)], dma=True, force=True)
            else:
                P.op("sp", lambda e, i=i: e.dma_start(out=yv[:, i, :], in_=h[:, i, :]),
                     reads=[("h", i)], writes=[("y", i)], dma=True, force=True)

    def finish(self):
        _DRAM_NAMES[id(self.nc)] = list(self.dram.keys())
        self.P.finalize()
        self.P.emit(self.nc, self.stack, final_waits="sp")
        self.stack.close()
        return self.nc

    def moe_setup(self, layers):
        self.moe_layers = list(layers)
        nl = len(self.moe_layers)
        self.din("router_w", [nl, D, 36])
        self.din("router_b", [nl, 36])
        for p in range(nl):
            self.din("expert_w_gu%d" % p, [NE, D, 2 * FF])
            self.din("expert_w_down%d" % p, [NE, FF, D])
        self.Xs = self.dscratch("Xs", [NSLOT, D], BF16)
        self.Ys = self.dscratch("Ys", [NSLOT, D], BF16)
        P = self.P
        self.arena_reset()
        self.zeros_b = self.ar("zeros_b", [128, D], BF16)
        P.op("dve", lambda e: e.memset(self.zeros_b[:], 0.0), writes=["zeros_b"])
        self.zeroed = False

    def moe_layer(self, layer):
        li = self.moe_layers.index(layer)
        P, h, ps = self.P, self.h, self.ps
        rstd, gb = self.rstd, self.gb
        P.barrier()
        self.arena_reset()
        ar = self.ar
        wgu = [ar("wgu%d" % s, [128, 8, 2 * FF], BF16) for s in range(2)]
        wdn = [ar("wdn%d" % s, [128, 4, D], BF16) for s in range(2)]
        NYG = 8
        yg = [self.nc.alloc_sbuf_tensor_at("yg%d_%d" % (layer, s), [128, D], BF16, offset=self.arena_base + s * 2048)
              for s in range(NYG)]
        save = self.arena_off
        NRB = 3
        hn32 = [ar("hn32_%d" % s, [128, D], F32) for s in range(NRB)]
        hhi = [ar("hhi%d" % s, [128, D], BF16) for s in range(NRB)]
        hlo = [ar("hlo%d" % s, [128, D], BF16) for s in range(NRB)]
        hiT = [ar("hiT%d" % s, [128, 8, 128], BF16) for s in range(NRB)]
        loT = [ar("loT%d" % s, [128, 8, 128], BF16) for s in range(NRB)]
        end1 = self.arena_off
        self.arena_off = save
        xg = [ar("xg%d" % s, [128, 2, D], BF16) for s in range(2)]
        xT = [ar("xT%d" % s, [128, 8, CAP], BF16) for s in range(2)]
        hT = [ar("hT%d" % s, [128, 4, CAP], BF16) for s in range(2)]
        sA = [ar("sA%d" % s, [128, CAP], F32) for s in range(2)]
        yt = [ar("yt%d" % s, [128, 2, D], BF16) for s in range(2)]
        self.arena_off = max(self.arena_off, end1)
        NHB = 4
        hnb = [ar("hnb%d" % s, [128, D], BF16) for s in range(NHB)]
        fence = ar("fence", [128, 8], F32)
        fence2 = ar("fence2", [128, D], BF16)
        wr_hi = ar("wr_hi", [128, 8, 36], BF16)
        wr_lo = ar("wr_lo", [128, 8, 36], BF16)
        wr32 = ar("wr32", [128, 8, 36], F32)
        brb = ar("brb", [128, 36], F32)
        LG = ar("LG", [128, NT, 36], F32)
        Em = ar("Em", [128, NT, 32], F32)
        T1 = ar("T1", [128, NT, 32], F32)
        oh1 = ar("oh1", [128, NT, 32], F32)
        oh2 = ar("oh2", [128, NT, 32], F32)
        Em2 = ar("Em2", [128, NT, 32], F32)
        RK = ar("RK", [128, NT, 32], F32)
        Obf = ar("Obf", [128, NT, 32], BF16)
        Gm = ar("Gm", [128, NT, 4], F32)
        ohG = ar("ohG", [128, NT, 4], F32)
        pen = ar("pen", [128, NT, 4], F32)
        sm = {n: ar("sm_" + n, [128, NT], F32) for n in
              ("gmax", "sumG", "pg", "m1", "m2", "r", "den", "g1", "g2", "rk", "base", "valid", "sl")}
        slot_i = ar("slot_i", [128, 2, NT], I32)

        wr = self.dram["router_w"]
        br = self.dram["router_b"]
        wgu_d = self.dram["expert_w_gu%d" % li]
        wdn_d = self.dram["expert_w_down%d" % li]
        Xs, Ys = self.Xs, self.Ys

        self.rms_stats(self.dram["norm_ffn"][layer:layer + 1, :])
        P.op("sp", lambda e: e.dma_start(out=wr32[:], in_=wr[li].rearrange("(c p) j -> p c j", p=128)),
             writes=["wr32"], dma=True)
        P.op("sp", lambda e: e.dma_start(out=brb[:], in_=br[li:li + 1, :].partition_broadcast(128)),
             writes=["brb"], dma=True)

        def load_w(e_idx):
            s = e_idx % 2
            P.op("pool", lambda e: e.dma_start(out=wgu[s][:], in_=wgu_d[e_idx].rearrange("(c p) f -> p c f", p=128)),
                 writes=[("wgu", s)], dma=True)
            P.op("pool", lambda e: e.dma_start(out=wdn[s][:], in_=wdn_d[e_idx].rearrange("(c p) f -> p c f", p=128)),
                 writes=[("wdn", s)], dma=True)

        load_w(0)
        load_w(1)
        nr = NSLOT // 128
        xs_z_keys = [("Xs_z", r0) for r0 in range(0, nr, 13)] + ["Ys_z"]
        if not self.zeroed:
            self.zeroed = True
            P.op("dve", lambda e: e.memset(fence2[:], 0.0), writes=["zeros_b"])
            xsv = Xs.rearrange("(r p) d -> p r d", p=128)
            ysv = Ys.rearrange("(r p) d -> p r d", p=128)
            for r0 in range(0, nr, 13):
                P.op("sp", lambda e, r0=r0: e.dma_start(
                    out=xsv[:, r0:r0 + 13, :], in_=fence2[:].unsqueeze(1).to_broadcast([128, 13, D])),
                    reads=["zeros_b"], writes=[("Xs_z", r0)], dma=True)
            P.op("sp", lambda e: e.dma_start(out=ysv[:, nr - 1, :], in_=fence2[:]),
                 reads=["zeros_b"], writes=["Ys_z"], dma=True)

        P.op("act", lambda e: e.activation(out=wr_hi[:], in_=wr32[:], func=AF.Copy), reads=["wr32"], writes=["wr_hi"])
        P.op("pool", lambda e: e.tensor_tensor(out=wr_lo[:], in0=wr32[:], in1=wr_hi[:], op=ALU.subtract),
             reads=["wr32", "wr_hi"], writes=["wr_lo"])
        def st1(t):
            b = t % NRB
            P.op("dve", lambda e: e.scalar_tensor_tensor(
                out=hn32[b][:], in0=h[:, t, :], scalar=rstd[:, t:t + 1], in1=gb[:], op0=ALU.mult, op1=ALU.mult),
                reads=[("h", t), "rstd", "gb"], writes=[("hn32", b)])
            P.op("act", lambda e: e.activation(out=hhi[b][:], in_=hn32[b][:], func=AF.Copy),
                 reads=[("hn32", b)], writes=[("hhi", b)])
            P.op("pool", lambda e: e.tensor_tensor(out=hlo[b][:], in0=hn32[b][:], in1=hhi[b][:], op=ALU.subtract),
                 reads=[("hn32", b), ("hhi", b)], writes=[("hlo", b)])

        def st2(t):
            b = t % NRB
            for w, (src, sk, dstT, dk) in enumerate(((hhi, "hhi", hiT, "hiT"), (hlo, "hlo", loT, "loT"))):
                bi = 2 * (t % 2) + w

                def tr(e, src=src, bi=bi):
                    pv = ps[bi][:].bitcast(BF16)
                    for c in range(8):
                        inst = e.transpose(out=pv[:, c * 128:(c + 1) * 128], in_=src[b][:, c * 128:(c + 1) * 128],
                                           identity=self.ident_b[:])
                    return inst
                P.op("pe", tr, reads=[(sk, b), "ident_b"], writes=[("ps", bi)])
                srcv = lambda bi=bi: ps[bi][:].bitcast(BF16).rearrange("p (c t) -> p c t", c=8)
                if w == 0:
                    P.op("act", lambda e, srcv=srcv, dstT=dstT: e.activation(out=dstT[b][:], in_=srcv(), func=AF.Copy),
                         reads=[("ps", bi)], writes=[(dk, b)])
                else:
                    P.op("dve", lambda e, srcv=srcv, dstT=dstT: e.tensor_copy(out=dstT[b][:], in_=srcv()),
                         reads=[("ps", bi)], writes=[(dk, b)])

        def st3(t):
            b = t % NRB
            pb = 4 + t % 2

            def rl(e):
                k = 0
                for (xt, wt) in ((hiT, wr_hi), (loT, wr_hi), (hiT, wr_lo)):
                    for c in range(8):
                        inst = e.matmul(ps[pb][:, 0:36], xt[b][:, c, :], wt[:, c, :], start=(k == 0), stop=(k == 23))
                        k += 1
                return inst
            P.op("pe", rl, reads=[("hiT", b), ("loT", b), "wr_hi", "wr_lo"], writes=[("ps", pb)])
            P.op("dve", lambda e: e.tensor_tensor(out=LG[:, t, :], in0=ps[pb][:, 0:36], in1=brb[:], op=ALU.add),
                 reads=[("ps", pb), "brb"], writes=[("LG", t)])

        for i in range(NT + 2):
            if i < NT:
                st1(i)
            if 0 <= i - 1 < NT:
                st2(i - 1)
            if 0 <= i - 2 < NT:
                st3(i - 2)

        if int(os.environ.get('MOE_STOP', '99')) <= 1:
            return
        G = LG[:, :, 0:4]
        E4 = LG[:, :, 4:36].rearrange("p n (g e) -> p n g e", g=4)

        def bc(t2, n):
            return t2[:].unsqueeze(2).to_broadcast([128, NT, n])

        def dve(fn, reads, writes):
            P.op("dve", fn, reads=reads, writes=writes)

        P.op("dve", lambda e: e.memset(fence[:], 0.0), reads=[("LG", t) for t in range(NT)], writes=["LG"])
        dve(lambda e: e.tensor_reduce(out=sm["gmax"][:], in_=G, axis=AX.X, op=ALU.max), ["LG"], ["gmax"])
        dve(lambda e: e.tensor_tensor(out=Gm[:], in0=G, in1=bc(sm["gmax"], 4), op=ALU.subtract), ["LG", "gmax"], ["Gm"])
        dve(lambda e: e.tensor_single_scalar(out=ohG[:], in_=Gm[:], scalar=0.0, op=ALU.is_ge), ["Gm"], ["ohG"])
        P.op("act", lambda e: e.activation(out=Gm[:], in_=Gm[:], func=AF.Exp), reads=["Gm", "ohG"], writes=["Gm"])
        dve(lambda e: e.tensor_reduce(out=sm["sumG"][:], in_=Gm[:], axis=AX.X, op=ALU.add), ["Gm"], ["sumG"])
        dve(lambda e: e.reciprocal(out=sm["pg"][:], in_=sm["sumG"][:]), ["sumG"], ["pg"])
        dve(lambda e: e.tensor_scalar(out=pen[:], in0=ohG[:], scalar1=BIG, scalar2=-BIG, op0=ALU.mult, op1=ALU.add),
            ["ohG"], ["pen"])
        dve(lambda e: e.tensor_tensor(out=Em[:].rearrange("p n (g e) -> p n g e", g=4), in0=E4,
                                      in1=pen[:].unsqueeze(3).to_broadcast([128, NT, 4, 8]), op=ALU.add),
            ["LG", "pen"], ["Em"])
        dve(lambda e: e.tensor_reduce(out=sm["m1"][:], in_=Em[:], axis=AX.X, op=ALU.max), ["Em"], ["m1"])
        dve(lambda e: e.tensor_tensor(out=T1[:], in0=Em[:], in1=bc(sm["m1"], 32), op=ALU.subtract), ["Em", "m1"], ["T1"])
        dve(lambda e: e.tensor_single_scalar(out=oh1[:], in_=T1[:], scalar=0.0, op=ALU.is_ge), ["T1"], ["oh1"])
        dve(lambda e: e.scalar_tensor_tensor(out=Em2[:], in0=oh1[:], scalar=-BIG, in1=Em[:], op0=ALU.mult, op1=ALU.add),
            ["oh1", "Em"], ["Em2"])
        dve(lambda e: e.tensor_reduce(out=sm["m2"][:], in_=Em2[:], axis=AX.X, op=ALU.max), ["Em2"], ["m2"])
        dve(lambda e: e.tensor_tensor(out=T1[:], in0=Em2[:], in1=bc(sm["m2"], 32), op=ALU.subtract), ["Em2", "m2"], ["T1"])
        dve(lambda e: e.tensor_single_scalar(out=oh2[:], in_=T1[:], scalar=0.0, op=ALU.is_ge), ["T1"], ["oh2"])
        dve(lambda e: e.tensor_tensor(out=sm["r"][:], in0=sm["m2"][:], in1=sm["m1"][:], op=ALU.subtract), ["m1", "m2"], ["r"])
        P.op("act", lambda e: e.activation(out=sm["r"][:], in_=sm["r"][:], func=AF.Exp), reads=["r"], writes=["r"])
        dve(lambda e: e.tensor_scalar(out=sm["den"][:], in0=sm["r"][:], scalar1=1.0, scalar2=None, op0=ALU.add), ["r"], ["den"])
        dve(lambda e: e.reciprocal(out=sm["den"][:], in_=sm["den"][:]), ["den"], ["den"])
        dve(lambda e: e.tensor_tensor(out=sm["g1"][:], in0=sm["pg"][:], in1=sm["den"][:], op=ALU.mult), ["pg", "den"], ["g1"])
        dve(lambda e: e.tensor_tensor(out=sm["g2"][:], in0=sm["g1"][:], in1=sm["r"][:], op=ALU.mult), ["g1", "r"], ["g2"])
        dve(lambda e: e.tensor_tensor(out=Obf[:], in0=oh1[:], in1=oh2[:], op=ALU.add), ["oh1", "oh2"], ["Obf"])

        if int(os.environ.get('MOE_STOP', '99')) <= 2:
            return
        def ranks(e):
            for t in range(NT):
                o = ps[7][:, t * 32:(t + 1) * 32]
                inst = e.matmul(o, self.lst_b[:], Obf[:, t, :], start=True, stop=(t == 0))
                for j in range(t):
                    inst = e.matmul(o, self.ones_b[:], Obf[:, j, :], start=False, stop=(j == t - 1))
            return inst
        P.op("pe", ranks, reads=["Obf", "lst_b", "ones_b"], writes=[("ps", 7)])
        dve(lambda e: e.tensor_copy(out=RK[:], in_=ps[7][:].rearrange("p (n e) -> p n e", e=32)), [("ps", 7)], ["RK"])
        ebc = self.ebase[:].unsqueeze(1).to_broadcast([128, NT, 32])
        for k, (oh, ohn, gn) in enumerate(((oh1, "oh1", "g1"), (oh2, "oh2", "g2"))):
            dve(lambda e, oh=oh: e.tensor_tensor(out=T1[:], in0=oh[:], in1=RK[:], op=ALU.mult), [ohn, "RK"], ["T1"])
            dve(lambda e: e.tensor_reduce(out=sm["rk"][:], in_=T1[:], axis=AX.X, op=ALU.add), ["T1"], ["rk"])
            dve(lambda e, oh=oh: e.tensor_tensor(out=T1[:], in0=oh[:], in1=ebc, op=ALU.mult), [ohn, "ebase"], ["T1"])
            dve(lambda e: e.tensor_reduce(out=sm["base"][:], in_=T1[:], axis=AX.X, op=ALU.add), ["T1"], ["base"])
            dve(lambda e: e.tensor_single_scalar(out=sm["valid"][:], in_=sm["rk"][:], scalar=float(CAP), op=ALU.is_lt),
                ["rk"], ["valid"])
            dve(lambda e: e.scalar_tensor_tensor(out=sm["sl"][:], in0=sm["rk"][:], scalar=-float(TRASH), in1=sm["base"][:],
                                                 op0=ALU.add, op1=ALU.add), ["rk", "base"], ["sl"])
            dve(lambda e: e.tensor_tensor(out=sm["sl"][:], in0=sm["sl"][:], in1=sm["valid"][:], op=ALU.mult), ["sl", "valid"], ["sl"])
            dve(lambda e: e.tensor_scalar(out=sm["sl"][:], in0=sm["sl"][:], scalar1=float(TRASH), scalar2=None, op0=ALU.add),
                ["sl"], ["sl"])
            dve(lambda e, k=k: e.tensor_copy(out=slot_i[:, k, :], in_=sm["sl"][:]), ["sl"], [("slot", k)])
            dve(lambda e, gn=gn: e.tensor_tensor(out=sm[gn][:], in0=sm[gn][:], in1=sm["valid"][:], op=ALU.mult),
                [gn, "valid"], [gn])

        if int(os.environ.get('MOE_STOP', '99')) <= 3:
            return
        for t in range(NT):
            b = t % NHB
            P.op("dve", lambda e, t=t, b=b: e.scalar_tensor_tensor(
                out=hnb[b][:], in0=h[:, t, :], scalar=rstd[:, t:t + 1], in1=gb[:], op0=ALU.mult, op1=ALU.mult),
                reads=[("h", t), "rstd", "gb"], writes=[("hnb", b)])
            for k in range(2):
                P.op("pool", lambda e, t=t, b=b, k=k: e.indirect_dma_start(
                    out=Xs[:, :], out_offset=bass.IndirectOffsetOnAxis(ap=slot_i[:, k, t:t + 1], axis=0),
                    in_=hnb[b][:], in_offset=None),
                    reads=[("hnb", b), ("slot", k)] + xs_z_keys, writes=[("Xs_w", t, k)], dma=True)

        if int(os.environ.get('MOE_STOP', '99')) <= 4:
            dbg = [sm["sl"], sm["g1"], sm["g2"], sm["rk"], sm["base"], sm["valid"]]
            for q, tl in enumerate(dbg):
                P.op("dve", lambda e, q=q, tl=tl: e.tensor_copy(out=h[:, 0, q * 16:(q + 1) * 16], in_=tl[:]),
                     reads=["sl", "g1", "g2", "rk", "base", "valid"], writes=[("h", 0)])
            for k in range(2):
                P.op("dve", lambda e, k=k: e.tensor_copy(out=h[:, 0, 96 + k * 16:112 + k * 16], in_=slot_i[:, k, :]),
                     reads=[("slot", k)], writes=[("h", 0)])
            P.op("dve", lambda e: e.tensor_copy(out=h[:, 1, 0:576], in_=LG[:].rearrange("p n j -> p (n j)")),
                 reads=["LG"], writes=[("h", 1)])
            return
        P.barrier()
        xs_keys = [("Xs_w", t, k) for t in range(NT) for k in range(2)]

        def load_x(ex):
            s = ex % 2
            P.op("sp", lambda e: e.dma_start(
                out=xg[s][:], in_=Xs[ex * CAP:(ex + 1) * CAP, :].rearrange("(r p) d -> p r d", p=128)),
                reads=xs_keys, writes=[("xg", s)], dma=True)

        load_x(0)
        for ex in range(NE):
            s = ex % 2
            if ex + 1 < NE:
                load_x(ex + 1)
            for r in range(2):
                bank = ps[r]

                def tr2(e, r=r, s=s, bank=bank):
                    pv = bank[:].bitcast(BF16)
                    for c in range(8):
                        inst = e.transpose(out=pv[:, c * 128:(c + 1) * 128], in_=xg[s][:, r, c * 128:(c + 1) * 128],
                                           identity=self.ident_b[:])
                    return inst
                P.op("pe", tr2, reads=[("xg", s), "ident_b"], writes=[("ps", r)])
                src = lambda bank=bank: bank[:].bitcast(BF16).rearrange("p (c t) -> p c t", c=8)
                if r == 0:
                    P.op("act", lambda e, s=s, src=src: e.activation(out=xT[s][:, :, 0:128], in_=src(), func=AF.Copy),
                         reads=[("ps", 0)], writes=[("xT", s, 0)])
                else:
                    P.op("dve", lambda e, s=s, src=src: e.tensor_copy(out=xT[s][:, :, 128:256], in_=src()),
                         reads=[("ps", 1)], writes=[("xT", s, 1)])
            for m in range(4):
                bank = ps[2 + m % 2]
                bk = ("ps", 2 + m % 2)

                def gu(e, m=m, s=s, bank=bank):
                    for half in range(2):
                        col = half * FF + m * 128
                        for c in range(8):
                            inst = e.matmul(bank[:, half * CAP:(half + 1) * CAP], wgu[s][:, c, col:col + 128],
                                            xT[s][:, c, :], start=(c == 0), stop=(c == 7))
                    return inst
                P.op("pe", gu, reads=[("wgu", s), ("xT", s, 0), ("xT", s, 1)], writes=[bk])
                P.op("act", lambda e, m=m, bank=bank: e.activation(out=sA[m % 2][:], in_=bank[:, 0:CAP], func=AF.Silu),
                     reads=[bk], writes=[("sA", m % 2)])
                P.op("dve", lambda e, m=m, s=s, bank=bank: e.tensor_tensor(out=hT[s][:, m, :], in0=sA[m % 2][:],
                                                                          in1=bank[:, CAP:2 * CAP], op=ALU.mult),
                     reads=[bk, ("sA", m % 2)], writes=[("hT", s, m)])
            for r in range(2):
                for n in range(2):
                    q = r * 2 + n
                    bank = ps[4 + q % 2]
                    bk = ("ps", 4 + q % 2)

                    def dn(e, r=r, n=n, s=s, bank=bank):
                        for m in range(4):
                            inst = e.matmul(bank[:, :], hT[s][:, m, r * 128:(r + 1) * 128],
                                            wdn[s][:, m, n * 512:(n + 1) * 512], start=(m == 0), stop=(m == 3))
                        return inst
                    P.op("pe", dn, reads=[("wdn", s)] + [("hT", s, m) for m in range(4)], writes=[bk])
                    if q % 2 == 0:
                        P.op("act", lambda e, r=r, n=n, s=s, bank=bank: e.activation(
                            out=yt[s][:, r, n * 512:(n + 1) * 512], in_=bank[:, :], func=AF.Copy),
                            reads=[bk], writes=[("yt", s, q)])
                    else:
                        P.op("dve", lambda e, r=r, n=n, s=s, bank=bank: e.tensor_copy(
                            out=yt[s][:, r, n * 512:(n + 1) * 512], in_=bank[:, :]),
                            reads=[bk], writes=[("yt", s, q)])
            P.op("sp", lambda e, ex=ex, s=s: e.dma_start(
                out=Ys[ex * CAP:(ex + 1) * CAP, :].rearrange("(r p) d -> p r d", p=128), in_=yt[s][:]),
                reads=[("yt", s, q) for q in range(4)], writes=[("Ys_w", ex)], dma=True)
            if ex + 2 < NE:
                load_w(ex + 2)

        if int(os.environ.get('MOE_STOP', '99')) <= 5:
            return
        P.op("pool", lambda e: e.memset(fence[:], 0.0), writes=[("wgu", 0)] + [("yg", b) for b in range(NYG)])
        for t in range(NT):
            for k, gn in enumerate(("g1", "g2")):
                b = (t * 2 + k) % NYG
                P.op("pool", lambda e, t=t, b=b, k=k: e.indirect_dma_start(
                    out=yg[b][:], out_offset=None, in_=Ys[:, :],
                    in_offset=bass.IndirectOffsetOnAxis(ap=slot_i[:, k, t:t + 1], axis=0)),
                    reads=[("Ys_w", ex) for ex in range(NE)] + [("slot", k), "Ys_z"], writes=[("yg", b)], dma=True)
                P.op("dve", lambda e, t=t, b=b, gn=gn: e.scalar_tensor_tensor(
                    out=h[:, t, :], in0=yg[b][:], scalar=sm[gn][:, t:t + 1], in1=h[:, t, :], op0=ALU.mult, op1=ALU.add),
                    reads=[("yg", b), gn, ("h", t)], writes=[("h", t)])
                if k == 1:
                    self.sq(t)

    def norm_transpose(self, hnT, hnb):
        P, h, ps = self.P, self.h, self.ps
        def st1(t):
            b = t % 2
            P.op("dve", lambda e: e.scalar_tensor_tensor(
                out=hnb[b][:], in0=h[:, t, :], scalar=self.rstd[:, t:t + 1], in1=self.gb[:], op0=ALU.mult, op1=ALU.mult),
                reads=[("h", t), "rstd", "gb"], writes=[("hnb", b)])
        st1(0)
        for t in range(NT):
            if t + 1 < NT:
                st1(t + 1)
            self.transpose_tile(hnb[t % 2], ("hnb", t % 2), hnT, t, t % 2)

    def transpose_tile(self, src, src_key, dstT, t, b):
        P, ps = self.P, self.ps
        bank = ps[b]

        def tr(e):
            pv = bank[:].bitcast(BF16)
            for c in range(8):
                inst = e.transpose(out=pv[:, c * 128:(c + 1) * 128], in_=src[:, c * 128:(c + 1) * 128],
                                   identity=self.ident_b[:])
            return inst
        P.op("pe", tr, reads=[src_key, "ident_b"], writes=[("ps", b)])
        srcv = lambda: bank[:].bitcast(BF16).rearrange("p (c t) -> p c t", c=8)
        if b == 0:
            P.op("act", lambda e: e.activation(out=dstT[:, :, t * 128:(t + 1) * 128], in_=srcv(), func=AF.Copy),
                 reads=[("ps", b)], writes=[("xT", t)])
        else:
            P.op("dve", lambda e: e.tensor_copy(out=dstT[:, :, t * 128:(t + 1) * 128], in_=srcv()),
                 reads=[("ps", b)], writes=[("xT", t)])

    def out_proj(self, xT, kc, wout, n_k):
        P, h, ps = self.P, self.h, self.ps
        for t in range(NT):
            for n in range(2):
                bi = 2 + (t * 2 + n) % 2
                bank = ps[bi]

                def mm(e, t=t, n=n, bank=bank):
                    for c in range(kc):
                        inst = e.matmul(bank[:, :], xT[:, c, t * 128:(t + 1) * 128], wout[:, c, n * 512:(n + 1) * 512],
                                        start=(c == 0), stop=(c == kc - 1))
                    return inst
                P.op("pe", mm, reads=[("xT", t), "wout"] if n_k is None else n_k(t) + ["wout"], writes=[("ps", bi)])
                P.op("dve", lambda e, t=t, n=n, bank=bank: e.tensor_tensor(
                    out=h[:, t, n * 512:(n + 1) * 512], in0=h[:, t, n * 512:(n + 1) * 512], in1=bank[:, :], op=ALU.add),
                    reads=[("ps", bi), ("h", t)], writes=[("h", t)])
                if n == 1:
                    self.sq(t)

    def fox_setup(self, layers):
        self.fox_layers = list(layers)
        nl = len(layers)
        self.din("fox_w_in", [nl, D, 4112])
        self.din("fox_b_f", [nl, 16])
        self.din("fox_w_out", [nl, D, D])
        self.din("c_mask", [128, 128])
        self.cum3_d = self.dscratch("cum3_d", [16, 3, S], BF16)
        self.Og_d = self.dscratch("Og_d", [S, D], BF16)
        self.mask_b = self.sb("mask_b", [128, 128], BF16)
        self.P.op("pool", lambda e: e.dma_start(out=self.mask_b[:], in_=self.dram["c_mask"]), writes=["mask_b"], dma=True)

    def fox_layer(self, layer):
        j = self.fox_layers.index(layer)
        P, h, ps, ar = self.P, self.h, self.ps, self.ar
        w_in = self.dram["fox_w_in"]
        P.barrier()
        self.arena_reset()
        hnT = ar("f_hnT", [128, 8, S], BF16)
        hnb = [ar("f_hnb%d" % s, [128, D], BF16) for s in range(2)]
        wf = ar("f_wf", [128, 8, 16], BF16)
        bft = ar("f_bft", [128, 1], F32)
        negbf = ar("f_negbf", [128, 1], F32)
        save = self.arena_off
        Ft = ar("f_Ft", [128, S], F32)
        cumP = ar("f_cumP", [128, S], F32)
        r1 = ar("f_r1", [128, S], F32)
        cum3 = ar("f_cum3", [128, 3, S], BF16)
        self.arena_off = save
        wgrp = [ar("f_wgrp%d" % s, [128, 8, 4, 128], BF16) for s in range(2)]
        QTa = [ar("f_QTa%d" % s, [128, S], BF16) for s in range(2)]
        KTa = [ar("f_KTa%d" % s, [128, S], BF16) for s in range(2)]
        Tst = [ar("f_Tst%d" % s, [128, S], BF16) for s in range(2)]
        Vaug = ar("f_Vaug", [128, NT, 2, 65], BF16)
        Gs = ar("f_Gs", [128, NT, 128], BF16)
        NPT = 8
        PT = [ar("f_PT%d" % s, [128, 512], BF16) for s in range(NPT)]
        Og = [ar("f_Og%d" % s, [128, NT, 128], BF16) for s in range(2)]
        wout = ar("f_wout", [128, 8, D], BF16)
        rden = ar("f_rden", [128, 8], F32)

        self.rms_stats(self.dram["norm_mix"][layer:layer + 1, :])
        P.op("pool", lambda e: e.dma_start(out=wf[:], in_=w_in[j][:, 4096:4112].rearrange("(c p) f -> p c f", p=128)),
             writes=["wf"], dma=True)
        P.op("sp", lambda e: e.dma_start(out=bft[0:16, :], in_=self.dram["fox_b_f"][j].rearrange("(h o) -> h o", o=1)),
             writes=["bft"], dma=True)
        P.op("dve", lambda e: e.tensor_scalar(out=negbf[0:16, :], in0=bft[0:16, :], scalar1=-1.0, scalar2=None, op0=ALU.mult),
             reads=["bft"], writes=["negbf"])
        self.norm_transpose(hnT, hnb)
        xT_all = [("xT", t) for t in range(NT)]

        if int(os.environ.get('FOX_STOP', '99')) <= 1:
            return
        for qd in range(4):
            bank = ps[2 + qd % 2]
            bk = ("ps", 2 + qd % 2)

            def fm(e, qd=qd, bank=bank):
                for c in range(8):
                    inst = e.matmul(bank[0:16, :], wf[:, c, :], hnT[:, c, qd * 512:(qd + 1) * 512], start=(c == 0), stop=(c == 7))
                return inst
            P.op("pe", fm, reads=xT_all + ["wf"], writes=[bk])
            P.op("act", lambda e, qd=qd, bank=bank: e.activation(out=Ft[0:16, qd * 512:(qd + 1) * 512], in_=bank[0:16, :],
                                                                func=AF.Exp, scale=-1.0, bias=negbf[0:16, :]),
                 reads=[bk, "negbf"], writes=["Ft"])
        P.op("act", lambda e: e.activation(out=Ft[0:16, :], in_=Ft[0:16, :], func=AF.Ln, bias=1.0), reads=["Ft"], writes=["Ft"])
        P.op("dve", lambda e: e.tensor_scalar(out=Ft[0:16, :], in0=Ft[0:16, :], scalar1=0.5, scalar2=None, op0=ALU.mult),
             reads=["Ft"], writes=["Ft"])
        P.op("dve", lambda e: e.tensor_tensor_scan(out=cumP[0:16, :], data0=Ft[0:16, :], data1=Ft[0:16, :], initial=0.0,
                                                   op0=ALU.add, op1=ALU.add), reads=["Ft"], writes=["cumP"])
        P.op("dve", lambda e: e.tensor_copy(out=cum3[0:16, 0, :], in_=cumP[0:16, :]), reads=["cumP"], writes=["cum3"])
        P.op("dve", lambda e: e.tensor_tensor(out=r1[0:16, :], in0=cumP[0:16, :], in1=cum3[0:16, 0, :], op=ALU.subtract),
             reads=["cumP", "cum3"], writes=["r1"])
        P.op("dve", lambda e: e.tensor_copy(out=cum3[0:16, 1, :], in_=r1[0:16, :]), reads=["r1"], writes=["cum3"])
        P.op("dve", lambda e: e.tensor_tensor(out=r1[0:16, :], in0=r1[0:16, :], in1=cum3[0:16, 1, :], op=ALU.subtract),
             reads=["r1", "cum3"], writes=["r1"])
        P.op("dve", lambda e: e.tensor_copy(out=cum3[0:16, 2, :], in_=r1[0:16, :]), reads=["r1"], writes=["cum3"])
        P.op("sp", lambda e: e.dma_start(out=self.cum3_d, in_=cum3[0:16, :, :]), reads=["cum3"], writes=["cum3_d"], dma=True)
        if int(os.environ.get('FOX_STOP', '99')) <= 2:
            return
        P.barrier()

        P.op("pool", lambda e: e.dma_start(out=wout[:], in_=self.dram["fox_w_out"][j].rearrange("(c p) f -> p c f", p=128)),
             writes=["wout"], dma=True)
        for hh in range(2):
            P.op("dve", lambda e, hh=hh: e.memset(QTa[hh][64:128, :], 0.0), writes=[("QTa", hh, "aug")])
            P.op("dve", lambda e, hh=hh: e.memset(KTa[hh][64:128, :], 0.0), writes=[("KTa", hh, "aug")])
            P.op("dve", lambda e, hh=hh: e.memset(QTa[hh][64:70, :], 1.0), writes=[("QTa", hh, "aug")])
            P.op("dve", lambda e, hh=hh: e.memset(KTa[hh][64:70, :], -1.0), writes=[("KTa", hh, "aug")])
        P.op("dve", lambda e: e.memset(Vaug[:, :, :, 64:65], 1.0), writes=["Vaug1"])

        def load_grp(g):
            s = g % 2
            for seg in range(4):
                P.op("pool", lambda e, seg=seg: e.dma_start(
                    out=wgrp[s][:, :, seg, :],
                    in_=w_in[j][:, seg * 1024 + g * 128: seg * 1024 + (g + 1) * 128].rearrange("(c p) f -> p c f", p=128)),
                    writes=[("wgrp", s, seg)], dma=True)

        if int(os.environ.get('FOX_STOP', '99')) <= 3:
            return
        load_grp(0)
        pt_rr = [0]
        ogv = self.Og_d.rearrange("(n p) d -> p n d", p=128)
        NG = int(os.environ.get('FOX_NG', '8'))
        for g in range(NG):
            s = g % 2
            if g + 1 < NG:
                load_grp(g + 1)
            for qk in range(2):
                dstT = QTa if qk == 0 else KTa
                dn = "QTa" if qk == 0 else "KTa"
                for qd in range(4):
                    bi = (qk * 4 + qd) % 2
                    bank = ps[bi]
                    cs = slice(qd * 512, (qd + 1) * 512)

                    def pm(e, qk=qk, qd=qd, bank=bank, s=s):
                        for c in range(8):
                            inst = e.matmul(bank[:, :], wgrp[s][:, c, qk, :],
                                            hnT[:, c, qd * 512:(qd + 1) * 512], start=(c == 0), stop=(c == 7))
                        return inst
                    P.op("pe", pm, reads=xT_all + [("wgrp", s, qk)], writes=[("ps", bi)])
                    if qk == 0:
                        P.op("act", lambda e, cs=cs, bank=bank: e.activation(
                            out=QTa[0][0:64, cs], in_=bank[0:64, :], func=AF.Identity, scale=0.125),
                            reads=[("ps", bi)], writes=[("QTa", 0, qd)])
                        P.op("act", lambda e, cs=cs, bank=bank: e.activation(
                            out=Tst[0][64:128, cs], in_=bank[64:128, :], func=AF.Identity, scale=0.125),
                            reads=[("ps", bi)], writes=[("Tst", 0, qd)])
                    else:
                        P.op("dve", lambda e, cs=cs, bank=bank: e.tensor_copy(out=KTa[0][0:64, cs], in_=bank[0:64, :]),
                             reads=[("ps", bi)], writes=[("KTa", 0, qd)])
                        P.op("dve", lambda e, cs=cs, bank=bank: e.tensor_copy(out=Tst[1][64:128, cs], in_=bank[64:128, :]),
                             reads=[("ps", bi)], writes=[("Tst", 1, qd)])
                P.op("sp", lambda e, qk=qk, dstT=dstT: e.dma_start(out=dstT[1][0:64, :], in_=Tst[qk][64:128, :]),
                     reads=[("Tst", qk, qd) for qd in range(4)], writes=[(dn, 1, qd) for qd in range(4)], dma=True)
            for hh in range(2):
                head = 2 * g + hh
                P.op("sp", lambda e, hh=hh, head=head: e.dma_start(out=QTa[hh][64:67, :], in_=self.cum3_d[head]),
                     reads=["cum3_d"], writes=[("QTa", hh, "aug")], dma=True)
                P.op("sp", lambda e, hh=hh, head=head: e.dma_start(out=KTa[hh][67:70, :], in_=self.cum3_d[head]),
                     reads=["cum3_d"], writes=[("KTa", hh, "aug")], dma=True)
            if int(os.environ.get('FOX_STOP', '99')) <= 4:
                return
            for t in range(NT):
                bi = t % 2
                bank = ps[bi]

                def vm(e, t=t, bank=bank, s=s):
                    for c in range(8):
                        inst = e.matmul(bank[:, 0:256], hnT[:, c, t * 128:(t + 1) * 128], wgrp[s][:, c, 2:4, :].rearrange("p a b -> p (a b)"),
                                        start=(c == 0), stop=(c == 7))
                    return inst
                P.op("pe", vm, reads=[("xT", t), ("wgrp", s, 2), ("wgrp", s, 3)], writes=[("ps", bi)])
                if os.environ.get("FOX_DBG", "") != "noV":
                    P.op("dve", lambda e, t=t, bank=bank: e.tensor_copy(
                        out=Vaug[:, t, :, 0:64], in_=bank[:, 0:128].rearrange("p (a b) -> p a b", a=2)),
                        reads=[("ps", bi)], writes=[("Vaug", t)])
                if os.environ.get("FOX_DBG", "") != "noG":
                    P.op("act", lambda e, t=t, bank=bank: e.activation(out=Gs[:, t, :], in_=bank[:, 128:256], func=AF.Sigmoid),
                         reads=[("ps", bi)], writes=[("Gs", t)])
            if int(os.environ.get('FOX_STOP', '99')) <= 5:
                allk = [("QTa", 0, q) for q in range(4)] + [("KTa", 0, q) for q in range(4)] + [("QTa", 0, "aug"), ("KTa", 0, "aug"), "Vaug1"] + [("Vaug", t) for t in range(NT)] + [("Gs", t) for t in range(NT)]
                def dd(dst, src):
                    P.op("dve", lambda e: e.tensor_copy(out=dst, in_=src), reads=allk, writes=[("h", i) for i in range(NT)])
                dd(h[:, 0, :], QTa[0][:, 0:1024]); dd(h[:, 1, :], QTa[0][:, 1024:2048])
                dd(h[:, 2, :], KTa[0][:, 0:1024]); dd(h[:, 3, :], KTa[0][:, 1024:2048])
                dd(h[:, 4, 0:130], Vaug[:, 0, :, :].rearrange("p a b -> p (a b)")); dd(h[:, 4, 256:384], Gs[:, 0, :])
                return
            items = []
            for hh in range(2):
                for qg in range(4):
                    for kb in range(4 * (qg + 1)):
                        items.append((hh, qg, kb))

            def rec_qk(it):
                hh, qg, kb = it
                q0 = max(0, kb - 4 * qg) * 128
                slot = pt_rr[0] % NPT
                pt_rr[0] += 1
                bi = slot % 4
                bank = ps[bi]
                P.op("pe", lambda e: e.matmul(bank[:, q0:512], KTa[hh][0:96, kb * 128:(kb + 1) * 128],
                                             QTa[hh][0:96, qg * 512 + q0:(qg + 1) * 512], start=True, stop=True),
                     reads=[("KTa", hh, kb // 4), ("KTa", hh, "aug"), ("QTa", hh, qg), ("QTa", hh, "aug")],
                     writes=[("ps", bi)])
                P.op("act", lambda e: e.activation(out=PT[slot][:, q0:512], in_=bank[:, q0:512], func=AF.Exp),
                     reads=[("ps", bi)], writes=[("PT", slot)])
                if kb >= 4 * qg:
                    P.op("dve", lambda e: e.tensor_tensor(out=PT[slot][:, q0:q0 + 128], in0=PT[slot][:, q0:q0 + 128],
                                                          in1=self.mask_b[:], op=ALU.mult),
                         reads=[("PT", slot), "mask_b"], writes=[("PT", slot)])
                return slot

            def rec_pv(it, slot):
                hh, qg, kb = it
                for qt in range(4):
                    i = 4 * qg + qt
                    if kb > i:
                        continue
                    bi = 4 + qt
                    P.op("pe", lambda e, qt=qt, i=i, bi=bi: e.matmul(
                        ps[bi][:, 0:65], PT[slot][:, qt * 128:(qt + 1) * 128], Vaug[:, kb, hh, :],
                        start=(kb == 0), stop=(kb == i)),
                        reads=[("PT", slot), ("Vaug", kb), "Vaug1"], writes=[("ps", bi)])
                    if kb == i:
                        rs = (i + hh) % 8
                        P.op("dve", lambda e, bi=bi, rs=rs: e.reciprocal(out=rden[:, rs:rs + 1], in_=ps[bi][:, 64:65]),
                             reads=[("ps", bi)], writes=[("rden", rs)])
                        P.op("dve", lambda e, bi=bi, rs=rs, i=i, s=s: e.scalar_tensor_tensor(
                            out=Og[s][:, i, hh * 64:(hh + 1) * 64], in0=ps[bi][:, 0:64], scalar=rden[:, rs:rs + 1],
                            in1=Gs[:, i, hh * 64:(hh + 1) * 64], op0=ALU.mult, op1=ALU.mult),
                            reads=[("ps", bi), ("rden", rs), ("Gs", i)], writes=[("Og", s, i)])

            slots = {}
            LA = int(os.environ.get('FOX_LA', '4'))
            for n in range(min(LA, len(items))):
                slots[n] = rec_qk(items[n])
            for n, it in enumerate(items):
                if n + LA < len(items):
                    slots[n + LA] = rec_qk(items[n + LA])
                rec_pv(it, slots[n])
            P.op("sp", lambda e, g=g, s=s: e.dma_start(out=ogv[:, :, g * 128:(g + 1) * 128], in_=Og[s][:]),
                 reads=[("Og", s, i) for i in range(NT)], writes=[("Og_d", g)], dma=True)
            if int(os.environ.get('FOX_STOP', '99')) <= 6:
                allk = [("Og", s, i) for i in range(NT)]
                P.op("dve", lambda e: e.tensor_copy(out=h[:, 0, :], in_=Og[0][:, 0:8, :].rearrange("p a b -> p (a b)")),
                     reads=allk, writes=[("h", 0)])
                P.op("dve", lambda e: e.tensor_copy(out=h[:, 1, :], in_=Og[0][:, 8:16, :].rearrange("p a b -> p (a b)")),
                     reads=allk, writes=[("h", 1)])
                return

        if int(os.environ.get('FOX_STOP', '99')) <= 7:
            return
        for t in range(NT):
            b = t % 2
            P.op("sp", lambda e, t=t, b=b: e.dma_start(out=hnb[b][:], in_=ogv[:, t, :]),
                 reads=[("Og_d", g) for g in range(8)], writes=[("hnb", b)], dma=True)
            self.transpose_tile(hnb[b], ("hnb", b), hnT, t, b)
        self.out_proj(hnT, NG, wout, None)

    def ret_setup(self, layers):
        self.ret_layers = list(layers)
        nl = len(layers)
        self.din("ret_w_in", [nl, D, 6144])
        self.din("ret_gn_gain", [nl, 2048])
        self.din("ret_w_out", [nl, 2048, D])
        self.din("c_cos", [128, S])
        self.din("c_sin", [128, S])
        self.din("c_intraT", [128, 4, 128])
        self.din("c_rdec", [128, 12])
        self.Og2_d = self.dscratch("Og2_d", [S, 2048], BF16)

    def ret_layer(self, layer):
        j = self.ret_layers.index(layer)
        P, h, ps, ar = self.P, self.h, self.ps, self.ar
        w_in = self.dram["ret_w_in"]
        P.barrier()
        self.arena_reset()
        hnT = ar("r_hnT", [128, 8, S], BF16)
        hnb = [ar("r_hnb%d" % s, [128, D], BF16) for s in range(2)]
        wqk = [ar("r_wqk%d" % s, [128, 8, 512], BF16) for s in range(2)]
        wvg = ar("r_wvg", [128, 8, 1024], BF16)
        QT = ar("r_QT", [128, 2, S], BF16)
        KT = ar("r_KT", [128, 2, S], BF16)
        cosb = ar("r_cos", [128, S], BF16)
        sinb = ar("r_sin", [128, S], BF16)
        rt = [ar("r_rt%d" % s, [128, 512], F32) for s in range(4)]
        intraT = ar("r_intraT", [128, 4, 128], BF16)
        rdec = ar("r_rdec", [128, 12], F32)
        R32 = ar("r_R32", [128, 2, 512], F32)
        Rb = [ar("r_Rb%d" % s, [128, 2, 512], BF16) for s in range(2)]
        Vt = [ar("r_Vt%d" % s, [128, 512], BF16) for s in range(2)]
        Kd = [ar("r_Kd%d" % s, [128, 256], BF16) for s in range(2)]
        PTt = [ar("r_PT%d" % s, [128, 128], BF16) for s in range(2)]
        on = [ar("r_on%d" % s, [128, 512], F32) for s in range(2)]
        sg = [ar("r_sg%d" % s, [128, 512], BF16) for s in range(2)]
        ogo = [ar("r_ogo%d" % s, [128, 512], BF16) for s in range(2)]
        st6 = ar("r_st6", [128, 2, 6], F32)
        mv = ar("r_mv", [128, 2, 2], F32)
        sm = ar("r_sm", [128, 2, 4], F32)

        self.rms_stats(self.dram["norm_mix"][layer:layer + 1, :])
        P.op("pool", lambda e: e.dma_start(out=cosb[:], in_=self.dram["c_cos"]), writes=["cosb"], dma=True)
        P.op("pool", lambda e: e.dma_start(out=sinb[:], in_=self.dram["c_sin"]), writes=["sinb"], dma=True)
        P.op("pool", lambda e: e.dma_start(out=intraT[:], in_=self.dram["c_intraT"]), writes=["intraT"], dma=True)
        P.op("sp", lambda e: e.dma_start(out=rdec[:], in_=self.dram["c_rdec"]), writes=["rdec"], dma=True)
        self.norm_transpose(hnT, hnb)
        xT_all = [("xT", t) for t in range(NT)]
        og2v = self.Og2_d

        def load_qk(hd):
            s = hd % 2
            for qk in range(2):
                P.op("pool", lambda e, qk=qk: e.dma_start(
                    out=wqk[s][:, :, qk * 256:(qk + 1) * 256],
                    in_=w_in[j][:, qk * 1024 + hd * 256: qk * 1024 + (hd + 1) * 256].rearrange("(c p) f -> p c f", p=128)),
                    writes=[("wqk", s, qk)], dma=True)

        def load_vg(hd):
            for vg in range(2):
                P.op("pool", lambda e, vg=vg: e.dma_start(
                    out=wvg[:, :, vg * 512:(vg + 1) * 512],
                    in_=w_in[j][:, 2048 + vg * 2048 + hd * 512: 2048 + vg * 2048 + (hd + 1) * 512].rearrange("(c p) f -> p c f", p=128)),
                    writes=[("wvg", vg)], dma=True)

        load_qk(0)
        for hd in range(4):
            s = hd % 2
            gam = 1.0 - 2.0 ** (-5.0 - hd)
            gamC = float(np.float32(np.exp(np.float32(np.log(np.float32(gam))) * np.float32(128.0))))
            load_vg(hd)
            if hd + 1 < 4:
                load_qk(hd + 1)
            for qk in range(2):
                dst = QT if qk == 0 else KT
                dname = "QT" if qk == 0 else "KT"
                sc = 1.0 if qk == 0 else 0.0625
                for qd in range(4):
                    cs = slice(qd * 512, (qd + 1) * 512)
                    for half in range(2):
                        def pm(e, half=half, qk=qk, qd=qd, s=s):
                            col = qk * 256 + half * 128
                            for c in range(8):
                                inst = e.matmul(ps[half][:, :], wqk[s][:, c, col:col + 128], hnT[:, c, qd * 512:(qd + 1) * 512],
                                                start=(c == 0), stop=(c == 7))
                            return inst
                        P.op("pe", pm, reads=xT_all + [("wqk", s, qk)], writes=[("ps", half)])
                    for k4, (bank, tab, tn) in enumerate(((0, cosb, "cosb"), (1, sinb, "sinb"), (0, sinb, "sinb"), (1, cosb, "cosb"))):
                        P.op("dve", lambda e, k4=k4, bank=bank, tab=tab, cs=cs, sc=sc: e.scalar_tensor_tensor(
                            out=rt[k4][:], in0=ps[bank][:, :], scalar=sc, in1=tab[:, cs], op0=ALU.mult, op1=ALU.mult),
                            reads=[("ps", bank), tn], writes=[("rt", k4)])
                    P.op("pool", lambda e, dst=dst, cs=cs: e.tensor_tensor(out=dst[:, 0, cs], in0=rt[0][:], in1=rt[1][:], op=ALU.subtract),
                         reads=[("rt", 0), ("rt", 1)], writes=[(dname, qd)])
                    P.op("pool", lambda e, dst=dst, cs=cs: e.tensor_tensor(out=dst[:, 1, cs], in0=rt[2][:], in1=rt[3][:], op=ALU.add),
                         reads=[("rt", 2), ("rt", 3)], writes=[(dname, qd)])
            P.op("dve", lambda e: e.memset(R32[:], 0.0), writes=[("R32", 0), ("R32", 1)])
            def partA(n):
                b = n % 2
                cols = slice(n * 128, (n + 1) * 128)
                qdk = n // 4

                def ktr(e, cols=cols, b=b):
                    pv = ps[0][:].bitcast(BF16)
                    for c in range(2):
                        inst = e.transpose(out=pv[:, c * 128:(c + 1) * 128], in_=KT[:, c, cols], identity=self.ident_b[:])
                    return inst
                P.op("pe", ktr, reads=[("KT", qdk), "ident_b"], writes=[("ps", 0)])
                P.op("act", lambda e, b=b, hd=hd: e.activation(out=Kd[b][:], in_=ps[0][:].bitcast(BF16)[:, 0:256], func=AF.Identity,
                                                               scale=rdec[:, 8 + hd:9 + hd]),
                     reads=[("ps", 0), "rdec"], writes=[("Kd", b)])

                def smm(e, cols=cols):
                    for c in range(2):
                        inst = e.matmul(ps[1][:, 0:128], KT[:, c, cols], QT[:, c, cols], start=(c == 0), stop=(c == 1))
                    return inst
                P.op("pe", smm, reads=[("KT", qdk), ("QT", qdk)], writes=[("ps", 1)])
                P.op("dve", lambda e, b=b, hd=hd: e.tensor_tensor(out=PTt[b][:], in0=ps[1][:, 0:128], in1=intraT[:, hd, :], op=ALU.mult),
                     reads=[("ps", 1), "intraT"], writes=[("PTt", b)])

                def vmm(e, cols=cols):
                    for c in range(8):
                        inst = e.matmul(ps[2][:, :], hnT[:, c, cols], wvg[:, c, 0:512], start=(c == 0), stop=(c == 7))
                    return inst
                P.op("pe", vmm, reads=[("xT", n), ("wvg", 0)], writes=[("ps", 2)])
                P.op("act", lambda e, b=b: e.activation(out=Vt[b][:], in_=ps[2][:, :], func=AF.Copy),
                     reads=[("ps", 2)], writes=[("Vt", b)])

                def gmm(e, cols=cols):
                    for c in range(8):
                        inst = e.matmul(ps[3][:, :], hnT[:, c, cols], wvg[:, c, 512:1024], start=(c == 0), stop=(c == 7))
                    return inst
                P.op("pe", gmm, reads=[("xT", n), ("wvg", 1)], writes=[("ps", 3)])
                P.op("act", lambda e, b=b: e.activation(out=sg[b][:], in_=ps[3][:, :], func=AF.Silu),
                     reads=[("ps", 3)], writes=[("sg", b)])


            def partB(n):
                b = n % 2
                cols = slice(n * 128, (n + 1) * 128)
                qdk = n // 4
                if n < NT - 1:
                    rbn = (n + 1) % 2
                    for c in range(2):
                        P.op("pe", lambda e, c=c, b=b: e.matmul(ps[5 + c][:, :], Kd[b][:, c * 128:(c + 1) * 128], Vt[b][:],
                                                               start=True, stop=True),
                             reads=[("Kd", b), ("Vt", b)], writes=[("ps", 5 + c)])
                        P.op("dve", lambda e, c=c, gamC=gamC: e.scalar_tensor_tensor(
                            out=R32[:, c, :], in0=R32[:, c, :], scalar=gamC, in1=ps[5 + c][:, :], op0=ALU.mult, op1=ALU.add),
                            reads=[("ps", 5 + c), ("R32", c)], writes=[("R32", c)])
                        P.op("act", lambda e, c=c, rbn=rbn: e.activation(out=Rb[rbn][:, c, :], in_=R32[:, c, :], func=AF.Copy),
                             reads=[("R32", c)], writes=[("Rb", rbn, c)])

                def omm(e, cols=cols, b=b, n=n):
                    inst = e.matmul(ps[4][:, :], PTt[b][:], Vt[b][:], start=True, stop=(n == 0))
                    if n > 0:
                        for c in range(2):
                            inst = e.matmul(ps[4][:, :], QT[:, c, cols], Rb[n % 2][:, c, :], start=False, stop=(c == 1))
                    return inst
                P.op("pe", omm, reads=[("PTt", b), ("Vt", b), ("QT", qdk), ("Rb", n % 2, 0), ("Rb", n % 2, 1)], writes=[("ps", 4)])
                P.op("dve", lambda e, b=b: e.bn_stats(out=st6[:, b, :], in_=ps[4][:, :]), reads=[("ps", 4)], writes=[("st6", b)])
                P.op("dve", lambda e, b=b: e.bn_aggr(out=mv[:, b, :], in_=st6[:, b, :]), reads=[("st6", b)], writes=[("mv", b)])
                P.op("dve", lambda e, b=b, hd=hd: e.tensor_scalar(out=sm[:, b, 0:1], in0=mv[:, b, 1:2], scalar1=rdec[:, 4 + hd:5 + hd],
                                                                 scalar2=EPS, op0=ALU.mult, op1=ALU.add),
                     reads=[("mv", b), "rdec"], writes=[("sm", b, 0)])
                P.op("pool", lambda e, b=b: e.tensor_tensor(out=sm[:, b, 1:2], in0=sm[:, b, 0:1], in1=self.negh[:, 0:1], op=ALU.pow),
                     reads=[("sm", b, 0), "negh"], writes=[("sm", b, 1)])
                P.op("dve", lambda e, b=b, hd=hd: e.tensor_tensor(out=sm[:, b, 2:3], in0=sm[:, b, 1:2], in1=rdec[:, hd:hd + 1], op=ALU.mult),
                     reads=[("sm", b, 1), "rdec"], writes=[("sm", b, 2)])
                P.op("dve", lambda e, b=b: e.scalar_tensor_tensor(out=sm[:, b, 3:4], in0=mv[:, b, 0:1], scalar=-1.0, in1=sm[:, b, 2:3],
                                                                 op0=ALU.mult, op1=ALU.mult),
                     reads=[("mv", b), ("sm", b, 2)], writes=[("sm", b, 3)])
                P.op("act", lambda e, b=b: e.activation(out=on[b][:], in_=ps[4][:, :], func=AF.Identity,
                                                        scale=sm[:, b, 2:3], bias=sm[:, b, 3:4]),
                     reads=[("ps", 4), ("sm", b, 2), ("sm", b, 3)], writes=[("on", b)])
                P.op("pool", lambda e, b=b: e.tensor_tensor(out=ogo[b][:], in0=on[b][:], in1=sg[b][:], op=ALU.mult),
                     reads=[("on", b), ("sg", b)], writes=[("ogo", b)])
                P.op("sp", lambda e, b=b, n=n, hd=hd: e.dma_start(out=og2v[n * 128:(n + 1) * 128, hd * 512:(hd + 1) * 512], in_=ogo[b][:]),
                     reads=[("ogo", b)], writes=[("Og2_d", n, hd)], dma=True)

            partA(0)
            for n in range(NT):
                if n + 1 < NT:
                    partA(n + 1)
                partB(n)

        P.barrier()
        self.arena_reset()
        wout = ar("r_wout", [128, 16, D], BF16)
        ogt = [ar("r_ogt%d" % s, [128, 2048], BF16) for s in range(2)]
        OgT = [ar("r_OgT%d" % s, [128, 16, 128], BF16) for s in range(2)]
        gcol = ar("r_gcol", [128, 16], F32)
        P.op("pool", lambda e: e.dma_start(out=wout[:], in_=self.dram["ret_w_out"][j].rearrange("(c p) f -> p c f", p=128)),
             writes=["wout"], dma=True)
        P.op("sp", lambda e: e.dma_start(out=gcol[:], in_=self.dram["ret_gn_gain"][j].rearrange("(c p) -> p c", p=128),
                                         allow_slow_non_contiguous=True), writes=["gcol"], dma=True)
        for c in range(16):
            P.op("dve", lambda e, c=c: e.tensor_scalar(out=wout[:, c, :], in0=wout[:, c, :], scalar1=gcol[:, c:c + 1], scalar2=None,
                                                     op0=ALU.mult), reads=["wout", "gcol"], writes=["wout"])
        for t in range(NT):
            b = t % 2
            P.op("sp", lambda e, t=t, b=b: e.dma_start(out=ogt[b][:], in_=og2v[t * 128:(t + 1) * 128, :]),
                 writes=[("ogt", b)], dma=True)
            for half in range(2):
                def tr(e, half=half, b=b):
                    pv = ps[half][:].bitcast(BF16)
                    for c in range(8):
                        cc = half * 8 + c
                        inst = e.transpose(out=pv[:, c * 128:(c + 1) * 128], in_=ogt[b][:, cc * 128:(cc + 1) * 128],
                                           identity=self.ident_b[:])
                    return inst
                P.op("pe", tr, reads=[("ogt", b), "ident_b"], writes=[("ps", half)])
                srcv = lambda half=half: ps[half][:].bitcast(BF16).rearrange("p (c t) -> p c t", c=8)
                if half == 0:
                    P.op("act", lambda e, b=b, srcv=srcv: e.activation(out=OgT[b][:, 0:8, :], in_=srcv(), func=AF.Copy),
                         reads=[("ps", 0)], writes=[("OgT", b, 0)])
                else:
                    P.op("dve", lambda e, b=b, srcv=srcv: e.tensor_copy(out=OgT[b][:, 8:16, :], in_=srcv()),
                         reads=[("ps", 1)], writes=[("OgT", b, 1)])
            for nn in range(2):
                bi = 2 + (t * 2 + nn) % 2

                def mm(e, nn=nn, b=b, bi=bi):
                    for c in range(16):
                        inst = e.matmul(ps[bi][:, :], OgT[b][:, c, :], wout[:, c, nn * 512:(nn + 1) * 512],
                                        start=(c == 0), stop=(c == 15))
                    return inst
                P.op("pe", mm, reads=[("OgT", b, 0), ("OgT", b, 1), "wout"], writes=[("ps", bi)])
                P.op("dve", lambda e, t=t, nn=nn, bi=bi: e.tensor_tensor(
                    out=h[:, t, nn * 512:(nn + 1) * 512], in0=h[:, t, nn * 512:(nn + 1) * 512], in1=ps[bi][:, :], op=ALU.add),
                    reads=[("ps", bi), ("h", t)], writes=[("h", t)])
                if nn == 1:
                    self.sq(t)


def build(stages=None, final_norm=True):
    B = Builder()
    B.prologue()
    if stages is None:
        stages = []
        for i in range(DEPTH):
            stages += ["mix%d" % i, "moe%d" % i]
    moe_layers = sorted(set(int(s[3:]) for s in stages if s.startswith("moe")))
    if moe_layers:
        B.moe_setup(moe_layers)
    fox_layers = sorted(set(int(s[3:]) for s in stages if s.startswith("mix") and int(s[3:]) % 2 == 0))
    if fox_layers:
        B.fox_setup(fox_layers)
    ret_layers = sorted(set(int(s[3:]) for s in stages if s.startswith("mix") and int(s[3:]) % 2 == 1))
    if ret_layers:
        B.ret_setup(ret_layers)
    B.layer_sets = {"moe": moe_layers, "fox": fox_layers, "ret": ret_layers}
    for s in stages:
        li = int(s[3:])
        if s.startswith("moe"):
            B.moe_layer(li)
        elif li % 2 == 0:
            B.fox_layer(li)
        else:
            B.ret_layer(li)
    B.epilogue(final_norm=final_norm)
    nc = B.finish()
    _LAYER_SETS[id(nc)] = B.layer_sets
    return nc


def _consts():
    ident = np.eye(128, dtype=np.float32)
    lst = np.triu(np.ones((128, 128), np.float32), k=1)
    ebase = (np.arange(NE, dtype=np.float32) * CAP).reshape(1, NE)
    mask = np.triu(np.ones((128, 128), np.float32), k=0)
    inv = (np.float32(1.0) / (np.float32(10000.0) ** np.linspace(0.0, 1.0, 128, dtype=np.float32))).astype(np.float32)
    ang = (np.arange(S, dtype=np.float32)[None, :] * inv[:, None]).astype(np.float32)
    cosT = np.cos(ang).astype(np.float32)
    sinT = np.sin(ang).astype(np.float32)
    log_g = np.log(np.float32(1.0) - np.float32(2.0) ** (np.float32(-5.0) - np.arange(4, dtype=np.float32))).astype(np.float32)
    idx = np.arange(128, dtype=np.float32)
    intraT = np.zeros((128, 4, 128), np.float32)
    rdec = np.zeros((128, 12), np.float32)
    for hd in range(4):
        col = np.exp(-log_g[hd] * (idx + 1.0)).astype(np.float32)
        intraT[:, hd, :] = np.where(idx[None, :] >= idx[:, None], col[:, None], 0.0)
        qd = np.exp(log_g[hd] * (idx + 1.0)).astype(np.float32)
        rdec[:, hd] = qd
        rdec[:, 4 + hd] = qd * qd
        rdec[:, 8 + hd] = np.exp(log_g[hd] * (127.0 - idx)).astype(np.float32)
    return {"c_ident": ident, "c_lst": lst, "c_ebase": ebase, "c_mask": mask,
            "c_cos": cosT, "c_sin": sinT, "c_intraT": intraT, "c_rdec": rdec}


_CACHE = {}


def prep_inputs(inputs, nc_inputs, layer_sets):
    f = lambda a: np.ascontiguousarray(np.asarray(a), dtype=np.float32)
    shared = {}
    shared["norm_mix"] = f(inputs["norm_mix"])
    shared["norm_ffn"] = f(inputs["norm_ffn"])
    shared["norm_final"] = f(inputs["norm_final"]).reshape(1, D)
    shared.update(_consts())
    if "router_w" in nc_inputs:
        ml = layer_sets["moe"]
        shared["router_w"] = np.ascontiguousarray(
            np.concatenate([f(inputs["router_group_w"]), f(inputs["router_expert_w"])], axis=-1)[ml])
        shared["router_b"] = np.ascontiguousarray(
            np.concatenate([f(inputs["router_group_b"]), f(inputs["router_expert_b"])], axis=-1)[ml])
        for p, l in enumerate(ml):
            shared["expert_w_gu%d" % p] = f(inputs["expert_w_gu"][l])
            shared["expert_w_down%d" % p] = f(inputs["expert_w_down"][l])
    if "fox_w_in" in nc_inputs:
        fl = [l // 2 for l in layer_sets["fox"]]
        shared["fox_w_in"] = np.ascontiguousarray(f(inputs["fox_w_in"])[fl])
        shared["fox_b_f"] = np.ascontiguousarray(f(inputs["fox_b_f"])[fl])
        shared["fox_w_out"] = np.ascontiguousarray(f(inputs["fox_w_out"])[fl])
    if "ret_w_in" in nc_inputs:
        rl = [l // 2 for l in layer_sets["ret"]]
        shared["ret_w_in"] = np.ascontiguousarray(f(inputs["ret_w_in"])[rl])
        shared["ret_gn_gain"] = np.ascontiguousarray(f(inputs["ret_gn_gain"])[rl])
        shared["ret_w_out"] = np.ascontiguousarray(f(inputs["ret_w_out"])[rl])
    x = f(inputs["x"])
    in_maps = []
    for b in range(NCORES):
        m = {k: v for k, v in shared.items() if k in nc_inputs}
        m["x"] = x[b]
        in_maps.append(m)
    return in_maps


def run(inputs, stages=None, final_norm=True, trace=False):
    key = (tuple(stages) if stages is not None else None, final_norm)
    if key not in _CACHE:
        B_nc = build(stages, final_norm)
        _CACHE[key] = B_nc
    nc = _CACHE[key]
    names = set(_DRAM_NAMES[id(nc)])
    in_maps = prep_inputs(inputs, names, _LAYER_SETS[id(nc)])
    res = run_bass_kernel_spmd(nc, in_maps, core_ids=list(range(NCORES)), trace=trace)
    out = np.stack([np.asarray(r["y"]) for r in res.results], axis=0).astype(np.float32)
    return out, res


def kernel(**inputs):
    out, _ = run(inputs)
    return out
```
